# Optimizing a Trainium2 kernel written in Bass

```python
import math
import jax
import jax.numpy as jnp
from jax import lax
import numpy as np

D_MODEL = 1024
BATCH = 8
SEQ = 4096
DEPTH = 2

GRID_W = 64
CTX_LEN = 256
N_MIXERS = 2
N_GDN_LAYERS = (DEPTH + 1) // 2
N_SWA_LAYERS = DEPTH // 2

GDN_HEADS = 8
GDN_DK = D_MODEL // GDN_HEADS
GDN_DV = GDN_DK
GDN_WIDTH = GDN_HEADS * GDN_DK
GDN_CONV = 5
GDN_CHUNK = 64
GDN_IN_WIDTH = 4 * GDN_WIDTH + 4 * GDN_HEADS

SWA_Q_HEADS = 16
SWA_KV_HEADS = 4
SWA_GROUP = SWA_Q_HEADS // SWA_KV_HEADS
SWA_HEAD_DIM = D_MODEL // SWA_Q_HEADS
SWA_Q_WIDTH = SWA_Q_HEADS * SWA_HEAD_DIM
SWA_KV_WIDTH = SWA_KV_HEADS * SWA_HEAD_DIM
WINDOW = 128
ATTN_BLOCK = 128
ROPE_BASE = 10000.0
ROPE_AXIS_DIM = SWA_HEAD_DIM // 2

PEER_HEADS = 8
N_KEYS = 128
N_EXPERTS = N_KEYS * N_KEYS
PEER_TOPK = 16
PEER_D_KEY = 256
PEER_HALF = PEER_D_KEY // 2
PEER_BLOCK = 128

ADA_CHUNKS = 6
RMS_EPS = 1e-6
NEG_INF = -1e30
F32 = jnp.float32

kernel_name = 'hybrid_gdn_swa_peer_dit'


def rmsnorm(x, g):
    xf = x.astype(F32)
    y = xf * lax.rsqrt(jnp.mean(xf * xf, axis=-1, keepdims=True) + RMS_EPS)
    return (y * g.astype(F32)).astype(x.dtype)


def adaln_params(cvec, w, b):
    m = jax.nn.silu(cvec) @ w + b
    return [mi[..., None, :] for mi in jnp.split(m, ADA_CHUNKS, axis=-1)]


def modulate(x, g, shift, scale):
    return rmsnorm(x, g) * (1 + scale) + shift


def l2norm(x):
    return x * lax.rsqrt(jnp.sum(x * x, axis=-1, keepdims=True) + 1e-6)


def centred_depthwise_conv(x, w):
    k = w.shape[0]
    return lax.conv_general_dilated(x, w[:, None, :].astype(x.dtype), window_strides=(1,),
                                    padding=[(k // 2, k // 2)],
                                    dimension_numbers=('NWC', 'WIO', 'NWC'),
                                    feature_group_count=x.shape[-1])


def gdn_project(h, w_in, conv_w, a_log, dt_bias):
    b, l, _ = h.shape
    p = h @ w_in
    qkv = jax.nn.silu(centred_depthwise_conv(p[..., :3 * GDN_WIDTH], conv_w))
    z = p[..., 3 * GDN_WIDTH:4 * GDN_WIDTH]
    o0 = 4 * GDN_WIDTH
    a = p[..., o0:o0 + 2 * GDN_HEADS].astype(F32).reshape(b, l, 2, GDN_HEADS).transpose(2, 0, 3, 1)
    bt = p[..., o0 + 2 * GDN_HEADS:].astype(F32).reshape(b, l, 2, GDN_HEADS).transpose(2, 0, 3, 1)
    g = -jnp.exp(a_log.astype(F32))[:, None, :, None] * jax.nn.softplus(a + dt_bias.astype(F32)[:, None, :, None])
    beta = jax.nn.sigmoid(bt)

    def heads(t):
        return t.astype(F32).reshape(b, l, GDN_HEADS, -1).transpose(0, 2, 1, 3)

    q = l2norm(heads(qkv[..., :GDN_WIDTH])) * GDN_DK ** -0.5
    k = l2norm(heads(qkv[..., GDN_WIDTH:2 * GDN_WIDTH]))
    v = heads(qkv[..., 2 * GDN_WIDTH:])
    return q, k, v, z, g, beta


def delta_chunked(q, k, v, g, beta, s0, with_output):
    b, h, l, dk = q.shape
    dv = v.shape[-1]
    c = GDN_CHUNK
    n = l // c
    q = q.reshape(b, h, n, c, dk)
    k = k.reshape(b, h, n, c, dk)
    v = v.reshape(b, h, n, c, dv)
    beta = beta.reshape(b, h, n, c)
    big_g = jnp.cumsum(g.reshape(b, h, n, c), axis=-1)
    incl = jnp.tril(jnp.ones((c, c), bool))
    strict = jnp.tril(jnp.ones((c, c), bool), -1)
    diff = big_g[..., :, None] - big_g[..., None, :]
    decay = jnp.where(incl, jnp.exp(jnp.where(incl, diff, 0.0)), 0.0)
    kb = k * beta[..., None]
    lmat = jnp.where(strict, jnp.einsum('bhnid,bhnjd->bhnij', kb, k) * decay, 0.0)
    eye = jnp.eye(c, dtype=F32)
    t_inv = lax.linalg.triangular_solve(lmat + eye, jnp.broadcast_to(eye, lmat.shape),
                                        left_side=True, lower=True, unit_diagonal=True)
    u = jnp.einsum('bhnij,bhnjd->bhnid', t_inv, v * beta[..., None])
    w = jnp.einsum('bhnij,bhnjd->bhnid', t_inv, kb * jnp.exp(big_g)[..., None])
    g_last = big_g[..., -1]
    k_dec = k * jnp.exp(g_last[..., None] - big_g)[..., None]
    chunk_decay = jnp.exp(g_last)
    xs = [k_dec, w, u, chunk_decay]
    if with_output:
        a_qk = jnp.where(incl, jnp.einsum('bhnid,bhnjd->bhnij', q, k) * decay, 0.0)
        q_dec = q * jnp.exp(big_g)[..., None]
        xs = xs + [q_dec, a_qk]
    xs = tuple(jnp.moveaxis(t, 2, 0) for t in xs)

    def step(state, xc):
        kd, wc, uc, cd = xc[:4]
        v_new = uc - jnp.einsum('bhcd,bhde->bhce', wc, state)
        new_state = state * cd[..., None, None] + jnp.einsum('bhcd,bhce->bhde', kd, v_new)
        if with_output:
            qd, aqk = xc[4:]
            o = jnp.einsum('bhcd,bhde->bhce', qd, state) + jnp.einsum('bhcj,bhje->bhce', aqk, v_new)
            return new_state, o
        return new_state, None

    s_final, o = lax.scan(step, s0, xs)
    if not with_output:
        return s_final, None
    return s_final, jnp.moveaxis(o, 0, 2).reshape(b, h, l, dv)


def gdn_bidir(q, k, v, g, beta, s0_f, s0_b, with_output):
    s_f, o_f = delta_chunked(q, k, v, g[0], beta[0], s0_f, with_output)
    fl = lambda t: jnp.flip(t, axis=2)
    s_b, o_b = delta_chunked(fl(q), fl(k), fl(v), jnp.flip(g[1], axis=-1), jnp.flip(beta[1], axis=-1),
                             s0_b, with_output)
    o = o_f + fl(o_b) if with_output else None
    return s_f, s_b, o


def gdn_out(o, z, norm_g, w_out):
    b, h, l, dv = o.shape
    o = o.transpose(0, 2, 1, 3).astype(z.dtype)
    o = rmsnorm(o, norm_g) * jax.nn.silu(z.reshape(b, l, h, dv))
    return o.reshape(b, l, h * dv) @ w_out


def gdn_mixer(h, hc, w_in, conv_w, a_log, dt_bias, norm_g, w_out, ctx_out):
    q, k, v, z, g, beta = gdn_project(h, w_in, conv_w, a_log, dt_bias)
    qc, kc, vc, zc, gc, betac = gdn_project(hc, w_in, conv_w, a_log, dt_bias)
    s0 = jnp.zeros((h.shape[0], GDN_HEADS, GDN_DK, GDN_DV), F32)
    sf_c, sb_c, oc = gdn_bidir(qc, kc, vc, gc, betac, s0, s0, ctx_out)
    _, _, o = gdn_bidir(q, k, v, g, beta, sf_c, sb_c, True)
    y = gdn_out(o, z, norm_g, w_out)
    yc = gdn_out(oc, zc, norm_g, w_out) if ctx_out else None
    return y, yc


def axial_rope_tables(n_lat):
    rows = n_lat // GRID_W
    row = jnp.broadcast_to(jnp.arange(rows)[:, None], (rows, GRID_W)).reshape(-1).astype(F32)
    col = jnp.broadcast_to(jnp.arange(GRID_W)[None, :], (rows, GRID_W)).reshape(-1).astype(F32)
    inv = ROPE_BASE ** (-jnp.arange(0, ROPE_AXIS_DIM, 2, dtype=F32) / ROPE_AXIS_DIM)
    ang_r = row[:, None] * inv
    ang_c = col[:, None] * inv
    return jnp.cos(ang_r), jnp.sin(ang_r), jnp.cos(ang_c), jnp.sin(ang_c)


def rotate(x, cos, sin):
    m = x.shape[-1] // 2
    x1, x2 = x[..., :m], x[..., m:]
    c = cos[:, None, :].astype(x.dtype)
    s = sin[:, None, :].astype(x.dtype)
    return jnp.concatenate([x1 * c - x2 * s, x2 * c + x1 * s], axis=-1)


def axial_rope(x, tables):
    cr, sr, cc, sc = tables
    return jnp.concatenate([rotate(x[..., :ROPE_AXIS_DIM], cr, sr),
                            rotate(x[..., ROPE_AXIS_DIM:], cc, sc)], axis=-1)


def window_attention(q, k, v, kc, vc, sinks):
    b, l = q.shape[0], q.shape[1]
    blk = ATTN_BLOCK
    nb = l // blk
    nw = 3 * blk
    qb = q.reshape(b, nb, blk, SWA_KV_HEADS, SWA_GROUP, SWA_HEAD_DIM)

    def band(t):
        tp = jnp.pad(t, ((0, 0), (blk, blk), (0, 0), (0, 0))).reshape(b, nb + 2, blk, SWA_KV_HEADS, SWA_HEAD_DIM)
        return jnp.concatenate([tp[:, :nb], tp[:, 1:nb + 1], tp[:, 2:]], axis=2)

    kw, vw = band(k), band(v)
    scale = SWA_HEAD_DIM ** -0.5
    s_loc = jnp.einsum('bnqhgd,bnkhd->bnhgqk', qb, kw).astype(F32) * scale
    qpos = jnp.arange(nb)[:, None] * blk + jnp.arange(blk)[None, :]
    kpos = jnp.arange(nb)[:, None] * blk - blk + jnp.arange(nw)[None, :]
    valid = ((jnp.abs(qpos[:, :, None] - kpos[:, None, :]) <= WINDOW)
             & (kpos[:, None, :] >= 0) & (kpos[:, None, :] < l))
    s_loc = jnp.where(valid[None, :, None, None], s_loc, NEG_INF)
    s_ctx = jnp.einsum('bnqhgd,bchd->bnhgqc', qb, kc).astype(F32) * scale
    sink = jnp.broadcast_to(sinks.astype(F32).reshape(SWA_KV_HEADS, SWA_GROUP)[None, None, :, :, None, None],
                            s_loc.shape[:-1] + (1,))
    p = jax.nn.softmax(jnp.concatenate([s_loc, s_ctx, sink], axis=-1), axis=-1)
    p_loc = p[..., :nw].astype(v.dtype)
    p_ctx = p[..., nw:nw + kc.shape[1]].astype(v.dtype)
    o = (jnp.einsum('bnhgqk,bnkhd->bnqhgd', p_loc, vw)
         + jnp.einsum('bnhgqc,bchd->bnqhgd', p_ctx, vc))
    return o.reshape(b, l, SWA_Q_WIDTH)


def context_attention(qc, kc, vc, sinks):
    b, c = qc.shape[0], qc.shape[1]
    qg = qc.reshape(b, c, SWA_KV_HEADS, SWA_GROUP, SWA_HEAD_DIM)
    s = jnp.einsum('bqhgd,bkhd->bhgqk', qg, kc).astype(F32) * SWA_HEAD_DIM ** -0.5
    sink = jnp.broadcast_to(sinks.astype(F32).reshape(SWA_KV_HEADS, SWA_GROUP)[None, :, :, None, None],
                            s.shape[:-1] + (1,))
    p = jax.nn.softmax(jnp.concatenate([s, sink], axis=-1), axis=-1)
    o = jnp.einsum('bhgqk,bkhd->bqhgd', p[..., :c].astype(vc.dtype), vc)
    return o.reshape(b, c, SWA_Q_WIDTH)


def swa_mixer(h, hc, w_in, sinks, w_out, rope_tables, ctx_out):
    b, l, _ = h.shape
    nc = hc.shape[1]
    p = h @ w_in
    q = axial_rope(p[..., :SWA_Q_WIDTH].reshape(b, l, SWA_Q_HEADS, SWA_HEAD_DIM), rope_tables)
    k = axial_rope(p[..., SWA_Q_WIDTH:SWA_Q_WIDTH + SWA_KV_WIDTH].reshape(b, l, SWA_KV_HEADS, SWA_HEAD_DIM),
                   rope_tables)
    v = p[..., SWA_Q_WIDTH + SWA_KV_WIDTH:].reshape(b, l, SWA_KV_HEADS, SWA_HEAD_DIM)
    pc = hc @ (w_in if ctx_out else w_in[:, SWA_Q_WIDTH:])
    kvc = pc[..., -2 * SWA_KV_WIDTH:]
    kc = kvc[..., :SWA_KV_WIDTH].reshape(b, nc, SWA_KV_HEADS, SWA_HEAD_DIM)
    vc = kvc[..., SWA_KV_WIDTH:].reshape(b, nc, SWA_KV_HEADS, SWA_HEAD_DIM)
    y = window_attention(q, k, v, kc, vc, sinks) @ w_out
    yc = None
    if ctx_out:
        qc = pc[..., :SWA_Q_WIDTH].reshape(b, nc, SWA_Q_HEADS, SWA_HEAD_DIM)
        yc = context_attention(qc, kc, vc, sinks) @ w_out
    return y, yc


def peer_ffn(h, w_query, sub_keys, expert_u, expert_v):
    b, l, d = h.shape
    t_count = b * l
    t = h.reshape(t_count, d)
    q = (t @ w_query).reshape(t_count, PEER_HEADS, 2, PEER_HALF)
    s = jnp.einsum('thpd,hpkd->thpk', q, sub_keys).astype(F32)
    s1, i1 = lax.top_k(s[:, :, 0], PEER_TOPK)
    s2, i2 = lax.top_k(s[:, :, 1], PEER_TOPK)
    cand_s = (s1[..., :, None] + s2[..., None, :]).reshape(t_count, PEER_HEADS, PEER_TOPK * PEER_TOPK)
    cand_i = (i1[..., :, None] * N_KEYS + i2[..., None, :]).reshape(t_count, PEER_HEADS, PEER_TOPK * PEER_TOPK)
    top_s, pos = lax.top_k(cand_s, PEER_TOPK)
    idx = jnp.take_along_axis(cand_i, pos, axis=-1).reshape(t_count, PEER_HEADS * PEER_TOPK)
    gate = jax.nn.softmax(top_s, axis=-1).reshape(t_count, PEER_HEADS * PEER_TOPK).astype(h.dtype)
    nblk = t_count // PEER_BLOCK

    def block(args):
        tb, ib, gb = args
        act = jax.nn.gelu(jnp.einsum('pkd,pd->pk', expert_u[ib], tb), approximate=False) * gb
        return jnp.einsum('pk,pkd->pd', act, expert_v[ib])

    out = lax.map(block, (t.reshape(nblk, PEER_BLOCK, d),
                          idx.reshape(nblk, PEER_BLOCK, -1),
                          gate.reshape(nblk, PEER_BLOCK, -1)))
    return out.reshape(b, l, d)


def setup_inputs(seed: int = 0) -> dict:
    key = jax.random.key(seed)
    ks = jax.random.split(key, 22)

    def nrm(k, shape, scale):
        return jax.random.normal(k, shape, jnp.float32) * scale

    dt = jnp.exp(jax.random.uniform(ks[11], (N_GDN_LAYERS, 2, GDN_HEADS), jnp.float32,
                                    math.log(1e-3), math.log(1e-1)))
    return {
        'x': nrm(ks[0], (BATCH, SEQ, D_MODEL), 1.0),
        'c': nrm(ks[1], (BATCH, D_MODEL), 1.0),
        'ctx': nrm(ks[2], (BATCH, CTX_LEN, D_MODEL), 1.0),
        'c_ctx': nrm(ks[3], (D_MODEL,), 1.0),
        'ada_w': nrm(ks[4], (DEPTH, D_MODEL, ADA_CHUNKS * D_MODEL), 0.5 * D_MODEL ** -0.5),
        'ada_b': nrm(ks[5], (DEPTH, ADA_CHUNKS * D_MODEL), 0.02),
        'norm1_g': 1.0 + nrm(ks[6], (DEPTH, D_MODEL), 0.02),
        'norm2_g': 1.0 + nrm(ks[7], (DEPTH, D_MODEL), 0.02),
        'gdn_w_in': nrm(ks[8], (N_GDN_LAYERS, D_MODEL, GDN_IN_WIDTH), D_MODEL ** -0.5),
        'gdn_conv_w': nrm(ks[9], (N_GDN_LAYERS, GDN_CONV, 3 * GDN_WIDTH), GDN_CONV ** -0.5),
        'gdn_a_log': jnp.log(jax.random.uniform(ks[10], (N_GDN_LAYERS, 2, GDN_HEADS), jnp.float32, 1.0, 16.0)),
        'gdn_dt_bias': dt + jnp.log(-jnp.expm1(-dt)),
        'gdn_norm_g': 1.0 + nrm(ks[12], (N_GDN_LAYERS, GDN_DV), 0.02),
        'gdn_w_out': nrm(ks[13], (N_GDN_LAYERS, GDN_WIDTH, D_MODEL), GDN_WIDTH ** -0.5),
        'swa_w_in': nrm(ks[14], (N_SWA_LAYERS, D_MODEL, SWA_Q_WIDTH + 2 * SWA_KV_WIDTH), D_MODEL ** -0.5),
        'swa_sinks': nrm(ks[15], (N_SWA_LAYERS, SWA_Q_HEADS), 0.5),
        'swa_w_out': nrm(ks[16], (N_SWA_LAYERS, SWA_Q_WIDTH, D_MODEL), SWA_Q_WIDTH ** -0.5),
        'peer_w_query': nrm(ks[17], (DEPTH, D_MODEL, PEER_HEADS * PEER_D_KEY), D_MODEL ** -0.5),
        'peer_sub_keys': nrm(ks[18], (DEPTH, PEER_HEADS, 2, N_KEYS, PEER_HALF), PEER_HALF ** -0.5),
        'peer_u': nrm(ks[19], (DEPTH, N_EXPERTS, D_MODEL), D_MODEL ** -0.5),
        'peer_v': nrm(ks[20], (DEPTH, N_EXPERTS, D_MODEL), 0.5),
        'final_g': 1.0 + nrm(ks[21], (D_MODEL,), 0.02),
    }


def reference(x, c, ctx, c_ctx, ada_w, ada_b, norm1_g, norm2_g, gdn_w_in, gdn_conv_w, gdn_a_log,
              gdn_dt_bias, gdn_norm_g, gdn_w_out, swa_w_in, swa_sinks, swa_w_out, peer_w_query,
              peer_sub_keys, peer_u, peer_v, final_g):
    n_lat = x.shape[1]
    rope_tables = axial_rope_tables(n_lat)
    xc = ctx
    for i in range(DEPTH):
        last = i == DEPTH - 1
        sh1, sc1, g1, sh2, sc2, g2 = adaln_params(c, ada_w[i], ada_b[i])
        csh1, csc1, cg1, csh2, csc2, cg2 = adaln_params(c_ctx, ada_w[i], ada_b[i])
        h = modulate(x, norm1_g[i], sh1, sc1)
        hc = modulate(xc, norm1_g[i], csh1, csc1)
        j = i // N_MIXERS
        if i % N_MIXERS == 0:
            y, yc = gdn_mixer(h, hc, gdn_w_in[j], gdn_conv_w[j], gdn_a_log[j], gdn_dt_bias[j],
                              gdn_norm_g[j], gdn_w_out[j], not last)
        else:
            y, yc = swa_mixer(h, hc, swa_w_in[j], swa_sinks[j], swa_w_out[j], rope_tables, not last)
        x = x + g1 * y
        x = x + g2 * peer_ffn(modulate(x, norm2_g[i], sh2, sc2), peer_w_query[i], peer_sub_keys[i],
                              peer_u[i], peer_v[i])
        if not last:
            xc = xc + cg1 * yc
            xc = xc + cg2 * peer_ffn(modulate(xc, norm2_g[i], csh2, csc2), peer_w_query[i],
                                     peer_sub_keys[i], peer_u[i], peer_v[i])
    return rmsnorm(x, final_g)
```

```python
import numpy as np
import concourse.bass as bass
import concourse.mybir as mybir
from concourse.bass_utils import run_bass_kernel_spmd

F32 = mybir.dt.float32
BF16 = mybir.dt.bfloat16
U32 = mybir.dt.uint32
AF = mybir.ActivationFunctionType
ALU = mybir.AluOpType
AX = mybir.AxisListType

D = 1024
L = 4096
CTX = 256
NTOK = L + CTX
NT = NTOK // 128
EPS = 1e-6
NEXP_T = 128


class K:
    def __init__(self):
        self.nc = bass.Bass("TRN2", target_bir_lowering=False)
        nc = self.nc
        self.E = {"pe": nc.tensor, "act": nc.scalar, "dve": nc.vector, "pool": nc.gpsimd, "sp": nc.sync}
        self.sem = {e: nc.alloc_semaphore("s_" + e) for e in self.E}
        self.cnt = {e: 0 for e in self.E}
        self.seen = {e: {} for e in self.E}
        self.st = {}
        self.NDMA = 24
        self.dsem = [nc.alloc_semaphore("s_dma%d" % i) for i in range(self.NDMA)]
        self.dval = [0] * self.NDMA
        self.dnext = 0
        self.nsb = 0
        self.stack = []

    def sb(self, shape, dtype=F32, name=None):
        self.nsb += 1
        name = "sb%d_%s" % (self.nsb, name or "t")
        cm = self.nc.sbuf_tensor(name, list(shape), dtype)
        t = cm.__enter__()
        self.stack.append(cm)
        return t

    def mark(self):
        return len(self.stack)

    def release(self, mark):
        self.barrier()
        while len(self.stack) > mark:
            self.stack.pop().__exit__(None, None, None)

    def _semh(self, sk):
        return self.sem[sk] if isinstance(sk, str) else self.dsem[sk]

    def wait(self, e, tok):
        sk, v = tok
        if sk == e and e == "pe":
            return
        if self.seen[e].get(sk, 0) >= v:
            return
        self.E[e].wait_ge(self._semh(sk), v)
        self.seen[e][sk] = v

    @staticmethod
    def _key(x):
        if isinstance(x, tuple):
            return x[1]
        if isinstance(x, str):
            return x
        return x.tensor.name

    def _deps(self, e, outs, ins):
        deps = []
        for x in ins:
            s = self.st.get(self._key(x))
            if s and s[0]:
                deps.append(s[0])
        for x in outs:
            s = self.st.get(self._key(x))
            if s:
                if s[0] and s[0][0] != e:
                    deps.append(s[0])
                for re_, rt in s[1].items():
                    if re_ != e:
                        deps.append(rt)
        for t in deps:
            self.wait(e, t)

    def _commit(self, tok, e, outs, ins):
        for x in ins:
            s = self.st.setdefault(self._key(x), [None, {}])
            s[1][e] = tok
        for x in outs:
            s = self.st.setdefault(self._key(x), [None, {}])
            s[0] = tok
            s[1] = {}

    def op(self, e, fn, outs, ins, inc=True):
        self._deps(e, outs, ins)
        inst = fn()
        tok = (e, self.cnt[e] + 1)
        if inc:
            inst.then_inc(self.sem[e], 1)
            self.cnt[e] += 1
        self._commit(tok, e, outs, ins)
        return inst

    def dma(self, q, out, in_, outs, ins, **kw):
        self._deps(q, outs, ins)
        i = self.dnext
        self.dnext = (self.dnext + 1) % self.NDMA
        if self.dval[i]:
            self.wait(q, (i, self.dval[i]))
        self.dval[i] += 16
        self.E[q].dma_start(out=out, in_=in_, **kw).then_inc(self.dsem[i], 16)
        tok = (i, self.dval[i])
        for x in ins:
            s = self.st.setdefault(self._key(x), [None, {}])
            s[1]["d%d" % i] = tok
        for x in outs:
            s = self.st.setdefault(self._key(x), [None, {}])
            s[0] = tok
            s[1] = {}
        return tok

    def barrier(self):
        toks = [(e, self.cnt[e]) for e in self.E if self.cnt[e]]
        toks += [(i, self.dval[i]) for i in range(self.NDMA) if self.dval[i]]
        for e in self.E:
            for t in toks:
                if t[0] != e:
                    self.wait(e, t)
        self.st = {}

    def finish(self):
        for i in range(self.NDMA):
            if self.dval[i]:
                self.wait("sp", (i, self.dval[i]))
        for e in self.E:
            if e != "sp" and self.cnt[e]:
                self.wait("sp", (e, self.cnt[e]))


def V(ap, *dims):
    return bass.AP(ap.tensor, ap.offset, [list(ap.ap[0])] + [list(d) for d in dims])


C_ID, C_IOTA, C_EPS, C_DSELB, C_ONES, C_NMLS, C_NMUS, C_NMLI, C_NMUI, C_NMP, C_NMN, C_NCOL = 0, 128, 256, 264, 272, 400, 464, 528, 592, 656, 784, 912


def host_consts():
    c = np.zeros((128, C_NCOL), np.float32)
    c[:, C_ID:C_ID + 128] = np.eye(128, dtype=np.float32)
    c[:, C_IOTA:C_IOTA + 128] = np.arange(128, dtype=np.float32)[None, :]
    c[:, C_EPS] = EPS
    c[:, C_EPS + 1] = -0.5
    c[:, C_EPS + 2] = np.e
    c[8:16, C_DSELB] = 1.0
    c[:, C_ONES:C_ONES + 128] = 1.0
    p = np.arange(128)[:, None]
    f = np.arange(64)[None, :]
    NEG = -1e4
    c[:, C_NMLS:C_NMLS + 64] = np.where(p > f, 0.0, NEG)
    c[:, C_NMUS:C_NMUS + 64] = np.where(f > p, 0.0, NEG)
    c[:, C_NMLI:C_NMLI + 64] = np.where(p >= f, 0.0, NEG)
    c[:, C_NMUI:C_NMUI + 64] = np.where(f >= p, 0.0, NEG)
    f2 = np.arange(128)[None, :]
    c[:, C_NMP:C_NMP + 128] = np.where(p >= f2, 0.0, NEG)
    c[:, C_NMN:C_NMN + 128] = np.where(p <= f2, 0.0, NEG)
    return c


class Ctx:
    pass


def load_consts(k, g):
    g.cst = k.sb([128, C_NCOL], F32, "cst_sb")
    k.dma("sp", g.cst[:], g.d_cst, [g.cst[:]], [])
    g.ident = g.cst[:, C_ID:C_ID + 128]
    g.iota = g.cst[:, C_IOTA:C_IOTA + 128]
    g.epsT = g.cst[:, C_EPS:C_EPS + 1]
    g.ones = g.cst[:, C_ONES:C_ONES + 128]
    g.psb = [k.nc.alloc_psum_tensor("psb%d" % i, [128, 512], F32) for i in range(8)]


def bcast_row_load(k, dst, row_ap, n):
    P = dst.shape[0]
    src = bass.AP(row_ap.tensor, row_ap.offset, [[0, P], [1, n]])
    k.dma("sp", dst, src, [dst], [])


def peer_prepass_early(k, g, layers):
    nc = k.nc
    for li in layers:
        sem = nc.alloc_semaphore("s_pp%d" % li)
        k.dsem.append(sem)
        k.dval.append(0)
        si = len(k.dsem) - 1
        n = 0
        for src, dst in ((g.d_ut[li], g.d_utb[li]), (g.d_v[li], g.d_vb[li])):
            sv = src.rearrange("c p f -> (c p) f").rearrange("(a b) f -> a (b f)", b=8)
            dv = dst.rearrange("c p f -> (c p) f").rearrange("(a b) f -> a (b f)", b=8)
            for r0 in range(0, 2048, 128):
                nc.gpsimd.dma_start(out=dv[r0:r0 + 128, :], in_=sv[r0:r0 + 128, :]).then_inc(sem, 16)
                n += 1
        for blk in range(4):
            nc.gpsimd.dma_start(out=g.d_wqb[li][blk], in_=g.d_wq[li][:, blk * 512:(blk + 1) * 512].rearrange("(k p) f -> p k f", p=128)
                                ).then_inc(sem, 16)
            n += 1
        k.dval[si] = 16 * n
        k.st[("wb", li)] = [(si, 16 * n), {}]
        k.pp_tok = getattr(k, "pp_tok", {})
        k.pp_tok[li] = (si, 16 * n)


def peer_prepass(k, g, li):
    m = k.mark()
    CB = 8
    bufs = [k.sb([128, CB, 1024], BF16, "ppb%d" % i) for i in range(3)]
    n = 0
    for src, dst in ((g.d_ut[li], g.d_utb), (g.d_v[li], g.d_vb)):
        for c0 in range(0, 128, CB):
            b = bufs[n % 3]
            n += 1
            k.dma("pool", b[:], src[c0:c0 + CB].rearrange("c p f -> p c f"), [b[:]], [])
            for c1 in range(0, CB, 2):
                k.dma("pool", dst[(c0 + c1) // 2], b[:, c1:c1 + 2, :], [("x", ("wb", li))], [b[:]])
    for blk in range(4):
        b = bufs[n % 3]
        n += 1
        bv = b[:].rearrange("p c (a f) -> p (c a) f", a=2)[:, 0:8, :]
        for kk in range(8):
            k.dma("pool", bv[:, kk, :], g.d_wq[li, kk * 128:(kk + 1) * 128, blk * 512:(blk + 1) * 512], [b[:]], [])
        k.dma("pool", g.d_wqb[blk], bv, [("x", ("wb", li))], [b[:]])
    k.release(m)


def peer_layer(k, g, li, ntiles, final):
    nc = k.nc
    m0 = k.mark()
    TG = 2
    NTOKG = TG * 128
    vec, act, pe, pool = nc.vector, nc.scalar, nc.tensor, nc.gpsimd
    wqb = [k.sb([128, 8, 512], BF16, "wqb%d" % i) for i in range(2)]
    wk = k.sb([128, 16, 128], F32, "wk")
    skn = wk
    k.dma("sp", skn[:], g.d_sk[li].rearrange("h p k j -> k (h p) j"), [skn[:]], [])
    skt = k.sb([128, 16, 128], F32, "skt")
    for hp in range(16):
        ps = g.psb[hp % 2]
        k.op("pe", lambda: pe.transpose(ps[:, 0:128], skn[:, hp, :], g.ident), [ps[:]], [skn[:], g.cst[:]])
        k.op("act", lambda: act.copy(out=skt[:, hp, :], in_=ps[:, 0:128]), [skt[:]], [ps[:]])
    GS = k.sb([128, 1024], F32, "modgs")
    SH = k.sb([128, 1024], F32, "modsh")
    GT = k.sb([128, 1024], F32, "modgt")
    cur_a, cur_e = [-1], [-1]

    def set_mods_a(r):
        if cur_a[0] != r:
            cur_a[0] = r
            bcast_row_load(k, GS[:], g.d_mod[li, r, 3], 1024)
            bcast_row_load(k, SH[:], g.d_mod[li, r, 4], 1024)

    def set_mods_e(r):
        if cur_e[0] != r:
            cur_e[0] = r
            bcast_row_load(k, GT[:], g.d_mod[li, r, 5], 1024)
    if final:
        fg = k.sb([128, 1024], F32, "fing")
        bcast_row_load(k, fg[:], g.d_fg, 1024)
    gbuf = k.sb([128, NTOKG, 128], BF16, "gbuf")
    h2T = [k.sb([128, 8, NTOKG], BF16, "h2T%d" % i) for i in range(2)]
    xt = [[k.sb([128, 1024], F32, "xt%d_%d" % (p, i)) for i in range(TG)] for p in range(2)]
    selT = [[k.sb([128, 3, 128], F32, "selT%d_%d" % (p, i)) for i in range(TG)] for p in range(2)]
    hh = k.sb([128, 1024], F32, "hh")
    ss = k.sb([128, 4], F32, "ss")
    ss2 = k.sb([128, 4], F32, "ss2")
    qT = k.sb([128, 2, 256], F32, "qT")
    top2 = [k.sb([128, 16, 16], F32, "top%d" % i) for i in range(TG)]
    idx2 = [k.sb([128, 16, 16], U32, "idx%d" % i) for i in range(TG)]
    idxf = k.sb([128, 16, 16], F32, "idxf")
    cand = k.sb([128, 8, 256], F32, "cand")
    wk2 = wk[:].rearrange("p (a b) k -> p a (b k)", b=2)
    ctop = k.sb([128, 8, 16], F32, "ctop")
    pos = k.sb([128, 8, 16], U32, "pos")
    posi = k.sb([128, 2, 128], U32, "posi")
    posf = k.sb([128, 2, 128], F32, "posf")
    ee = k.sb([128, 8, 16], F32, "ee")
    zz = k.sb([128, 16], F32, "zz")
    eq = wk[:].rearrange("p a (b i) -> p (a b) i", i=16)
    sel = k.sb([128, 3, 128], F32, "sel")
    TB = 8
    ohA = [k.sb([128, TB, 128], BF16, "ohA%d" % i) for i in range(2)]
    ohB = [k.sb([128, TB, 128], BF16, "ohB%d" % i) for i in range(2)]
    iotab = k.sb([128, 128], BF16, "iotab")
    k.op("dve", lambda: vec.tensor_copy(out=iotab[:], in_=g.iota), [iotab[:]], [g.cst[:]])
    CB = 2
    NWB = 3
    utb = [k.sb([128, CB, 1024], BF16, "utb%d" % i) for i in range(NWB)]
    vbb = [k.sb([128, CB, 1024], BF16, "vbb%d" % i) for i in range(NWB)]
    actS = [k.sb([128, NTOKG], F32, "actS%d" % i) for i in range(2)]
    ga = [k.sb([128, NTOKG], BF16, "ga%d" % i) for i in range(2)]
    osb = hh

    psO = g.psb[0:4]
    psA = g.psb[4:6]
    psX = g.psb[6]
    psS = g.psb[7]

    ngroups = (ntiles + TG - 1) // TG

    def group_tiles(gi):
        return list(range(gi * TG, min(ntiles, (gi + 1) * TG)))

    def A1(gi):
        par = gi % 2
        tiles = group_tiles(gi)
        ntk = len(tiles) * 128
        set_mods_a(0 if tiles[0] < L // 128 else 1)
        hT_ = h2T[par]
        for ti, t in enumerate(tiles):
            x = xt[par][ti]
            k.dma("sp", x[:], g.d_xs[t * 128:(t + 1) * 128, :], [x[:]], [("x", ("xs", t))])
            norm_mod(k, g, x, hh, ss, GS, SH)
            yield
            for hf in range(2):
                for kk in range(4):
                    k.op("pe", lambda: pe.transpose(psX[:, kk * 128:(kk + 1) * 128], hh[:, (hf * 4 + kk) * 128:(hf * 4 + kk + 1) * 128],
                                                    g.ident), [psX[:]], [hh[:], g.cst[:]], inc=(kk == 3))
                k.op("act", lambda: act.copy(out=hT_[:, hf * 4:hf * 4 + 4, ti * 128:(ti + 1) * 128],
                                             in_=psX[:].rearrange("p (a b) -> p a b", a=4)), [hT_[:]], [psX[:]])
                yield
        k.dma("sp", wqb[0][:], g.d_wqb[li][0], [wqb[0][:]], [("x", ("wb", li))])
        for blk in range(4):
            wq = wqb[blk % 2]
            if blk + 1 < 4:
                k.dma("sp", wqb[(blk + 1) % 2][:], g.d_wqb[li][blk + 1], [wqb[(blk + 1) % 2][:]], [("x", ("wb", li))])
            for half in range(2):
                for h2 in range(2):
                    h4 = half * 2 + h2
                    for kk in range(8):
                        k.op("pe", lambda: pe.matmul(psX[:, h2 * 256:h2 * 256 + ntk], lhsT=wq[:, kk, h4 * 128:(h4 + 1) * 128],
                                                     rhs=hT_[:, kk, 0:ntk], start=(kk == 0), stop=(kk == 7)),
                             [psX[:]], [wq[:], hT_[:]], inc=(kk == 7 and h2 == 1))
                    yield
                k.op("act", lambda: act.copy(out=qT[:], in_=psX[:].rearrange("p (a b) -> p a b", a=2)), [qT[:]], [psX[:]])
                for ti in range(len(tiles)):
                    for h2 in range(2):
                        k.op("pe", lambda: pe.matmul(psS[:, (ti * 2 + h2) * 128:(ti * 2 + h2 + 1) * 128],
                                                     lhsT=qT[:, h2, ti * 128:(ti + 1) * 128], rhs=skt[:, blk * 4 + half * 2 + h2, :],
                                                     start=True, stop=True), [psS[:]], [qT[:], skt[:]],
                             inc=(h2 == 1 and ti == len(tiles) - 1))
                yield
                for ti in range(len(tiles)):
                    tp, ix = top2[ti], idx2[ti]
                    hps = [(blk * 4 + half * 2 + h2, (ti * 2 + h2)) for h2 in range(2)]

                    def S(c_):
                        return psS[:, c_ * 128:(c_ + 1) * 128]

                    for hp, c_ in hps:
                        k.op("dve", lambda: vec.max(out=tp[:, hp, 0:8], in_=S(c_)), [("x", ("topa", ti))], [psS[:]])
                    for hp, c_ in hps:
                        k.op("dve", lambda: vec.match_replace(out=wk[:, hp, :], in_to_replace=tp[:, hp, 0:8], in_values=S(c_),
                                                              imm_value=-1e30), [wk[:]], [("x", ("topa", ti)), psS[:]])
                    yield
                    for hp, c_ in hps:
                        k.op("dve", lambda: vec.max(out=tp[:, hp, 8:16], in_=wk[:, hp, :]), [("x", ("topb", ti))], [wk[:]])
                    for hp, c_ in hps:
                        k.op("dve", lambda: vec.max_index(out=ix[:, hp, 0:8], in_max=tp[:, hp, 0:8], in_values=S(c_)),
                             [("x", ("idxa", ti))], [("x", ("topa", ti)), psS[:]])
                    yield
                    for hp, c_ in hps:
                        k.op("dve", lambda: vec.max_index(out=ix[:, hp, 8:16], in_max=tp[:, hp, 8:16], in_values=S(c_)),
                             [("x", ("idxb", ti))], [("x", ("topb", ti)), psS[:]])
                    yield
        for ti, t in enumerate(tiles):
            top, idx = top2[ti], idx2[ti]
            k.op("dve", lambda: vec.tensor_copy(out=idxf[:], in_=idx[:]), [idxf[:]], [("x", ("idxa", ti)), ("x", ("idxb", ti))])
            t1 = V(top[:, 0, :], (32, 8), (1, 16), (0, 16))
            t2 = V(top[:, 1, :], (32, 8), (0, 16), (1, 16))
            k.op("dve", lambda: vec.tensor_tensor(out=cand[:].rearrange("p h (i j) -> p h i j", i=16), in0=t1, in1=t2,
                                                  op=ALU.add), [cand[:]], [("x", ("topa", ti)), ("x", ("topb", ti))])
            yield
            for h in range(8):
                k.op("dve", lambda: vec.max(out=ctop[:, h, 0:8], in_=cand[:, h, :]), [("x", "ctopa")], [cand[:]])
            yield
            for h in range(8):
                k.op("dve", lambda: vec.match_replace(out=wk2[:, h, :], in_to_replace=ctop[:, h, 0:8], in_values=cand[:, h, :],
                                                      imm_value=-1e30), [wk[:]], [("x", "ctopa"), cand[:]])
            yield
            for h in range(8):
                k.op("dve", lambda: vec.max(out=ctop[:, h, 8:16], in_=wk2[:, h, :]), [("x", "ctopb")], [wk[:]])
            yield
            for h in range(8):
                k.op("dve", lambda: vec.max_index(out=pos[:, h, 0:8], in_max=ctop[:, h, 0:8], in_values=cand[:, h, :]),
                     [("x", "posa")], [("x", "ctopa"), cand[:]])
            yield
            for h in range(8):
                k.op("dve", lambda: vec.max_index(out=pos[:, h, 8:16], in_max=ctop[:, h, 8:16], in_values=cand[:, h, :]),
                     [("x", "posb")], [("x", "ctopb"), cand[:]])
            yield
            k.op("dve", lambda: vec.tensor_tensor(out=ee[:], in0=ctop[:], in1=V(ctop[:, 0, 0:1], (16, 8), (0, 16)),
                                                  op=ALU.subtract), [ee[:]], [("x", "ctopa"), ("x", "ctopb")])
            k.op("act", lambda: act.activation(out=ee[:], in_=ee[:], func=AF.Exp), [ee[:]], [ee[:]])
            k.op("dve", lambda: vec.tensor_reduce(out=zz[:, 0:8], in_=ee[:], axis=AX.X, op=ALU.add), [("x", "zz0")], [ee[:]])
            k.op("dve", lambda: vec.reciprocal(out=zz[:, 8:16], in_=zz[:, 0:8]), [("x", "zz1")], [("x", "zz0")])
            k.op("dve", lambda: vec.tensor_tensor(out=sel[:, 2, :].rearrange("p (h r) -> p h r", h=8), in0=ee[:],
                                                  in1=V(zz[:, 8:9], (1, 8), (0, 16)), op=ALU.mult),
                 [("x", "sel2")], [ee[:], ("x", "zz1")])
            yield
            posv = pos[:].rearrange("p h r -> p (h r)")
            k.op("dve", lambda: vec.tensor_single_scalar(out=posi[:, 0, :], in_=posv, scalar=4, op=ALU.logical_shift_right),
                 [("x", "posi0")], [("x", "posa"), ("x", "posb")])
            k.op("dve", lambda: vec.tensor_single_scalar(out=posi[:, 1, :], in_=posv, scalar=15, op=ALU.bitwise_and),
                 [("x", "posi1")], [("x", "posa"), ("x", "posb")])
            k.op("dve", lambda: vec.tensor_copy(out=posf[:], in_=posi[:]), [posf[:]], [("x", "posi0"), ("x", "posi1")])
            yield
            for p_ in range(2):
                k.op("dve", lambda: vec.tensor_tensor(out=eq, in0=V(posf[:, p_, :], (1, 128), (0, 16)),
                                                      in1=V(g.iota[:, 0:16], (0, 128), (1, 16)), op=ALU.is_equal),
                     [wk[:]], [posf[:], g.cst[:]])
                yield
                k.op("pool", lambda: pool.tensor_tensor(out=eq.rearrange("p (h r) i -> p h r i", h=8),
                                                        in0=eq.rearrange("p (h r) i -> p h r i", h=8),
                                                        in1=V(idxf[:, p_, :], (32, 8), (0, 16), (1, 16)), op=ALU.mult),
                     [wk[:]], [wk[:], idxf[:]])
                k.op("dve", lambda: vec.tensor_reduce(out=sel[:, p_, :], in_=eq, axis=AX.X, op=ALU.add),
                     [("x", ("sel", p_))], [wk[:]])
                yield
            for q in range(3):
                k.op("pe", lambda: pe.transpose(psX[:, q * 128:(q + 1) * 128], sel[:, q, :], g.ident), [psX[:]],
                     [("x", ("sel", 0)), ("x", ("sel", 1)), ("x", "sel2"), g.cst[:]], inc=(q == 2))
            sT = selT[par][ti]
            k.op("act", lambda: act.copy(out=sT[:], in_=psX[:, 0:384].rearrange("p (a b) -> p a b", a=3)), [sT[:]], [psX[:]])
            yield

    def A2(gi):
        par = gi % 2
        tiles = group_tiles(gi)
        nq = 0
        for ti, t in enumerate(tiles):
            sT = selT[par][ti]
            for b in range(128 // TB):
                A = ohA[b % 2]
                B = ohB[b % 2]
                t0 = b * TB
                for tt in range(TB):
                    k.op("dve", lambda: vec.tensor_scalar(out=A[:, tt, :], in0=iotab[:], scalar1=sT[:, 0, t0 + tt:t0 + tt + 1],
                                                          scalar2=sT[:, 2, t0 + tt:t0 + tt + 1], op0=ALU.is_equal, op1=ALU.mult),
                         [A[:]], [sT[:], iotab[:]])
                    if False:
                        k.op("pool", lambda: pool.tensor_scalar(out=B[:, tt, :], in0=iotab[:], scalar1=sT[:, 1, t0 + tt:t0 + tt + 1],
                                                                scalar2=None, op0=ALU.is_equal), [("x", ("ohB", b % 2, "p"))],
                             [sT[:], iotab[:], ("x", ("ohBr", b % 2))])
                    else:
                        k.op("dve", lambda: vec.tensor_scalar(out=B[:, tt, :], in0=iotab[:], scalar1=sT[:, 1, t0 + tt:t0 + tt + 1],
                                                              scalar2=None, op0=ALU.is_equal), [("x", ("ohB", b % 2, "d"))],
                             [sT[:], iotab[:], ("x", ("ohBr", b % 2))])
                for q in range(TB // 4):
                    ps = psA[nq % 2]
                    nq += 1
                    for u in range(4):
                        tt = q * 4 + u
                        k.op("pe", lambda: pe.matmul(ps[:, u * 128:(u + 1) * 128], lhsT=B[:, tt, :], rhs=A[:, tt, :], start=True,
                                                     stop=True), [ps[:], ("x", ("ohBr", b % 2))],
                             [A[:], ("x", ("ohB", b % 2, "p")), ("x", ("ohB", b % 2, "d"))], inc=(u == 3))
                    tg0 = ti * 128 + t0 + q * 4
                    k.op("act", lambda: act.copy(out=gbuf[:, tg0:tg0 + 4, :], in_=ps[:].rearrange("p (a b) -> p a b", a=4)),
                         [("x", "gbufW")], [ps[:], ("x", "gbufR"), ("x", "gbufR2")])

    def B(gi, nxt):
        par = gi % 2
        tiles = group_tiles(gi)
        ntg = len(tiles)
        ntok = ntg * 128
        hT_ = h2T[par]

        def load_w(cb):
            b = (cb // CB) % NWB
            k.dma("sp", utb[b][:], g.d_utb[li][cb:cb + CB].rearrange("c p f -> p c f"), [utb[b][:]], [("x", ("wb", li))])
            k.dma("sp", vbb[b][:], g.d_vb[li][cb:cb + CB].rearrange("c p f -> p c f"), [vbb[b][:]], [("x", ("wb", li))])

        def umm(c):
            b = (c // CB) % NWB
            ps = psA[c % 2]
            for kk in range(8):
                k.op("pe", lambda: pe.matmul(ps[:, 0:ntok], lhsT=utb[b][:, c % CB, kk * 128:(kk + 1) * 128],
                                             rhs=hT_[:, kk, 0:ntok], start=(kk == 0), stop=(kk == 7)),
                     [ps[:]], [utb[b][:], hT_[:]], inc=(kk == 7))

        load_w(0)
        load_w(CB)
        umm(0)
        for c in range(128):
            if c % CB == 0 and c + 2 * CB < 128:
                load_w(c + 2 * CB)
            if c + 1 < 128:
                umm(c + 1)
            b = (c // CB) % NWB
            ps = psA[c % 2]
            a_ = actS[c % 2]
            g_ = ga[c % 2]
            k.op("act", lambda: act.activation(out=a_[:, 0:ntok], in_=ps[:, 0:ntok], func=AF.Gelu), [a_[:]], [ps[:]])
            if c % 2 == 0:
                k.op("dve", lambda: vec.tensor_tensor(out=g_[:, 0:ntok], in0=a_[:, 0:ntok], in1=V(gbuf[:, 0, c:c + 1], (128, ntok)),
                                                      op=ALU.mult), [g_[:], ("x", "gbufR")], [a_[:], ("x", "gbufW")])
            else:
                k.op("pool", lambda: pool.tensor_tensor(out=g_[:, 0:ntok], in0=a_[:, 0:ntok], in1=V(gbuf[:, 0, c:c + 1], (128, ntok)),
                                                        op=ALU.mult), [g_[:], ("x", "gbufR2")], [a_[:], ("x", "gbufW")])
            for ti in range(ntg):
                for hf in range(2):
                    k.op("pe", lambda: pe.matmul(psO[ti * 2 + hf][:], lhsT=g_[:, ti * 128:(ti + 1) * 128],
                                                 rhs=vbb[b][:, c % CB, hf * 512:(hf + 1) * 512], start=(c == 0), stop=(c == 127)),
                         [psO[ti * 2 + hf][:]], [g_[:], vbb[b][:]], inc=(ti == ntg - 1 and hf == 1))
            if nxt is not None and c >= 2:
                for _ in range(STEPS):
                    next(nxt, None)
        if nxt is not None:
            for _ in nxt:
                pass
        set_mods_e(0 if tiles[0] < L // 128 else 1)
        for ti, t in enumerate(tiles):
            x = xt[par][ti]
            for hf in range(2):
                sl = slice(hf * 512, (hf + 1) * 512)
                k.op("dve", lambda: vec.tensor_tensor(out=osb[:, sl], in0=psO[ti * 2 + hf][:], in1=GT[:, sl], op=ALU.mult),
                     [osb[:]], [psO[ti * 2 + hf][:], GT[:]])
            k.op("pool", lambda: pool.tensor_tensor(out=osb[:], in0=osb[:], in1=x[:], op=ALU.add), [osb[:]], [osb[:], x[:]])
            if not final:
                k.dma("pool", g.d_xs[t * 128:(t + 1) * 128, :], osb[:], [("x", ("xs", t))], [osb[:]])
            else:
                norm_mod_final(k, g, osb, x, ss2, fg)
                k.dma("pool", g.d_out[t * 128:(t + 1) * 128, :], x[:], [("x", ("out", t))], [x[:]])

    STEPS = 1
    n_a1 = 6 + 24 + 32 * TG * 3 // 3 + 18 * TG
    STEPS = 1
    for _ in A1(0):
        pass
    for gi in range(ngroups):
        A2(gi)
        nxt = A1(gi + 1) if gi + 1 < ngroups else None
        B(gi, nxt)
    k.release(m0)


def norm_mod_final(k, g, x, out, ss, fg):
    nc = k.nc
    vec, act = nc.vector, nc.scalar
    k.op("dve", lambda: vec.scalar_tensor_tensor(out=out[:], in0=x[:], scalar=1.0, in1=x[:], op0=ALU.mult, op1=ALU.mult,
                                                accum_out=ss[:, 0:1]), [out[:], ss[:]], [x[:]])
    k.op("act", lambda: act.activation(out=ss[:, 1:2], in_=ss[:, 0:1], func=AF.Sqrt, scale=1.0 / D, bias=g.epsT), [("x", "fss1")],
         [ss[:]])
    k.op("dve", lambda: vec.reciprocal(out=ss[:, 2:3], in_=ss[:, 1:2]), [("x", "fss2")], [("x", "fss1")])
    k.op("dve", lambda: vec.scalar_tensor_tensor(out=out[:], in0=x[:], scalar=ss[:, 2:3], in1=fg[:], op0=ALU.mult, op1=ALU.mult),
         [out[:]], [x[:], ("x", "fss2"), fg[:]])


def EPS_AP(g):
    return g.epsT[:, 0:1]


def build(phases=("all",), dev=None):
    dev = dev or {}
    k = K()
    nc = k.nc
    g = Ctx()

    k.ext_inputs = []

    def din(name, shape, dt=F32):
        k.ext_inputs.append(name)
        return nc.dram_tensor(name, list(shape), dt, kind="ExternalInput").ap()

    def dint(name, shape, dt=F32):
        return nc.dram_tensor(name, list(shape), dt, kind="Internal").ap()

    g.d_x = din("x", [L, D])
    g.d_ctx = din("ctx", [CTX, D])
    g.d_cst = din("cst", [128, C_NCOL])
    g.d_wq = din("peer_wq", [2, D, 2048])
    g.d_sk = din("peer_sk", [2, 8, 2, 128, 128])
    g.d_ut = din("peer_ut", [2, 128, 128, 1024])
    g.d_v = din("peer_v", [2, 128, 128, 1024])
    g.d_fg = din("final_g", [D])
    g.d_cT = din("cT", [128, 8, 2])
    g.d_adaw = din("ada_w", [2, D, 6144])
    g.d_adab = din("ada_b", [2, 6144])
    g.d_n1g = din("norm1_g", [2, D])
    g.d_n2g = din("norm2_g", [2, D])
    g.d_cst2 = din("cst2", [16, NTOK])
    g.d_gwin = din("gdn_w_in", [D, 4128])
    g.d_gconv = din("gdn_conv", [24, 128, 5])
    g.d_galog = din("gdn_a_log", [2, 8])
    g.d_gdtb = din("gdn_dt_bias", [2, 8])
    g.d_gng = din("gdn_norm_g", [128])
    g.d_gwout = din("gdn_w_out", [D, D])
    g.d_swin = din("swa_w_in", [D, 1536])
    g.d_swout = din("swa_w_out", [D, D])
    g.d_ssink = din("swa_sinks", [16])
    g.d_rope = din("rope", [2, L, 64])
    g.d_hT = dint("hT", [NT, 128, 8, 128], BF16)
    g.d_qkv = dint("qkv", [24, 128, NTOK], BF16)
    g.d_rows = dint("rows", [2, 16, NTOK])
    g.d_cd = dint("cd", [16, NCH])
    g.d_o = dint("gdn_o", [2, NTOK, D])
    if dev.get("mod_ext"):
        g.d_mod = din("mod", [2, 2, 6, D])
    else:
        g.d_mod = dint("mod", [2, 2, 6, D])
    g.d_xs = dint("xs", [NTOK, D])
    g.d_utb = dint("utb", [2, 128, 128, 1024], BF16)
    g.d_vb = dint("vb", [2, 128, 128, 1024], BF16)
    g.d_wqb = dint("wqb", [2, 4, 128, 8, 512], BF16)
    g.d_out = nc.dram_tensor("out", [L, D], F32, kind="ExternalOutput").ap()
    if dev.get("xs_out"):
        g.d_xso = nc.dram_tensor("xs_out", [NTOK, D], F32, kind="ExternalOutput").ap()

    load_consts(k, g)
    peer_prepass_early(k, g, [ph[1] for ph in phases if ph[0] == "peer"])
    for r0 in range(0, L, 512):
        k.dma("sp", g.d_xs[r0:r0 + 512, :], g.d_x[r0:r0 + 512, :], [("x", ("xs", r0 // 128 + i)) for i in range(4)], [])
    k.dma("sp", g.d_xs[L:NTOK, :], g.d_ctx, [("x", ("xs", 32)), ("x", ("xs", 33))], [])

    for ph in phases:
        if ph[0] == "adaln":
            adaln_phase(k, g)
        elif ph[0] == "gdn":
            m_ = k.mark()
            build_hT(k, g, 0, NT)
            gdn_rows(k, g)
            gdn_qkv(k, g)
            gdn_scan(k, g)
            k.release(m_)
            gdn_out(k, g, 0, NT)
        elif ph[0] == "swa":
            build_hT(k, g, 1, NT)
            swa_phase(k, g, 1)
        elif ph[0] == "peer":
            _, li, ntiles, final = ph
            k.st[("wb", li)] = [k.pp_tok[li], {}]
            peer_layer(k, g, li, ntiles, final)
    if dev.get("mod_out"):
        k.barrier()
        d_mo = nc.dram_tensor("mod_out", [2, 2, 6, D], F32, kind="ExternalOutput").ap()
        k.dma("sp", d_mo.rearrange("a b c d -> (a b c) d"), g.d_mod.rearrange("a b c d -> (a b c) d"), [("x", "modo")], [])
    if dev.get("xs_out"):
        k.barrier()
        for r0 in range(0, NTOK, 256):
            k.dma("sp", g.d_xso[r0:r0 + 256, :], g.d_xs[r0:r0 + 256, :], [("x", ("xso", r0))], [])
    k.finish()
    return k


def adaln_phase(k, g):
    nc = k.nc
    m = k.mark()
    cT = k.sb([128, 8, 2], F32, "cT")
    k.dma("sp", cT[:], g.d_cT, [cT[:]], [])
    sc = k.sb([128, 8, 2], F32, "scT")
    k.op("act", lambda: nc.scalar.activation(out=sc[:], in_=cT[:], func=AF.Silu), [sc[:]], [cT[:]])
    bb = k.sb([2, 6144], F32, "adab")
    msb = k.sb([2, 6144], F32, "adam")
    n1 = k.sb([2, 1024], F32, "n1g")
    n2 = k.sb([2, 1024], F32, "n2g")
    o1 = k.sb([2, 1024], F32, "ado1")
    o2 = k.sb([2, 1024], F32, "ado2")
    wb = [k.sb([128, 8, 512], F32, "adaw%d" % i) for i in range(2)]
    for li in range(2):
        bcast_row_load(k, bb[:], g.d_adab[li], 6144)
        bcast_row_load(k, n1[:], g.d_n1g[li], 1024)
        bcast_row_load(k, n2[:], g.d_n2g[li], 1024)
        for n in range(12):
            w = wb[n % 2]
            k.dma("sp", w[:], g.d_adaw[li][:, n * 512:(n + 1) * 512].rearrange("(k p) f -> p k f", p=128), [w[:]], [])
            ps = g.psb[n % 2]
            for kk in range(8):
                k.op("pe", lambda: nc.tensor.matmul(ps[0:2, :], lhsT=sc[:, kk, :], rhs=w[:, kk, :], start=(kk == 0), stop=(kk == 7)),
                     [ps[:]], [sc[:], w[:]], inc=(kk == 7))
            k.op("dve", lambda: nc.vector.tensor_tensor(out=msb[:, n * 512:(n + 1) * 512], in0=ps[0:2, :],
                                                        in1=bb[:, n * 512:(n + 1) * 512], op=ALU.add), [msb[:]], [ps[:], bb[:]])
        k.op("dve", lambda: nc.vector.scalar_tensor_tensor(out=o1[:], in0=msb[:, 1024:2048], scalar=1.0, in1=n1[:], op0=ALU.add,
                                                          op1=ALU.mult), [o1[:]], [msb[:], n1[:]])
        k.op("dve", lambda: nc.vector.scalar_tensor_tensor(out=o2[:], in0=msb[:, 4096:5120], scalar=1.0, in1=n2[:], op0=ALU.add,
                                                          op1=ALU.mult), [o2[:]], [msb[:], n2[:]])
        srcs = [o1[:], msb[:, 0:1024], msb[:, 2048:3072], o2[:], msb[:, 3072:4096], msb[:, 5120:6144]]
        for j in range(6):
            k.dma("sp", g.d_mod[li, :, j, :], srcs[j], [("x", ("mod", li))], [o1[:], o2[:], msb[:]])
    k.release(m)


def norm_mod(k, g, x, hh, ss, GS, SH):
    nc = k.nc
    vec, act, pool = nc.vector, nc.scalar, nc.gpsimd
    k.op("dve", lambda: vec.scalar_tensor_tensor(out=hh[:], in0=x[:], scalar=1.0, in1=x[:], op0=ALU.mult, op1=ALU.mult,
                                                accum_out=ss[:, 0:1]), [hh[:], ss[:]], [x[:]])
    k.op("dve", lambda: vec.tensor_scalar(out=ss[:, 1:2], in0=ss[:, 0:1], scalar1=1.0 / D, scalar2=EPS, op0=ALU.mult, op1=ALU.add),
         [("x", "ss1")], [ss[:]])
    k.op("pool", lambda: pool.tensor_tensor(out=ss[:, 2:3], in0=ss[:, 1:2], in1=g.cst[:, C_EPS + 1:C_EPS + 2], op=ALU.pow),
         [("x", "ss2")], [("x", "ss1"), g.cst[:]])
    k.op("dve", lambda: vec.scalar_tensor_tensor(out=hh[:], in0=x[:], scalar=ss[:, 2:3], in1=GS[:], op0=ALU.mult, op1=ALU.mult),
         [hh[:]], [x[:], ("x", "ss2"), GS[:]])
    k.op("pool", lambda: pool.tensor_tensor(out=hh[:], in0=hh[:], in1=SH[:], op=ALU.add), [hh[:]], [hh[:], SH[:]])


def build_hT(k, g, li, ntiles):
    nc = k.nc
    m = k.mark()
    GS = k.sb([128, 1024], F32, "hgs")
    SH = k.sb([128, 1024], F32, "hsh")
    xt = [k.sb([128, 1024], F32, "hxt%d" % i) for i in range(2)]
    hh = k.sb([128, 1024], F32, "hhh")
    ss = k.sb([128, 4], F32, "hss")
    hTt = [k.sb([128, 8, 128], BF16, "hTt%d" % i) for i in range(2)]
    cur = -1
    for t in range(ntiles):
        r = 0 if t < L // 128 else 1
        if r != cur:
            cur = r
            bcast_row_load(k, GS[:], g.d_mod[li, r, 0], 1024)
            bcast_row_load(k, SH[:], g.d_mod[li, r, 1], 1024)
        x = xt[t % 2]
        k.dma("sp", x[:], g.d_xs[t * 128:(t + 1) * 128, :], [x[:]], [("x", ("xs", t))])
        norm_mod(k, g, x, hh, ss, GS, SH)
        o = hTt[t % 2]
        for kk in range(8):
            ps = g.psb[kk // 4]
            k.op("pe", lambda: nc.tensor.transpose(ps[:, (kk % 4) * 128:(kk % 4 + 1) * 128], hh[:, kk * 128:(kk + 1) * 128], g.ident),
                 [ps[:]], [hh[:], g.cst[:]], inc=(kk % 4 == 3))
            if kk % 4 == 3:
                h0 = kk // 4
                if h0 == 0:
                    k.op("act", lambda: nc.scalar.copy(out=o[:, 0:4, :], in_=ps[:].rearrange("p (a b) -> p a b", a=4)), [o[:]], [ps[:]])
                else:
                    k.op("dve", lambda: nc.vector.tensor_copy(out=o[:, 4:8, :], in_=ps[:].rearrange("p (a b) -> p a b", a=4)),
                         [o[:]], [ps[:]])
        k.dma("pool", g.d_hT[t], o[:], [("x", ("hT", t))], [o[:]])
    k.release(m)


NCH = NTOK // 64


def gdn_rows(k, g):
    nc = k.nc
    vec, act, pe = nc.vector, nc.scalar, nc.tensor
    g.TM = k.sb([64, NCH, 4, 16], F32, "gdnTM")
    m = k.mark()
    wab = k.sb([128, 8, 32], BF16, "wab")
    for kk in range(8):
        k.dma("pool", wab[:, kk, :], g.d_gwin[kk * 128:(kk + 1) * 128, 4096:4128], [wab[:]], [])
    alog = k.sb([16, 4], F32, "alog")
    k.dma("sp", alog[:, 0:1], g.d_galog.rearrange("d (h o) -> (d h) o", o=1), [alog[:]], [])
    k.dma("sp", alog[:, 1:2], g.d_gdtb.rearrange("d (h o) -> (d h) o", o=1), [alog[:]], [])
    k.op("act", lambda: act.activation(out=alog[:, 2:3], in_=alog[:, 0:1], func=AF.Exp), [("x", "alog2")], [alog[:]])
    k.op("dve", lambda: vec.tensor_scalar(out=alog[:, 3:4], in0=alog[:, 2:3], scalar1=-1.0, scalar2=None, op0=ALU.mult),
         [("x", "alog3")], [("x", "alog2")])
    rows = {nm: k.sb([16, NTOK], F32, "row_" + nm) for nm in ("g", "beta", "Gf", "Gb", "eG", "kdf")}
    cm = k.sb([16, NTOK], F32, "row_cm")
    k.dma("sp", cm[:], g.d_cst2, [cm[:]], [])
    hb = [k.sb([128, 4, 8, 128], BF16, "rhb%d" % i) for i in range(2)]
    for blk in range((NT + 3) // 4):
        t0 = blk * 4
        nt = min(4, NT - t0)
        h = hb[blk % 2]
        k.dma("sp", h[:, 0:nt], g.d_hT[t0:t0 + nt].rearrange("t p k n -> p t k n"), [h[:]], [("x", ("hT", t0 + i)) for i in range(nt)])
        psa, psb_ = g.psb[(blk % 2) * 2], g.psb[(blk % 2) * 2 + 1]
        for half, ps in ((0, psa), (1, psb_)):
            for t in range(nt):
                for kk in range(8):
                    k.op("pe", lambda: pe.matmul(ps[0:16, t * 128:(t + 1) * 128], lhsT=wab[:, kk, half * 16:(half + 1) * 16],
                                                 rhs=h[:, t, kk, :], start=(kk == 0), stop=(kk == 7)), [ps[:]], [wab[:], h[:]],
                         inc=(kk == 7 and t == nt - 1))
        sl = slice(t0 * 128, (t0 + nt) * 128)
        w = nt * 128
        k.op("act", lambda: act.activation(out=rows["g"][:, sl], in_=psa[0:16, 0:w], func=AF.Exp, bias=alog[:, 1:2]),
             [rows["g"][:]], [psa[:], alog[:]])
        k.op("act", lambda: act.activation(out=rows["g"][:, sl], in_=rows["g"][:, sl], func=AF.Ln, bias=1.0), [rows["g"][:]],
             [rows["g"][:]])
        k.op("dve", lambda: vec.tensor_scalar(out=rows["g"][:, sl], in0=rows["g"][:, sl], scalar1=alog[:, 3:4], scalar2=None,
                                              op0=ALU.mult), [rows["g"][:]], [rows["g"][:], ("x", "alog3")])
        k.op("act", lambda: act.activation(out=rows["beta"][:, sl], in_=psb_[0:16, 0:w], func=AF.Sigmoid), [rows["beta"][:]],
             [psb_[:]])
    G, Gf, Gb, B, eG, kdf = rows["g"], rows["Gf"], rows["Gb"], rows["beta"], rows["eG"], rows["kdf"]
    k.op("dve", lambda: vec.tensor_tensor_scan(out=Gf[:], data0=cm[:], data1=G[:], initial=0.0, op0=ALU.mult, op1=ALU.add),
         [Gf[:]], [cm[:], G[:]])
    tot = V(Gf[:, 63:64], (64, NCH), (0, 64))
    v3 = lambda t: t[:].rearrange("p (c j) -> p c j", j=64)
    k.op("dve", lambda: vec.tensor_tensor(out=v3(Gb), in0=tot, in1=v3(Gf), op=ALU.subtract), [Gb[:]], [Gf[:]])
    k.op("dve", lambda: vec.tensor_tensor(out=Gb[:], in0=Gb[:], in1=G[:], op=ALU.add), [Gb[:]], [Gb[:], G[:]])
    k.op("dve", lambda: vec.tensor_tensor(out=Gb[:], in0=Gb[:], in1=Gf[:], op=ALU.subtract), [Gb[:]], [Gb[:], Gf[:]])
    k.op("dve", lambda: vec.scalar_tensor_tensor(out=Gb[:], in0=Gb[:], scalar=g.cst[0:16, C_DSELB:C_DSELB + 1], in1=Gf[:],
                                                op0=ALU.mult, op1=ALU.add), [Gb[:]], [Gb[:], Gf[:], g.cst[:]])
    GD = Gb
    k.op("act", lambda: act.activation(out=eG[:], in_=GD[:], func=AF.Exp), [eG[:]], [GD[:]])
    k.op("dve", lambda: vec.tensor_tensor(out=v3(kdf), in0=tot, in1=v3(GD), op=ALU.subtract), [kdf[:]], [Gf[:], GD[:]])
    k.op("act", lambda: act.activation(out=kdf[:], in_=kdf[:], func=AF.Exp), [kdf[:]], [kdf[:]])
    cd = k.sb([16, NCH], F32, "row_cd")
    k.op("act", lambda: act.activation(out=cd[:], in_=V(Gf[:, 63:64], (64, NCH)), func=AF.Exp), [cd[:]], [Gf[:]])
    k.dma("sp", g.d_rows[0], GD[:], [("x", "rows")], [GD[:]])
    k.dma("sp", g.d_rows[1], B[:], [("x", "rows")], [B[:]])
    k.dma("sp", g.d_cd, cd[:], [("x", "rows")], [cd[:]])
    for c in range(NCH):
        ps = g.psb[4 + (c // 8) % 2]
        for q, src in enumerate((GD, B, eG, kdf)):
            col = (c % 8) * 64 + q * 16
            k.op("pe", lambda: pe.transpose(ps[0:64, col:col + 16], src[:, c * 64:(c + 1) * 64], g.ident[0:16, 0:16]), [ps[:]],
                 [src[:], g.cst[:]], inc=(q == 3 and (c % 8 == 7 or c == NCH - 1)))
        if c % 8 == 7 or c == NCH - 1:
            c0 = (c // 8) * 8
            n = c - c0 + 1
            k.op("act", lambda: act.copy(out=g.TM[:, c0:c0 + n].rearrange("p c q d -> p (c q d)"), in_=ps[0:64, 0:n * 64]),
                 [g.TM[:]], [ps[:]])
    k.release(m)


def gdn_qkv(k, g):
    nc = k.nc
    vec, act, pe, pool = nc.vector, nc.scalar, nc.tensor, nc.gpsimd
    m = k.mark()
    PW = NTOK + 8
    Ps = [k.sb([128, PW], F32, "gP%d" % i) for i in range(2)]
    for P in Ps:
        k.op("pool", lambda: pool.memset(P[:], 0.0), [P[:]], [])
    R = [k.sb([128, NTOK], F32, "gR%d" % i) for i in range(2)]
    RB = [k.sb([128, NTOK], BF16, "gRB%d" % i) for i in range(2)]
    cw = k.sb([128, 24, 5], F32, "gcw")
    k.dma("sp", cw[:], g.d_gconv.rearrange("f p j -> p f j"), [cw[:]], [])
    wf = [k.sb([128, 8, 128], BF16, "gwf%d" % i) for i in range(2)]
    hb = [k.sb([128, 4, 8, 128], BF16, "ghb%d" % i) for i in range(2)]
    sq = [k.sb([128, 512], F32, "gsq%d" % i) for i in range(2)]
    rn = [k.sb([128, 512], F32, "grn%d" % i) for i in range(2)]
    nblk = (NT + 3) // 4
    for fc in range(24):
        w = wf[fc % 2]
        P = Ps[fc % 2]
        for kk in range(8):
            k.dma("pool", w[:, kk, :], g.d_gwin[kk * 128:(kk + 1) * 128, fc * 128:(fc + 1) * 128], [w[:]], [])
        for blk in range(nblk):
            t0 = blk * 4
            nt = min(4, NT - t0)
            h = hb[blk % 2]
            k.dma("sp", h[:, 0:nt], g.d_hT[t0:t0 + nt].rearrange("t p k n -> p t k n"), [h[:]],
                  [("x", ("hT", t0 + i)) for i in range(nt)])
            ps = g.psb[blk % 2]
            for t in range(nt):
                for kk in range(8):
                    k.op("pe", lambda: pe.matmul(ps[:, t * 128:(t + 1) * 128], lhsT=w[:, kk, :], rhs=h[:, t, kk, :], start=(kk == 0),
                                                 stop=(kk == 7)), [ps[:]], [w[:], h[:]], inc=(kk == 7 and t == nt - 1))
            off = t0 * 128 + (2 if t0 < 32 else 6)
            k.op("act", lambda: act.copy(out=P[:, off:off + nt * 128], in_=ps[:, 0:nt * 128]), [P[:]], [ps[:]])
        r = R[fc % 2]
        for (o0, n, p0) in ((0, L, 0), (L, CTX, L + 4)):
            k.op("dve", lambda: vec.tensor_scalar(out=r[:, o0:o0 + n], in0=P[:, p0:p0 + n], scalar1=cw[:, fc, 0:1], scalar2=None,
                                                  op0=ALU.mult), [r[:]], [P[:], cw[:]])
            for j in range(1, 5):
                k.op("dve", lambda: vec.scalar_tensor_tensor(out=r[:, o0:o0 + n], in0=P[:, p0 + j:p0 + j + n], scalar=cw[:, fc, j:j + 1],
                                                            in1=r[:, o0:o0 + n], op0=ALU.mult, op1=ALU.add), [r[:]], [P[:], cw[:], r[:]])
        rb = RB[fc % 2]
        if fc >= 16:
            k.op("act", lambda: act.activation(out=rb[:], in_=r[:], func=AF.Silu), [rb[:]], [r[:]])
        else:
            k.op("act", lambda: act.activation(out=r[:], in_=r[:], func=AF.Silu), [r[:]], [r[:]])
        if fc < 16:
            scl = (128.0 ** -0.5) if fc < 8 else 1.0
            for b in range((NTOK + 511) // 512):
                c0 = b * 512
                n = min(512, NTOK - c0)
                s_, r_ = sq[b % 2], rn[b % 2]
                ps = g.psb[2 + b % 2]
                k.op("pool", lambda: pool.tensor_tensor(out=s_[:, 0:n], in0=r[:, c0:c0 + n], in1=r[:, c0:c0 + n], op=ALU.mult), [s_[:]],
                     [r[:]])
                k.op("pe", lambda: pe.matmul(ps[:, 0:n], lhsT=g.ones, rhs=s_[:, 0:n], start=True, stop=True), [ps[:]], [s_[:], g.cst[:]])
                k.op("act", lambda: act.activation(out=r_[:, 0:n], in_=ps[:, 0:n], func=AF.Sqrt, bias=g.epsT), [r_[:]], [ps[:], g.cst[:]])
                k.op("dve", lambda: vec.reciprocal(out=r_[:, 0:n], in_=r_[:, 0:n]), [r_[:]], [r_[:]])
                k.op("dve", lambda: vec.scalar_tensor_tensor(out=rb[:, c0:c0 + n], in0=r[:, c0:c0 + n], scalar=scl, in1=r_[:, 0:n],
                                                            op0=ALU.mult, op1=ALU.mult), [rb[:]], [r[:], r_[:]])
        k.dma("pool", g.d_qkv[fc], rb[:], [("x", ("qkv", fc))], [rb[:]])
    k.release(m)


def gdn_scan(k, g):
    nc = k.nc
    vec, act, pe, pool = nc.vector, nc.scalar, nc.tensor, nc.gpsimd
    m = k.mark()
    BC = 8
    BW = BC * 64
    id64 = g.cst[0:64, C_ID:C_ID + 64]
    TM = g.TM
    masks = {nm: g.cst[0:64, c0:c0 + 64] for nm, c0 in (("ls", C_NMLS), ("us", C_NMUS), ("li", C_NMLI), ("ui", C_NMUI))}
    idb = k.sb([128, 128], BF16, "gidb")
    k.op("dve", lambda: vec.tensor_copy(out=idb[:], in_=g.ident), [idb[:]], [g.cst[:]])

    class BS:
        pass

    sets = []
    for d in range(2):
        b_ = BS()
        n_ = "d%d" % d
        b_.qTb = k.sb([128, BW], BF16, "qTb" + n_)
        b_.kTb = k.sb([128, BW], BF16, "kTb" + n_)
        b_.vTb = k.sb([128, BW], BF16, "vTb" + n_)
        b_.grow = k.sb([64, BC, 64], F32, "grow" + n_)
        b_.brow = k.sb([64, BC, 64], F32, "brow" + n_)
        b_.cdb = k.sb([128, BC], F32, "cdb" + n_)
        b_.ktm = k.sb([64, BC, 128], BF16, "ktm" + n_)
        b_.vtm = k.sb([64, BC, 128], BF16, "vtm" + n_)
        b_.kdec = k.sb([64, BC, 128], BF16, "kdec" + n_)
        b_.dif = k.sb([64, BC, 64], F32, "dif" + n_)
        b_.E = [k.sb([64, BC, 64], F32, "gE%d%s" % (i, n_)) for i in range(3)]
        b_.Y = [k.sb([64, BC, 64], BF16, "gY%d%s" % (i, n_)) for i in range(2)]
        b_.YT = [k.sb([64, BC, 64], BF16, "gYT%d%s" % (i, n_)) for i in range(2)]
        b_.PT = [k.sb([64, BC, 64], BF16, "gPT%d%s" % (i, n_)) for i in range(2)]
        b_.AQ = k.sb([64, BC, 64], BF16, "gAQ" + n_)
        b_.ob = k.sb([64, BC, 128], F32, "gob" + n_)
        b_.S = k.sb([128, 128], F32, "gS" + n_)
        b_.Sb = k.sb([128, 128], BF16, "gSb" + n_)
        b_.t2a = k.sb([64, 128], F32, "gt2a" + n_)
        b_.t2b = k.sb([64, 128], BF16, "gt2b" + n_)
        b_.vnew = k.sb([64, 128], BF16, "gvnew" + n_)
        b_.t3 = k.sb([64, 128], F32, "gt3" + n_)
        b_.pb = [g.psb[4 * d + i] for i in range(4)]
        sets.append(b_)

    def evac(i, out, in_):
        if i % 2 == 0:
            k.op("act", lambda: act.copy(out=out, in_=in_), [out], [in_])
        else:
            k.op("dve", lambda: vec.tensor_copy(out=out, in_=in_), [out], [in_])

    def run(h, d):
        B_ = sets[d]
        qTb, kTb, vTb, grow, brow, cdb, ktm, vtm, kdec, dif = (B_.qTb, B_.kTb, B_.vTb, B_.grow, B_.brow, B_.cdb, B_.ktm, B_.vtm,
                                                               B_.kdec, B_.dif)
        E, Y, YT, PT, AQ, ob, S, Sb, t2a, t2b, vnew, t3, pb = (B_.E, B_.Y, B_.YT, B_.PT, B_.AQ, B_.ob, B_.S, B_.Sb, B_.t2a, B_.t2b,
                                                              B_.vnew, B_.t3, B_.pb)
        dh = d * 8 + h
        lat = [(c0, 8) for c0 in range(0, 64, 8)]
        batches = [(64, 4)] + (lat if d == 0 else lat[::-1])
        k.op("pool", lambda: pool.memset(S[:], 0.0), [S[:]], [])
        k.op("pool", lambda: pool.memset(Sb[:], 0.0), [Sb[:]], [])

        def rk(r):
            return ("x", ("sc", d, r))

        for (c0, nch) in batches:
            tok0 = c0 * 64
            W = nch * 64
            k.dma("sp", qTb[:, 0:W], g.d_qkv[h][:, tok0:tok0 + W], [qTb[:]], [("x", ("qkv", h))])
            k.dma("sp", kTb[:, 0:W], g.d_qkv[8 + h][:, tok0:tok0 + W], [kTb[:]], [("x", ("qkv", 8 + h))])
            k.dma("sp", vTb[:, 0:W], g.d_qkv[16 + h][:, tok0:tok0 + W], [vTb[:]], [("x", ("qkv", 16 + h))])
            for dst, ri in ((grow, 0), (brow, 1)):
                src = g.d_rows[ri, dh, tok0:tok0 + W]
                k.dma("sp", dst[:, 0:nch].rearrange("p c j -> p (c j)"), bass.AP(src.tensor, src.offset, [[0, 64], [1, W]]),
                      [dst[:]], [("x", "rows")])
            srcc = g.d_cd[dh, c0:c0 + nch]
            k.dma("sp", cdb[:, 0:nch], bass.AP(srcc.tensor, srcc.offset, [[0, 128], [1, nch]]), [cdb[:]], [("x", "rows")])
            yield

            def tmc(q, inner):
                return V(TM[:, c0, q, dh:dh + 1], (64, nch), (0, inner))

            for si, (src, dst) in enumerate(((kTb, ktm), (vTb, vtm))):
                psv = pb[si][:].bitcast(BF16)
                for c in range(nch):
                    k.op("pe", lambda: pe.transpose(psv[0:64, c * 128:(c + 1) * 128], src[:, c * 64:(c + 1) * 64], idb[:]), [pb[si][:]],
                         [src[:], idb[:]], inc=(c == nch - 1))
                evac(si, dst[:, 0:nch].rearrange("p c f -> p (c f)"), psv[0:64, 0:nch * 128])
                yield
            k.op("dve", lambda: vec.tensor_tensor(out=kdec[:, 0:nch], in0=ktm[:, 0:nch], in1=tmc(3, 128), op=ALU.mult), [kdec[:]],
                 [ktm[:], TM[:]])
            yield
            for c in range(nch):
                k.op("pe", lambda: pe.matmul(pb[1][0:64, c * 64:(c + 1) * 64], lhsT=kTb[:, c * 64:(c + 1) * 64],
                                             rhs=kTb[:, c * 64:(c + 1) * 64], start=True, stop=True), [pb[1][:]], [kTb[:]],
                     inc=(c == nch - 1))
            for c in range(nch):
                k.op("pe", lambda: pe.matmul(pb[2][0:64, c * 64:(c + 1) * 64], lhsT=kTb[:, c * 64:(c + 1) * 64],
                                             rhs=qTb[:, c * 64:(c + 1) * 64], start=True, stop=True), [pb[2][:]],
                     [kTb[:], qTb[:]], inc=(c == nch - 1))
            yield
            k.op("dve", lambda: vec.tensor_tensor(out=dif[:, 0:nch], in0=grow[:, 0:nch], in1=tmc(0, 64), op=ALU.subtract), [dif[:]],
                 [grow[:], TM[:]])
            mk = ("ls", "us", "ui") if d == 0 else ("us", "ls", "li")
            for i in range(3):
                mv = V(masks[mk[i]], (0, nch), (1, 64))
                k.op("dve", lambda: vec.tensor_tensor(out=E[i][:, 0:nch], in0=dif[:, 0:nch], in1=mv,
                                                      op=(ALU.subtract if i == 0 else ALU.add)), [E[i][:]], [dif[:], g.cst[:]])
                k.op("act", lambda: act.activation(out=E[i][:, 0:nch], in_=E[i][:, 0:nch], func=AF.Exp,
                                                   scale=(-1.0 if i == 0 else 1.0)), [E[i][:]], [E[i][:]])
                yield
            cs = slice(0, nch)
            kk3 = pb[1][0:64, 0:nch * 64].rearrange("p (c j) -> p c j", j=64)
            qk3 = pb[2][0:64, 0:nch * 64].rearrange("p (c j) -> p c j", j=64)
            k.op("dve", lambda: vec.tensor_tensor(out=E[0][:, cs], in0=kk3, in1=E[0][:, cs], op=ALU.mult), [E[0][:]],
                 [pb[1][:], E[0][:]])
            k.op("dve", lambda: vec.scalar_tensor_tensor(out=Y[0][:, cs], in0=E[0][:, cs], scalar=-1.0, in1=tmc(1, 64),
                                                        op0=ALU.mult, op1=ALU.mult), [Y[0][:]], [E[0][:], TM[:]])
            yield
            k.op("dve", lambda: vec.tensor_tensor(out=E[1][:, cs], in0=kk3, in1=E[1][:, cs], op=ALU.mult), [E[1][:]],
                 [pb[1][:], E[1][:]])
            k.op("dve", lambda: vec.scalar_tensor_tensor(out=YT[0][:, cs], in0=E[1][:, cs], scalar=-1.0, in1=brow[:, cs],
                                                        op0=ALU.mult, op1=ALU.mult), [YT[0][:]], [E[1][:], brow[:]])
            yield
            k.op("dve", lambda: vec.tensor_tensor(out=AQ[:, cs], in0=qk3, in1=E[2][:, cs], op=ALU.mult), [AQ[:]],
                 [pb[2][:], E[2][:]])
            k.op("pool", lambda: pool.tensor_tensor(out=PT[0][:, 0:nch], in0=YT[0][:, 0:nch], in1=V(id64, (0, nch), (1, 64)),
                                                    op=ALU.add), [PT[0][:]], [YT[0][:], g.cst[:]])
            yield
            cy, cp = 0, 0
            for lvl in range(5):
                ny = 1 - cy
                for c in range(nch):
                    k.op("pe", lambda: pe.matmul(pb[0][0:64, c * 64:(c + 1) * 64], lhsT=YT[cy][:, c, :], rhs=Y[cy][:, c, :],
                                                 start=True, stop=True), [pb[0][:]], [YT[cy][:], Y[cy][:]], inc=(c == nch - 1))
                evac(0, Y[ny][:, 0:nch].rearrange("p c j -> p (c j)"), pb[0][0:64, 0:nch * 64])
                yield
                if lvl < 4:
                    for c in range(nch):
                        k.op("pe", lambda: pe.matmul(pb[1][0:64, c * 64:(c + 1) * 64], lhsT=Y[cy][:, c, :], rhs=YT[cy][:, c, :],
                                                     start=True, stop=True), [pb[1][:]], [YT[cy][:], Y[cy][:]], inc=(c == nch - 1))
                    evac(1, YT[ny][:, 0:nch].rearrange("p c j -> p (c j)"), pb[1][0:64, 0:nch * 64])
                    yield
                np_ = 1 - cp
                for c in range(nch):
                    k.op("pe", lambda: pe.matmul(pb[2][0:64, c * 64:(c + 1) * 64], lhsT=Y[ny][:, c, :], rhs=PT[cp][:, c, :],
                                                 start=True, stop=True), [pb[2][:]], [Y[ny][:], PT[cp][:]], inc=(c == nch - 1))
                k.op("dve", lambda: vec.tensor_tensor(out=PT[np_][:, 0:nch].rearrange("p c j -> p (c j)"), in0=pb[2][0:64, 0:nch * 64],
                                                      in1=PT[cp][:, 0:nch].rearrange("p c j -> p (c j)"), op=ALU.add),
                     [PT[np_][:]], [pb[2][:], PT[cp][:]])
                yield
                cy, cp = ny, np_
            TT = PT[cp]
            order = range(nch) if d == 0 else range(nch - 1, -1, -1)
            R = pb[3]
            for c in order:
                gc = c0 + c
                eGc = TM[:, gc, 2, dh:dh + 1]
                bc_ = TM[:, gc, 1, dh:dh + 1]
                k.op("pe", lambda: pe.matmul(R[0:64, 0:128], lhsT=kTb[:, c * 64:(c + 1) * 64], rhs=Sb[:], start=True, stop=True),
                     [rk(0)], [kTb[:], Sb[:]])
                k.op("pe", lambda: pe.matmul(R[0:64, 384:512], lhsT=qTb[:, c * 64:(c + 1) * 64], rhs=Sb[:], start=True, stop=True),
                     [rk(3)], [qTb[:], Sb[:]])
                k.op("dve", lambda: vec.scalar_tensor_tensor(out=t2a[:], in0=R[0:64, 0:128], scalar=eGc, in1=vtm[:, c, :],
                                                            op0=ALU.mult, op1=ALU.subtract), [t2a[:]], [rk(0), TM[:], vtm[:]])
                k.op("dve", lambda: vec.tensor_scalar(out=t2b[:], in0=t2a[:], scalar1=bc_, scalar2=-1.0, op0=ALU.mult, op1=ALU.mult),
                     [t2b[:]], [t2a[:], TM[:]])
                yield
                k.op("pe", lambda: pe.matmul(R[0:64, 128:256], lhsT=TT[:, c, :], rhs=t2b[:], start=True, stop=True), [rk(1)],
                     [TT[:], t2b[:]])
                k.op("act", lambda: act.copy(out=vnew[:], in_=R[0:64, 128:256]), [vnew[:]], [rk(1)])
                yield
                k.op("pe", lambda: pe.matmul(R[:, 256:384], lhsT=kdec[:, c, :], rhs=vnew[:], start=True, stop=True), [rk(2)],
                     [kdec[:], vnew[:]])
                k.op("pe", lambda: pe.matmul(pb[2][0:64, 0:128], lhsT=AQ[:, c, :], rhs=vnew[:], start=True, stop=True), [pb[2][:]],
                     [AQ[:], vnew[:]])
                k.op("dve", lambda: vec.scalar_tensor_tensor(out=S[:], in0=S[:], scalar=cdb[:, c:c + 1], in1=R[:, 256:384],
                                                            op0=ALU.mult, op1=ALU.add), [S[:]], [S[:], cdb[:], rk(2)])
                k.op("act", lambda: act.copy(out=Sb[:], in_=S[:]), [Sb[:]], [S[:]])
                k.op("act", lambda: act.activation(out=t3[:], in_=R[0:64, 384:512], func=AF.Identity, scale=eGc), [t3[:]],
                     [rk(3), TM[:]])
                k.op("dve", lambda: vec.tensor_tensor(out=ob[:, c, :], in0=pb[2][0:64, 0:128], in1=t3[:], op=ALU.add), [ob[:]],
                     [pb[2][:], t3[:]])
                yield
            dsto = g.d_o[d][tok0:tok0 + W, h * 128:(h + 1) * 128].rearrange("(c p) f -> p c f", p=64)
            k.dma("pool", dsto, ob[:, 0:nch], [("x", ("o", d, c0))], [ob[:]])
            yield

    for h in range(8):
        gens = [run(h, 0), run(h, 1)]
        alive = [True, True]
        while any(alive):
            for i in range(2):
                if alive[i]:
                    try:
                        next(gens[i])
                    except StopIteration:
                        alive[i] = False
    k.release(m)


def gdn_out(k, g, li, ntiles):
    nc = k.nc
    vec, act, pe, pool = nc.vector, nc.scalar, nc.tensor, nc.gpsimd
    m = k.mark()
    wz = k.sb([128, 8, 1024], BF16, "wz")
    wo = k.sb([128, 8, 1024], BF16, "wo")
    for kk in range(8):
        k.dma("pool", wz[:, kk, :], g.d_gwin[kk * 128:(kk + 1) * 128, 3072:4096], [wz[:]], [])
        k.dma("pool", wo[:, kk, :], g.d_gwout[kk * 128:(kk + 1) * 128, :], [wo[:]], [])
    ng = k.sb([128, 128], F32, "gng")
    bcast_row_load(k, ng[:], g.d_gng, 128)
    GT = k.sb([128, 1024], F32, "ggt")
    of = [k.sb([128, 1024], F32, "gof%d" % i) for i in range(2)]
    obw = [k.sb([128, 1024], F32, "gobw%d" % i) for i in range(2)]
    xt = [k.sb([128, 1024], F32, "gxt%d" % i) for i in range(2)]
    hT = [k.sb([128, 8, 128], BF16, "ghT%d" % i) for i in range(2)]
    sq = k.sb([128, 1024], F32, "gsq")
    zs = k.sb([128, 1024], F32, "gzs")
    st = k.sb([128, 24], F32, "gst")
    ogT = k.sb([128, 8, 128], BF16, "gogT")
    cur = -1
    for t in range(ntiles):
        r = 0 if t < L // 128 else 1
        if r != cur:
            cur = r
            bcast_row_load(k, GT[:], g.d_mod[li, r, 2], 1024)
        o, o2, x, h = of[t % 2], obw[t % 2], xt[t % 2], hT[t % 2]
        k.dma("sp", o[:], g.d_o[0][t * 128:(t + 1) * 128, :], [o[:]], [("x", ("o", 0, c)) for c in range(0, 72, 8)])
        k.dma("sp", o2[:], g.d_o[1][t * 128:(t + 1) * 128, :], [o2[:]], [("x", ("o", 1, c)) for c in range(0, 72, 8)])
        k.dma("sp", x[:], g.d_xs[t * 128:(t + 1) * 128, :], [x[:]], [("x", ("xs", t))])
        k.dma("sp", h[:], g.d_hT[t], [h[:]], [("x", ("hT", t))])
        for hf in range(2):
            ps = g.psb[hf]
            for kk in range(8):
                k.op("pe", lambda: pe.matmul(ps[:], lhsT=h[:, kk, :], rhs=wz[:, kk, hf * 512:(hf + 1) * 512], start=(kk == 0),
                                             stop=(kk == 7)), [ps[:]], [h[:], wz[:]], inc=(kk == 7))
            k.op("act", lambda: act.activation(out=zs[:, hf * 512:(hf + 1) * 512], in_=ps[:], func=AF.Silu), [zs[:]], [ps[:]])
        k.op("pool", lambda: pool.tensor_tensor(out=o[:], in0=o[:], in1=o2[:], op=ALU.add), [o[:]], [o[:], o2[:]])
        k.op("pool", lambda: pool.tensor_tensor(out=sq[:], in0=o[:], in1=o[:], op=ALU.mult), [sq[:]], [o[:]])
        k.op("dve", lambda: vec.tensor_reduce(out=st[:, 0:8], in_=sq[:].rearrange("p (h f) -> p h f", f=128), axis=AX.X, op=ALU.add),
             [("x", "gst0")], [sq[:]])
        k.op("act", lambda: act.activation(out=st[:, 8:16], in_=st[:, 0:8], func=AF.Sqrt, scale=1.0 / 128, bias=g.epsT), [("x", "gst1")],
             [("x", "gst0"), g.cst[:]])
        k.op("dve", lambda: vec.reciprocal(out=st[:, 16:24], in_=st[:, 8:16]), [("x", "gst2")], [("x", "gst1")])
        o3 = o[:].rearrange("p (h f) -> p h f", f=128)
        k.op("dve", lambda: vec.tensor_tensor(out=o3, in0=o3, in1=V(st[:, 16:17], (1, 8), (0, 128)), op=ALU.mult), [o[:]],
             [o[:], ("x", "gst2")])
        k.op("pool", lambda: pool.tensor_tensor(out=o3, in0=o3, in1=V(ng[:, 0:128], (0, 8), (1, 128)), op=ALU.mult), [o[:]], [o[:], ng[:]])
        k.op("dve", lambda: vec.tensor_tensor(out=o[:], in0=o[:], in1=zs[:], op=ALU.mult), [o[:]], [o[:], zs[:]])
        for kk in range(8):
            ps = g.psb[2 + kk // 4]
            k.op("pe", lambda: pe.transpose(ps[:, (kk % 4) * 128:(kk % 4 + 1) * 128], o[:, kk * 128:(kk + 1) * 128], g.ident), [ps[:]],
                 [o[:], g.cst[:]], inc=(kk % 4 == 3))
            if kk % 4 == 3:
                h0 = kk // 4
                k.op("act", lambda: act.copy(out=ogT[:, h0 * 4:h0 * 4 + 4, :], in_=ps[:].rearrange("p (a b) -> p a b", a=4)), [ogT[:]],
                     [ps[:]])
        for hf in range(2):
            ps = g.psb[4 + hf]
            for kk in range(8):
                k.op("pe", lambda: pe.matmul(ps[:], lhsT=ogT[:, kk, :], rhs=wo[:, kk, hf * 512:(hf + 1) * 512], start=(kk == 0),
                                             stop=(kk == 7)), [ps[:]], [ogT[:], wo[:]], inc=(kk == 7))
            sl = slice(hf * 512, (hf + 1) * 512)
            k.op("dve", lambda: vec.tensor_tensor(out=o2[:, sl], in0=ps[:], in1=GT[:, sl], op=ALU.mult), [o2[:]], [ps[:], GT[:]])
        k.op("pool", lambda: pool.tensor_tensor(out=o2[:], in0=o2[:], in1=x[:], op=ALU.add), [o2[:]], [o2[:], x[:]])
        k.dma("pool", g.d_xs[t * 128:(t + 1) * 128, :], o2[:], [("x", ("xs", t))], [o2[:]])
    k.release(m)


def host_cst2():
    r = np.ones((16, NTOK), np.float32)
    r[:, ::64] = 0.0
    return r


def prep_shared(inp):
    sh = {}
    sh["cst"] = host_consts()
    sh["cst2"] = host_cst2()
    sh["peer_wq"] = np.ascontiguousarray(inp["peer_w_query"], dtype=np.float32)
    sh["peer_sk"] = np.ascontiguousarray(inp["peer_sub_keys"], dtype=np.float32)
    u = np.asarray(inp["peer_u"], dtype=np.float32).reshape(2, 128, 128, 8, 128)
    sh["peer_ut"] = np.ascontiguousarray(u.transpose(0, 1, 4, 3, 2)).reshape(2, 128, 128, 1024)
    sh["peer_v"] = np.ascontiguousarray(inp["peer_v"], dtype=np.float32).reshape(2, 128, 128, 1024)
    sh["final_g"] = np.ascontiguousarray(inp["final_g"], dtype=np.float32)
    sh["ada_w"] = np.ascontiguousarray(inp["ada_w"], dtype=np.float32)
    sh["ada_b"] = np.ascontiguousarray(inp["ada_b"], dtype=np.float32)
    sh["norm1_g"] = np.ascontiguousarray(inp["norm1_g"], dtype=np.float32)
    sh["norm2_g"] = np.ascontiguousarray(inp["norm2_g"], dtype=np.float32)
    sh["gdn_w_in"] = np.ascontiguousarray(inp["gdn_w_in"][0], dtype=np.float32)
    cw = np.asarray(inp["gdn_conv_w"][0], dtype=np.float32)
    sh["gdn_conv"] = np.ascontiguousarray(cw.T.reshape(24, 128, 5))
    sh["gdn_a_log"] = np.ascontiguousarray(inp["gdn_a_log"][0], dtype=np.float32)
    sh["gdn_dt_bias"] = np.ascontiguousarray(inp["gdn_dt_bias"][0], dtype=np.float32)
    sh["gdn_norm_g"] = np.ascontiguousarray(inp["gdn_norm_g"][0], dtype=np.float32)
    sh["gdn_w_out"] = np.ascontiguousarray(inp["gdn_w_out"][0], dtype=np.float32)
    sh["swa_w_in"] = np.ascontiguousarray(inp["swa_w_in"][0], dtype=np.float32)
    sh["swa_w_out"] = np.ascontiguousarray(inp["swa_w_out"][0], dtype=np.float32)
    sh["swa_sinks"] = np.ascontiguousarray(inp["swa_sinks"][0], dtype=np.float32)
    sh["rope"] = host_rope()
    return sh


def prep_core(inp, b):
    pc = {}
    pc["x"] = np.ascontiguousarray(inp["x"][b], dtype=np.float32)
    pc["ctx"] = np.ascontiguousarray(inp["ctx"][b], dtype=np.float32)
    cc = np.stack([np.asarray(inp["c"][b], np.float32), np.asarray(inp["c_ctx"], np.float32)], axis=-1)
    pc["cT"] = np.ascontiguousarray(cc.reshape(8, 128, 2).transpose(1, 0, 2))
    return pc


def host_rope():
    rows = L // 64
    row = np.repeat(np.arange(rows), 64).astype(np.float32)
    col = np.tile(np.arange(64), rows).astype(np.float32)
    inv = (10000.0 ** (-np.arange(0, 32, 2, dtype=np.float32) / 32.0)).astype(np.float32)
    ar = row[:, None] * inv
    ac = col[:, None] * inv
    cos = np.concatenate([np.cos(ar), np.cos(ar), np.cos(ac), np.cos(ac)], axis=1)
    sin = np.concatenate([-np.sin(ar), np.sin(ar), -np.sin(ac), np.sin(ac)], axis=1)
    return np.ascontiguousarray(np.stack([cos, sin]).astype(np.float32))


def swa_phase(k, g, li):
    nc = k.nc
    vec, act, pe, pool = nc.vector, nc.scalar, nc.tensor, nc.gpsimd
    m = k.mark()
    NLT = L // 128
    win = k.sb([128, 8, 1536], BF16, "swin")
    wo = k.sb([128, 8, 1024], BF16, "swo")
    for kk in range(8):
        k.dma("pool", win[:, kk, :], g.d_swin[kk * 128:(kk + 1) * 128, :], [win[:]], [])
        k.dma("pool", wo[:, kk, :], g.d_swout[kk * 128:(kk + 1) * 128, :], [wo[:]], [])
    QT = k.sb([128, 8, L], BF16, "sQT")
    KT = k.sb([128, 4, NTOK], BF16, "sKT")
    V1 = k.sb([128, NT, 4, 65], BF16, "sV1")
    k.op("pool", lambda: pool.memset(V1[:], 1.0), [V1[:]], [])
    idb = k.sb([128, 128], BF16, "sidb")
    nmp = k.sb([128, 128], BF16, "snmp")
    nmn = k.sb([128, 128], BF16, "snmn")
    k.op("dve", lambda: vec.tensor_copy(out=idb[:], in_=g.ident), [idb[:]], [g.cst[:]])
    k.op("dve", lambda: vec.tensor_copy(out=nmp[:], in_=g.cst[:, C_NMP:C_NMP + 128]), [nmp[:]], [g.cst[:]])
    k.op("dve", lambda: vec.tensor_copy(out=nmn[:], in_=g.cst[:, C_NMN:C_NMN + 128]), [nmn[:]], [g.cst[:]])
    esink = k.sb([128, 16], F32, "sesink")
    bcast_row_load(k, esink[:], g.d_ssink, 16)
    k.op("act", lambda: act.activation(out=esink[:], in_=esink[:], func=AF.Exp), [esink[:]], [esink[:]])
    GT = k.sb([128, 1024], F32, "sgt")
    bcast_row_load(k, GT[:], g.d_mod[li, 0, 2], 1024)
    hT = [k.sb([128, 8, 128], BF16, "shT%d" % i) for i in range(2)]
    cs = [k.sb([128, 2, 64], F32, "scs%d" % i) for i in range(2)]
    tmp = k.sb([128, 512], F32, "stmp")
    tmp2 = k.sb([128, 512], F32, "stmp2")
    qr = k.sb([128, 1024], BF16, "sqr")
    kr = k.sb([128, 4, 2, 64], BF16, "skr")
    pb = g.psb
    for t in range(NT):
        lat = t < NLT
        h = hT[t % 2]
        k.dma("sp", h[:], g.d_hT[t], [h[:]], [("x", ("hT", t))])
        if lat:
            c_ = cs[t % 2]
            k.dma("sp", c_[:], g.d_rope[:, t * 128:(t + 1) * 128, :].rearrange("a p e -> p a e"), [c_[:]], [])
        banks = [0, 1, 2] if lat else [2]
        for b in banks:
            for kk in range(8):
                k.op("pe", lambda: pe.matmul(pb[b][:], lhsT=h[:, kk, :], rhs=win[:, kk, b * 512:(b + 1) * 512], start=(kk == 0),
                                             stop=(kk == 7)), [pb[b][:]], [h[:], win[:]], inc=(kk == 7))
        if lat:
            for b, H, dst in ((0, 8, qr[:, 0:512]), (1, 8, qr[:, 512:1024]), (2, 4, None)):
                W = H * 64
                src = pb[b][:, 0:W]
                for hf in range(2):
                    pv = V(pb[b][:, (1 - hf) * 16:(1 - hf) * 16 + 1], (32, 2 * H), (1, 16))
                    sv = V(c_[:, 1, hf * 16:hf * 16 + 1], (0, H), (32, 2), (1, 16))
                    ov = V(tmp[:, hf * 16:hf * 16 + 1], (32, 2 * H), (1, 16))
                    pv4 = V(pb[b][:, (1 - hf) * 16:(1 - hf) * 16 + 1], (64, H), (32, 2), (1, 16))
                    ov4 = V(tmp[:, hf * 16:hf * 16 + 1], (64, H), (32, 2), (1, 16))
                    k.op("dve", lambda: vec.tensor_tensor(out=ov4, in0=pv4, in1=sv, op=ALU.mult), [tmp[:]], [pb[b][:], c_[:]])
                k.op("dve", lambda: vec.tensor_tensor(out=tmp2[:, 0:W].rearrange("p (h e) -> p h e", e=64),
                                                      in0=src.rearrange("p (h e) -> p h e", e=64), in1=V(c_[:, 0, 0:1], (0, H), (1, 64)),
                                                      op=ALU.mult), [tmp2[:]], [pb[b][:], c_[:]])
                if b < 2:
                    k.op("pool", lambda: pool.tensor_tensor(out=dst, in0=tmp[:, 0:W], in1=tmp2[:, 0:W], op=ALU.add), [qr[:]],
                         [tmp[:], tmp2[:]])
                else:
                    for dup in range(2):
                        k.op("pool", lambda: pool.tensor_tensor(out=kr[:, :, dup, :], in0=tmp[:, 0:W].rearrange("p (h e) -> p h e", e=64),
                                                                in1=tmp2[:, 0:W].rearrange("p (h e) -> p h e", e=64), op=ALU.add),
                             [kr[:]], [tmp[:], tmp2[:]])
            psq = pb[3][:].bitcast(BF16)
            for pr in range(8):
                k.op("pe", lambda: pe.transpose(psq[:, pr * 128:(pr + 1) * 128], qr[:, pr * 128:(pr + 1) * 128], idb[:]), [pb[3][:]],
                     [qr[:], idb[:]], inc=(pr == 7))
            k.op("act", lambda: act.copy(out=QT[:, :, t * 128:(t + 1) * 128], in_=psq[:, 0:1024].rearrange("p (a b) -> p a b", a=8)),
                 [QT[:]], [pb[3][:]])
        else:
            for dup in range(2):
                k.op("act", lambda: act.copy(out=kr[:, :, dup, :], in_=pb[2][:, 0:256].rearrange("p (h e) -> p h e", e=64)), [kr[:]],
                     [pb[2][:]])
        psk = pb[4][:].bitcast(BF16)
        for j in range(4):
            k.op("pe", lambda: pe.transpose(psk[:, j * 128:(j + 1) * 128], kr[:, j].rearrange("p a e -> p (a e)"), idb[:]), [pb[4][:]],
                 [kr[:], idb[:]], inc=(j == 3))
        k.op("dve", lambda: vec.tensor_copy(out=KT[:, :, t * 128:(t + 1) * 128], in_=psk[:, 0:512].rearrange("p (a b) -> p a b", a=4)),
             [KT[:]], [pb[4][:]])
        k.op("act", lambda: act.copy(out=V1[:, t, :, 0:64], in_=pb[2][:, 256:512].rearrange("p (a b) -> p a b", a=4)), [V1[:]], [pb[2][:]])
    PT = [k.sb([128, 640], BF16, "sPT%d" % i) for i in range(2)]
    otm = k.sb([128, 1024], BF16, "sotm")
    den = k.sb([128, 8], F32, "sden")
    ogT = k.sb([128, 8, 128], BF16, "sogT")
    xt = [k.sb([128, 1024], F32, "sxt%d" % i) for i in range(2)]
    ysb = k.sb([128, 1024], F32, "sysb")
    psqT = pb[6][:].bitcast(BF16)
    n = 0
    for t in range(NLT):
        x = xt[t % 2]
        k.dma("sp", x[:], g.d_xs[t * 128:(t + 1) * 128, :], [x[:]], [("x", ("xs", t))])
        kbs = ([(t - 1, "p")] if t > 0 else []) + [(t, "c")] + ([(t + 1, "n")] if t < NLT - 1 else []) + [(NLT, "c"), (NLT + 1, "c")]
        nkb = len(kbs)
        for j in range(4):
            pso = pb[4 + j % 2]
            for hh in range(4):
                hq = 4 * j + hh
                pair, u = hq // 2, hq % 2
                sl = slice(u * 64, (u + 1) * 64)
                bs = (n % 2) * 2
                n += 1
                for i, (kt, kind) in enumerate(kbs):
                    dst = pb[bs + i // 4][:, (i % 4) * 128:(i % 4 + 1) * 128]
                    last = kind == "c"
                    k.op("pe", lambda: pe.matmul(dst, lhsT=KT[sl, j, kt * 128:(kt + 1) * 128], rhs=QT[sl, pair, t * 128:(t + 1) * 128],
                                                 start=True, stop=last), [pb[bs + i // 4][:]], [KT[:], QT[:]],
                         inc=(last and (i == nkb - 1 or i == 3)))
                    if not last:
                        mk_ = nmp if kind == "p" else nmn
                        k.op("pe", lambda: pe.matmul(dst, lhsT=idb[:], rhs=mk_[:], start=False, stop=True), [pb[bs + i // 4][:]],
                             [idb[:], mk_[:]], inc=(i == nkb - 1 or i == 3))
                P = PT[n % 2]
                n0 = min(nkb, 4) * 128
                k.op("act", lambda: act.activation(out=P[:, 0:n0], in_=pb[bs][:, 0:n0], func=AF.Exp, scale=0.125), [P[:]], [pb[bs][:]])
                if nkb > 4:
                    k.op("act", lambda: act.activation(out=P[:, 512:640], in_=pb[bs + 1][:, 0:128], func=AF.Exp, scale=0.125), [P[:]],
                         [pb[bs + 1][:]])
                for i, (kt, kind) in enumerate(kbs):
                    k.op("pe", lambda: pe.matmul(pso[:, hh * 128:hh * 128 + 65], lhsT=P[:, i * 128:(i + 1) * 128], rhs=V1[:, kt, j, :],
                                                 start=(i == 0), stop=(i == nkb - 1)), [pso[:]], [P[:], V1[:]],
                         inc=(i == nkb - 1 and hh == 3))
            k.op("dve", lambda: vec.tensor_tensor(out=den[:, 0:4], in0=V(pso[:, 64:65], (128, 4)), in1=esink[:, 4 * j:4 * j + 4], op=ALU.add),
                 [("x", "sden0")], [pso[:], esink[:]])
            k.op("dve", lambda: vec.reciprocal(out=den[:, 4:8], in_=den[:, 0:4]), [("x", "sden1")], [("x", "sden0")])
            k.op("dve", lambda: vec.tensor_tensor(out=otm[:, j * 256:(j + 1) * 256].rearrange("p (a e) -> p a e", e=64),
                                                  in0=V(pso[:, 0:1], (128, 4), (1, 64)), in1=V(den[:, 4:5], (1, 4), (0, 64)), op=ALU.mult),
                 [otm[:]], [pso[:], ("x", "sden1")])
        for kk in range(8):
            k.op("pe", lambda: pe.transpose(psqT[:, kk * 128:(kk + 1) * 128], otm[:, kk * 128:(kk + 1) * 128], idb[:]), [pb[6][:]],
                 [otm[:], idb[:]], inc=(kk == 7))
        k.op("act", lambda: act.copy(out=ogT[:], in_=psqT[:, 0:1024].rearrange("p (a b) -> p a b", a=8)), [ogT[:]], [pb[6][:]])
        for hf in range(2):
            ps = pb[7 - hf]
            for kk in range(8):
                k.op("pe", lambda: pe.matmul(ps[:], lhsT=ogT[:, kk, :], rhs=wo[:, kk, hf * 512:(hf + 1) * 512], start=(kk == 0), stop=(kk == 7)),
                     [ps[:]], [ogT[:], wo[:]], inc=(kk == 7))
            sl2 = slice(hf * 512, (hf + 1) * 512)
            k.op("dve", lambda: vec.tensor_tensor(out=ysb[:, sl2], in0=ps[:], in1=GT[:, sl2], op=ALU.mult), [ysb[:]], [ps[:], GT[:]])
        k.op("pool", lambda: pool.tensor_tensor(out=ysb[:], in0=ysb[:], in1=x[:], op=ALU.add), [ysb[:]], [ysb[:], x[:]])
        k.dma("pool", g.d_xs[t * 128:(t + 1) * 128, :], ysb[:], [("x", ("xs", t))], [ysb[:]])
    k.release(m)


FULL_PHASES = (("adaln",), ("gdn",), ("peer", 0, NT, False), ("swa",), ("peer", 1, L // 128, True))


def kernel(**inputs):
    k = build(phases=FULL_PHASES)
    sh = prep_shared(inputs)
    names = set(k.ext_inputs)
    in_maps = []
    for b in range(8):
        d_ = dict(sh)
        d_.update(prep_core(inputs, b))
        in_maps.append({a: v for a, v in d_.items() if a in names})
    res = run_bass_kernel_spmd(k.nc, in_maps, core_ids=list(range(8)))
    return np.stack([np.asarray(r["out"], dtype=np.float32) for r in res.results], axis=0)
```

```python
import numpy as np
import concourse.bass as bass
import concourse.mybir as mybir
from concourse.bass_utils import run_bass_kernel_spmd

F32 = mybir.dt.float32
BF16 = mybir.dt.bfloat16
U32 = mybir.dt.uint32
AF = mybir.ActivationFunctionType
ALU = mybir.AluOpType
AX = mybir.AxisListType

D = 1024
L = 4096
CTX = 256
NTOK = L + CTX
NT = NTOK // 128
EPS = 1e-6
NEXP_T = 128


class K:
    def __init__(self):
        self.nc = bass.Bass("TRN2", target_bir_lowering=False)
        nc = self.nc
        self.E = {"pe": nc.tensor, "act": nc.scalar, "dve": nc.vector, "pool": nc.gpsimd, "sp": nc.sync}
        self.sem = {e: nc.alloc_semaphore("s_" + e) for e in self.E}
        self.cnt = {e: 0 for e in self.E}
        self.seen = {e: {} for e in self.E}
        self.st = {}
        self.NDMA = 24
        self.dsem = [nc.alloc_semaphore("s_dma%d" % i) for i in range(self.NDMA)]
        self.dval = [0] * self.NDMA
        self.dnext = 0
        self.nsb = 0
        self.stack = []

    def sb(self, shape, dtype=F32, name=None):
        self.nsb += 1
        name = "sb%d_%s" % (self.nsb, name or "t")
        cm = self.nc.sbuf_tensor(name, list(shape), dtype)
        t = cm.__enter__()
        self.stack.append(cm)
        return t

    def mark(self):
        return len(self.stack)

    def release(self, mark):
        self.barrier()
        while len(self.stack) > mark:
            self.stack.pop().__exit__(None, None, None)

    def _semh(self, sk):
        return self.sem[sk] if isinstance(sk, str) else self.dsem[sk]

    def wait(self, e, tok):
        sk, v = tok
        if sk == e and e == "pe":
            return
        if self.seen[e].get(sk, 0) >= v:
            return
        self.E[e].wait_ge(self._semh(sk), v)
        self.seen[e][sk] = v

    @staticmethod
    def _key(x):
        if isinstance(x, tuple):
            return x[1]
        if isinstance(x, str):
            return x
        return x.tensor.name

    def _deps(self, e, outs, ins):
        deps = []
        for x in ins:
            s = self.st.get(self._key(x))
            if s and s[0]:
                deps.append(s[0])
        for x in outs:
            s = self.st.get(self._key(x))
            if s:
                if s[0] and s[0][0] != e:
                    deps.append(s[0])
                for re_, rt in s[1].items():
                    if re_ != e:
                        deps.append(rt)
        for t in deps:
            self.wait(e, t)

    def _commit(self, tok, e, outs, ins):
        for x in ins:
            s = self.st.setdefault(self._key(x), [None, {}])
            s[1][e] = tok
        for x in outs:
            s = self.st.setdefault(self._key(x), [None, {}])
            s[0] = tok
            s[1] = {}

    def op(self, e, fn, outs, ins, inc=True):
        self._deps(e, outs, ins)
        inst = fn()
        tok = (e, self.cnt[e] + 1)
        if inc:
            inst.then_inc(self.sem[e], 1)
            self.cnt[e] += 1
        self._commit(tok, e, outs, ins)
        return inst

    def dma(self, q, out, in_, outs, ins, **kw):
        self._deps(q, outs, ins)
        i = self.dnext
        self.dnext = (self.dnext + 1) % self.NDMA
        if self.dval[i]:
            self.wait(q, (i, self.dval[i]))
        self.dval[i] += 16
        self.E[q].dma_start(out=out, in_=in_, **kw).then_inc(self.dsem[i], 16)
        tok = (i, self.dval[i])
        for x in ins:
            s = self.st.setdefault(self._key(x), [None, {}])
            s[1]["d%d" % i] = tok
        for x in outs:
            s = self.st.setdefault(self._key(x), [None, {}])
            s[0] = tok
            s[1] = {}
        return tok

    def barrier(self):
        toks = [(e, self.cnt[e]) for e in self.E if self.cnt[e]]
        toks += [(i, self.dval[i]) for i in range(self.NDMA) if self.dval[i]]
        for e in self.E:
            for t in toks:
                if t[0] != e:
                    self.wait(e, t)
        self.st = {}

    def finish(self):
        for i in range(self.NDMA):
            if self.dval[i]:
                self.wait("sp", (i, self.dval[i]))
        for e in self.E:
            if e != "sp" and self.cnt[e]:
                self.wait("sp", (e, self.cnt[e]))


def V(ap, *dims):
    return bass.AP(ap.tensor, ap.offset, [list(ap.ap[0])] + [list(d) for d in dims])


C_ID, C_IOTA, C_EPS, C_DSELB, C_ONES, C_NMLS, C_NMUS, C_NMLI, C_NMUI, C_NMP, C_NMN, C_NCOL = 0, 128, 256, 264, 272, 400, 464, 528, 592, 656, 784, 912


def host_consts():
    c = np.zeros((128, C_NCOL), np.float32)
    c[:, C_ID:C_ID + 128] = np.eye(128, dtype=np.float32)
    c[:, C_IOTA:C_IOTA + 128] = np.arange(128, dtype=np.float32)[None, :]
    c[:, C_EPS] = EPS
    c[:, C_EPS + 1] = -0.5
    c[:, C_EPS + 2] = np.e
    c[8:16, C_DSELB] = 1.0
    c[:, C_ONES:C_ONES + 128] = 1.0
    p = np.arange(128)[:, None]
    f = np.arange(64)[None, :]
    NEG = -1e4
    c[:, C_NMLS:C_NMLS + 64] = np.where(p > f, 0.0, NEG)
    c[:, C_NMUS:C_NMUS + 64] = np.where(f > p, 0.0, NEG)
    c[:, C_NMLI:C_NMLI + 64] = np.where(p >= f, 0.0, NEG)
    c[:, C_NMUI:C_NMUI + 64] = np.where(f >= p, 0.0, NEG)
    f2 = np.arange(128)[None, :]
    c[:, C_NMP:C_NMP + 128] = np.where(p >= f2, 0.0, NEG)
    c[:, C_NMN:C_NMN + 128] = np.where(p <= f2, 0.0, NEG)
    return c


class Ctx:
    pass


def load_consts(k, g):
    g.cst = k.sb([128, C_NCOL], F32, "cst_sb")
    k.dma("sp", g.cst[:], g.d_cst, [g.cst[:]], [])
    g.ident = g.cst[:, C_ID:C_ID + 128]
    g.iota = g.cst[:, C_IOTA:C_IOTA + 128]
    g.epsT = g.cst[:, C_EPS:C_EPS + 1]
    g.ones = g.cst[:, C_ONES:C_ONES + 128]
    g.psb = [k.nc.alloc_psum_tensor("psb%d" % i, [128, 512], F32) for i in range(8)]


def bcast_row_load(k, dst, row_ap, n):
    P = dst.shape[0]
    src = bass.AP(row_ap.tensor, row_ap.offset, [[0, P], [1, n]])
    k.dma("sp", dst, src, [dst], [])


def peer_prepass_early(k, g, layers):
    nc = k.nc
    for li in layers:
        sem = nc.alloc_semaphore("s_pp%d" % li)
        k.dsem.append(sem)
        k.dval.append(0)
        si = len(k.dsem) - 1
        n = 0
        for src, dst in ((g.d_ut[li], g.d_utb[li]), (g.d_v[li], g.d_vb[li])):
            sv = src.rearrange("c p f -> (c p) f").rearrange("(a b) f -> a (b f)", b=8)
            dv = dst.rearrange("c p f -> (c p) f").rearrange("(a b) f -> a (b f)", b=8)
            for r0 in range(0, 2048, 128):
                nc.gpsimd.dma_start(out=dv[r0:r0 + 128, :], in_=sv[r0:r0 + 128, :]).then_inc(sem, 16)
                n += 1
        for blk in range(4):
            nc.gpsimd.dma_start(out=g.d_wqb[li][blk], in_=g.d_wq[li][:, blk * 512:(blk + 1) * 512].rearrange("(k p) f -> p k f", p=128)
                                ).then_inc(sem, 16)
            n += 1
        k.dval[si] = 16 * n
        k.st[("wb", li)] = [(si, 16 * n), {}]
        k.pp_tok = getattr(k, "pp_tok", {})
        k.pp_tok[li] = (si, 16 * n)


def peer_prepass(k, g, li):
    m = k.mark()
    CB = 8
    bufs = [k.sb([128, CB, 1024], BF16, "ppb%d" % i) for i in range(3)]
    n = 0
    for src, dst in ((g.d_ut[li], g.d_utb), (g.d_v[li], g.d_vb)):
        for c0 in range(0, 128, CB):
            b = bufs[n % 3]
            n += 1
            k.dma("pool", b[:], src[c0:c0 + CB].rearrange("c p f -> p c f"), [b[:]], [])
            for c1 in range(0, CB, 2):
                k.dma("pool", dst[(c0 + c1) // 2], b[:, c1:c1 + 2, :], [("x", ("wb", li))], [b[:]])
    for blk in range(4):
        b = bufs[n % 3]
        n += 1
        bv = b[:].rearrange("p c (a f) -> p (c a) f", a=2)[:, 0:8, :]
        for kk in range(8):
            k.dma("pool", bv[:, kk, :], g.d_wq[li, kk * 128:(kk + 1) * 128, blk * 512:(blk + 1) * 512], [b[:]], [])
        k.dma("pool", g.d_wqb[blk], bv, [("x", ("wb", li))], [b[:]])
    k.release(m)


def peer_layer(k, g, li, ntiles, final):
    nc = k.nc
    m0 = k.mark()
    TG = 2
    NTOKG = TG * 128
    vec, act, pe, pool = nc.vector, nc.scalar, nc.tensor, nc.gpsimd
    wqb = [k.sb([128, 8, 512], BF16, "wqb%d" % i) for i in range(2)]
    wk = k.sb([128, 16, 128], F32, "wk")
    skn = wk
    k.dma("sp", skn[:], g.d_sk[li].rearrange("h p k j -> k (h p) j"), [skn[:]], [])
    skt = k.sb([128, 16, 128], F32, "skt")
    for hp in range(16):
        ps = g.psb[hp % 2]
        k.op("pe", lambda: pe.transpose(ps[:, 0:128], skn[:, hp, :], g.ident), [ps[:]], [skn[:], g.cst[:]])
        k.op("act", lambda: act.copy(out=skt[:, hp, :], in_=ps[:, 0:128]), [skt[:]], [ps[:]])
    GS = k.sb([128, 1024], F32, "modgs")
    SH = k.sb([128, 1024], F32, "modsh")
    GT = k.sb([128, 1024], F32, "modgt")
    cur_a, cur_e = [-1], [-1]

    def set_mods_a(r):
        if cur_a[0] != r:
            cur_a[0] = r
            bcast_row_load(k, GS[:], g.d_mod[li, r, 3], 1024)
            bcast_row_load(k, SH[:], g.d_mod[li, r, 4], 1024)

    def set_mods_e(r):
        if cur_e[0] != r:
            cur_e[0] = r
            bcast_row_load(k, GT[:], g.d_mod[li, r, 5], 1024)
    if final:
        fg = k.sb([128, 1024], F32, "fing")
        bcast_row_load(k, fg[:], g.d_fg, 1024)
    gbuf = k.sb([128, NTOKG, 128], BF16, "gbuf")
    h2T = [k.sb([128, 8, NTOKG], BF16, "h2T%d" % i) for i in range(2)]
    xt = [[k.sb([128, 1024], F32, "xt%d_%d" % (p, i)) for i in range(TG)] for p in range(2)]
    selT = [[k.sb([128, 3, 128], F32, "selT%d_%d" % (p, i)) for i in range(TG)] for p in range(2)]
    hh = k.sb([128, 1024], F32, "hh")
    ss = k.sb([128, 4], F32, "ss")
    ss2 = k.sb([128, 4], F32, "ss2")
    qT = k.sb([128, 2, 256], F32, "qT")
    top2 = [k.sb([128, 16, 16], F32, "top%d" % i) for i in range(TG)]
    idx2 = [k.sb([128, 16, 16], U32, "idx%d" % i) for i in range(TG)]
    idxf = k.sb([128, 16, 16], F32, "idxf")
    cand = k.sb([128, 8, 256], F32, "cand")
    wk2 = wk[:].rearrange("p (a b) k -> p a (b k)", b=2)
    ctop = k.sb([128, 8, 16], F32, "ctop")
    pos = k.sb([128, 8, 16], U32, "pos")
    posi = k.sb([128, 2, 128], U32, "posi")
    posf = k.sb([128, 2, 128], F32, "posf")
    ee = k.sb([128, 8, 16], F32, "ee")
    zz = k.sb([128, 16], F32, "zz")
    eq = wk[:].rearrange("p a (b i) -> p (a b) i", i=16)
    sel = k.sb([128, 3, 128], F32, "sel")
    TB = 8
    ohA = [k.sb([128, TB, 128], BF16, "ohA%d" % i) for i in range(2)]
    ohB = [k.sb([128, TB, 128], BF16, "ohB%d" % i) for i in range(2)]
    iotab = k.sb([128, 128], BF16, "iotab")
    k.op("dve", lambda: vec.tensor_copy(out=iotab[:], in_=g.iota), [iotab[:]], [g.cst[:]])
    CB = 2
    NWB = 3
    utb = [k.sb([128, CB, 1024], BF16, "utb%d" % i) for i in range(NWB)]
    vbb = [k.sb([128, CB, 1024], BF16, "vbb%d" % i) for i in range(NWB)]
    actS = [k.sb([128, NTOKG], F32, "actS%d" % i) for i in range(2)]
    ga = [k.sb([128, NTOKG], BF16, "ga%d" % i) for i in range(2)]
    osb = hh

    psO = g.psb[0:4]
    psA = g.psb[4:6]
    psX = g.psb[6]
    psS = g.psb[7]

    ngroups = (ntiles + TG - 1) // TG

    def group_tiles(gi):
        return list(range(gi * TG, min(ntiles, (gi + 1) * TG)))

    def A1(gi):
        par = gi % 2
        tiles = group_tiles(gi)
        ntk = len(tiles) * 128
        set_mods_a(0 if tiles[0] < L // 128 else 1)
        hT_ = h2T[par]
        for ti, t in enumerate(tiles):
            x = xt[par][ti]
            k.dma("sp", x[:], g.d_xs[t * 128:(t + 1) * 128, :], [x[:]], [("x", ("xs", t))])
            norm_mod(k, g, x, hh, ss, GS, SH)
            yield
            for hf in range(2):
                for kk in range(4):
                    k.op("pe", lambda: pe.transpose(psX[:, kk * 128:(kk + 1) * 128], hh[:, (hf * 4 + kk) * 128:(hf * 4 + kk + 1) * 128],
                                                    g.ident), [psX[:]], [hh[:], g.cst[:]], inc=(kk == 3))
                k.op("act", lambda: act.copy(out=hT_[:, hf * 4:hf * 4 + 4, ti * 128:(ti + 1) * 128],
                                             in_=psX[:].rearrange("p (a b) -> p a b", a=4)), [hT_[:]], [psX[:]])
                yield
        k.dma("sp", wqb[0][:], g.d_wqb[li][0], [wqb[0][:]], [("x", ("wb", li))])
        for blk in range(4):
            wq = wqb[blk % 2]
            if blk + 1 < 4:
                k.dma("sp", wqb[(blk + 1) % 2][:], g.d_wqb[li][blk + 1], [wqb[(blk + 1) % 2][:]], [("x", ("wb", li))])
            for half in range(2):
                for h2 in range(2):
                    h4 = half * 2 + h2
                    for kk in range(8):
                        k.op("pe", lambda: pe.matmul(psX[:, h2 * 256:h2 * 256 + ntk], lhsT=wq[:, kk, h4 * 128:(h4 + 1) * 128],
                                                     rhs=hT_[:, kk, 0:ntk], start=(kk == 0), stop=(kk == 7)),
                             [psX[:]], [wq[:], hT_[:]], inc=(kk == 7 and h2 == 1))
                    yield
                k.op("act", lambda: act.copy(out=qT[:], in_=psX[:].rearrange("p (a b) -> p a b", a=2)), [qT[:]], [psX[:]])
                for ti in range(len(tiles)):
                    for h2 in range(2):
                        k.op("pe", lambda: pe.matmul(psS[:, (ti * 2 + h2) * 128:(ti * 2 + h2 + 1) * 128],
                                                     lhsT=qT[:, h2, ti * 128:(ti + 1) * 128], rhs=skt[:, blk * 4 + half * 2 + h2, :],
                                                     start=True, stop=True), [psS[:]], [qT[:], skt[:]],
                             inc=(h2 == 1 and ti == len(tiles) - 1))
                yield
                for ti in range(len(tiles)):
                    tp, ix = top2[ti], idx2[ti]
                    hps = [(blk * 4 + half * 2 + h2, (ti * 2 + h2)) for h2 in range(2)]

                    def S(c_):
                        return psS[:, c_ * 128:(c_ + 1) * 128]

                    for hp, c_ in hps:
                        k.op("dve", lambda: vec.max(out=tp[:, hp, 0:8], in_=S(c_)), [("x", ("topa", ti))], [psS[:]])
                    for hp, c_ in hps:
                        k.op("dve", lambda: vec.match_replace(out=wk[:, hp, :], in_to_replace=tp[:, hp, 0:8], in_values=S(c_),
                                                              imm_value=-1e30), [wk[:]], [("x", ("topa", ti)), psS[:]])
                    yield
                    for hp, c_ in hps:
                        k.op("dve", lambda: vec.max(out=tp[:, hp, 8:16], in_=wk[:, hp, :]), [("x", ("topb", ti))], [wk[:]])
                    for hp, c_ in hps:
                        k.op("dve", lambda: vec.max_index(out=ix[:, hp, 0:8], in_max=tp[:, hp, 0:8], in_values=S(c_)),
                             [("x", ("idxa", ti))], [("x", ("topa", ti)), psS[:]])
                    yield
                    for hp, c_ in hps:
                        k.op("dve", lambda: vec.max_index(out=ix[:, hp, 8:16], in_max=tp[:, hp, 8:16], in_values=S(c_)),
                             [("x", ("idxb", ti))], [("x", ("topb", ti)), psS[:]])
                    yield
        for ti, t in enumerate(tiles):
            top, idx = top2[ti], idx2[ti]
            k.op("dve", lambda: vec.tensor_copy(out=idxf[:], in_=idx[:]), [idxf[:]], [("x", ("idxa", ti)), ("x", ("idxb", ti))])
            t1 = V(top[:, 0, :], (32, 8), (1, 16), (0, 16))
            t2 = V(top[:, 1, :], (32, 8), (0, 16), (1, 16))
            k.op("dve", lambda: vec.tensor_tensor(out=cand[:].rearrange("p h (i j) -> p h i j", i=16), in0=t1, in1=t2,
                                                  op=ALU.add), [cand[:]], [("x", ("topa", ti)), ("x", ("topb", ti))])
            yield
            for h in range(8):
                k.op("dve", lambda: vec.max(out=ctop[:, h, 0:8], in_=cand[:, h, :]), [("x", "ctopa")], [cand[:]])
            yield
            for h in range(8):
                k.op("dve", lambda: vec.match_replace(out=wk2[:, h, :], in_to_replace=ctop[:, h, 0:8], in_values=cand[:, h, :],
                                                      imm_value=-1e30), [wk[:]], [("x", "ctopa"), cand[:]])
            yield
            for h in range(8):
                k.op("dve", lambda: vec.max(out=ctop[:, h, 8:16], in_=wk2[:, h, :]), [("x", "ctopb")], [wk[:]])
            yield
            for h in range(8):
                k.op("dve", lambda: vec.max_index(out=pos[:, h, 0:8], in_max=ctop[:, h, 0:8], in_values=cand[:, h, :]),
                     [("x", "posa")], [("x", "ctopa"), cand[:]])
            yield
            for h in range(8):
                k.op("dve", lambda: vec.max_index(out=pos[:, h, 8:16], in_max=ctop[:, h, 8:16], in_values=cand[:, h, :]),
                     [("x", "posb")], [("x", "ctopb"), cand[:]])
            yield
            k.op("dve", lambda: vec.tensor_tensor(out=ee[:], in0=ctop[:], in1=V(ctop[:, 0, 0:1], (16, 8), (0, 16)),
                                                  op=ALU.subtract), [ee[:]], [("x", "ctopa"), ("x", "ctopb")])
            k.op("act", lambda: act.activation(out=ee[:], in_=ee[:], func=AF.Exp), [ee[:]], [ee[:]])
            k.op("dve", lambda: vec.tensor_reduce(out=zz[:, 0:8], in_=ee[:], axis=AX.X, op=ALU.add), [("x", "zz0")], [ee[:]])
            k.op("dve", lambda: vec.reciprocal(out=zz[:, 8:16], in_=zz[:, 0:8]), [("x", "zz1")], [("x", "zz0")])
            k.op("dve", lambda: vec.tensor_tensor(out=sel[:, 2, :].rearrange("p (h r) -> p h r", h=8), in0=ee[:],
                                                  in1=V(zz[:, 8:9], (1, 8), (0, 16)), op=ALU.mult),
                 [("x", "sel2")], [ee[:], ("x", "zz1")])
            yield
            posv = pos[:].rearrange("p h r -> p (h r)")
            k.op("dve", lambda: vec.tensor_single_scalar(out=posi[:, 0, :], in_=posv, scalar=4, op=ALU.logical_shift_right),
                 [("x", "posi0")], [("x", "posa"), ("x", "posb")])
            k.op("dve", lambda: vec.tensor_single_scalar(out=posi[:, 1, :], in_=posv, scalar=15, op=ALU.bitwise_and),
                 [("x", "posi1")], [("x", "posa"), ("x", "posb")])
            k.op("dve", lambda: vec.tensor_copy(out=posf[:], in_=posi[:]), [posf[:]], [("x", "posi0"), ("x", "posi1")])
            yield
            for p_ in range(2):
                k.op("dve", lambda: vec.tensor_tensor(out=eq, in0=V(posf[:, p_, :], (1, 128), (0, 16)),
                                                      in1=V(g.iota[:, 0:16], (0, 128), (1, 16)), op=ALU.is_equal),
                     [wk[:]], [posf[:], g.cst[:]])
                yield
                k.op("pool", lambda: pool.tensor_tensor(out=eq.rearrange("p (h r) i -> p h r i", h=8),
                                                        in0=eq.rearrange("p (h r) i -> p h r i", h=8),
                                                        in1=V(idxf[:, p_, :], (32, 8), (0, 16), (1, 16)), op=ALU.mult),
                     [wk[:]], [wk[:], idxf[:]])
                k.op("dve", lambda: vec.tensor_reduce(out=sel[:, p_, :], in_=eq, axis=AX.X, op=ALU.add),
                     [("x", ("sel", p_))], [wk[:]])
                yield
            for q in range(3):
                k.op("pe", lambda: pe.transpose(psX[:, q * 128:(q + 1) * 128], sel[:, q, :], g.ident), [psX[:]],
                     [("x", ("sel", 0)), ("x", ("sel", 1)), ("x", "sel2"), g.cst[:]], inc=(q == 2))
            sT = selT[par][ti]
            k.op("act", lambda: act.copy(out=sT[:], in_=psX[:, 0:384].rearrange("p (a b) -> p a b", a=3)), [sT[:]], [psX[:]])
            yield

    def A2(gi):
        par = gi % 2
        tiles = group_tiles(gi)
        nq = 0
        for ti, t in enumerate(tiles):
            sT = selT[par][ti]
            for b in range(128 // TB):
                A = ohA[b % 2]
                B = ohB[b % 2]
                t0 = b * TB
                for tt in range(TB):
                    k.op("dve", lambda: vec.tensor_scalar(out=A[:, tt, :], in0=iotab[:], scalar1=sT[:, 0, t0 + tt:t0 + tt + 1],
                                                          scalar2=sT[:, 2, t0 + tt:t0 + tt + 1], op0=ALU.is_equal, op1=ALU.mult),
                         [A[:]], [sT[:], iotab[:]])
                    if False:
                        k.op("pool", lambda: pool.tensor_scalar(out=B[:, tt, :], in0=iotab[:], scalar1=sT[:, 1, t0 + tt:t0 + tt + 1],
                                                                scalar2=None, op0=ALU.is_equal), [("x", ("ohB", b % 2, "p"))],
                             [sT[:], iotab[:], ("x", ("ohBr", b % 2))])
                    else:
                        k.op("dve", lambda: vec.tensor_scalar(out=B[:, tt, :], in0=iotab[:], scalar1=sT[:, 1, t0 + tt:t0 + tt + 1],
                                                              scalar2=None, op0=ALU.is_equal), [("x", ("ohB", b % 2, "d"))],
                             [sT[:], iotab[:], ("x", ("ohBr", b % 2))])
                for q in range(TB // 4):
                    ps = psA[nq % 2]
                    nq += 1
                    for u in range(4):
                        tt = q * 4 + u
                        k.op("pe", lambda: pe.matmul(ps[:, u * 128:(u + 1) * 128], lhsT=B[:, tt, :], rhs=A[:, tt, :], start=True,
                                                     stop=True), [ps[:], ("x", ("ohBr", b % 2))],
                             [A[:], ("x", ("ohB", b % 2, "p")), ("x", ("ohB", b % 2, "d"))], inc=(u == 3))
                    tg0 = ti * 128 + t0 + q * 4
                    k.op("act", lambda: act.copy(out=gbuf[:, tg0:tg0 + 4, :], in_=ps[:].rearrange("p (a b) -> p a b", a=4)),
                         [("x", "gbufW")], [ps[:], ("x", "gbufR"), ("x", "gbufR2")])

    def B(gi, nxt):
        par = gi % 2
        tiles = group_tiles(gi)
        ntg = len(tiles)
        ntok = ntg * 128
        hT_ = h2T[par]

        def load_w(cb):
            b = (cb // CB) % NWB
            k.dma("sp", utb[b][:], g.d_utb[li][cb:cb + CB].rearrange("c p f -> p c f"), [utb[b][:]], [("x", ("wb", li))])
            k.dma("sp", vbb[b][:], g.d_vb[li][cb:cb + CB].rearrange("c p f -> p c f"), [vbb[b][:]], [("x", ("wb", li))])

        def umm(c):
            b = (c // CB) % NWB
            ps = psA[c % 2]
            for kk in range(8):
                k.op("pe", lambda: pe.matmul(ps[:, 0:ntok], lhsT=utb[b][:, c % CB, kk * 128:(kk + 1) * 128],
                                             rhs=hT_[:, kk, 0:ntok], start=(kk == 0), stop=(kk == 7)),
                     [ps[:]], [utb[b][:], hT_[:]], inc=(kk == 7))

        load_w(0)
        load_w(CB)
        umm(0)
        for c in range(128):
            if c % CB == 0 and c + 2 * CB < 128:
                load_w(c + 2 * CB)
            if c + 1 < 128:
                umm(c + 1)
            b = (c // CB) % NWB
            ps = psA[c % 2]
            a_ = actS[c % 2]
            g_ = ga[c % 2]
            k.op("act", lambda: act.activation(out=a_[:, 0:ntok], in_=ps[:, 0:ntok], func=AF.Gelu), [a_[:]], [ps[:]])
            if c % 2 == 0:
                k.op("dve", lambda: vec.tensor_tensor(out=g_[:, 0:ntok], in0=a_[:, 0:ntok], in1=V(gbuf[:, 0, c:c + 1], (128, ntok)),
                                                      op=ALU.mult), [g_[:], ("x", "gbufR")], [a_[:], ("x", "gbufW")])
            else:
                k.op("pool", lambda: pool.tensor_tensor(out=g_[:, 0:ntok], in0=a_[:, 0:ntok], in1=V(gbuf[:, 0, c:c + 1], (128, ntok)),
                                                        op=ALU.mult), [g_[:], ("x", "gbufR2")], [a_[:], ("x", "gbufW")])
            for ti in range(ntg):
                for hf in range(2):
                    k.op("pe", lambda: pe.matmul(psO[ti * 2 + hf][:], lhsT=g_[:, ti * 128:(ti + 1) * 128],
                                                 rhs=vbb[b][:, c % CB, hf * 512:(hf + 1) * 512], start=(c == 0), stop=(c == 127)),
                         [psO[ti * 2 + hf][:]], [g_[:], vbb[b][:]], inc=(ti == ntg - 1 and hf == 1))
            if nxt is not None and c >= 2:
                for _ in range(STEPS):
                    next(nxt, None)
        if nxt is not None:
            for _ in nxt:
                pass
        set_mods_e(0 if tiles[0] < L // 128 else 1)
        for ti, t in enumerate(tiles):
            x = xt[par][ti]
            for hf in range(2):
                sl = slice(hf * 512, (hf + 1) * 512)
                k.op("dve", lambda: vec.tensor_tensor(out=osb[:, sl], in0=psO[ti * 2 + hf][:], in1=GT[:, sl], op=ALU.mult),
                     [osb[:]], [psO[ti * 2 + hf][:], GT[:]])
            k.op("pool", lambda: pool.tensor_tensor(out=osb[:], in0=osb[:], in1=x[:], op=ALU.add), [osb[:]], [osb[:], x[:]])
            if not final:
                k.dma("pool", g.d_xs[t * 128:(t + 1) * 128, :], osb[:], [("x", ("xs", t))], [osb[:]])
            else:
                norm_mod_final(k, g, osb, x, ss2, fg)
                k.dma("pool", g.d_out[t * 128:(t + 1) * 128, :], x[:], [("x", ("out", t))], [x[:]])

    STEPS = 1
    n_a1 = 6 + 24 + 32 * TG * 3 // 3 + 18 * TG
    STEPS = 1
    for _ in A1(0):
        pass
    for gi in range(ngroups):
        A2(gi)
        nxt = A1(gi + 1) if gi + 1 < ngroups else None
        B(gi, nxt)
    k.release(m0)


def norm_mod_final(k, g, x, out, ss, fg):
    nc = k.nc
    vec, act = nc.vector, nc.scalar
    k.op("dve", lambda: vec.scalar_tensor_tensor(out=out[:], in0=x[:], scalar=1.0, in1=x[:], op0=ALU.mult, op1=ALU.mult,
                                                accum_out=ss[:, 0:1]), [out[:], ss[:]], [x[:]])
    k.op("act", lambda: act.activation(out=ss[:, 1:2], in_=ss[:, 0:1], func=AF.Sqrt, scale=1.0 / D, bias=g.epsT), [("x", "fss1")],
         [ss[:]])
    k.op("dve", lambda: vec.reciprocal(out=ss[:, 2:3], in_=ss[:, 1:2]), [("x", "fss2")], [("x", "fss1")])
    k.op("dve", lambda: vec.scalar_tensor_tensor(out=out[:], in0=x[:], scalar=ss[:, 2:3], in1=fg[:], op0=ALU.mult, op1=ALU.mult),
         [out[:]], [x[:], ("x", "fss2"), fg[:]])


def EPS_AP(g):
    return g.epsT[:, 0:1]


def build(phases=("all",), dev=None):
    dev = dev or {}
    k = K()
    nc = k.nc
    g = Ctx()

    k.ext_inputs = []

    def din(name, shape, dt=F32):
        k.ext_inputs.append(name)
        return nc.dram_tensor(name, list(shape), dt, kind="ExternalInput").ap()

    def dint(name, shape, dt=F32):
        return nc.dram_tensor(name, list(shape), dt, kind="Internal").ap()

    g.d_x = din("x", [L, D])
    g.d_ctx = din("ctx", [CTX, D])
    g.d_cst = din("cst", [128, C_NCOL])
    g.d_wq = din("peer_wq", [2, D, 2048])
    g.d_sk = din("peer_sk", [2, 8, 2, 128, 128])
    g.d_ut = din("peer_ut", [2, 128, 128, 1024])
    g.d_v = din("peer_v", [2, 128, 128, 1024])
    g.d_fg = din("final_g", [D])
    g.d_cT = din("cT", [128, 8, 2])
    g.d_adaw = din("ada_w", [2, D, 6144])
    g.d_adab = din("ada_b", [2, 6144])
    g.d_n1g = din("norm1_g", [2, D])
    g.d_n2g = din("norm2_g", [2, D])
    g.d_cst2 = din("cst2", [16, NTOK])
    g.d_gwin = din("gdn_w_in", [D, 4128])
    g.d_gconv = din("gdn_conv", [24, 128, 5])
    g.d_galog = din("gdn_a_log", [2, 8])
    g.d_gdtb = din("gdn_dt_bias", [2, 8])
    g.d_gng = din("gdn_norm_g", [128])
    g.d_gwout = din("gdn_w_out", [D, D])
    g.d_swin = din("swa_w_in", [D, 1536])
    g.d_swout = din("swa_w_out", [D, D])
    g.d_ssink = din("swa_sinks", [16])
    g.d_rope = din("rope", [2, L, 64])
    g.d_hT = dint("hT", [NT, 128, 8, 128], BF16)
    g.d_qkv = dint("qkv", [24, 128, NTOK], BF16)
    g.d_rows = dint("rows", [2, 16, NTOK])
    g.d_cd = dint("cd", [16, NCH])
    g.d_o = dint("gdn_o", [2, NTOK, D])
    if dev.get("mod_ext"):
        g.d_mod = din("mod", [2, 2, 6, D])
    else:
        g.d_mod = dint("mod", [2, 2, 6, D])
    g.d_xs = dint("xs", [NTOK, D])
    g.d_utb = dint("utb", [2, 128, 128, 1024], BF16)
    g.d_vb = dint("vb", [2, 128, 128, 1024], BF16)
    g.d_wqb = dint("wqb", [2, 4, 128, 8, 512], BF16)
    g.d_out = nc.dram_tensor("out", [L, D], F32, kind="ExternalOutput").ap()
    if dev.get("xs_out"):
        g.d_xso = nc.dram_tensor("xs_out", [NTOK, D], F32, kind="ExternalOutput").ap()

    load_consts(k, g)
    peer_prepass_early(k, g, [ph[1] for ph in phases if ph[0] == "peer"])
    for r0 in range(0, L, 512):
        k.dma("sp", g.d_xs[r0:r0 + 512, :], g.d_x[r0:r0 + 512, :], [("x", ("xs", r0 // 128 + i)) for i in range(4)], [])
    k.dma("sp", g.d_xs[L:NTOK, :], g.d_ctx, [("x", ("xs", 32)), ("x", ("xs", 33))], [])

    for ph in phases:
        if ph[0] == "adaln":
            adaln_phase(k, g)
        elif ph[0] == "gdn":
            m_ = k.mark()
            build_hT(k, g, 0, NT)
            gdn_rows(k, g)
            gdn_qkv(k, g)
            gdn_scan(k, g)
            k.release(m_)
            gdn_out(k, g, 0, NT)
        elif ph[0] == "swa":
            build_hT(k, g, 1, NT)
            swa_phase(k, g, 1)
        elif ph[0] == "peer":
            _, li, ntiles, final = ph
            k.st[("wb", li)] = [k.pp_tok[li], {}]
            peer_layer(k, g, li, ntiles, final)
    if dev.get("mod_out"):
        k.barrier()
        d_mo = nc.dram_tensor("mod_out", [2, 2, 6, D], F32, kind="ExternalOutput").ap()
        k.dma("sp", d_mo.rearrange("a b c d -> (a b c) d"), g.d_mod.rearrange("a b c d -> (a b c) d"), [("x", "modo")], [])
    if dev.get("xs_out"):
        k.barrier()
        for r0 in range(0, NTOK, 256):
            k.dma("sp", g.d_xso[r0:r0 + 256, :], g.d_xs[r0:r0 + 256, :], [("x", ("xso", r0))], [])
    k.finish()
    return k


def adaln_phase(k, g):
    nc = k.nc
    m = k.mark()
    cT = k.sb([128, 8, 2], F32, "cT")
    k.dma("sp", cT[:], g.d_cT, [cT[:]], [])
    sc = k.sb([128, 8, 2], F32, "scT")
    k.op("act", lambda: nc.scalar.activation(out=sc[:], in_=cT[:], func=AF.Silu), [sc[:]], [cT[:]])
    bb = k.sb([2, 6144], F32, "adab")
    msb = k.sb([2, 6144], F32, "adam")
    n1 = k.sb([2, 1024], F32, "n1g")
    n2 = k.sb([2, 1024], F32, "n2g")
    o1 = k.sb([2, 1024], F32, "ado1")
    o2 = k.sb([2, 1024], F32, "ado2")
    wb = [k.sb([128, 8, 512], F32, "adaw%d" % i) for i in range(2)]
    for li in range(2):
        bcast_row_load(k, bb[:], g.d_adab[li], 6144)
        bcast_row_load(k, n1[:], g.d_n1g[li], 1024)
        bcast_row_load(k, n2[:], g.d_n2g[li], 1024)
        for n in range(12):
            w = wb[n % 2]
            k.dma("sp", w[:], g.d_adaw[li][:, n * 512:(n + 1) * 512].rearrange("(k p) f -> p k f", p=128), [w[:]], [])
            ps = g.psb[n % 2]
            for kk in range(8):
                k.op("pe", lambda: nc.tensor.matmul(ps[0:2, :], lhsT=sc[:, kk, :], rhs=w[:, kk, :], start=(kk == 0), stop=(kk == 7)),
                     [ps[:]], [sc[:], w[:]], inc=(kk == 7))
            k.op("dve", lambda: nc.vector.tensor_tensor(out=msb[:, n * 512:(n + 1) * 512], in0=ps[0:2, :],
                                                        in1=bb[:, n * 512:(n + 1) * 512], op=ALU.add), [msb[:]], [ps[:], bb[:]])
        k.op("dve", lambda: nc.vector.scalar_tensor_tensor(out=o1[:], in0=msb[:, 1024:2048], scalar=1.0, in1=n1[:], op0=ALU.add,
                                                          op1=ALU.mult), [o1[:]], [msb[:], n1[:]])
        k.op("dve", lambda: nc.vector.scalar_tensor_tensor(out=o2[:], in0=msb[:, 4096:5120], scalar=1.0, in1=n2[:], op0=ALU.add,
                                                          op1=ALU.mult), [o2[:]], [msb[:], n2[:]])
        srcs = [o1[:], msb[:, 0:1024], msb[:, 2048:3072], o2[:], msb[:, 3072:4096], msb[:, 5120:6144]]
        for j in range(6):
            k.dma("sp", g.d_mod[li, :, j, :], srcs[j], [("x", ("mod", li))], [o1[:], o2[:], msb[:]])
    k.release(m)


def norm_mod(k, g, x, hh, ss, GS, SH):
    nc = k.nc
    vec, act, pool = nc.vector, nc.scalar, nc.gpsimd
    k.op("dve", lambda: vec.scalar_tensor_tensor(out=hh[:], in0=x[:], scalar=1.0, in1=x[:], op0=ALU.mult, op1=ALU.mult,
                                                accum_out=ss[:, 0:1]), [hh[:], ss[:]], [x[:]])
    k.op("dve", lambda: vec.tensor_scalar(out=ss[:, 1:2], in0=ss[:, 0:1], scalar1=1.0 / D, scalar2=EPS, op0=ALU.mult, op1=ALU.add),
         [("x", "ss1")], [ss[:]])
    k.op("pool", lambda: pool.tensor_tensor(out=ss[:, 2:3], in0=ss[:, 1:2], in1=g.cst[:, C_EPS + 1:C_EPS + 2], op=ALU.pow),
         [("x", "ss2")], [("x", "ss1"), g.cst[:]])
    k.op("dve", lambda: vec.scalar_tensor_tensor(out=hh[:], in0=x[:], scalar=ss[:, 2:3], in1=GS[:], op0=ALU.mult, op1=ALU.mult),
         [hh[:]], [x[:], ("x", "ss2"), GS[:]])
    k.op("pool", lambda: pool.tensor_tensor(out=hh[:], in0=hh[:], in1=SH[:], op=ALU.add), [hh[:]], [hh[:], SH[:]])


def build_hT(k, g, li, ntiles):
    nc = k.nc
    m = k.mark()
    GS = k.sb([128, 1024], F32, "hgs")
    SH = k.sb([128, 1024], F32, "hsh")
    xt = [k.sb([128, 1024], F32, "hxt%d" % i) for i in range(2)]
    hh = k.sb([128, 1024], F32, "hhh")
    ss = k.sb([128, 4], F32, "hss")
    hTt = [k.sb([128, 8, 128], BF16, "hTt%d" % i) for i in range(3)]
    cur = -1
    pend = None
    for t in range(ntiles):
        r = 0 if t < L // 128 else 1
        if r != cur:
            cur = r
            bcast_row_load(k, GS[:], g.d_mod[li, r, 0], 1024)
            bcast_row_load(k, SH[:], g.d_mod[li, r, 1], 1024)
        x = xt[t % 2]
        k.dma("sp", x[:], g.d_xs[t * 128:(t + 1) * 128, :], [x[:]], [("x", ("xs", t))])
        norm_mod(k, g, x, hh, ss, GS, SH)
        o = hTt[t % 3]
        for kk in range(8):
            ps = g.psb[kk // 4]
            k.op("pe", lambda: nc.tensor.transpose(ps[:, (kk % 4) * 128:(kk % 4 + 1) * 128], hh[:, kk * 128:(kk + 1) * 128], g.ident),
                 [ps[:]], [hh[:], g.cst[:]], inc=(kk % 4 == 3))
            if kk % 4 == 3:
                h0 = kk // 4
                if h0 == 0:
                    k.op("act", lambda: nc.scalar.copy(out=o[:, 0:4, :], in_=ps[:].rearrange("p (a b) -> p a b", a=4)), [o[:]], [ps[:]])
                else:
                    k.op("dve", lambda: nc.vector.tensor_copy(out=o[:, 4:8, :], in_=ps[:].rearrange("p (a b) -> p a b", a=4)),
                         [o[:]], [ps[:]])
        if pend is not None:
            k.dma("pool", g.d_hT[pend[0]], pend[1][:], [("x", ("hT", pend[0]))], [pend[1][:]])
        pend = (t, o)
    k.dma("pool", g.d_hT[pend[0]], pend[1][:], [("x", ("hT", pend[0]))], [pend[1][:]])
    k.release(m)


NCH = NTOK // 64


def gdn_rows(k, g):
    nc = k.nc
    vec, act, pe = nc.vector, nc.scalar, nc.tensor
    g.TM = k.sb([64, NCH, 4, 16], F32, "gdnTM")
    m = k.mark()
    wab = k.sb([128, 8, 32], BF16, "wab")
    for kk in range(8):
        k.dma("pool", wab[:, kk, :], g.d_gwin[kk * 128:(kk + 1) * 128, 4096:4128], [wab[:]], [])
    alog = k.sb([16, 4], F32, "alog")
    k.dma("sp", alog[:, 0:1], g.d_galog.rearrange("d (h o) -> (d h) o", o=1), [alog[:]], [])
    k.dma("sp", alog[:, 1:2], g.d_gdtb.rearrange("d (h o) -> (d h) o", o=1), [alog[:]], [])
    k.op("act", lambda: act.activation(out=alog[:, 2:3], in_=alog[:, 0:1], func=AF.Exp), [("x", "alog2")], [alog[:]])
    k.op("dve", lambda: vec.tensor_scalar(out=alog[:, 3:4], in0=alog[:, 2:3], scalar1=-1.0, scalar2=None, op0=ALU.mult),
         [("x", "alog3")], [("x", "alog2")])
    rows = {nm: k.sb([16, NTOK], F32, "row_" + nm) for nm in ("g", "beta", "Gf", "Gb", "eG", "kdf")}
    cm = k.sb([16, NTOK], F32, "row_cm")
    k.dma("sp", cm[:], g.d_cst2, [cm[:]], [])
    hb = [k.sb([128, 4, 8, 128], BF16, "rhb%d" % i) for i in range(2)]
    for blk in range((NT + 3) // 4):
        t0 = blk * 4
        nt = min(4, NT - t0)
        h = hb[blk % 2]
        k.dma("sp", h[:, 0:nt], g.d_hT[t0:t0 + nt].rearrange("t p k n -> p t k n"), [h[:]], [("x", ("hT", t0 + i)) for i in range(nt)])
        psa, psb_ = g.psb[(blk % 2) * 2], g.psb[(blk % 2) * 2 + 1]
        for half, ps in ((0, psa), (1, psb_)):
            for t in range(nt):
                for kk in range(8):
                    k.op("pe", lambda: pe.matmul(ps[0:16, t * 128:(t + 1) * 128], lhsT=wab[:, kk, half * 16:(half + 1) * 16],
                                                 rhs=h[:, t, kk, :], start=(kk == 0), stop=(kk == 7)), [ps[:]], [wab[:], h[:]],
                         inc=(kk == 7 and t == nt - 1))
        sl = slice(t0 * 128, (t0 + nt) * 128)
        w = nt * 128
        k.op("act", lambda: act.activation(out=rows["g"][:, sl], in_=psa[0:16, 0:w], func=AF.Exp, bias=alog[:, 1:2]),
             [rows["g"][:]], [psa[:], alog[:]])
        k.op("act", lambda: act.activation(out=rows["g"][:, sl], in_=rows["g"][:, sl], func=AF.Ln, bias=1.0), [rows["g"][:]],
             [rows["g"][:]])
        k.op("dve", lambda: vec.tensor_scalar(out=rows["g"][:, sl], in0=rows["g"][:, sl], scalar1=alog[:, 3:4], scalar2=None,
                                              op0=ALU.mult), [rows["g"][:]], [rows["g"][:], ("x", "alog3")])
        k.op("act", lambda: act.activation(out=rows["beta"][:, sl], in_=psb_[0:16, 0:w], func=AF.Sigmoid), [rows["beta"][:]],
             [psb_[:]])
    G, Gf, Gb, B, eG, kdf = rows["g"], rows["Gf"], rows["Gb"], rows["beta"], rows["eG"], rows["kdf"]
    k.op("dve", lambda: vec.tensor_tensor_scan(out=Gf[:], data0=cm[:], data1=G[:], initial=0.0, op0=ALU.mult, op1=ALU.add),
         [Gf[:]], [cm[:], G[:]])
    tot = V(Gf[:, 63:64], (64, NCH), (0, 64))
    v3 = lambda t: t[:].rearrange("p (c j) -> p c j", j=64)
    k.op("dve", lambda: vec.tensor_tensor(out=v3(Gb), in0=tot, in1=v3(Gf), op=ALU.subtract), [Gb[:]], [Gf[:]])
    k.op("dve", lambda: vec.tensor_tensor(out=Gb[:], in0=Gb[:], in1=G[:], op=ALU.add), [Gb[:]], [Gb[:], G[:]])
    k.op("dve", lambda: vec.tensor_tensor(out=Gb[:], in0=Gb[:], in1=Gf[:], op=ALU.subtract), [Gb[:]], [Gb[:], Gf[:]])
    k.op("dve", lambda: vec.scalar_tensor_tensor(out=Gb[:], in0=Gb[:], scalar=g.cst[0:16, C_DSELB:C_DSELB + 1], in1=Gf[:],
                                                op0=ALU.mult, op1=ALU.add), [Gb[:]], [Gb[:], Gf[:], g.cst[:]])
    GD = Gb
    k.op("act", lambda: act.activation(out=eG[:], in_=GD[:], func=AF.Exp), [eG[:]], [GD[:]])
    k.op("dve", lambda: vec.tensor_tensor(out=v3(kdf), in0=tot, in1=v3(GD), op=ALU.subtract), [kdf[:]], [Gf[:], GD[:]])
    k.op("act", lambda: act.activation(out=kdf[:], in_=kdf[:], func=AF.Exp), [kdf[:]], [kdf[:]])
    cd = k.sb([16, NCH], F32, "row_cd")
    k.op("act", lambda: act.activation(out=cd[:], in_=V(Gf[:, 63:64], (64, NCH)), func=AF.Exp), [cd[:]], [Gf[:]])
    k.dma("sp", g.d_rows[0], GD[:], [("x", "rows")], [GD[:]])
    k.dma("sp", g.d_rows[1], B[:], [("x", "rows")], [B[:]])
    k.dma("sp", g.d_cd, cd[:], [("x", "rows")], [cd[:]])
    for c in range(NCH):
        ps = g.psb[4 + (c // 8) % 2]
        for q, src in enumerate((GD, B, eG, kdf)):
            col = (c % 8) * 64 + q * 16
            k.op("pe", lambda: pe.transpose(ps[0:64, col:col + 16], src[:, c * 64:(c + 1) * 64], g.ident[0:16, 0:16]), [ps[:]],
                 [src[:], g.cst[:]], inc=(q == 3 and (c % 8 == 7 or c == NCH - 1)))
        if c % 8 == 7 or c == NCH - 1:
            c0 = (c // 8) * 8
            n = c - c0 + 1
            k.op("act", lambda: act.copy(out=g.TM[:, c0:c0 + n].rearrange("p c q d -> p (c q d)"), in_=ps[0:64, 0:n * 64]),
                 [g.TM[:]], [ps[:]])
    k.release(m)


def gdn_qkv(k, g):
    nc = k.nc
    vec, act, pe, pool = nc.vector, nc.scalar, nc.tensor, nc.gpsimd
    m = k.mark()
    PW = NTOK + 8
    Ps = [k.sb([128, PW], F32, "gP%d" % i) for i in range(2)]
    for P in Ps:
        k.op("pool", lambda: pool.memset(P[:], 0.0), [P[:]], [])
    R = [k.sb([128, NTOK], F32, "gR%d" % i) for i in range(2)]
    RB = [k.sb([128, NTOK], BF16, "gRB%d" % i) for i in range(2)]
    cw = k.sb([128, 24, 5], F32, "gcw")
    k.dma("sp", cw[:], g.d_gconv.rearrange("f p j -> p f j"), [cw[:]], [])
    wf = [k.sb([128, 8, 128], BF16, "gwf%d" % i) for i in range(2)]
    hb = [k.sb([128, 4, 8, 128], BF16, "ghb%d" % i) for i in range(2)]
    sq = [k.sb([128, 512], F32, "gsq%d" % i) for i in range(2)]
    rn = [k.sb([128, 512], F32, "grn%d" % i) for i in range(2)]
    nblk = (NT + 3) // 4
    def load_w(fc_):
        w_ = wf[fc_ % 2]
        for kk in range(8):
            k.dma("pool", w_[:, kk, :], g.d_gwin[kk * 128:(kk + 1) * 128, fc_ * 128:(fc_ + 1) * 128], [w_[:]], [])

    load_w(0)
    pending_store = None
    for fc in range(24):
        w = wf[fc % 2]
        P = Ps[fc % 2]
        for blk in range(nblk):
            t0 = blk * 4
            nt = min(4, NT - t0)
            h = hb[blk % 2]
            k.dma("sp", h[:, 0:nt], g.d_hT[t0:t0 + nt].rearrange("t p k n -> p t k n"), [h[:]],
                  [("x", ("hT", t0 + i)) for i in range(nt)])
            ps = g.psb[blk % 2]
            for t in range(nt):
                for kk in range(8):
                    k.op("pe", lambda: pe.matmul(ps[:, t * 128:(t + 1) * 128], lhsT=w[:, kk, :], rhs=h[:, t, kk, :], start=(kk == 0),
                                                 stop=(kk == 7)), [ps[:]], [w[:], h[:]], inc=(kk == 7 and t == nt - 1))
            off = t0 * 128 + (2 if t0 < 32 else 6)
            k.op("act", lambda: act.copy(out=P[:, off:off + nt * 128], in_=ps[:, 0:nt * 128]), [P[:]], [ps[:]])
        if fc + 1 < 24:
            load_w(fc + 1)
        if pending_store is not None:
            pf, prb = pending_store
            k.dma("pool", g.d_qkv[pf], prb[:], [("x", ("qkv", pf))], [prb[:]])
            pending_store = None
        r = R[fc % 2]
        for (o0, n, p0) in ((0, L, 0), (L, CTX, L + 4)):
            k.op("dve", lambda: vec.tensor_scalar(out=r[:, o0:o0 + n], in0=P[:, p0:p0 + n], scalar1=cw[:, fc, 0:1], scalar2=None,
                                                  op0=ALU.mult), [r[:]], [P[:], cw[:]])
            for j in range(1, 5):
                k.op("dve", lambda: vec.scalar_tensor_tensor(out=r[:, o0:o0 + n], in0=P[:, p0 + j:p0 + j + n], scalar=cw[:, fc, j:j + 1],
                                                            in1=r[:, o0:o0 + n], op0=ALU.mult, op1=ALU.add), [r[:]], [P[:], cw[:], r[:]])
        rb = RB[fc % 2]
        if fc >= 16:
            k.op("act", lambda: act.activation(out=rb[:], in_=r[:], func=AF.Silu), [rb[:]], [r[:]])
        else:
            k.op("act", lambda: act.activation(out=r[:], in_=r[:], func=AF.Silu), [r[:]], [r[:]])
        if fc < 16:
            scl = (128.0 ** -0.5) if fc < 8 else 1.0
            for b in range((NTOK + 511) // 512):
                c0 = b * 512
                n = min(512, NTOK - c0)
                s_, r_ = sq[b % 2], rn[b % 2]
                ps = g.psb[2 + b % 2]
                k.op("pool", lambda: pool.tensor_tensor(out=s_[:, 0:n], in0=r[:, c0:c0 + n], in1=r[:, c0:c0 + n], op=ALU.mult), [s_[:]],
                     [r[:]])
                k.op("pe", lambda: pe.matmul(ps[:, 0:n], lhsT=g.ones, rhs=s_[:, 0:n], start=True, stop=True), [ps[:]], [s_[:], g.cst[:]])
                k.op("act", lambda: act.activation(out=r_[:, 0:n], in_=ps[:, 0:n], func=AF.Sqrt, bias=g.epsT), [r_[:]], [ps[:], g.cst[:]])
                k.op("dve", lambda: vec.reciprocal(out=r_[:, 0:n], in_=r_[:, 0:n]), [r_[:]], [r_[:]])
                k.op("dve", lambda: vec.scalar_tensor_tensor(out=rb[:, c0:c0 + n], in0=r[:, c0:c0 + n], scalar=scl, in1=r_[:, 0:n],
                                                            op0=ALU.mult, op1=ALU.mult), [rb[:]], [r[:], r_[:]])
        pending_store = (fc, rb)
    pf, prb = pending_store
    k.dma("pool", g.d_qkv[pf], prb[:], [("x", ("qkv", pf))], [prb[:]])
    k.release(m)


def gdn_scan(k, g):
    nc = k.nc
    vec, act, pe, pool = nc.vector, nc.scalar, nc.tensor, nc.gpsimd
    m = k.mark()
    BC = 8
    BW = BC * 64
    id64 = g.cst[0:64, C_ID:C_ID + 64]
    TM = g.TM
    masks = {nm: g.cst[0:64, c0:c0 + 64] for nm, c0 in (("ls", C_NMLS), ("us", C_NMUS), ("li", C_NMLI), ("ui", C_NMUI))}
    idb = k.sb([128, 128], BF16, "gidb")
    k.op("dve", lambda: vec.tensor_copy(out=idb[:], in_=g.ident), [idb[:]], [g.cst[:]])

    class BS:
        pass

    sets = []
    for d in range(2):
        b_ = BS()
        n_ = "d%d" % d
        b_.qTb = k.sb([128, BW], BF16, "qTb" + n_)
        b_.kTb = k.sb([128, BW], BF16, "kTb" + n_)
        b_.vTb = k.sb([128, BW], BF16, "vTb" + n_)
        b_.grow = k.sb([64, BC, 64], F32, "grow" + n_)
        b_.brow = k.sb([64, BC, 64], F32, "brow" + n_)
        b_.cdb = k.sb([128, BC], F32, "cdb" + n_)
        b_.ktm = k.sb([64, BC, 128], BF16, "ktm" + n_)
        b_.vtm = k.sb([64, BC, 128], BF16, "vtm" + n_)
        b_.kdec = k.sb([64, BC, 128], BF16, "kdec" + n_)
        b_.dif = k.sb([64, BC, 64], F32, "dif" + n_)
        b_.E = [k.sb([64, BC, 64], F32, "gE%d%s" % (i, n_)) for i in range(3)]
        b_.Y = [k.sb([64, BC, 64], BF16, "gY%d%s" % (i, n_)) for i in range(2)]
        b_.YT = [k.sb([64, BC, 64], BF16, "gYT%d%s" % (i, n_)) for i in range(2)]
        b_.PT = [k.sb([64, BC, 64], BF16, "gPT%d%s" % (i, n_)) for i in range(2)]
        b_.AQ = k.sb([64, BC, 64], BF16, "gAQ" + n_)
        b_.ob = k.sb([64, BC, 128], F32, "gob" + n_)
        b_.S = k.sb([128, 128], F32, "gS" + n_)
        b_.Sb = k.sb([128, 128], BF16, "gSb" + n_)
        b_.t2a = k.sb([64, 128], F32, "gt2a" + n_)
        b_.t2b = k.sb([64, 128], BF16, "gt2b" + n_)
        b_.vnew = k.sb([64, 128], BF16, "gvnew" + n_)
        b_.t3 = k.sb([64, 128], F32, "gt3" + n_)
        b_.pb = [g.psb[4 * d + i] for i in range(4)]
        sets.append(b_)

    def evac(i, out, in_):
        if i % 2 == 0:
            k.op("act", lambda: act.copy(out=out, in_=in_), [out], [in_])
        else:
            k.op("dve", lambda: vec.tensor_copy(out=out, in_=in_), [out], [in_])

    def run(h, d):
        B_ = sets[d]
        qTb, kTb, vTb, grow, brow, cdb, ktm, vtm, kdec, dif = (B_.qTb, B_.kTb, B_.vTb, B_.grow, B_.brow, B_.cdb, B_.ktm, B_.vtm,
                                                               B_.kdec, B_.dif)
        E, Y, YT, PT, AQ, ob, S, Sb, t2a, t2b, vnew, t3, pb = (B_.E, B_.Y, B_.YT, B_.PT, B_.AQ, B_.ob, B_.S, B_.Sb, B_.t2a, B_.t2b,
                                                              B_.vnew, B_.t3, B_.pb)
        dh = d * 8 + h
        lat = [(c0, 8) for c0 in range(0, 64, 8)]
        batches = [(64, 4)] + (lat if d == 0 else lat[::-1])
        k.op("pool", lambda: pool.memset(S[:], 0.0), [S[:]], [])
        k.op("pool", lambda: pool.memset(Sb[:], 0.0), [Sb[:]], [])

        def rk(r):
            return ("x", ("sc", d, r))

        for (c0, nch) in batches:
            tok0 = c0 * 64
            W = nch * 64
            k.dma("sp", qTb[:, 0:W], g.d_qkv[h][:, tok0:tok0 + W], [qTb[:]], [("x", ("qkv", h))])
            k.dma("sp", kTb[:, 0:W], g.d_qkv[8 + h][:, tok0:tok0 + W], [kTb[:]], [("x", ("qkv", 8 + h))])
            k.dma("sp", vTb[:, 0:W], g.d_qkv[16 + h][:, tok0:tok0 + W], [vTb[:]], [("x", ("qkv", 16 + h))])
            for dst, ri in ((grow, 0), (brow, 1)):
                src = g.d_rows[ri, dh, tok0:tok0 + W]
                k.dma("sp", dst[:, 0:nch].rearrange("p c j -> p (c j)"), bass.AP(src.tensor, src.offset, [[0, 64], [1, W]]),
                      [dst[:]], [("x", "rows")])
            srcc = g.d_cd[dh, c0:c0 + nch]
            k.dma("sp", cdb[:, 0:nch], bass.AP(srcc.tensor, srcc.offset, [[0, 128], [1, nch]]), [cdb[:]], [("x", "rows")])
            yield

            def tmc(q, inner):
                return V(TM[:, c0, q, dh:dh + 1], (64, nch), (0, inner))

            for si, (src, dst) in enumerate(((kTb, ktm), (vTb, vtm))):
                psv = pb[si][:].bitcast(BF16)
                for c in range(nch):
                    k.op("pe", lambda: pe.transpose(psv[0:64, c * 128:(c + 1) * 128], src[:, c * 64:(c + 1) * 64], idb[:]), [pb[si][:]],
                         [src[:], idb[:]], inc=(c == nch - 1))
                evac(si, dst[:, 0:nch].rearrange("p c f -> p (c f)"), psv[0:64, 0:nch * 128])
                yield
            k.op("dve", lambda: vec.tensor_tensor(out=kdec[:, 0:nch], in0=ktm[:, 0:nch], in1=tmc(3, 128), op=ALU.mult), [kdec[:]],
                 [ktm[:], TM[:]])
            yield
            for c in range(nch):
                k.op("pe", lambda: pe.matmul(pb[1][0:64, c * 64:(c + 1) * 64], lhsT=kTb[:, c * 64:(c + 1) * 64],
                                             rhs=kTb[:, c * 64:(c + 1) * 64], start=True, stop=True), [pb[1][:]], [kTb[:]],
                     inc=(c == nch - 1))
            for c in range(nch):
                k.op("pe", lambda: pe.matmul(pb[2][0:64, c * 64:(c + 1) * 64], lhsT=kTb[:, c * 64:(c + 1) * 64],
                                             rhs=qTb[:, c * 64:(c + 1) * 64], start=True, stop=True), [pb[2][:]],
                     [kTb[:], qTb[:]], inc=(c == nch - 1))
            yield
            k.op("dve", lambda: vec.tensor_tensor(out=dif[:, 0:nch], in0=grow[:, 0:nch], in1=tmc(0, 64), op=ALU.subtract), [dif[:]],
                 [grow[:], TM[:]])
            mk = ("ls", "us", "ui") if d == 0 else ("us", "ls", "li")
            for i in range(3):
                mv = V(masks[mk[i]], (0, nch), (1, 64))
                k.op("dve", lambda: vec.tensor_tensor(out=E[i][:, 0:nch], in0=dif[:, 0:nch], in1=mv,
                                                      op=(ALU.subtract if i == 0 else ALU.add)), [E[i][:]], [dif[:], g.cst[:]])
                k.op("act", lambda: act.activation(out=E[i][:, 0:nch], in_=E[i][:, 0:nch], func=AF.Exp,
                                                   scale=(-1.0 if i == 0 else 1.0)), [E[i][:]], [E[i][:]])
                yield
            cs = slice(0, nch)
            kk3 = pb[1][0:64, 0:nch * 64].rearrange("p (c j) -> p c j", j=64)
            qk3 = pb[2][0:64, 0:nch * 64].rearrange("p (c j) -> p c j", j=64)
            k.op("dve", lambda: vec.tensor_tensor(out=E[0][:, cs], in0=kk3, in1=E[0][:, cs], op=ALU.mult), [E[0][:]],
                 [pb[1][:], E[0][:]])
            k.op("dve", lambda: vec.scalar_tensor_tensor(out=Y[0][:, cs], in0=E[0][:, cs], scalar=-1.0, in1=tmc(1, 64),
                                                        op0=ALU.mult, op1=ALU.mult), [Y[0][:]], [E[0][:], TM[:]])
            yield
            k.op("dve", lambda: vec.tensor_tensor(out=E[1][:, cs], in0=kk3, in1=E[1][:, cs], op=ALU.mult), [E[1][:]],
                 [pb[1][:], E[1][:]])
            k.op("dve", lambda: vec.scalar_tensor_tensor(out=YT[0][:, cs], in0=E[1][:, cs], scalar=-1.0, in1=brow[:, cs],
                                                        op0=ALU.mult, op1=ALU.mult), [YT[0][:]], [E[1][:], brow[:]])
            yield
            k.op("dve", lambda: vec.tensor_tensor(out=AQ[:, cs], in0=qk3, in1=E[2][:, cs], op=ALU.mult), [AQ[:]],
                 [pb[2][:], E[2][:]])
            k.op("pool", lambda: pool.tensor_tensor(out=PT[0][:, 0:nch], in0=YT[0][:, 0:nch], in1=V(id64, (0, nch), (1, 64)),
                                                    op=ALU.add), [PT[0][:]], [YT[0][:], g.cst[:]])
            yield
            cy, cp = 0, 0
            for lvl in range(5):
                ny = 1 - cy
                for c in range(nch):
                    k.op("pe", lambda: pe.matmul(pb[0][0:64, c * 64:(c + 1) * 64], lhsT=YT[cy][:, c, :], rhs=Y[cy][:, c, :],
                                                 start=True, stop=True), [pb[0][:]], [YT[cy][:], Y[cy][:]], inc=(c == nch - 1))
                evac(0, Y[ny][:, 0:nch].rearrange("p c j -> p (c j)"), pb[0][0:64, 0:nch * 64])
                yield
                if lvl < 4:
                    for c in range(nch):
                        k.op("pe", lambda: pe.matmul(pb[1][0:64, c * 64:(c + 1) * 64], lhsT=Y[cy][:, c, :], rhs=YT[cy][:, c, :],
                                                     start=True, stop=True), [pb[1][:]], [YT[cy][:], Y[cy][:]], inc=(c == nch - 1))
                    evac(1, YT[ny][:, 0:nch].rearrange("p c j -> p (c j)"), pb[1][0:64, 0:nch * 64])
                    yield
                np_ = 1 - cp
                for c in range(nch):
                    k.op("pe", lambda: pe.matmul(pb[2][0:64, c * 64:(c + 1) * 64], lhsT=Y[ny][:, c, :], rhs=PT[cp][:, c, :],
                                                 start=True, stop=True), [pb[2][:]], [Y[ny][:], PT[cp][:]], inc=(c == nch - 1))
                k.op("dve", lambda: vec.tensor_tensor(out=PT[np_][:, 0:nch].rearrange("p c j -> p (c j)"), in0=pb[2][0:64, 0:nch * 64],
                                                      in1=PT[cp][:, 0:nch].rearrange("p c j -> p (c j)"), op=ALU.add),
                     [PT[np_][:]], [pb[2][:], PT[cp][:]])
                yield
                cy, cp = ny, np_
            TT = PT[cp]
            order = range(nch) if d == 0 else range(nch - 1, -1, -1)
            R = pb[3]
            for c in order:
                gc = c0 + c
                eGc = TM[:, gc, 2, dh:dh + 1]
                bc_ = TM[:, gc, 1, dh:dh + 1]
                k.op("pe", lambda: pe.matmul(R[0:64, 0:128], lhsT=kTb[:, c * 64:(c + 1) * 64], rhs=Sb[:], start=True, stop=True),
                     [rk(0)], [kTb[:], Sb[:]])
                k.op("pe", lambda: pe.matmul(R[0:64, 384:512], lhsT=qTb[:, c * 64:(c + 1) * 64], rhs=Sb[:], start=True, stop=True),
                     [rk(3)], [qTb[:], Sb[:]])
                k.op("dve", lambda: vec.scalar_tensor_tensor(out=t2a[:], in0=R[0:64, 0:128], scalar=eGc, in1=vtm[:, c, :],
                                                            op0=ALU.mult, op1=ALU.subtract), [t2a[:]], [rk(0), TM[:], vtm[:]])
                k.op("dve", lambda: vec.tensor_scalar(out=t2b[:], in0=t2a[:], scalar1=bc_, scalar2=-1.0, op0=ALU.mult, op1=ALU.mult),
                     [t2b[:]], [t2a[:], TM[:]])
                yield
                k.op("pe", lambda: pe.matmul(R[0:64, 128:256], lhsT=TT[:, c, :], rhs=t2b[:], start=True, stop=True), [rk(1)],
                     [TT[:], t2b[:]])
                k.op("act", lambda: act.copy(out=vnew[:], in_=R[0:64, 128:256]), [vnew[:]], [rk(1)])
                yield
                k.op("pe", lambda: pe.matmul(R[:, 256:384], lhsT=kdec[:, c, :], rhs=vnew[:], start=True, stop=True), [rk(2)],
                     [kdec[:], vnew[:]])
                k.op("pe", lambda: pe.matmul(pb[2][0:64, 0:128], lhsT=AQ[:, c, :], rhs=vnew[:], start=True, stop=True), [pb[2][:]],
                     [AQ[:], vnew[:]])
                k.op("dve", lambda: vec.scalar_tensor_tensor(out=S[:], in0=S[:], scalar=cdb[:, c:c + 1], in1=R[:, 256:384],
                                                            op0=ALU.mult, op1=ALU.add), [S[:]], [S[:], cdb[:], rk(2)])
                k.op("act", lambda: act.copy(out=Sb[:], in_=S[:]), [Sb[:]], [S[:]])
                k.op("act", lambda: act.activation(out=t3[:], in_=R[0:64, 384:512], func=AF.Identity, scale=eGc), [t3[:]],
                     [rk(3), TM[:]])
                k.op("dve", lambda: vec.tensor_tensor(out=ob[:, c, :], in0=pb[2][0:64, 0:128], in1=t3[:], op=ALU.add), [ob[:]],
                     [pb[2][:], t3[:]])
                yield
            dsto = g.d_o[d][tok0:tok0 + W, h * 128:(h + 1) * 128].rearrange("(c p) f -> p c f", p=64)
            k.dma("pool", dsto, ob[:, 0:nch], [("x", ("o", d, c0))], [ob[:]])
            yield

    for h in range(8):
        gens = [run(h, 0), run(h, 1)]
        alive = [True, True]
        while any(alive):
            for i in range(2):
                if alive[i]:
                    try:
                        next(gens[i])
                    except StopIteration:
                        alive[i] = False
    k.release(m)


def gdn_out(k, g, li, ntiles):
    nc = k.nc
    vec, act, pe, pool = nc.vector, nc.scalar, nc.tensor, nc.gpsimd
    m = k.mark()
    wz = k.sb([128, 8, 1024], BF16, "wz")
    wo = k.sb([128, 8, 1024], BF16, "wo")
    for kk in range(8):
        k.dma("pool", wz[:, kk, :], g.d_gwin[kk * 128:(kk + 1) * 128, 3072:4096], [wz[:]], [])
        k.dma("pool", wo[:, kk, :], g.d_gwout[kk * 128:(kk + 1) * 128, :], [wo[:]], [])
    ng = k.sb([128, 128], F32, "gng")
    bcast_row_load(k, ng[:], g.d_gng, 128)
    GT = k.sb([128, 1024], F32, "ggt")
    of = [k.sb([128, 1024], F32, "gof%d" % i) for i in range(2)]
    obw = [k.sb([128, 1024], F32, "gobw%d" % i) for i in range(2)]
    xt = [k.sb([128, 1024], F32, "gxt%d" % i) for i in range(2)]
    hT = [k.sb([128, 8, 128], BF16, "ghT%d" % i) for i in range(2)]
    sq = k.sb([128, 1024], F32, "gsq")
    zs = k.sb([128, 1024], F32, "gzs")
    st = k.sb([128, 24], F32, "gst")
    ogT = k.sb([128, 8, 128], BF16, "gogT")
    cur = -1
    for t in range(ntiles):
        r = 0 if t < L // 128 else 1
        if r != cur:
            cur = r
            bcast_row_load(k, GT[:], g.d_mod[li, r, 2], 1024)
        o, o2, x, h = of[t % 2], obw[t % 2], xt[t % 2], hT[t % 2]
        k.dma("sp", o[:], g.d_o[0][t * 128:(t + 1) * 128, :], [o[:]], [("x", ("o", 0, c)) for c in range(0, 72, 8)])
        k.dma("sp", o2[:], g.d_o[1][t * 128:(t + 1) * 128, :], [o2[:]], [("x", ("o", 1, c)) for c in range(0, 72, 8)])
        k.dma("sp", x[:], g.d_xs[t * 128:(t + 1) * 128, :], [x[:]], [("x", ("xs", t))])
        k.dma("sp", h[:], g.d_hT[t], [h[:]], [("x", ("hT", t))])
        for hf in range(2):
            ps = g.psb[hf]
            for kk in range(8):
                k.op("pe", lambda: pe.matmul(ps[:], lhsT=h[:, kk, :], rhs=wz[:, kk, hf * 512:(hf + 1) * 512], start=(kk == 0),
                                             stop=(kk == 7)), [ps[:]], [h[:], wz[:]], inc=(kk == 7))
            k.op("act", lambda: act.activation(out=zs[:, hf * 512:(hf + 1) * 512], in_=ps[:], func=AF.Silu), [zs[:]], [ps[:]])
        k.op("pool", lambda: pool.tensor_tensor(out=o[:], in0=o[:], in1=o2[:], op=ALU.add), [o[:]], [o[:], o2[:]])
        k.op("pool", lambda: pool.tensor_tensor(out=sq[:], in0=o[:], in1=o[:], op=ALU.mult), [sq[:]], [o[:]])
        k.op("dve", lambda: vec.tensor_reduce(out=st[:, 0:8], in_=sq[:].rearrange("p (h f) -> p h f", f=128), axis=AX.X, op=ALU.add),
             [("x", "gst0")], [sq[:]])
        k.op("act", lambda: act.activation(out=st[:, 8:16], in_=st[:, 0:8], func=AF.Sqrt, scale=1.0 / 128, bias=g.epsT), [("x", "gst1")],
             [("x", "gst0"), g.cst[:]])
        k.op("dve", lambda: vec.reciprocal(out=st[:, 16:24], in_=st[:, 8:16]), [("x", "gst2")], [("x", "gst1")])
        o3 = o[:].rearrange("p (h f) -> p h f", f=128)
        k.op("dve", lambda: vec.tensor_tensor(out=o3, in0=o3, in1=V(st[:, 16:17], (1, 8), (0, 128)), op=ALU.mult), [o[:]],
             [o[:], ("x", "gst2")])
        k.op("pool", lambda: pool.tensor_tensor(out=o3, in0=o3, in1=V(ng[:, 0:128], (0, 8), (1, 128)), op=ALU.mult), [o[:]], [o[:], ng[:]])
        k.op("dve", lambda: vec.tensor_tensor(out=o[:], in0=o[:], in1=zs[:], op=ALU.mult), [o[:]], [o[:], zs[:]])
        for kk in range(8):
            ps = g.psb[2 + kk // 4]
            k.op("pe", lambda: pe.transpose(ps[:, (kk % 4) * 128:(kk % 4 + 1) * 128], o[:, kk * 128:(kk + 1) * 128], g.ident), [ps[:]],
                 [o[:], g.cst[:]], inc=(kk % 4 == 3))
            if kk % 4 == 3:
                h0 = kk // 4
                k.op("act", lambda: act.copy(out=ogT[:, h0 * 4:h0 * 4 + 4, :], in_=ps[:].rearrange("p (a b) -> p a b", a=4)), [ogT[:]],
                     [ps[:]])
        for hf in range(2):
            ps = g.psb[4 + hf]
            for kk in range(8):
                k.op("pe", lambda: pe.matmul(ps[:], lhsT=ogT[:, kk, :], rhs=wo[:, kk, hf * 512:(hf + 1) * 512], start=(kk == 0),
                                             stop=(kk == 7)), [ps[:]], [ogT[:], wo[:]], inc=(kk == 7))
            sl = slice(hf * 512, (hf + 1) * 512)
            k.op("dve", lambda: vec.tensor_tensor(out=o2[:, sl], in0=ps[:], in1=GT[:, sl], op=ALU.mult), [o2[:]], [ps[:], GT[:]])
        k.op("pool", lambda: pool.tensor_tensor(out=o2[:], in0=o2[:], in1=x[:], op=ALU.add), [o2[:]], [o2[:], x[:]])
        k.dma("pool", g.d_xs[t * 128:(t + 1) * 128, :], o2[:], [("x", ("xs", t))], [o2[:]])
    k.release(m)


def host_cst2():
    r = np.ones((16, NTOK), np.float32)
    r[:, ::64] = 0.0
    return r


def prep_shared(inp):
    sh = {}
    sh["cst"] = host_consts()
    sh["cst2"] = host_cst2()
    sh["peer_wq"] = np.ascontiguousarray(inp["peer_w_query"], dtype=np.float32)
    sh["peer_sk"] = np.ascontiguousarray(inp["peer_sub_keys"], dtype=np.float32)
    u = np.asarray(inp["peer_u"], dtype=np.float32).reshape(2, 128, 128, 8, 128)
    sh["peer_ut"] = np.ascontiguousarray(u.transpose(0, 1, 4, 3, 2)).reshape(2, 128, 128, 1024)
    sh["peer_v"] = np.ascontiguousarray(inp["peer_v"], dtype=np.float32).reshape(2, 128, 128, 1024)
    sh["final_g"] = np.ascontiguousarray(inp["final_g"], dtype=np.float32)
    sh["ada_w"] = np.ascontiguousarray(inp["ada_w"], dtype=np.float32)
    sh["ada_b"] = np.ascontiguousarray(inp["ada_b"], dtype=np.float32)
    sh["norm1_g"] = np.ascontiguousarray(inp["norm1_g"], dtype=np.float32)
    sh["norm2_g"] = np.ascontiguousarray(inp["norm2_g"], dtype=np.float32)
    sh["gdn_w_in"] = np.ascontiguousarray(inp["gdn_w_in"][0], dtype=np.float32)
    cw = np.asarray(inp["gdn_conv_w"][0], dtype=np.float32)
    sh["gdn_conv"] = np.ascontiguousarray(cw.T.reshape(24, 128, 5))
    sh["gdn_a_log"] = np.ascontiguousarray(inp["gdn_a_log"][0], dtype=np.float32)
    sh["gdn_dt_bias"] = np.ascontiguousarray(inp["gdn_dt_bias"][0], dtype=np.float32)
    sh["gdn_norm_g"] = np.ascontiguousarray(inp["gdn_norm_g"][0], dtype=np.float32)
    sh["gdn_w_out"] = np.ascontiguousarray(inp["gdn_w_out"][0], dtype=np.float32)
    sh["swa_w_in"] = np.ascontiguousarray(inp["swa_w_in"][0], dtype=np.float32)
    sh["swa_w_out"] = np.ascontiguousarray(inp["swa_w_out"][0], dtype=np.float32)
    sh["swa_sinks"] = np.ascontiguousarray(inp["swa_sinks"][0], dtype=np.float32)
    sh["rope"] = host_rope()
    return sh


def prep_core(inp, b):
    pc = {}
    pc["x"] = np.ascontiguousarray(inp["x"][b], dtype=np.float32)
    pc["ctx"] = np.ascontiguousarray(inp["ctx"][b], dtype=np.float32)
    cc = np.stack([np.asarray(inp["c"][b], np.float32), np.asarray(inp["c_ctx"], np.float32)], axis=-1)
    pc["cT"] = np.ascontiguousarray(cc.reshape(8, 128, 2).transpose(1, 0, 2))
    return pc


def host_rope():
    rows = L // 64
    row = np.repeat(np.arange(rows), 64).astype(np.float32)
    col = np.tile(np.arange(64), rows).astype(np.float32)
    inv = (10000.0 ** (-np.arange(0, 32, 2, dtype=np.float32) / 32.0)).astype(np.float32)
    ar = row[:, None] * inv
    ac = col[:, None] * inv
    cos = np.concatenate([np.cos(ar), np.cos(ar), np.cos(ac), np.cos(ac)], axis=1)
    sin = np.concatenate([-np.sin(ar), np.sin(ar), -np.sin(ac), np.sin(ac)], axis=1)
    return np.ascontiguousarray(np.stack([cos, sin]).astype(np.float32))


def swa_phase(k, g, li):
    nc = k.nc
    vec, act, pe, pool = nc.vector, nc.scalar, nc.tensor, nc.gpsimd
    m = k.mark()
    NLT = L // 128
    win = k.sb([128, 8, 1536], BF16, "swin")
    wo = k.sb([128, 8, 1024], BF16, "swo")
    for kk in range(8):
        k.dma("pool", win[:, kk, :], g.d_swin[kk * 128:(kk + 1) * 128, :], [win[:]], [])
        k.dma("pool", wo[:, kk, :], g.d_swout[kk * 128:(kk + 1) * 128, :], [wo[:]], [])
    QT = k.sb([128, 8, L], BF16, "sQT")
    KT = k.sb([128, 4, NTOK], BF16, "sKT")
    V1 = k.sb([128, NT, 4, 65], BF16, "sV1")
    k.op("pool", lambda: pool.memset(V1[:], 1.0), [V1[:]], [])
    idb = k.sb([128, 128], BF16, "sidb")
    nmp = k.sb([128, 128], BF16, "snmp")
    nmn = k.sb([128, 128], BF16, "snmn")
    k.op("dve", lambda: vec.tensor_copy(out=idb[:], in_=g.ident), [idb[:]], [g.cst[:]])
    k.op("dve", lambda: vec.tensor_copy(out=nmp[:], in_=g.cst[:, C_NMP:C_NMP + 128]), [nmp[:]], [g.cst[:]])
    k.op("dve", lambda: vec.tensor_copy(out=nmn[:], in_=g.cst[:, C_NMN:C_NMN + 128]), [nmn[:]], [g.cst[:]])
    esink = k.sb([128, 16], F32, "sesink")
    bcast_row_load(k, esink[:], g.d_ssink, 16)
    k.op("act", lambda: act.activation(out=esink[:], in_=esink[:], func=AF.Exp), [esink[:]], [esink[:]])
    GT = k.sb([128, 1024], F32, "sgt")
    bcast_row_load(k, GT[:], g.d_mod[li, 0, 2], 1024)
    hT = [k.sb([128, 8, 128], BF16, "shT%d" % i) for i in range(2)]
    cs = [k.sb([128, 2, 64], F32, "scs%d" % i) for i in range(2)]
    tmps = [k.sb([128, 512], F32, "stmp%d" % i) for i in range(3)]
    tmp2s = [k.sb([128, 512], F32, "stmp2%d" % i) for i in range(3)]
    qrs = [k.sb([128, 1024], BF16, "sqr%d" % i) for i in range(2)]
    krs = [k.sb([128, 4, 2, 64], BF16, "skr%d" % i) for i in range(2)]
    pb = g.psb
    for t in range(NT):
        lat = t < NLT
        h = hT[t % 2]
        k.dma("sp", h[:], g.d_hT[t], [h[:]], [("x", ("hT", t))])
        if lat:
            c_ = cs[t % 2]
            k.dma("sp", c_[:], g.d_rope[:, t * 128:(t + 1) * 128, :].rearrange("a p e -> p a e"), [c_[:]], [])
        qr, kr = qrs[t % 2], krs[t % 2]
        banks = [0, 1, 2] if lat else [2]
        for b in banks:
            for kk in range(8):
                k.op("pe", lambda: pe.matmul(pb[b][:], lhsT=h[:, kk, :], rhs=win[:, kk, b * 512:(b + 1) * 512], start=(kk == 0),
                                             stop=(kk == 7)), [pb[b][:]], [h[:], win[:]], inc=(kk == 7))
        if lat:
            for b, H, dst in ((0, 8, qr[:, 0:512]), (1, 8, qr[:, 512:1024]), (2, 4, None)):
                W = H * 64
                tmp, tmp2 = tmps[b], tmp2s[b]
                src = pb[b][:, 0:W]
                for hf in range(2):
                    pv = V(pb[b][:, (1 - hf) * 16:(1 - hf) * 16 + 1], (32, 2 * H), (1, 16))
                    sv = V(c_[:, 1, hf * 16:hf * 16 + 1], (0, H), (32, 2), (1, 16))
                    ov = V(tmp[:, hf * 16:hf * 16 + 1], (32, 2 * H), (1, 16))
                    pv4 = V(pb[b][:, (1 - hf) * 16:(1 - hf) * 16 + 1], (64, H), (32, 2), (1, 16))
                    ov4 = V(tmp[:, hf * 16:hf * 16 + 1], (64, H), (32, 2), (1, 16))
                    k.op("dve", lambda: vec.tensor_tensor(out=ov4, in0=pv4, in1=sv, op=ALU.mult), [tmp[:]], [pb[b][:], c_[:]])
                k.op("dve", lambda: vec.tensor_tensor(out=tmp2[:, 0:W].rearrange("p (h e) -> p h e", e=64),
                                                      in0=src.rearrange("p (h e) -> p h e", e=64), in1=V(c_[:, 0, 0:1], (0, H), (1, 64)),
                                                      op=ALU.mult), [tmp2[:]], [pb[b][:], c_[:]])
                if b < 2:
                    k.op("pool", lambda: pool.tensor_tensor(out=dst, in0=tmp[:, 0:W], in1=tmp2[:, 0:W], op=ALU.add), [qr[:]],
                         [tmp[:], tmp2[:]])
                else:
                    for dup in range(2):
                        k.op("pool", lambda: pool.tensor_tensor(out=kr[:, :, dup, :], in0=tmp[:, 0:W].rearrange("p (h e) -> p h e", e=64),
                                                                in1=tmp2[:, 0:W].rearrange("p (h e) -> p h e", e=64), op=ALU.add),
                             [kr[:]], [tmp[:], tmp2[:]])
            psq = pb[3][:].bitcast(BF16)
            for pr in range(8):
                k.op("pe", lambda: pe.transpose(psq[:, pr * 128:(pr + 1) * 128], qr[:, pr * 128:(pr + 1) * 128], idb[:]), [pb[3][:]],
                     [qr[:], idb[:]], inc=(pr == 7))
            k.op("act", lambda: act.copy(out=QT[:, :, t * 128:(t + 1) * 128], in_=psq[:, 0:1024].rearrange("p (a b) -> p a b", a=8)),
                 [QT[:]], [pb[3][:]])
        else:
            for dup in range(2):
                k.op("act", lambda: act.copy(out=kr[:, :, dup, :], in_=pb[2][:, 0:256].rearrange("p (h e) -> p h e", e=64)), [kr[:]],
                     [pb[2][:]])
        psk = pb[4][:].bitcast(BF16)
        for j in range(4):
            k.op("pe", lambda: pe.transpose(psk[:, j * 128:(j + 1) * 128], kr[:, j].rearrange("p a e -> p (a e)"), idb[:]), [pb[4][:]],
                 [kr[:], idb[:]], inc=(j == 3))
        k.op("dve", lambda: vec.tensor_copy(out=KT[:, :, t * 128:(t + 1) * 128], in_=psk[:, 0:512].rearrange("p (a b) -> p a b", a=4)),
             [KT[:]], [pb[4][:]])
        k.op("act", lambda: act.copy(out=V1[:, t, :, 0:64], in_=pb[2][:, 256:512].rearrange("p (a b) -> p a b", a=4)), [V1[:]], [pb[2][:]])
    PT = [k.sb([128, 640], BF16, "sPT%d" % i) for i in range(2)]
    otm = k.sb([128, 1024], BF16, "sotm")
    den = k.sb([128, 8], F32, "sden")
    ogT = k.sb([128, 8, 128], BF16, "sogT")
    xt = [k.sb([128, 1024], F32, "sxt%d" % i) for i in range(2)]
    ysb = k.sb([128, 1024], F32, "sysb")
    psqT = pb[6][:].bitcast(BF16)
    n = 0
    for t in range(NLT):
        x = xt[t % 2]
        k.dma("sp", x[:], g.d_xs[t * 128:(t + 1) * 128, :], [x[:]], [("x", ("xs", t))])
        kbs = ([(t - 1, "p")] if t > 0 else []) + [(t, "c")] + ([(t + 1, "n")] if t < NLT - 1 else []) + [(NLT, "c"), (NLT + 1, "c")]
        nkb = len(kbs)
        for j in range(4):
            pso = pb[4 + j % 2]
            for hh in range(4):
                hq = 4 * j + hh
                pair, u = hq // 2, hq % 2
                sl = slice(u * 64, (u + 1) * 64)
                bs = (n % 2) * 2
                n += 1
                for i, (kt, kind) in enumerate(kbs):
                    dst = pb[bs + i // 4][:, (i % 4) * 128:(i % 4 + 1) * 128]
                    last = kind == "c"
                    k.op("pe", lambda: pe.matmul(dst, lhsT=KT[sl, j, kt * 128:(kt + 1) * 128], rhs=QT[sl, pair, t * 128:(t + 1) * 128],
                                                 start=True, stop=last), [pb[bs + i // 4][:]], [KT[:], QT[:]],
                         inc=(last and (i == nkb - 1 or i == 3)))
                    if not last:
                        mk_ = nmp if kind == "p" else nmn
                        k.op("pe", lambda: pe.matmul(dst, lhsT=idb[:], rhs=mk_[:], start=False, stop=True), [pb[bs + i // 4][:]],
                             [idb[:], mk_[:]], inc=(i == nkb - 1 or i == 3))
                P = PT[n % 2]
                n0 = min(nkb, 4) * 128
                k.op("act", lambda: act.activation(out=P[:, 0:n0], in_=pb[bs][:, 0:n0], func=AF.Exp, scale=0.125), [P[:]], [pb[bs][:]])
                if nkb > 4:
                    k.op("act", lambda: act.activation(out=P[:, 512:640], in_=pb[bs + 1][:, 0:128], func=AF.Exp, scale=0.125), [P[:]],
                         [pb[bs + 1][:]])
                for i, (kt, kind) in enumerate(kbs):
                    k.op("pe", lambda: pe.matmul(pso[:, hh * 128:hh * 128 + 65], lhsT=P[:, i * 128:(i + 1) * 128], rhs=V1[:, kt, j, :],
                                                 start=(i == 0), stop=(i == nkb - 1)), [pso[:]], [P[:], V1[:]],
                         inc=(i == nkb - 1 and hh == 3))
            k.op("dve", lambda: vec.tensor_tensor(out=den[:, 0:4], in0=V(pso[:, 64:65], (128, 4)), in1=esink[:, 4 * j:4 * j + 4], op=ALU.add),
                 [("x", "sden0")], [pso[:], esink[:]])
            k.op("dve", lambda: vec.reciprocal(out=den[:, 4:8], in_=den[:, 0:4]), [("x", "sden1")], [("x", "sden0")])
            k.op("dve", lambda: vec.tensor_tensor(out=otm[:, j * 256:(j + 1) * 256].rearrange("p (a e) -> p a e", e=64),
                                                  in0=V(pso[:, 0:1], (128, 4), (1, 64)), in1=V(den[:, 4:5], (1, 4), (0, 64)), op=ALU.mult),
                 [otm[:]], [pso[:], ("x", "sden1")])
        for kk in range(8):
            k.op("pe", lambda: pe.transpose(psqT[:, kk * 128:(kk + 1) * 128], otm[:, kk * 128:(kk + 1) * 128], idb[:]), [pb[6][:]],
                 [otm[:], idb[:]], inc=(kk == 7))
        k.op("act", lambda: act.copy(out=ogT[:], in_=psqT[:, 0:1024].rearrange("p (a b) -> p a b", a=8)), [ogT[:]], [pb[6][:]])
        for hf in range(2):
            ps = pb[7 - hf]
            for kk in range(8):
                k.op("pe", lambda: pe.matmul(ps[:], lhsT=ogT[:, kk, :], rhs=wo[:, kk, hf * 512:(hf + 1) * 512], start=(kk == 0), stop=(kk == 7)),
                     [ps[:]], [ogT[:], wo[:]], inc=(kk == 7))
            sl2 = slice(hf * 512, (hf + 1) * 512)
            k.op("dve", lambda: vec.tensor_tensor(out=ysb[:, sl2], in0=ps[:], in1=GT[:, sl2], op=ALU.mult), [ysb[:]], [ps[:], GT[:]])
        k.op("pool", lambda: pool.tensor_tensor(out=ysb[:], in0=ysb[:], in1=x[:], op=ALU.add), [ysb[:]], [ysb[:], x[:]])
        k.dma("pool", g.d_xs[t * 128:(t + 1) * 128, :], ysb[:], [("x", ("xs", t))], [ysb[:]])
    k.release(m)


FULL_PHASES = (("adaln",), ("gdn",), ("peer", 0, NT, False), ("swa",), ("peer", 1, L // 128, True))


def kernel(**inputs):
    k = build(phases=FULL_PHASES)
    sh = prep_shared(inputs)
    names = set(k.ext_inputs)
    in_maps = []
    for b in range(8):
        d_ = dict(sh)
        d_.update(prep_core(inputs, b))
        in_maps.append({a: v for a, v in d_.items() if a in names})
    res = run_bass_kernel_spmd(k.nc, in_maps, core_ids=list(range(8)))
    return np.stack([np.asarray(r["out"], dtype=np.float32) for r in res.results], axis=0)
```

```python
import numpy as np
import concourse.bass as bass
import concourse.mybir as mybir
from concourse.bass_utils import run_bass_kernel_spmd

F32 = mybir.dt.float32
BF16 = mybir.dt.bfloat16
U32 = mybir.dt.uint32
AF = mybir.ActivationFunctionType
ALU = mybir.AluOpType
AX = mybir.AxisListType

D = 1024
L = 4096
CTX = 256
NTOK = L + CTX
NT = NTOK // 128
EPS = 1e-6
NEXP_T = 128


class K:
    def __init__(self):
        self.nc = bass.Bass("TRN2", target_bir_lowering=False)
        nc = self.nc
        self.E = {"pe": nc.tensor, "act": nc.scalar, "dve": nc.vector, "pool": nc.gpsimd, "sp": nc.sync}
        self.sem = {e: nc.alloc_semaphore("s_" + e) for e in self.E}
        self.cnt = {e: 0 for e in self.E}
        self.seen = {e: {} for e in self.E}
        self.st = {}
        self.NDMA = 24
        self.dsem = [nc.alloc_semaphore("s_dma%d" % i) for i in range(self.NDMA)]
        self.dval = [0] * self.NDMA
        self.dnext = 0
        self.nsb = 0
        self.stack = []

    def sb(self, shape, dtype=F32, name=None):
        self.nsb += 1
        name = "sb%d_%s" % (self.nsb, name or "t")
        cm = self.nc.sbuf_tensor(name, list(shape), dtype)
        t = cm.__enter__()
        self.stack.append(cm)
        return t

    def mark(self):
        return len(self.stack)

    def release(self, mark):
        self.barrier()
        while len(self.stack) > mark:
            self.stack.pop().__exit__(None, None, None)

    def _semh(self, sk):
        return self.sem[sk] if isinstance(sk, str) else self.dsem[sk]

    def wait(self, e, tok):
        sk, v = tok
        if sk == e and e == "pe":
            return
        if self.seen[e].get(sk, 0) >= v:
            return
        self.E[e].wait_ge(self._semh(sk), v)
        self.seen[e][sk] = v

    @staticmethod
    def _key(x):
        if isinstance(x, tuple):
            return x[1]
        if isinstance(x, str):
            return x
        return x.tensor.name

    def _deps(self, e, outs, ins):
        deps = []
        for x in ins:
            s = self.st.get(self._key(x))
            if s and s[0]:
                deps.append(s[0])
        for x in outs:
            s = self.st.get(self._key(x))
            if s:
                if s[0] and s[0][0] != e:
                    deps.append(s[0])
                for re_, rt in s[1].items():
                    if re_ != e:
                        deps.append(rt)
        for t in deps:
            self.wait(e, t)

    def _commit(self, tok, e, outs, ins):
        for x in ins:
            s = self.st.setdefault(self._key(x), [None, {}])
            s[1][e] = tok
        for x in outs:
            s = self.st.setdefault(self._key(x), [None, {}])
            s[0] = tok
            s[1] = {}

    def op(self, e, fn, outs, ins, inc=True):
        self._deps(e, outs, ins)
        inst = fn()
        tok = (e, self.cnt[e] + 1)
        if inc:
            inst.then_inc(self.sem[e], 1)
            self.cnt[e] += 1
        self._commit(tok, e, outs, ins)
        return inst

    def dma(self, q, out, in_, outs, ins, **kw):
        self._deps(q, outs, ins)
        i = self.dnext
        self.dnext = (self.dnext + 1) % self.NDMA
        if self.dval[i]:
            self.wait(q, (i, self.dval[i]))
        self.dval[i] += 16
        self.E[q].dma_start(out=out, in_=in_, **kw).then_inc(self.dsem[i], 16)
        tok = (i, self.dval[i])
        for x in ins:
            s = self.st.setdefault(self._key(x), [None, {}])
            s[1]["d%d" % i] = tok
        for x in outs:
            s = self.st.setdefault(self._key(x), [None, {}])
            s[0] = tok
            s[1] = {}
        return tok

    def barrier(self):
        toks = [(e, self.cnt[e]) for e in self.E if self.cnt[e]]
        toks += [(i, self.dval[i]) for i in range(self.NDMA) if self.dval[i]]
        for e in self.E:
            for t in toks:
                if t[0] != e:
                    self.wait(e, t)
        self.st = {}

    def finish(self):
        for i in range(self.NDMA):
            if self.dval[i]:
                self.wait("sp", (i, self.dval[i]))
        for e in self.E:
            if e != "sp" and self.cnt[e]:
                self.wait("sp", (e, self.cnt[e]))


def V(ap, *dims):
    return bass.AP(ap.tensor, ap.offset, [list(ap.ap[0])] + [list(d) for d in dims])


C_ID, C_IOTA, C_EPS, C_DSELB, C_ONES, C_NMLS, C_NMUS, C_NMLI, C_NMUI, C_NMP, C_NMN, C_NCOL = 0, 128, 256, 264, 272, 400, 464, 528, 592, 656, 784, 912


def host_consts():
    c = np.zeros((128, C_NCOL), np.float32)
    c[:, C_ID:C_ID + 128] = np.eye(128, dtype=np.float32)
    c[:, C_IOTA:C_IOTA + 128] = np.arange(128, dtype=np.float32)[None, :]
    c[:, C_EPS] = EPS
    c[:, C_EPS + 1] = -0.5
    c[:, C_EPS + 2] = np.e
    c[8:16, C_DSELB] = 1.0
    c[:, C_ONES:C_ONES + 128] = 1.0
    p = np.arange(128)[:, None]
    f = np.arange(64)[None, :]
    NEG = -1e4
    c[:, C_NMLS:C_NMLS + 64] = np.where(p > f, 0.0, NEG)
    c[:, C_NMUS:C_NMUS + 64] = np.where(f > p, 0.0, NEG)
    c[:, C_NMLI:C_NMLI + 64] = np.where(p >= f, 0.0, NEG)
    c[:, C_NMUI:C_NMUI + 64] = np.where(f >= p, 0.0, NEG)
    f2 = np.arange(128)[None, :]
    c[:, C_NMP:C_NMP + 128] = np.where(p >= f2, 0.0, NEG)
    c[:, C_NMN:C_NMN + 128] = np.where(p <= f2, 0.0, NEG)
    return c


class Ctx:
    pass


def load_consts(k, g):
    g.cst = k.sb([128, C_NCOL], F32, "cst_sb")
    k.dma("sp", g.cst[:], g.d_cst, [g.cst[:]], [])
    g.ident = g.cst[:, C_ID:C_ID + 128]
    g.iota = g.cst[:, C_IOTA:C_IOTA + 128]
    g.epsT = g.cst[:, C_EPS:C_EPS + 1]
    g.ones = g.cst[:, C_ONES:C_ONES + 128]
    g.psb = [k.nc.alloc_psum_tensor("psb%d" % i, [128, 512], F32) for i in range(8)]


def bcast_row_load(k, dst, row_ap, n):
    P = dst.shape[0]
    src = bass.AP(row_ap.tensor, row_ap.offset, [[0, P], [1, n]])
    k.dma("sp", dst, src, [dst], [])


def peer_prepass_early(k, g, layers):
    nc = k.nc
    for li in layers:
        sem = nc.alloc_semaphore("s_pp%d" % li)
        k.dsem.append(sem)
        k.dval.append(0)
        si = len(k.dsem) - 1
        n = 0
        for src, dst in ((g.d_ut[li], g.d_utb[li]), (g.d_v[li], g.d_vb[li])):
            sv = src.rearrange("c p f -> (c p) f").rearrange("(a b) f -> a (b f)", b=8)
            dv = dst.rearrange("c p f -> (c p) f").rearrange("(a b) f -> a (b f)", b=8)
            for r0 in range(0, 2048, 128):
                nc.gpsimd.dma_start(out=dv[r0:r0 + 128, :], in_=sv[r0:r0 + 128, :]).then_inc(sem, 16)
                n += 1
        for blk in range(4):
            nc.gpsimd.dma_start(out=g.d_wqb[li][blk], in_=g.d_wq[li][:, blk * 512:(blk + 1) * 512].rearrange("(k p) f -> p k f", p=128)
                                ).then_inc(sem, 16)
            n += 1
        k.dval[si] = 16 * n
        k.st[("wb", li)] = [(si, 16 * n), {}]
        k.pp_tok = getattr(k, "pp_tok", {})
        k.pp_tok[li] = (si, 16 * n)


def peer_prepass(k, g, li):
    m = k.mark()
    CB = 8
    bufs = [k.sb([128, CB, 1024], BF16, "ppb%d" % i) for i in range(3)]
    n = 0
    for src, dst in ((g.d_ut[li], g.d_utb), (g.d_v[li], g.d_vb)):
        for c0 in range(0, 128, CB):
            b = bufs[n % 3]
            n += 1
            k.dma("pool", b[:], src[c0:c0 + CB].rearrange("c p f -> p c f"), [b[:]], [])
            for c1 in range(0, CB, 2):
                k.dma("pool", dst[(c0 + c1) // 2], b[:, c1:c1 + 2, :], [("x", ("wb", li))], [b[:]])
    for blk in range(4):
        b = bufs[n % 3]
        n += 1
        bv = b[:].rearrange("p c (a f) -> p (c a) f", a=2)[:, 0:8, :]
        for kk in range(8):
            k.dma("pool", bv[:, kk, :], g.d_wq[li, kk * 128:(kk + 1) * 128, blk * 512:(blk + 1) * 512], [b[:]], [])
        k.dma("pool", g.d_wqb[blk], bv, [("x", ("wb", li))], [b[:]])
    k.release(m)


def peer_layer(k, g, li, ntiles, final):
    nc = k.nc
    m0 = k.mark()
    TG = 2
    NTOKG = TG * 128
    vec, act, pe, pool = nc.vector, nc.scalar, nc.tensor, nc.gpsimd
    wqb = [k.sb([128, 8, 512], BF16, "wqb%d" % i) for i in range(2)]
    wk = k.sb([128, 16, 128], F32, "wk")
    skn = wk
    k.dma("sp", skn[:], g.d_sk[li].rearrange("h p k j -> k (h p) j"), [skn[:]], [])
    skt = k.sb([128, 16, 128], F32, "skt")
    for hp in range(16):
        ps = g.psb[hp % 2]
        k.op("pe", lambda: pe.transpose(ps[:, 0:128], skn[:, hp, :], g.ident), [ps[:]], [skn[:], g.cst[:]])
        k.op("act", lambda: act.copy(out=skt[:, hp, :], in_=ps[:, 0:128]), [skt[:]], [ps[:]])
    GS = k.sb([128, 1024], F32, "modgs")
    SH = k.sb([128, 1024], F32, "modsh")
    GT = k.sb([128, 1024], F32, "modgt")
    cur_a, cur_e = [-1], [-1]

    def set_mods_a(r):
        if cur_a[0] != r:
            cur_a[0] = r
            bcast_row_load(k, GS[:], g.d_mod[li, r, 3], 1024)
            bcast_row_load(k, SH[:], g.d_mod[li, r, 4], 1024)

    def set_mods_e(r):
        if cur_e[0] != r:
            cur_e[0] = r
            bcast_row_load(k, GT[:], g.d_mod[li, r, 5], 1024)
    if final:
        fg = k.sb([128, 1024], F32, "fing")
        bcast_row_load(k, fg[:], g.d_fg, 1024)
    gbuf = k.sb([128, NTOKG, 128], BF16, "gbuf")
    h2T = [k.sb([128, 8, NTOKG], BF16, "h2T%d" % i) for i in range(2)]
    xt = [[k.sb([128, 1024], F32, "xt%d_%d" % (p, i)) for i in range(TG)] for p in range(2)]
    selT = [[k.sb([128, 3, 128], F32, "selT%d_%d" % (p, i)) for i in range(TG)] for p in range(2)]
    hh = k.sb([128, 1024], F32, "hh")
    ss = k.sb([128, 4], F32, "ss")
    ss2 = k.sb([128, 4], F32, "ss2")
    qT = k.sb([128, 2, 256], F32, "qT")
    top2 = [k.sb([128, 16, 16], F32, "top%d" % i) for i in range(TG)]
    idx2 = [k.sb([128, 16, 16], U32, "idx%d" % i) for i in range(TG)]
    idxf = k.sb([128, 16, 16], F32, "idxf")
    cand = k.sb([128, 8, 256], F32, "cand")
    wk2 = wk[:].rearrange("p (a b) k -> p a (b k)", b=2)
    ctop = k.sb([128, 8, 16], F32, "ctop")
    pos = k.sb([128, 8, 16], U32, "pos")
    posi = k.sb([128, 2, 128], U32, "posi")
    posf = k.sb([128, 2, 128], F32, "posf")
    ee = k.sb([128, 8, 16], F32, "ee")
    zz = k.sb([128, 16], F32, "zz")
    eq = wk[:].rearrange("p a (b i) -> p (a b) i", i=16)
    sel = k.sb([128, 3, 128], F32, "sel")
    TB = 8
    ohA = [k.sb([128, TB, 128], BF16, "ohA%d" % i) for i in range(2)]
    ohB = [k.sb([128, TB, 128], BF16, "ohB%d" % i) for i in range(2)]
    iotab = k.sb([128, 128], BF16, "iotab")
    k.op("dve", lambda: vec.tensor_copy(out=iotab[:], in_=g.iota), [iotab[:]], [g.cst[:]])
    CB = 2
    NWB = 3
    utb = [k.sb([128, CB, 1024], BF16, "utb%d" % i) for i in range(NWB)]
    vbb = [k.sb([128, CB, 1024], BF16, "vbb%d" % i) for i in range(NWB)]
    actS = [k.sb([128, NTOKG], F32, "actS%d" % i) for i in range(2)]
    ga = [k.sb([128, NTOKG], BF16, "ga%d" % i) for i in range(2)]
    osb = hh

    psO = g.psb[0:4]
    psA = g.psb[4:6]
    psX = g.psb[6]
    psS = g.psb[7]

    ngroups = (ntiles + TG - 1) // TG

    def group_tiles(gi):
        return list(range(gi * TG, min(ntiles, (gi + 1) * TG)))

    def A1(gi):
        par = gi % 2
        tiles = group_tiles(gi)
        ntk = len(tiles) * 128
        set_mods_a(0 if tiles[0] < L // 128 else 1)
        hT_ = h2T[par]
        for ti, t in enumerate(tiles):
            x = xt[par][ti]
            k.dma("sp", x[:], g.d_xs[t * 128:(t + 1) * 128, :], [x[:]], [("x", ("xs", t))])
            norm_mod(k, g, x, hh, ss, GS, SH)
            yield
            for hf in range(2):
                for kk in range(4):
                    k.op("pe", lambda: pe.transpose(psX[:, kk * 128:(kk + 1) * 128], hh[:, (hf * 4 + kk) * 128:(hf * 4 + kk + 1) * 128],
                                                    g.ident), [psX[:]], [hh[:], g.cst[:]], inc=(kk == 3))
                k.op("act", lambda: act.copy(out=hT_[:, hf * 4:hf * 4 + 4, ti * 128:(ti + 1) * 128],
                                             in_=psX[:].rearrange("p (a b) -> p a b", a=4)), [hT_[:]], [psX[:]])
                yield
        k.dma("sp", wqb[0][:], g.d_wqb[li][0], [wqb[0][:]], [("x", ("wb", li))])
        for blk in range(4):
            wq = wqb[blk % 2]
            if blk + 1 < 4:
                k.dma("sp", wqb[(blk + 1) % 2][:], g.d_wqb[li][blk + 1], [wqb[(blk + 1) % 2][:]], [("x", ("wb", li))])
            for half in range(2):
                for h2 in range(2):
                    h4 = half * 2 + h2
                    for kk in range(8):
                        k.op("pe", lambda: pe.matmul(psX[:, h2 * 256:h2 * 256 + ntk], lhsT=wq[:, kk, h4 * 128:(h4 + 1) * 128],
                                                     rhs=hT_[:, kk, 0:ntk], start=(kk == 0), stop=(kk == 7)),
                             [psX[:]], [wq[:], hT_[:]], inc=(kk == 7 and h2 == 1))
                    yield
                k.op("act", lambda: act.copy(out=qT[:], in_=psX[:].rearrange("p (a b) -> p a b", a=2)), [qT[:]], [psX[:]])
                for ti in range(len(tiles)):
                    for h2 in range(2):
                        k.op("pe", lambda: pe.matmul(psS[:, (ti * 2 + h2) * 128:(ti * 2 + h2 + 1) * 128],
                                                     lhsT=qT[:, h2, ti * 128:(ti + 1) * 128], rhs=skt[:, blk * 4 + half * 2 + h2, :],
                                                     start=True, stop=True), [psS[:]], [qT[:], skt[:]],
                             inc=(h2 == 1 and ti == len(tiles) - 1))
                yield
                for ti in range(len(tiles)):
                    tp, ix = top2[ti], idx2[ti]
                    hps = [(blk * 4 + half * 2 + h2, (ti * 2 + h2)) for h2 in range(2)]

                    def S(c_):
                        return psS[:, c_ * 128:(c_ + 1) * 128]

                    for hp, c_ in hps:
                        k.op("dve", lambda: vec.max(out=tp[:, hp, 0:8], in_=S(c_)), [("x", ("topa", ti))], [psS[:]])
                    for hp, c_ in hps:
                        k.op("dve", lambda: vec.match_replace(out=wk[:, hp, :], in_to_replace=tp[:, hp, 0:8], in_values=S(c_),
                                                              imm_value=-1e30), [wk[:]], [("x", ("topa", ti)), psS[:]])
                    yield
                    for hp, c_ in hps:
                        k.op("dve", lambda: vec.max(out=tp[:, hp, 8:16], in_=wk[:, hp, :]), [("x", ("topb", ti))], [wk[:]])
                    for hp, c_ in hps:
                        k.op("dve", lambda: vec.max_index(out=ix[:, hp, 0:8], in_max=tp[:, hp, 0:8], in_values=S(c_)),
                             [("x", ("idxa", ti))], [("x", ("topa", ti)), psS[:]])
                    yield
                    for hp, c_ in hps:
                        k.op("dve", lambda: vec.max_index(out=ix[:, hp, 8:16], in_max=tp[:, hp, 8:16], in_values=S(c_)),
                             [("x", ("idxb", ti))], [("x", ("topb", ti)), psS[:]])
                    yield
        for ti, t in enumerate(tiles):
            top, idx = top2[ti], idx2[ti]
            k.op("dve", lambda: vec.tensor_copy(out=idxf[:], in_=idx[:]), [idxf[:]], [("x", ("idxa", ti)), ("x", ("idxb", ti))])
            t1 = V(top[:, 0, :], (32, 8), (1, 16), (0, 16))
            t2 = V(top[:, 1, :], (32, 8), (0, 16), (1, 16))
            k.op("dve", lambda: vec.tensor_tensor(out=cand[:].rearrange("p h (i j) -> p h i j", i=16), in0=t1, in1=t2,
                                                  op=ALU.add), [cand[:]], [("x", ("topa", ti)), ("x", ("topb", ti))])
            yield
            for h in range(8):
                k.op("dve", lambda: vec.max(out=ctop[:, h, 0:8], in_=cand[:, h, :]), [("x", "ctopa")], [cand[:]])
            yield
            for h in range(8):
                k.op("dve", lambda: vec.match_replace(out=wk2[:, h, :], in_to_replace=ctop[:, h, 0:8], in_values=cand[:, h, :],
                                                      imm_value=-1e30), [wk[:]], [("x", "ctopa"), cand[:]])
            yield
            for h in range(8):
                k.op("dve", lambda: vec.max(out=ctop[:, h, 8:16], in_=wk2[:, h, :]), [("x", "ctopb")], [wk[:]])
            yield
            for h in range(8):
                k.op("dve", lambda: vec.max_index(out=pos[:, h, 0:8], in_max=ctop[:, h, 0:8], in_values=cand[:, h, :]),
                     [("x", "posa")], [("x", "ctopa"), cand[:]])
            yield
            for h in range(8):
                k.op("dve", lambda: vec.max_index(out=pos[:, h, 8:16], in_max=ctop[:, h, 8:16], in_values=cand[:, h, :]),
                     [("x", "posb")], [("x", "ctopb"), cand[:]])
            yield
            k.op("dve", lambda: vec.tensor_tensor(out=ee[:], in0=ctop[:], in1=V(ctop[:, 0, 0:1], (16, 8), (0, 16)),
                                                  op=ALU.subtract), [ee[:]], [("x", "ctopa"), ("x", "ctopb")])
            k.op("act", lambda: act.activation(out=ee[:], in_=ee[:], func=AF.Exp), [ee[:]], [ee[:]])
            k.op("dve", lambda: vec.tensor_reduce(out=zz[:, 0:8], in_=ee[:], axis=AX.X, op=ALU.add), [("x", "zz0")], [ee[:]])
            k.op("dve", lambda: vec.reciprocal(out=zz[:, 8:16], in_=zz[:, 0:8]), [("x", "zz1")], [("x", "zz0")])
            k.op("dve", lambda: vec.tensor_tensor(out=sel[:, 2, :].rearrange("p (h r) -> p h r", h=8), in0=ee[:],
                                                  in1=V(zz[:, 8:9], (1, 8), (0, 16)), op=ALU.mult),
                 [("x", "sel2")], [ee[:], ("x", "zz1")])
            yield
            posv = pos[:].rearrange("p h r -> p (h r)")
            k.op("dve", lambda: vec.tensor_single_scalar(out=posi[:, 0, :], in_=posv, scalar=4, op=ALU.logical_shift_right),
                 [("x", "posi0")], [("x", "posa"), ("x", "posb")])
            k.op("dve", lambda: vec.tensor_single_scalar(out=posi[:, 1, :], in_=posv, scalar=15, op=ALU.bitwise_and),
                 [("x", "posi1")], [("x", "posa"), ("x", "posb")])
            k.op("dve", lambda: vec.tensor_copy(out=posf[:], in_=posi[:]), [posf[:]], [("x", "posi0"), ("x", "posi1")])
            yield
            for p_ in range(2):
                k.op("dve", lambda: vec.tensor_tensor(out=eq, in0=V(posf[:, p_, :], (1, 128), (0, 16)),
                                                      in1=V(g.iota[:, 0:16], (0, 128), (1, 16)), op=ALU.is_equal),
                     [wk[:]], [posf[:], g.cst[:]])
                yield
                k.op("pool", lambda: pool.tensor_tensor(out=eq.rearrange("p (h r) i -> p h r i", h=8),
                                                        in0=eq.rearrange("p (h r) i -> p h r i", h=8),
                                                        in1=V(idxf[:, p_, :], (32, 8), (0, 16), (1, 16)), op=ALU.mult),
                     [wk[:]], [wk[:], idxf[:]])
                k.op("dve", lambda: vec.tensor_reduce(out=sel[:, p_, :], in_=eq, axis=AX.X, op=ALU.add),
                     [("x", ("sel", p_))], [wk[:]])
                yield
            for q in range(3):
                k.op("pe", lambda: pe.transpose(psX[:, q * 128:(q + 1) * 128], sel[:, q, :], g.ident), [psX[:]],
                     [("x", ("sel", 0)), ("x", ("sel", 1)), ("x", "sel2"), g.cst[:]], inc=(q == 2))
            sT = selT[par][ti]
            k.op("act", lambda: act.copy(out=sT[:], in_=psX[:, 0:384].rearrange("p (a b) -> p a b", a=3)), [sT[:]], [psX[:]])
            yield

    def A2(gi):
        par = gi % 2
        tiles = group_tiles(gi)
        nq = 0
        for ti, t in enumerate(tiles):
            sT = selT[par][ti]
            for b in range(128 // TB):
                A = ohA[b % 2]
                B = ohB[b % 2]
                t0 = b * TB
                for tt in range(TB):
                    k.op("dve", lambda: vec.tensor_scalar(out=A[:, tt, :], in0=iotab[:], scalar1=sT[:, 0, t0 + tt:t0 + tt + 1],
                                                          scalar2=sT[:, 2, t0 + tt:t0 + tt + 1], op0=ALU.is_equal, op1=ALU.mult),
                         [A[:]], [sT[:], iotab[:]])
                    if False:
                        k.op("pool", lambda: pool.tensor_scalar(out=B[:, tt, :], in0=iotab[:], scalar1=sT[:, 1, t0 + tt:t0 + tt + 1],
                                                                scalar2=None, op0=ALU.is_equal), [("x", ("ohB", b % 2, "p"))],
                             [sT[:], iotab[:], ("x", ("ohBr", b % 2))])
                    else:
                        k.op("dve", lambda: vec.tensor_scalar(out=B[:, tt, :], in0=iotab[:], scalar1=sT[:, 1, t0 + tt:t0 + tt + 1],
                                                              scalar2=None, op0=ALU.is_equal), [("x", ("ohB", b % 2, "d"))],
                             [sT[:], iotab[:], ("x", ("ohBr", b % 2))])
                for q in range(TB // 4):
                    ps = psA[nq % 2]
                    nq += 1
                    for u in range(4):
                        tt = q * 4 + u
                        k.op("pe", lambda: pe.matmul(ps[:, u * 128:(u + 1) * 128], lhsT=B[:, tt, :], rhs=A[:, tt, :], start=True,
                                                     stop=True), [ps[:], ("x", ("ohBr", b % 2))],
                             [A[:], ("x", ("ohB", b % 2, "p")), ("x", ("ohB", b % 2, "d"))], inc=(u == 3))
                    tg0 = ti * 128 + t0 + q * 4
                    k.op("act", lambda: act.copy(out=gbuf[:, tg0:tg0 + 4, :], in_=ps[:].rearrange("p (a b) -> p a b", a=4)),
                         [("x", "gbufW")], [ps[:], ("x", "gbufR"), ("x", "gbufR2")])

    def B(gi, nxt):
        par = gi % 2
        tiles = group_tiles(gi)
        ntg = len(tiles)
        ntok = ntg * 128
        hT_ = h2T[par]

        def load_w(cb):
            b = (cb // CB) % NWB
            k.dma("sp", utb[b][:], g.d_utb[li][cb:cb + CB].rearrange("c p f -> p c f"), [utb[b][:]], [("x", ("wb", li))])
            k.dma("sp", vbb[b][:], g.d_vb[li][cb:cb + CB].rearrange("c p f -> p c f"), [vbb[b][:]], [("x", ("wb", li))])

        def umm(c):
            b = (c // CB) % NWB
            ps = psA[c % 2]
            for kk in range(8):
                k.op("pe", lambda: pe.matmul(ps[:, 0:ntok], lhsT=utb[b][:, c % CB, kk * 128:(kk + 1) * 128],
                                             rhs=hT_[:, kk, 0:ntok], start=(kk == 0), stop=(kk == 7)),
                     [ps[:]], [utb[b][:], hT_[:]], inc=(kk == 7))

        load_w(0)
        load_w(CB)
        umm(0)
        for c in range(128):
            if c % CB == 0 and c + 2 * CB < 128:
                load_w(c + 2 * CB)
            if c + 1 < 128:
                umm(c + 1)
            b = (c // CB) % NWB
            ps = psA[c % 2]
            a_ = actS[c % 2]
            g_ = ga[c % 2]
            k.op("act", lambda: act.activation(out=a_[:, 0:ntok], in_=ps[:, 0:ntok], func=AF.Gelu), [a_[:]], [ps[:]])
            if c % 2 == 0:
                k.op("dve", lambda: vec.tensor_tensor(out=g_[:, 0:ntok], in0=a_[:, 0:ntok], in1=V(gbuf[:, 0, c:c + 1], (128, ntok)),
                                                      op=ALU.mult), [g_[:], ("x", "gbufR")], [a_[:], ("x", "gbufW")])
            else:
                k.op("pool", lambda: pool.tensor_tensor(out=g_[:, 0:ntok], in0=a_[:, 0:ntok], in1=V(gbuf[:, 0, c:c + 1], (128, ntok)),
                                                        op=ALU.mult), [g_[:], ("x", "gbufR2")], [a_[:], ("x", "gbufW")])
            for ti in range(ntg):
                for hf in range(2):
                    k.op("pe", lambda: pe.matmul(psO[ti * 2 + hf][:], lhsT=g_[:, ti * 128:(ti + 1) * 128],
                                                 rhs=vbb[b][:, c % CB, hf * 512:(hf + 1) * 512], start=(c == 0), stop=(c == 127)),
                         [psO[ti * 2 + hf][:]], [g_[:], vbb[b][:]], inc=(ti == ntg - 1 and hf == 1))
            if nxt is not None and c >= 2:
                for _ in range(STEPS):
                    next(nxt, None)
        if nxt is not None:
            for _ in nxt:
                pass
        set_mods_e(0 if tiles[0] < L // 128 else 1)
        for ti, t in enumerate(tiles):
            x = xt[par][ti]
            for hf in range(2):
                sl = slice(hf * 512, (hf + 1) * 512)
                k.op("dve", lambda: vec.tensor_tensor(out=osb[:, sl], in0=psO[ti * 2 + hf][:], in1=GT[:, sl], op=ALU.mult),
                     [osb[:]], [psO[ti * 2 + hf][:], GT[:]])
            k.op("pool", lambda: pool.tensor_tensor(out=osb[:], in0=osb[:], in1=x[:], op=ALU.add), [osb[:]], [osb[:], x[:]])
            if not final:
                k.dma("pool", g.d_xs[t * 128:(t + 1) * 128, :], osb[:], [("x", ("xs", t))], [osb[:]])
            else:
                norm_mod_final(k, g, osb, x, ss2, fg)
                k.dma("pool", g.d_out[t * 128:(t + 1) * 128, :], x[:], [("x", ("out", t))], [x[:]])

    STEPS = 1
    n_a1 = 6 + 24 + 32 * TG * 3 // 3 + 18 * TG
    STEPS = 1
    for _ in A1(0):
        pass
    for gi in range(ngroups):
        A2(gi)
        nxt = A1(gi + 1) if gi + 1 < ngroups else None
        B(gi, nxt)
    k.release(m0)


def norm_mod_final(k, g, x, out, ss, fg):
    nc = k.nc
    vec, act = nc.vector, nc.scalar
    k.op("dve", lambda: vec.scalar_tensor_tensor(out=out[:], in0=x[:], scalar=1.0, in1=x[:], op0=ALU.mult, op1=ALU.mult,
                                                accum_out=ss[:, 0:1]), [out[:], ss[:]], [x[:]])
    k.op("act", lambda: act.activation(out=ss[:, 1:2], in_=ss[:, 0:1], func=AF.Sqrt, scale=1.0 / D, bias=g.epsT), [("x", "fss1")],
         [ss[:]])
    k.op("dve", lambda: vec.reciprocal(out=ss[:, 2:3], in_=ss[:, 1:2]), [("x", "fss2")], [("x", "fss1")])
    k.op("dve", lambda: vec.scalar_tensor_tensor(out=out[:], in0=x[:], scalar=ss[:, 2:3], in1=fg[:], op0=ALU.mult, op1=ALU.mult),
         [out[:]], [x[:], ("x", "fss2"), fg[:]])


def EPS_AP(g):
    return g.epsT[:, 0:1]


def build(phases=("all",), dev=None):
    dev = dev or {}
    k = K()
    nc = k.nc
    g = Ctx()

    k.ext_inputs = []

    def din(name, shape, dt=F32):
        k.ext_inputs.append(name)
        return nc.dram_tensor(name, list(shape), dt, kind="ExternalInput").ap()

    def dint(name, shape, dt=F32):
        return nc.dram_tensor(name, list(shape), dt, kind="Internal").ap()

    g.d_x = din("x", [L, D])
    g.d_ctx = din("ctx", [CTX, D])
    g.d_cst = din("cst", [128, C_NCOL])
    g.d_wq = din("peer_wq", [2, D, 2048])
    g.d_sk = din("peer_sk", [2, 8, 2, 128, 128])
    g.d_ut = din("peer_ut", [2, 128, 128, 1024])
    g.d_v = din("peer_v", [2, 128, 128, 1024])
    g.d_fg = din("final_g", [D])
    g.d_cT = din("cT", [128, 8, 2])
    g.d_adaw = din("ada_w", [2, D, 6144])
    g.d_adab = din("ada_b", [2, 6144])
    g.d_n1g = din("norm1_g", [2, D])
    g.d_n2g = din("norm2_g", [2, D])
    g.d_cst2 = din("cst2", [16, NTOK])
    g.d_gwin = din("gdn_w_in", [D, 4128])
    g.d_gconv = din("gdn_conv", [24, 128, 5])
    g.d_galog = din("gdn_a_log", [2, 8])
    g.d_gdtb = din("gdn_dt_bias", [2, 8])
    g.d_gng = din("gdn_norm_g", [128])
    g.d_gwout = din("gdn_w_out", [D, D])
    g.d_swin = din("swa_w_in", [D, 1536])
    g.d_swout = din("swa_w_out", [D, D])
    g.d_ssink = din("swa_sinks", [16])
    g.d_rope = din("rope", [2, L, 64])
    g.d_hT = dint("hT", [NT, 128, 8, 128], BF16)
    g.d_qkv = dint("qkv", [24, 128, NTOK], BF16)
    g.d_rows = dint("rows", [2, 16, NTOK])
    g.d_cd = dint("cd", [16, NCH])
    g.d_o = dint("gdn_o", [2, NTOK, D])
    if dev.get("mod_ext"):
        g.d_mod = din("mod", [2, 2, 6, D])
    else:
        g.d_mod = dint("mod", [2, 2, 6, D])
    g.d_xs = dint("xs", [NTOK, D])
    g.d_utb = dint("utb", [2, 128, 128, 1024], BF16)
    g.d_vb = dint("vb", [2, 128, 128, 1024], BF16)
    g.d_wqb = dint("wqb", [2, 4, 128, 8, 512], BF16)
    g.d_out = nc.dram_tensor("out", [L, D], F32, kind="ExternalOutput").ap()
    if dev.get("xs_out"):
        g.d_xso = nc.dram_tensor("xs_out", [NTOK, D], F32, kind="ExternalOutput").ap()

    load_consts(k, g)
    peer_prepass_early(k, g, [ph[1] for ph in phases if ph[0] == "peer"])
    for r0 in range(0, L, 512):
        k.dma("sp", g.d_xs[r0:r0 + 512, :], g.d_x[r0:r0 + 512, :], [("x", ("xs", r0 // 128 + i)) for i in range(4)], [])
    k.dma("sp", g.d_xs[L:NTOK, :], g.d_ctx, [("x", ("xs", 32)), ("x", ("xs", 33))], [])

    for ph in phases:
        if ph[0] == "adaln":
            adaln_phase(k, g)
        elif ph[0] == "gdn":
            m_ = k.mark()
            build_hT(k, g, 0, NT)
            gdn_rows(k, g)
            gdn_qkv(k, g)
            gdn_scan(k, g)
            k.release(m_)
            gdn_out(k, g, 0, NT)
        elif ph[0] == "swa":
            build_hT(k, g, 1, NT)
            swa_phase(k, g, 1)
        elif ph[0] == "peer":
            _, li, ntiles, final = ph
            k.st[("wb", li)] = [k.pp_tok[li], {}]
            peer_layer(k, g, li, ntiles, final)
    if dev.get("mod_out"):
        k.barrier()
        d_mo = nc.dram_tensor("mod_out", [2, 2, 6, D], F32, kind="ExternalOutput").ap()
        k.dma("sp", d_mo.rearrange("a b c d -> (a b c) d"), g.d_mod.rearrange("a b c d -> (a b c) d"), [("x", "modo")], [])
    if dev.get("xs_out"):
        k.barrier()
        for r0 in range(0, NTOK, 256):
            k.dma("sp", g.d_xso[r0:r0 + 256, :], g.d_xs[r0:r0 + 256, :], [("x", ("xso", r0))], [])
    k.finish()
    return k


def adaln_phase(k, g):
    nc = k.nc
    m = k.mark()
    cT = k.sb([128, 8, 2], F32, "cT")
    k.dma("sp", cT[:], g.d_cT, [cT[:]], [])
    sc = k.sb([128, 8, 2], F32, "scT")
    k.op("act", lambda: nc.scalar.activation(out=sc[:], in_=cT[:], func=AF.Silu), [sc[:]], [cT[:]])
    bb = k.sb([2, 6144], F32, "adab")
    msb = k.sb([2, 6144], F32, "adam")
    n1 = k.sb([2, 1024], F32, "n1g")
    n2 = k.sb([2, 1024], F32, "n2g")
    o1 = k.sb([2, 1024], F32, "ado1")
    o2 = k.sb([2, 1024], F32, "ado2")
    wb = [k.sb([128, 8, 512], F32, "adaw%d" % i) for i in range(2)]
    for li in range(2):
        bcast_row_load(k, bb[:], g.d_adab[li], 6144)
        bcast_row_load(k, n1[:], g.d_n1g[li], 1024)
        bcast_row_load(k, n2[:], g.d_n2g[li], 1024)
        for n in range(12):
            w = wb[n % 2]
            k.dma("sp", w[:], g.d_adaw[li][:, n * 512:(n + 1) * 512].rearrange("(k p) f -> p k f", p=128), [w[:]], [])
            ps = g.psb[n % 2]
            for kk in range(8):
                k.op("pe", lambda: nc.tensor.matmul(ps[0:2, :], lhsT=sc[:, kk, :], rhs=w[:, kk, :], start=(kk == 0), stop=(kk == 7)),
                     [ps[:]], [sc[:], w[:]], inc=(kk == 7))
            k.op("dve", lambda: nc.vector.tensor_tensor(out=msb[:, n * 512:(n + 1) * 512], in0=ps[0:2, :],
                                                        in1=bb[:, n * 512:(n + 1) * 512], op=ALU.add), [msb[:]], [ps[:], bb[:]])
        k.op("dve", lambda: nc.vector.scalar_tensor_tensor(out=o1[:], in0=msb[:, 1024:2048], scalar=1.0, in1=n1[:], op0=ALU.add,
                                                          op1=ALU.mult), [o1[:]], [msb[:], n1[:]])
        k.op("dve", lambda: nc.vector.scalar_tensor_tensor(out=o2[:], in0=msb[:, 4096:5120], scalar=1.0, in1=n2[:], op0=ALU.add,
                                                          op1=ALU.mult), [o2[:]], [msb[:], n2[:]])
        srcs = [o1[:], msb[:, 0:1024], msb[:, 2048:3072], o2[:], msb[:, 3072:4096], msb[:, 5120:6144]]
        for j in range(6):
            k.dma("sp", g.d_mod[li, :, j, :], srcs[j], [("x", ("mod", li))], [o1[:], o2[:], msb[:]])
    k.release(m)


def norm_mod(k, g, x, hh, ss, GS, SH):
    nc = k.nc
    vec, act, pool = nc.vector, nc.scalar, nc.gpsimd
    k.op("dve", lambda: vec.scalar_tensor_tensor(out=hh[:], in0=x[:], scalar=1.0, in1=x[:], op0=ALU.mult, op1=ALU.mult,
                                                accum_out=ss[:, 0:1]), [hh[:], ss[:]], [x[:]])
    k.op("dve", lambda: vec.tensor_scalar(out=ss[:, 1:2], in0=ss[:, 0:1], scalar1=1.0 / D, scalar2=EPS, op0=ALU.mult, op1=ALU.add),
         [("x", "ss1")], [ss[:]])
    k.op("pool", lambda: pool.tensor_tensor(out=ss[:, 2:3], in0=ss[:, 1:2], in1=g.cst[:, C_EPS + 1:C_EPS + 2], op=ALU.pow),
         [("x", "ss2")], [("x", "ss1"), g.cst[:]])
    k.op("dve", lambda: vec.scalar_tensor_tensor(out=hh[:], in0=x[:], scalar=ss[:, 2:3], in1=GS[:], op0=ALU.mult, op1=ALU.mult),
         [hh[:]], [x[:], ("x", "ss2"), GS[:]])
    k.op("pool", lambda: pool.tensor_tensor(out=hh[:], in0=hh[:], in1=SH[:], op=ALU.add), [hh[:]], [hh[:], SH[:]])


def build_hT(k, g, li, ntiles):
    nc = k.nc
    m = k.mark()
    GS = k.sb([128, 1024], F32, "hgs")
    SH = k.sb([128, 1024], F32, "hsh")
    xt = [k.sb([128, 1024], F32, "hxt%d" % i) for i in range(2)]
    hh = k.sb([128, 1024], F32, "hhh")
    ss = k.sb([128, 4], F32, "hss")
    hTt = [k.sb([128, 8, 128], BF16, "hTt%d" % i) for i in range(3)]
    cur = -1
    pend = None
    for t in range(ntiles):
        r = 0 if t < L // 128 else 1
        if r != cur:
            cur = r
            bcast_row_load(k, GS[:], g.d_mod[li, r, 0], 1024)
            bcast_row_load(k, SH[:], g.d_mod[li, r, 1], 1024)
        x = xt[t % 2]
        k.dma("sp", x[:], g.d_xs[t * 128:(t + 1) * 128, :], [x[:]], [("x", ("xs", t))])
        norm_mod(k, g, x, hh, ss, GS, SH)
        o = hTt[t % 3]
        for kk in range(8):
            ps = g.psb[kk // 4]
            k.op("pe", lambda: nc.tensor.transpose(ps[:, (kk % 4) * 128:(kk % 4 + 1) * 128], hh[:, kk * 128:(kk + 1) * 128], g.ident),
                 [ps[:]], [hh[:], g.cst[:]], inc=(kk % 4 == 3))
            if kk % 4 == 3:
                h0 = kk // 4
                if h0 == 0:
                    k.op("act", lambda: nc.scalar.copy(out=o[:, 0:4, :], in_=ps[:].rearrange("p (a b) -> p a b", a=4)), [o[:]], [ps[:]])
                else:
                    k.op("dve", lambda: nc.vector.tensor_copy(out=o[:, 4:8, :], in_=ps[:].rearrange("p (a b) -> p a b", a=4)),
                         [o[:]], [ps[:]])
        if pend is not None:
            k.dma("pool", g.d_hT[pend[0]], pend[1][:], [("x", ("hT", pend[0]))], [pend[1][:]])
        pend = (t, o)
    k.dma("pool", g.d_hT[pend[0]], pend[1][:], [("x", ("hT", pend[0]))], [pend[1][:]])
    k.release(m)


NCH = NTOK // 64


def gdn_rows(k, g):
    nc = k.nc
    vec, act, pe = nc.vector, nc.scalar, nc.tensor
    g.TM = k.sb([64, NCH, 4, 16], F32, "gdnTM")
    m = k.mark()
    wab = k.sb([128, 8, 32], BF16, "wab")
    for kk in range(8):
        k.dma("pool", wab[:, kk, :], g.d_gwin[kk * 128:(kk + 1) * 128, 4096:4128], [wab[:]], [])
    alog = k.sb([16, 4], F32, "alog")
    k.dma("sp", alog[:, 0:1], g.d_galog.rearrange("d (h o) -> (d h) o", o=1), [alog[:]], [])
    k.dma("sp", alog[:, 1:2], g.d_gdtb.rearrange("d (h o) -> (d h) o", o=1), [alog[:]], [])
    k.op("act", lambda: act.activation(out=alog[:, 2:3], in_=alog[:, 0:1], func=AF.Exp), [("x", "alog2")], [alog[:]])
    k.op("dve", lambda: vec.tensor_scalar(out=alog[:, 3:4], in0=alog[:, 2:3], scalar1=-1.0, scalar2=None, op0=ALU.mult),
         [("x", "alog3")], [("x", "alog2")])
    rows = {nm: k.sb([16, NTOK], F32, "row_" + nm) for nm in ("g", "beta", "Gf", "Gb", "eG", "kdf")}
    cm = k.sb([16, NTOK], F32, "row_cm")
    k.dma("sp", cm[:], g.d_cst2, [cm[:]], [])
    hb = [k.sb([128, 4, 8, 128], BF16, "rhb%d" % i) for i in range(2)]
    for blk in range((NT + 3) // 4):
        t0 = blk * 4
        nt = min(4, NT - t0)
        h = hb[blk % 2]
        k.dma("sp", h[:, 0:nt], g.d_hT[t0:t0 + nt].rearrange("t p k n -> p t k n"), [h[:]], [("x", ("hT", t0 + i)) for i in range(nt)])
        psa, psb_ = g.psb[(blk % 2) * 2], g.psb[(blk % 2) * 2 + 1]
        for half, ps in ((0, psa), (1, psb_)):
            for t in range(nt):
                for kk in range(8):
                    k.op("pe", lambda: pe.matmul(ps[0:16, t * 128:(t + 1) * 128], lhsT=wab[:, kk, half * 16:(half + 1) * 16],
                                                 rhs=h[:, t, kk, :], start=(kk == 0), stop=(kk == 7)), [ps[:]], [wab[:], h[:]],
                         inc=(kk == 7 and t == nt - 1))
        sl = slice(t0 * 128, (t0 + nt) * 128)
        w = nt * 128
        k.op("act", lambda: act.activation(out=rows["g"][:, sl], in_=psa[0:16, 0:w], func=AF.Exp, bias=alog[:, 1:2]),
             [rows["g"][:]], [psa[:], alog[:]])
        k.op("act", lambda: act.activation(out=rows["g"][:, sl], in_=rows["g"][:, sl], func=AF.Ln, bias=1.0), [rows["g"][:]],
             [rows["g"][:]])
        k.op("dve", lambda: vec.tensor_scalar(out=rows["g"][:, sl], in0=rows["g"][:, sl], scalar1=alog[:, 3:4], scalar2=None,
                                              op0=ALU.mult), [rows["g"][:]], [rows["g"][:], ("x", "alog3")])
        k.op("act", lambda: act.activation(out=rows["beta"][:, sl], in_=psb_[0:16, 0:w], func=AF.Sigmoid), [rows["beta"][:]],
             [psb_[:]])
    G, Gf, Gb, B, eG, kdf = rows["g"], rows["Gf"], rows["Gb"], rows["beta"], rows["eG"], rows["kdf"]
    k.op("dve", lambda: vec.tensor_tensor_scan(out=Gf[:], data0=cm[:], data1=G[:], initial=0.0, op0=ALU.mult, op1=ALU.add),
         [Gf[:]], [cm[:], G[:]])
    tot = V(Gf[:, 63:64], (64, NCH), (0, 64))
    v3 = lambda t: t[:].rearrange("p (c j) -> p c j", j=64)
    k.op("dve", lambda: vec.tensor_tensor(out=v3(Gb), in0=tot, in1=v3(Gf), op=ALU.subtract), [Gb[:]], [Gf[:]])
    k.op("dve", lambda: vec.tensor_tensor(out=Gb[:], in0=Gb[:], in1=G[:], op=ALU.add), [Gb[:]], [Gb[:], G[:]])
    k.op("dve", lambda: vec.tensor_tensor(out=Gb[:], in0=Gb[:], in1=Gf[:], op=ALU.subtract), [Gb[:]], [Gb[:], Gf[:]])
    k.op("dve", lambda: vec.scalar_tensor_tensor(out=Gb[:], in0=Gb[:], scalar=g.cst[0:16, C_DSELB:C_DSELB + 1], in1=Gf[:],
                                                op0=ALU.mult, op1=ALU.add), [Gb[:]], [Gb[:], Gf[:], g.cst[:]])
    GD = Gb
    k.op("act", lambda: act.activation(out=eG[:], in_=GD[:], func=AF.Exp), [eG[:]], [GD[:]])
    k.op("dve", lambda: vec.tensor_tensor(out=v3(kdf), in0=tot, in1=v3(GD), op=ALU.subtract), [kdf[:]], [Gf[:], GD[:]])
    k.op("act", lambda: act.activation(out=kdf[:], in_=kdf[:], func=AF.Exp), [kdf[:]], [kdf[:]])
    cd = k.sb([16, NCH], F32, "row_cd")
    k.op("act", lambda: act.activation(out=cd[:], in_=V(Gf[:, 63:64], (64, NCH)), func=AF.Exp), [cd[:]], [Gf[:]])
    k.dma("sp", g.d_rows[0], GD[:], [("x", "rows")], [GD[:]])
    k.dma("sp", g.d_rows[1], B[:], [("x", "rows")], [B[:]])
    k.dma("sp", g.d_cd, cd[:], [("x", "rows")], [cd[:]])
    for c in range(NCH):
        ps = g.psb[4 + (c // 8) % 2]
        for q, src in enumerate((GD, B, eG, kdf)):
            col = (c % 8) * 64 + q * 16
            k.op("pe", lambda: pe.transpose(ps[0:64, col:col + 16], src[:, c * 64:(c + 1) * 64], g.ident[0:16, 0:16]), [ps[:]],
                 [src[:], g.cst[:]], inc=(q == 3 and (c % 8 == 7 or c == NCH - 1)))
        if c % 8 == 7 or c == NCH - 1:
            c0 = (c // 8) * 8
            n = c - c0 + 1
            k.op("act", lambda: act.copy(out=g.TM[:, c0:c0 + n].rearrange("p c q d -> p (c q d)"), in_=ps[0:64, 0:n * 64]),
                 [g.TM[:]], [ps[:]])
    k.release(m)


def gdn_qkv(k, g):
    nc = k.nc
    vec, act, pe, pool = nc.vector, nc.scalar, nc.tensor, nc.gpsimd
    m = k.mark()
    PW = NTOK + 8
    Ps = [k.sb([128, PW], F32, "gP%d" % i) for i in range(2)]
    for P in Ps:
        k.op("pool", lambda: pool.memset(P[:], 0.0), [P[:]], [])
    R = [k.sb([128, NTOK], F32, "gR%d" % i) for i in range(2)]
    RB = [k.sb([128, NTOK], BF16, "gRB%d" % i) for i in range(2)]
    cw = k.sb([128, 24, 5], F32, "gcw")
    k.dma("sp", cw[:], g.d_gconv.rearrange("f p j -> p f j"), [cw[:]], [])
    wf = [k.sb([128, 8, 128], BF16, "gwf%d" % i) for i in range(2)]
    hb = [k.sb([128, 4, 8, 128], BF16, "ghb%d" % i) for i in range(2)]
    sq = [k.sb([128, 512], F32, "gsq%d" % i) for i in range(2)]
    rn = [k.sb([128, 512], F32, "grn%d" % i) for i in range(2)]
    nblk = (NT + 3) // 4
    def load_w(fc_):
        w_ = wf[fc_ % 2]
        for kk in range(8):
            k.dma("pool", w_[:, kk, :], g.d_gwin[kk * 128:(kk + 1) * 128, fc_ * 128:(fc_ + 1) * 128], [w_[:]], [])

    load_w(0)
    pending_store = None
    for fc in range(24):
        w = wf[fc % 2]
        P = Ps[fc % 2]
        for blk in range(nblk):
            t0 = blk * 4
            nt = min(4, NT - t0)
            h = hb[blk % 2]
            k.dma("sp", h[:, 0:nt], g.d_hT[t0:t0 + nt].rearrange("t p k n -> p t k n"), [h[:]],
                  [("x", ("hT", t0 + i)) for i in range(nt)])
            ps = g.psb[blk % 2]
            for t in range(nt):
                for kk in range(8):
                    k.op("pe", lambda: pe.matmul(ps[:, t * 128:(t + 1) * 128], lhsT=w[:, kk, :], rhs=h[:, t, kk, :], start=(kk == 0),
                                                 stop=(kk == 7)), [ps[:]], [w[:], h[:]], inc=(kk == 7 and t == nt - 1))
            off = t0 * 128 + (2 if t0 < 32 else 6)
            k.op("act", lambda: act.copy(out=P[:, off:off + nt * 128], in_=ps[:, 0:nt * 128]), [P[:]], [ps[:]])
        if fc + 1 < 24:
            load_w(fc + 1)
        if pending_store is not None:
            pf, prb = pending_store
            k.dma("pool", g.d_qkv[pf], prb[:], [("x", ("qkv", pf))], [prb[:]])
            pending_store = None
        r = R[fc % 2]
        for (o0, n, p0) in ((0, L, 0), (L, CTX, L + 4)):
            k.op("dve", lambda: vec.tensor_scalar(out=r[:, o0:o0 + n], in0=P[:, p0:p0 + n], scalar1=cw[:, fc, 0:1], scalar2=None,
                                                  op0=ALU.mult), [r[:]], [P[:], cw[:]])
            for j in range(1, 5):
                k.op("dve", lambda: vec.scalar_tensor_tensor(out=r[:, o0:o0 + n], in0=P[:, p0 + j:p0 + j + n], scalar=cw[:, fc, j:j + 1],
                                                            in1=r[:, o0:o0 + n], op0=ALU.mult, op1=ALU.add), [r[:]], [P[:], cw[:], r[:]])
        rb = RB[fc % 2]
        if fc >= 16:
            k.op("act", lambda: act.activation(out=rb[:], in_=r[:], func=AF.Silu), [rb[:]], [r[:]])
        else:
            k.op("act", lambda: act.activation(out=r[:], in_=r[:], func=AF.Silu), [r[:]], [r[:]])
        if fc < 16:
            scl = (128.0 ** -0.5) if fc < 8 else 1.0
            for b in range((NTOK + 511) // 512):
                c0 = b * 512
                n = min(512, NTOK - c0)
                s_, r_ = sq[b % 2], rn[b % 2]
                ps = g.psb[2 + b % 2]
                k.op("pool", lambda: pool.tensor_tensor(out=s_[:, 0:n], in0=r[:, c0:c0 + n], in1=r[:, c0:c0 + n], op=ALU.mult), [s_[:]],
                     [r[:]])
                k.op("pe", lambda: pe.matmul(ps[:, 0:n], lhsT=g.ones, rhs=s_[:, 0:n], start=True, stop=True), [ps[:]], [s_[:], g.cst[:]])
                k.op("act", lambda: act.activation(out=r_[:, 0:n], in_=ps[:, 0:n], func=AF.Sqrt, bias=g.epsT), [r_[:]], [ps[:], g.cst[:]])
                k.op("dve", lambda: vec.reciprocal(out=r_[:, 0:n], in_=r_[:, 0:n]), [r_[:]], [r_[:]])
                k.op("dve", lambda: vec.scalar_tensor_tensor(out=rb[:, c0:c0 + n], in0=r[:, c0:c0 + n], scalar=scl, in1=r_[:, 0:n],
                                                            op0=ALU.mult, op1=ALU.mult), [rb[:]], [r[:], r_[:]])
        pending_store = (fc, rb)
    pf, prb = pending_store
    k.dma("pool", g.d_qkv[pf], prb[:], [("x", ("qkv", pf))], [prb[:]])
    k.release(m)


def gdn_scan(k, g):
    nc = k.nc
    vec, act, pe, pool = nc.vector, nc.scalar, nc.tensor, nc.gpsimd
    m = k.mark()
    BC = 8
    BW = BC * 64
    id64 = g.cst[0:64, C_ID:C_ID + 64]
    TM = g.TM
    masks = {nm: g.cst[0:64, c0:c0 + 64] for nm, c0 in (("ls", C_NMLS), ("us", C_NMUS), ("li", C_NMLI), ("ui", C_NMUI))}
    idb = k.sb([128, 128], BF16, "gidb")
    k.op("dve", lambda: vec.tensor_copy(out=idb[:], in_=g.ident), [idb[:]], [g.cst[:]])

    class BS:
        pass

    sets = []
    for d in range(4):
        b_ = BS()
        n_ = "d%d" % d
        b_.qTb = k.sb([128, BW], BF16, "qTb" + n_)
        b_.kTb = k.sb([128, BW], BF16, "kTb" + n_)
        b_.vTb = k.sb([128, BW], BF16, "vTb" + n_)
        b_.grow = k.sb([64, BC, 64], F32, "grow" + n_)
        b_.brow = k.sb([64, BC, 64], F32, "brow" + n_)
        b_.cdb = k.sb([128, BC], F32, "cdb" + n_)
        b_.ktm = k.sb([64, BC, 128], BF16, "ktm" + n_)
        b_.vtm = k.sb([64, BC, 128], BF16, "vtm" + n_)
        b_.kdec = k.sb([64, BC, 128], BF16, "kdec" + n_)
        b_.dif = k.sb([64, BC, 64], F32, "dif" + n_)
        b_.E = [k.sb([64, BC, 64], F32, "gE%d%s" % (i, n_)) for i in range(3)]
        b_.Y = [k.sb([64, BC, 64], BF16, "gY%d%s" % (i, n_)) for i in range(2)]
        b_.YT = [k.sb([64, BC, 64], BF16, "gYT%d%s" % (i, n_)) for i in range(2)]
        b_.PT = [k.sb([64, BC, 64], BF16, "gPT%d%s" % (i, n_)) for i in range(2)]
        b_.AQ = k.sb([64, BC, 64], BF16, "gAQ" + n_)
        b_.ob = k.sb([64, BC, 128], F32, "gob" + n_)
        b_.S = k.sb([128, 128], F32, "gS" + n_)
        b_.Sb = k.sb([128, 128], BF16, "gSb" + n_)
        b_.t2a = k.sb([64, 128], F32, "gt2a" + n_)
        b_.t2b = k.sb([64, 128], BF16, "gt2b" + n_)
        b_.vnew = k.sb([64, 128], BF16, "gvnew" + n_)
        b_.t3 = k.sb([64, 128], F32, "gt3" + n_)
        b_.pb = [g.psb[2 * d], g.psb[2 * d + 1], g.psb[2 * d], g.psb[2 * d + 1]]
        sets.append(b_)

    def evac(i, out, in_):
        if i % 2 == 0:
            k.op("act", lambda: act.copy(out=out, in_=in_), [out], [in_])
        else:
            k.op("dve", lambda: vec.tensor_copy(out=out, in_=in_), [out], [in_])

    def run(h, d, si):
        B_ = sets[si]
        qTb, kTb, vTb, grow, brow, cdb, ktm, vtm, kdec, dif = (B_.qTb, B_.kTb, B_.vTb, B_.grow, B_.brow, B_.cdb, B_.ktm, B_.vtm,
                                                               B_.kdec, B_.dif)
        E, Y, YT, PT, AQ, ob, S, Sb, t2a, t2b, vnew, t3, pb = (B_.E, B_.Y, B_.YT, B_.PT, B_.AQ, B_.ob, B_.S, B_.Sb, B_.t2a, B_.t2b,
                                                              B_.vnew, B_.t3, B_.pb)
        dh = d * 8 + h
        lat = [(c0, 8) for c0 in range(0, 64, 8)]
        batches = [(64, 4)] + (lat if d == 0 else lat[::-1])
        k.op("pool", lambda: pool.memset(S[:], 0.0), [S[:]], [])
        k.op("pool", lambda: pool.memset(Sb[:], 0.0), [Sb[:]], [])

        def rk(r):
            return pb[3][:]

        for (c0, nch) in batches:
            tok0 = c0 * 64
            W = nch * 64
            k.dma("sp", qTb[:, 0:W], g.d_qkv[h][:, tok0:tok0 + W], [qTb[:]], [("x", ("qkv", h))])
            k.dma("sp", kTb[:, 0:W], g.d_qkv[8 + h][:, tok0:tok0 + W], [kTb[:]], [("x", ("qkv", 8 + h))])
            k.dma("sp", vTb[:, 0:W], g.d_qkv[16 + h][:, tok0:tok0 + W], [vTb[:]], [("x", ("qkv", 16 + h))])
            for dst, ri in ((grow, 0), (brow, 1)):
                src = g.d_rows[ri, dh, tok0:tok0 + W]
                k.dma("sp", dst[:, 0:nch].rearrange("p c j -> p (c j)"), bass.AP(src.tensor, src.offset, [[0, 64], [1, W]]),
                      [dst[:]], [("x", "rows")])
            srcc = g.d_cd[dh, c0:c0 + nch]
            k.dma("sp", cdb[:, 0:nch], bass.AP(srcc.tensor, srcc.offset, [[0, 128], [1, nch]]), [cdb[:]], [("x", "rows")])
            yield

            def tmc(q, inner):
                return V(TM[:, c0, q, dh:dh + 1], (64, nch), (0, inner))

            for si, (src, dst) in enumerate(((kTb, ktm), (vTb, vtm))):
                psv = pb[si][:].bitcast(BF16)
                for c in range(nch):
                    k.op("pe", lambda: pe.transpose(psv[0:64, c * 128:(c + 1) * 128], src[:, c * 64:(c + 1) * 64], idb[:]), [pb[si][:]],
                         [src[:], idb[:]], inc=(c == nch - 1))
                evac(si, dst[:, 0:nch].rearrange("p c f -> p (c f)"), psv[0:64, 0:nch * 128])
                yield
            k.op("dve", lambda: vec.tensor_tensor(out=kdec[:, 0:nch], in0=ktm[:, 0:nch], in1=tmc(3, 128), op=ALU.mult), [kdec[:]],
                 [ktm[:], TM[:]])
            yield
            for c in range(nch):
                k.op("pe", lambda: pe.matmul(pb[1][0:64, c * 64:(c + 1) * 64], lhsT=kTb[:, c * 64:(c + 1) * 64],
                                             rhs=kTb[:, c * 64:(c + 1) * 64], start=True, stop=True), [pb[1][:]], [kTb[:]],
                     inc=(c == nch - 1))
            for c in range(nch):
                k.op("pe", lambda: pe.matmul(pb[2][0:64, c * 64:(c + 1) * 64], lhsT=kTb[:, c * 64:(c + 1) * 64],
                                             rhs=qTb[:, c * 64:(c + 1) * 64], start=True, stop=True), [pb[2][:]],
                     [kTb[:], qTb[:]], inc=(c == nch - 1))
            yield
            k.op("dve", lambda: vec.tensor_tensor(out=dif[:, 0:nch], in0=grow[:, 0:nch], in1=tmc(0, 64), op=ALU.subtract), [dif[:]],
                 [grow[:], TM[:]])
            mk = ("ls", "us", "ui") if d == 0 else ("us", "ls", "li")
            for i in range(3):
                mv = V(masks[mk[i]], (0, nch), (1, 64))
                k.op("dve", lambda: vec.tensor_tensor(out=E[i][:, 0:nch], in0=dif[:, 0:nch], in1=mv,
                                                      op=(ALU.subtract if i == 0 else ALU.add)), [E[i][:]], [dif[:], g.cst[:]])
                k.op("act", lambda: act.activation(out=E[i][:, 0:nch], in_=E[i][:, 0:nch], func=AF.Exp,
                                                   scale=(-1.0 if i == 0 else 1.0)), [E[i][:]], [E[i][:]])
                yield
            cs = slice(0, nch)
            kk3 = pb[1][0:64, 0:nch * 64].rearrange("p (c j) -> p c j", j=64)
            qk3 = pb[2][0:64, 0:nch * 64].rearrange("p (c j) -> p c j", j=64)
            k.op("dve", lambda: vec.tensor_tensor(out=E[0][:, cs], in0=kk3, in1=E[0][:, cs], op=ALU.mult), [E[0][:]],
                 [pb[1][:], E[0][:]])
            k.op("dve", lambda: vec.scalar_tensor_tensor(out=Y[0][:, cs], in0=E[0][:, cs], scalar=-1.0, in1=tmc(1, 64),
                                                        op0=ALU.mult, op1=ALU.mult), [Y[0][:]], [E[0][:], TM[:]])
            yield
            k.op("dve", lambda: vec.tensor_tensor(out=E[1][:, cs], in0=kk3, in1=E[1][:, cs], op=ALU.mult), [E[1][:]],
                 [pb[1][:], E[1][:]])
            k.op("dve", lambda: vec.scalar_tensor_tensor(out=YT[0][:, cs], in0=E[1][:, cs], scalar=-1.0, in1=brow[:, cs],
                                                        op0=ALU.mult, op1=ALU.mult), [YT[0][:]], [E[1][:], brow[:]])
            yield
            k.op("dve", lambda: vec.tensor_tensor(out=AQ[:, cs], in0=qk3, in1=E[2][:, cs], op=ALU.mult), [AQ[:]],
                 [pb[2][:], E[2][:]])
            k.op("pool", lambda: pool.tensor_tensor(out=PT[0][:, 0:nch], in0=YT[0][:, 0:nch], in1=V(id64, (0, nch), (1, 64)),
                                                    op=ALU.add), [PT[0][:]], [YT[0][:], g.cst[:]])
            yield
            cy, cp = 0, 0
            for lvl in range(5):
                ny = 1 - cy
                for c in range(nch):
                    k.op("pe", lambda: pe.matmul(pb[0][0:64, c * 64:(c + 1) * 64], lhsT=YT[cy][:, c, :], rhs=Y[cy][:, c, :],
                                                 start=True, stop=True), [pb[0][:]], [YT[cy][:], Y[cy][:]], inc=(c == nch - 1))
                evac(0, Y[ny][:, 0:nch].rearrange("p c j -> p (c j)"), pb[0][0:64, 0:nch * 64])
                yield
                if lvl < 4:
                    for c in range(nch):
                        k.op("pe", lambda: pe.matmul(pb[1][0:64, c * 64:(c + 1) * 64], lhsT=Y[cy][:, c, :], rhs=YT[cy][:, c, :],
                                                     start=True, stop=True), [pb[1][:]], [YT[cy][:], Y[cy][:]], inc=(c == nch - 1))
                    evac(1, YT[ny][:, 0:nch].rearrange("p c j -> p (c j)"), pb[1][0:64, 0:nch * 64])
                    yield
                np_ = 1 - cp
                for c in range(nch):
                    k.op("pe", lambda: pe.matmul(pb[2][0:64, c * 64:(c + 1) * 64], lhsT=Y[ny][:, c, :], rhs=PT[cp][:, c, :],
                                                 start=True, stop=True), [pb[2][:]], [Y[ny][:], PT[cp][:]], inc=(c == nch - 1))
                k.op("dve", lambda: vec.tensor_tensor(out=PT[np_][:, 0:nch].rearrange("p c j -> p (c j)"), in0=pb[2][0:64, 0:nch * 64],
                                                      in1=PT[cp][:, 0:nch].rearrange("p c j -> p (c j)"), op=ALU.add),
                     [PT[np_][:]], [pb[2][:], PT[cp][:]])
                yield
                cy, cp = ny, np_
            TT = PT[cp]
            order = range(nch) if d == 0 else range(nch - 1, -1, -1)
            R = pb[3]
            for c in order:
                gc = c0 + c
                eGc = TM[:, gc, 2, dh:dh + 1]
                bc_ = TM[:, gc, 1, dh:dh + 1]
                k.op("pe", lambda: pe.matmul(R[0:64, 0:128], lhsT=kTb[:, c * 64:(c + 1) * 64], rhs=Sb[:], start=True, stop=True),
                     [rk(0)], [kTb[:], Sb[:]])
                k.op("pe", lambda: pe.matmul(R[0:64, 384:512], lhsT=qTb[:, c * 64:(c + 1) * 64], rhs=Sb[:], start=True, stop=True),
                     [rk(3)], [qTb[:], Sb[:]])
                k.op("dve", lambda: vec.scalar_tensor_tensor(out=t2a[:], in0=R[0:64, 0:128], scalar=eGc, in1=vtm[:, c, :],
                                                            op0=ALU.mult, op1=ALU.subtract), [t2a[:]], [rk(0), TM[:], vtm[:]])
                k.op("dve", lambda: vec.tensor_scalar(out=t2b[:], in0=t2a[:], scalar1=bc_, scalar2=-1.0, op0=ALU.mult, op1=ALU.mult),
                     [t2b[:]], [t2a[:], TM[:]])
                yield
                k.op("pe", lambda: pe.matmul(R[0:64, 128:256], lhsT=TT[:, c, :], rhs=t2b[:], start=True, stop=True), [rk(1)],
                     [TT[:], t2b[:]])
                k.op("act", lambda: act.copy(out=vnew[:], in_=R[0:64, 128:256]), [vnew[:]], [rk(1)])
                yield
                k.op("pe", lambda: pe.matmul(R[:, 256:384], lhsT=kdec[:, c, :], rhs=vnew[:], start=True, stop=True), [rk(2)],
                     [kdec[:], vnew[:]])
                k.op("pe", lambda: pe.matmul(pb[2][0:64, 0:128], lhsT=AQ[:, c, :], rhs=vnew[:], start=True, stop=True), [pb[2][:]],
                     [AQ[:], vnew[:]])
                k.op("dve", lambda: vec.scalar_tensor_tensor(out=S[:], in0=S[:], scalar=cdb[:, c:c + 1], in1=R[:, 256:384],
                                                            op0=ALU.mult, op1=ALU.add), [S[:]], [S[:], cdb[:], rk(2)])
                k.op("act", lambda: act.copy(out=Sb[:], in_=S[:]), [Sb[:]], [S[:]])
                k.op("act", lambda: act.activation(out=t3[:], in_=R[0:64, 384:512], func=AF.Identity, scale=eGc), [t3[:]],
                     [rk(3), TM[:]])
                k.op("dve", lambda: vec.tensor_tensor(out=ob[:, c, :], in0=pb[2][0:64, 0:128], in1=t3[:], op=ALU.add), [ob[:]],
                     [pb[2][:], t3[:]])
                yield
            dsto = g.d_o[d][tok0:tok0 + W, h * 128:(h + 1) * 128].rearrange("(c p) f -> p c f", p=64)
            k.dma("pool", dsto, ob[:, 0:nch], [("x", ("o", d, c0))], [ob[:]])
            yield

    for h2 in range(4):
        gens = [run(2 * h2, 0, 0), run(2 * h2, 1, 1), run(2 * h2 + 1, 0, 2), run(2 * h2 + 1, 1, 3)]
        alive = [True] * 4
        while any(alive):
            for i in range(4):
                if alive[i]:
                    try:
                        next(gens[i])
                    except StopIteration:
                        alive[i] = False
    k.release(m)


def gdn_out(k, g, li, ntiles):
    nc = k.nc
    vec, act, pe, pool = nc.vector, nc.scalar, nc.tensor, nc.gpsimd
    m = k.mark()
    wz = k.sb([128, 8, 1024], BF16, "wz")
    wo = k.sb([128, 8, 1024], BF16, "wo")
    for kk in range(8):
        k.dma("pool", wz[:, kk, :], g.d_gwin[kk * 128:(kk + 1) * 128, 3072:4096], [wz[:]], [])
        k.dma("pool", wo[:, kk, :], g.d_gwout[kk * 128:(kk + 1) * 128, :], [wo[:]], [])
    ng = k.sb([128, 128], F32, "gng")
    bcast_row_load(k, ng[:], g.d_gng, 128)
    GT = k.sb([128, 1024], F32, "ggt")
    of = [k.sb([128, 1024], F32, "gof%d" % i) for i in range(2)]
    obw = [k.sb([128, 1024], F32, "gobw%d" % i) for i in range(2)]
    xt = [k.sb([128, 1024], F32, "gxt%d" % i) for i in range(2)]
    hT = [k.sb([128, 8, 128], BF16, "ghT%d" % i) for i in range(2)]
    sq = k.sb([128, 1024], F32, "gsq")
    zs = k.sb([128, 1024], F32, "gzs")
    st = k.sb([128, 24], F32, "gst")
    ogT = k.sb([128, 8, 128], BF16, "gogT")
    cur = -1
    for t in range(ntiles):
        r = 0 if t < L // 128 else 1
        if r != cur:
            cur = r
            bcast_row_load(k, GT[:], g.d_mod[li, r, 2], 1024)
        o, o2, x, h = of[t % 2], obw[t % 2], xt[t % 2], hT[t % 2]
        k.dma("sp", o[:], g.d_o[0][t * 128:(t + 1) * 128, :], [o[:]], [("x", ("o", 0, c)) for c in range(0, 72, 8)])
        k.dma("sp", o2[:], g.d_o[1][t * 128:(t + 1) * 128, :], [o2[:]], [("x", ("o", 1, c)) for c in range(0, 72, 8)])
        k.dma("sp", x[:], g.d_xs[t * 128:(t + 1) * 128, :], [x[:]], [("x", ("xs", t))])
        k.dma("sp", h[:], g.d_hT[t], [h[:]], [("x", ("hT", t))])
        for hf in range(2):
            ps = g.psb[hf]
            for kk in range(8):
                k.op("pe", lambda: pe.matmul(ps[:], lhsT=h[:, kk, :], rhs=wz[:, kk, hf * 512:(hf + 1) * 512], start=(kk == 0),
                                             stop=(kk == 7)), [ps[:]], [h[:], wz[:]], inc=(kk == 7))
            k.op("act", lambda: act.activation(out=zs[:, hf * 512:(hf + 1) * 512], in_=ps[:], func=AF.Silu), [zs[:]], [ps[:]])
        k.op("pool", lambda: pool.tensor_tensor(out=o[:], in0=o[:], in1=o2[:], op=ALU.add), [o[:]], [o[:], o2[:]])
        k.op("pool", lambda: pool.tensor_tensor(out=sq[:], in0=o[:], in1=o[:], op=ALU.mult), [sq[:]], [o[:]])
        k.op("dve", lambda: vec.tensor_reduce(out=st[:, 0:8], in_=sq[:].rearrange("p (h f) -> p h f", f=128), axis=AX.X, op=ALU.add),
             [("x", "gst0")], [sq[:]])
        k.op("act", lambda: act.activation(out=st[:, 8:16], in_=st[:, 0:8], func=AF.Sqrt, scale=1.0 / 128, bias=g.epsT), [("x", "gst1")],
             [("x", "gst0"), g.cst[:]])
        k.op("dve", lambda: vec.reciprocal(out=st[:, 16:24], in_=st[:, 8:16]), [("x", "gst2")], [("x", "gst1")])
        o3 = o[:].rearrange("p (h f) -> p h f", f=128)
        k.op("dve", lambda: vec.tensor_tensor(out=o3, in0=o3, in1=V(st[:, 16:17], (1, 8), (0, 128)), op=ALU.mult), [o[:]],
             [o[:], ("x", "gst2")])
        k.op("pool", lambda: pool.tensor_tensor(out=o3, in0=o3, in1=V(ng[:, 0:128], (0, 8), (1, 128)), op=ALU.mult), [o[:]], [o[:], ng[:]])
        k.op("dve", lambda: vec.tensor_tensor(out=o[:], in0=o[:], in1=zs[:], op=ALU.mult), [o[:]], [o[:], zs[:]])
        for kk in range(8):
            ps = g.psb[2 + kk // 4]
            k.op("pe", lambda: pe.transpose(ps[:, (kk % 4) * 128:(kk % 4 + 1) * 128], o[:, kk * 128:(kk + 1) * 128], g.ident), [ps[:]],
                 [o[:], g.cst[:]], inc=(kk % 4 == 3))
            if kk % 4 == 3:
                h0 = kk // 4
                k.op("act", lambda: act.copy(out=ogT[:, h0 * 4:h0 * 4 + 4, :], in_=ps[:].rearrange("p (a b) -> p a b", a=4)), [ogT[:]],
                     [ps[:]])
        for hf in range(2):
            ps = g.psb[4 + hf]
            for kk in range(8):
                k.op("pe", lambda: pe.matmul(ps[:], lhsT=ogT[:, kk, :], rhs=wo[:, kk, hf * 512:(hf + 1) * 512], start=(kk == 0),
                                             stop=(kk == 7)), [ps[:]], [ogT[:], wo[:]], inc=(kk == 7))
            sl = slice(hf * 512, (hf + 1) * 512)
            k.op("dve", lambda: vec.tensor_tensor(out=o2[:, sl], in0=ps[:], in1=GT[:, sl], op=ALU.mult), [o2[:]], [ps[:], GT[:]])
        k.op("pool", lambda: pool.tensor_tensor(out=o2[:], in0=o2[:], in1=x[:], op=ALU.add), [o2[:]], [o2[:], x[:]])
        k.dma("pool", g.d_xs[t * 128:(t + 1) * 128, :], o2[:], [("x", ("xs", t))], [o2[:]])
    k.release(m)


def host_cst2():
    r = np.ones((16, NTOK), np.float32)
    r[:, ::64] = 0.0
    return r


def prep_shared(inp):
    sh = {}
    sh["cst"] = host_consts()
    sh["cst2"] = host_cst2()
    sh["peer_wq"] = np.ascontiguousarray(inp["peer_w_query"], dtype=np.float32)
    sh["peer_sk"] = np.ascontiguousarray(inp["peer_sub_keys"], dtype=np.float32)
    u = np.asarray(inp["peer_u"], dtype=np.float32).reshape(2, 128, 128, 8, 128)
    sh["peer_ut"] = np.ascontiguousarray(u.transpose(0, 1, 4, 3, 2)).reshape(2, 128, 128, 1024)
    sh["peer_v"] = np.ascontiguousarray(inp["peer_v"], dtype=np.float32).reshape(2, 128, 128, 1024)
    sh["final_g"] = np.ascontiguousarray(inp["final_g"], dtype=np.float32)
    sh["ada_w"] = np.ascontiguousarray(inp["ada_w"], dtype=np.float32)
    sh["ada_b"] = np.ascontiguousarray(inp["ada_b"], dtype=np.float32)
    sh["norm1_g"] = np.ascontiguousarray(inp["norm1_g"], dtype=np.float32)
    sh["norm2_g"] = np.ascontiguousarray(inp["norm2_g"], dtype=np.float32)
    sh["gdn_w_in"] = np.ascontiguousarray(inp["gdn_w_in"][0], dtype=np.float32)
    cw = np.asarray(inp["gdn_conv_w"][0], dtype=np.float32)
    sh["gdn_conv"] = np.ascontiguousarray(cw.T.reshape(24, 128, 5))
    sh["gdn_a_log"] = np.ascontiguousarray(inp["gdn_a_log"][0], dtype=np.float32)
    sh["gdn_dt_bias"] = np.ascontiguousarray(inp["gdn_dt_bias"][0], dtype=np.float32)
    sh["gdn_norm_g"] = np.ascontiguousarray(inp["gdn_norm_g"][0], dtype=np.float32)
    sh["gdn_w_out"] = np.ascontiguousarray(inp["gdn_w_out"][0], dtype=np.float32)
    sh["swa_w_in"] = np.ascontiguousarray(inp["swa_w_in"][0], dtype=np.float32)
    sh["swa_w_out"] = np.ascontiguousarray(inp["swa_w_out"][0], dtype=np.float32)
    sh["swa_sinks"] = np.ascontiguousarray(inp["swa_sinks"][0], dtype=np.float32)
    sh["rope"] = host_rope()
    return sh


def prep_core(inp, b):
    pc = {}
    pc["x"] = np.ascontiguousarray(inp["x"][b], dtype=np.float32)
    pc["ctx"] = np.ascontiguousarray(inp["ctx"][b], dtype=np.float32)
    cc = np.stack([np.asarray(inp["c"][b], np.float32), np.asarray(inp["c_ctx"], np.float32)], axis=-1)
    pc["cT"] = np.ascontiguousarray(cc.reshape(8, 128, 2).transpose(1, 0, 2))
    return pc


def host_rope():
    rows = L // 64
    row = np.repeat(np.arange(rows), 64).astype(np.float32)
    col = np.tile(np.arange(64), rows).astype(np.float32)
    inv = (10000.0 ** (-np.arange(0, 32, 2, dtype=np.float32) / 32.0)).astype(np.float32)
    ar = row[:, None] * inv
    ac = col[:, None] * inv
    cos = np.concatenate([np.cos(ar), np.cos(ar), np.cos(ac), np.cos(ac)], axis=1)
    sin = np.concatenate([-np.sin(ar), np.sin(ar), -np.sin(ac), np.sin(ac)], axis=1)
    return np.ascontiguousarray(np.stack([cos, sin]).astype(np.float32))


def swa_phase(k, g, li):
    nc = k.nc
    vec, act, pe, pool = nc.vector, nc.scalar, nc.tensor, nc.gpsimd
    m = k.mark()
    NLT = L // 128
    win = k.sb([128, 8, 1536], BF16, "swin")
    wo = k.sb([128, 8, 1024], BF16, "swo")
    for kk in range(8):
        k.dma("pool", win[:, kk, :], g.d_swin[kk * 128:(kk + 1) * 128, :], [win[:]], [])
        k.dma("pool", wo[:, kk, :], g.d_swout[kk * 128:(kk + 1) * 128, :], [wo[:]], [])
    QT = k.sb([128, 8, L], BF16, "sQT")
    KT = k.sb([128, 4, NTOK], BF16, "sKT")
    V1 = k.sb([128, NT, 4, 65], BF16, "sV1")
    k.op("pool", lambda: pool.memset(V1[:], 1.0), [V1[:]], [])
    idb = k.sb([128, 128], BF16, "sidb")
    nmp = k.sb([128, 128], BF16, "snmp")
    nmn = k.sb([128, 128], BF16, "snmn")
    k.op("dve", lambda: vec.tensor_copy(out=idb[:], in_=g.ident), [idb[:]], [g.cst[:]])
    k.op("dve", lambda: vec.tensor_copy(out=nmp[:], in_=g.cst[:, C_NMP:C_NMP + 128]), [nmp[:]], [g.cst[:]])
    k.op("dve", lambda: vec.tensor_copy(out=nmn[:], in_=g.cst[:, C_NMN:C_NMN + 128]), [nmn[:]], [g.cst[:]])
    esink = k.sb([128, 16], F32, "sesink")
    bcast_row_load(k, esink[:], g.d_ssink, 16)
    k.op("act", lambda: act.activation(out=esink[:], in_=esink[:], func=AF.Exp), [esink[:]], [esink[:]])
    GT = k.sb([128, 1024], F32, "sgt")
    bcast_row_load(k, GT[:], g.d_mod[li, 0, 2], 1024)
    hT = [k.sb([128, 8, 128], BF16, "shT%d" % i) for i in range(2)]
    cs = [k.sb([128, 2, 64], F32, "scs%d" % i) for i in range(2)]
    tmps = [k.sb([128, 512], F32, "stmp%d" % i) for i in range(3)]
    tmp2s = [k.sb([128, 512], F32, "stmp2%d" % i) for i in range(3)]
    qrs = [k.sb([128, 1024], BF16, "sqr%d" % i) for i in range(2)]
    krs = [k.sb([128, 4, 2, 64], BF16, "skr%d" % i) for i in range(2)]
    pb = g.psb
    for t in range(NT):
        lat = t < NLT
        h = hT[t % 2]
        k.dma("sp", h[:], g.d_hT[t], [h[:]], [("x", ("hT", t))])
        if lat:
            c_ = cs[t % 2]
            k.dma("sp", c_[:], g.d_rope[:, t * 128:(t + 1) * 128, :].rearrange("a p e -> p a e"), [c_[:]], [])
        qr, kr = qrs[t % 2], krs[t % 2]
        banks = [0, 1, 2] if lat else [2]
        for b in banks:
            for kk in range(8):
                k.op("pe", lambda: pe.matmul(pb[b][:], lhsT=h[:, kk, :], rhs=win[:, kk, b * 512:(b + 1) * 512], start=(kk == 0),
                                             stop=(kk == 7)), [pb[b][:]], [h[:], win[:]], inc=(kk == 7))
        if lat:
            for b, H, dst in ((0, 8, qr[:, 0:512]), (1, 8, qr[:, 512:1024]), (2, 4, None)):
                W = H * 64
                tmp, tmp2 = tmps[b], tmp2s[b]
                src = pb[b][:, 0:W]
                for hf in range(2):
                    pv = V(pb[b][:, (1 - hf) * 16:(1 - hf) * 16 + 1], (32, 2 * H), (1, 16))
                    sv = V(c_[:, 1, hf * 16:hf * 16 + 1], (0, H), (32, 2), (1, 16))
                    ov = V(tmp[:, hf * 16:hf * 16 + 1], (32, 2 * H), (1, 16))
                    pv4 = V(pb[b][:, (1 - hf) * 16:(1 - hf) * 16 + 1], (64, H), (32, 2), (1, 16))
                    ov4 = V(tmp[:, hf * 16:hf * 16 + 1], (64, H), (32, 2), (1, 16))
                    k.op("dve", lambda: vec.tensor_tensor(out=ov4, in0=pv4, in1=sv, op=ALU.mult), [tmp[:]], [pb[b][:], c_[:]])
                k.op("dve", lambda: vec.tensor_tensor(out=tmp2[:, 0:W].rearrange("p (h e) -> p h e", e=64),
                                                      in0=src.rearrange("p (h e) -> p h e", e=64), in1=V(c_[:, 0, 0:1], (0, H), (1, 64)),
                                                      op=ALU.mult), [tmp2[:]], [pb[b][:], c_[:]])
                if b < 2:
                    k.op("pool", lambda: pool.tensor_tensor(out=dst, in0=tmp[:, 0:W], in1=tmp2[:, 0:W], op=ALU.add), [qr[:]],
                         [tmp[:], tmp2[:]])
                else:
                    for dup in range(2):
                        k.op("pool", lambda: pool.tensor_tensor(out=kr[:, :, dup, :], in0=tmp[:, 0:W].rearrange("p (h e) -> p h e", e=64),
                                                                in1=tmp2[:, 0:W].rearrange("p (h e) -> p h e", e=64), op=ALU.add),
                             [kr[:]], [tmp[:], tmp2[:]])
            psq = pb[3][:].bitcast(BF16)
            for pr in range(8):
                k.op("pe", lambda: pe.transpose(psq[:, pr * 128:(pr + 1) * 128], qr[:, pr * 128:(pr + 1) * 128], idb[:]), [pb[3][:]],
                     [qr[:], idb[:]], inc=(pr == 7))
            k.op("act", lambda: act.copy(out=QT[:, :, t * 128:(t + 1) * 128], in_=psq[:, 0:1024].rearrange("p (a b) -> p a b", a=8)),
                 [QT[:]], [pb[3][:]])
        else:
            for dup in range(2):
                k.op("act", lambda: act.copy(out=kr[:, :, dup, :], in_=pb[2][:, 0:256].rearrange("p (h e) -> p h e", e=64)), [kr[:]],
                     [pb[2][:]])
        psk = pb[4][:].bitcast(BF16)
        for j in range(4):
            k.op("pe", lambda: pe.transpose(psk[:, j * 128:(j + 1) * 128], kr[:, j].rearrange("p a e -> p (a e)"), idb[:]), [pb[4][:]],
                 [kr[:], idb[:]], inc=(j == 3))
        k.op("dve", lambda: vec.tensor_copy(out=KT[:, :, t * 128:(t + 1) * 128], in_=psk[:, 0:512].rearrange("p (a b) -> p a b", a=4)),
             [KT[:]], [pb[4][:]])
        k.op("act", lambda: act.copy(out=V1[:, t, :, 0:64], in_=pb[2][:, 256:512].rearrange("p (a b) -> p a b", a=4)), [V1[:]], [pb[2][:]])
    PT = [k.sb([128, 640], BF16, "sPT%d" % i) for i in range(2)]
    otm = k.sb([128, 1024], BF16, "sotm")
    den = k.sb([128, 8], F32, "sden")
    ogT = k.sb([128, 8, 128], BF16, "sogT")
    xt = [k.sb([128, 1024], F32, "sxt%d" % i) for i in range(2)]
    ysb = k.sb([128, 1024], F32, "sysb")
    psqT = pb[6][:].bitcast(BF16)
    n = 0
    for t in range(NLT):
        x = xt[t % 2]
        k.dma("sp", x[:], g.d_xs[t * 128:(t + 1) * 128, :], [x[:]], [("x", ("xs", t))])
        kbs = ([(t - 1, "p")] if t > 0 else []) + [(t, "c")] + ([(t + 1, "n")] if t < NLT - 1 else []) + [(NLT, "c"), (NLT + 1, "c")]
        nkb = len(kbs)
        for j in range(4):
            pso = pb[4 + j % 2]
            for hh in range(4):
                hq = 4 * j + hh
                pair, u = hq // 2, hq % 2
                sl = slice(u * 64, (u + 1) * 64)
                bs = (n % 2) * 2
                n += 1
                for i, (kt, kind) in enumerate(kbs):
                    dst = pb[bs + i // 4][:, (i % 4) * 128:(i % 4 + 1) * 128]
                    last = kind == "c"
                    k.op("pe", lambda: pe.matmul(dst, lhsT=KT[sl, j, kt * 128:(kt + 1) * 128], rhs=QT[sl, pair, t * 128:(t + 1) * 128],
                                                 start=True, stop=last), [pb[bs + i // 4][:]], [KT[:], QT[:]],
                         inc=(last and (i == nkb - 1 or i == 3)))
                    if not last:
                        mk_ = nmp if kind == "p" else nmn
                        k.op("pe", lambda: pe.matmul(dst, lhsT=idb[:], rhs=mk_[:], start=False, stop=True), [pb[bs + i // 4][:]],
                             [idb[:], mk_[:]], inc=(i == nkb - 1 or i == 3))
                P = PT[n % 2]
                n0 = min(nkb, 4) * 128
                k.op("act", lambda: act.activation(out=P[:, 0:n0], in_=pb[bs][:, 0:n0], func=AF.Exp, scale=0.125), [P[:]], [pb[bs][:]])
                if nkb > 4:
                    k.op("act", lambda: act.activation(out=P[:, 512:640], in_=pb[bs + 1][:, 0:128], func=AF.Exp, scale=0.125), [P[:]],
                         [pb[bs + 1][:]])
                for i, (kt, kind) in enumerate(kbs):
                    k.op("pe", lambda: pe.matmul(pso[:, hh * 128:hh * 128 + 65], lhsT=P[:, i * 128:(i + 1) * 128], rhs=V1[:, kt, j, :],
                                                 start=(i == 0), stop=(i == nkb - 1)), [pso[:]], [P[:], V1[:]],
                         inc=(i == nkb - 1 and hh == 3))
            k.op("dve", lambda: vec.tensor_tensor(out=den[:, 0:4], in0=V(pso[:, 64:65], (128, 4)), in1=esink[:, 4 * j:4 * j + 4], op=ALU.add),
                 [("x", "sden0")], [pso[:], esink[:]])
            k.op("dve", lambda: vec.reciprocal(out=den[:, 4:8], in_=den[:, 0:4]), [("x", "sden1")], [("x", "sden0")])
            k.op("dve", lambda: vec.tensor_tensor(out=otm[:, j * 256:(j + 1) * 256].rearrange("p (a e) -> p a e", e=64),
                                                  in0=V(pso[:, 0:1], (128, 4), (1, 64)), in1=V(den[:, 4:5], (1, 4), (0, 64)), op=ALU.mult),
                 [otm[:]], [pso[:], ("x", "sden1")])
        for kk in range(8):
            k.op("pe", lambda: pe.transpose(psqT[:, kk * 128:(kk + 1) * 128], otm[:, kk * 128:(kk + 1) * 128], idb[:]), [pb[6][:]],
                 [otm[:], idb[:]], inc=(kk == 7))
        k.op("act", lambda: act.copy(out=ogT[:], in_=psqT[:, 0:1024].rearrange("p (a b) -> p a b", a=8)), [ogT[:]], [pb[6][:]])
        for hf in range(2):
            ps = pb[7 - hf]
            for kk in range(8):
                k.op("pe", lambda: pe.matmul(ps[:], lhsT=ogT[:, kk, :], rhs=wo[:, kk, hf * 512:(hf + 1) * 512], start=(kk == 0), stop=(kk == 7)),
                     [ps[:]], [ogT[:], wo[:]], inc=(kk == 7))
            sl2 = slice(hf * 512, (hf + 1) * 512)
            k.op("dve", lambda: vec.tensor_tensor(out=ysb[:, sl2], in0=ps[:], in1=GT[:, sl2], op=ALU.mult), [ysb[:]], [ps[:], GT[:]])
        k.op("pool", lambda: pool.tensor_tensor(out=ysb[:], in0=ysb[:], in1=x[:], op=ALU.add), [ysb[:]], [ysb[:], x[:]])
        k.dma("pool", g.d_xs[t * 128:(t + 1) * 128, :], ysb[:], [("x", ("xs", t))], [ysb[:]])
    k.release(m)


FULL_PHASES = (("adaln",), ("gdn",), ("peer", 0, NT, False), ("swa",), ("peer", 1, L // 128, True))


def kernel(**inputs):
    k = build(phases=FULL_PHASES)
    sh = prep_shared(inputs)
    names = set(k.ext_inputs)
    in_maps = []
    for b in range(8):
        d_ = dict(sh)
        d_.update(prep_core(inputs, b))
        in_maps.append({a: v for a, v in d_.items() if a in names})
    res = run_bass_kernel_spmd(k.nc, in_maps, core_ids=list(range(8)))
    return np.stack([np.asarray(r["out"], dtype=np.float32) for r in res.results], axis=0)
```

```python
import numpy as np
import concourse.bass as bass
import concourse.mybir as mybir
from concourse.bass_utils import run_bass_kernel_spmd

F32 = mybir.dt.float32
BF16 = mybir.dt.bfloat16
U32 = mybir.dt.uint32
AF = mybir.ActivationFunctionType
ALU = mybir.AluOpType
AX = mybir.AxisListType

D = 1024
L = 4096
CTX = 256
NTOK = L + CTX
NT = NTOK // 128
EPS = 1e-6
NEXP_T = 128


class K:
    def __init__(self):
        self.nc = bass.Bass("TRN2", target_bir_lowering=False)
        nc = self.nc
        self.E = {"pe": nc.tensor, "act": nc.scalar, "dve": nc.vector, "pool": nc.gpsimd, "sp": nc.sync}
        self.sem = {e: nc.alloc_semaphore("s_" + e) for e in self.E}
        self.cnt = {e: 0 for e in self.E}
        self.seen = {e: {} for e in self.E}
        self.st = {}
        self.NDMA = 24
        self.dsem = [nc.alloc_semaphore("s_dma%d" % i) for i in range(self.NDMA)]
        self.dval = [0] * self.NDMA
        self.dnext = 0
        self.nsb = 0
        self.stack = []

    def sb(self, shape, dtype=F32, name=None):
        self.nsb += 1
        name = "sb%d_%s" % (self.nsb, name or "t")
        cm = self.nc.sbuf_tensor(name, list(shape), dtype)
        t = cm.__enter__()
        self.stack.append(cm)
        return t

    def mark(self):
        return len(self.stack)

    def release(self, mark):
        self.barrier()
        while len(self.stack) > mark:
            self.stack.pop().__exit__(None, None, None)

    def _semh(self, sk):
        return self.sem[sk] if isinstance(sk, str) else self.dsem[sk]

    def wait(self, e, tok):
        sk, v = tok
        if sk == e and e == "pe":
            return
        if self.seen[e].get(sk, 0) >= v:
            return
        self.E[e].wait_ge(self._semh(sk), v)
        self.seen[e][sk] = v

    @staticmethod
    def _key(x):
        if isinstance(x, tuple):
            return x[1]
        if isinstance(x, str):
            return x
        return x.tensor.name

    def _deps(self, e, outs, ins):
        deps = []
        for x in ins:
            s = self.st.get(self._key(x))
            if s and s[0]:
                deps.append(s[0])
        for x in outs:
            s = self.st.get(self._key(x))
            if s:
                if s[0] and s[0][0] != e:
                    deps.append(s[0])
                for re_, rt in s[1].items():
                    if re_ != e:
                        deps.append(rt)
        for t in deps:
            self.wait(e, t)

    def _commit(self, tok, e, outs, ins):
        for x in ins:
            s = self.st.setdefault(self._key(x), [None, {}])
            s[1][e] = tok
        for x in outs:
            s = self.st.setdefault(self._key(x), [None, {}])
            s[0] = tok
            s[1] = {}

    def op(self, e, fn, outs, ins, inc=True):
        self._deps(e, outs, ins)
        inst = fn()
        tok = (e, self.cnt[e] + 1)
        if inc:
            inst.then_inc(self.sem[e], 1)
            self.cnt[e] += 1
        self._commit(tok, e, outs, ins)
        return inst

    def dma(self, q, out, in_, outs, ins, **kw):
        self._deps(q, outs, ins)
        i = self.dnext
        self.dnext = (self.dnext + 1) % self.NDMA
        if self.dval[i]:
            self.wait(q, (i, self.dval[i]))
        self.dval[i] += 16
        self.E[q].dma_start(out=out, in_=in_, **kw).then_inc(self.dsem[i], 16)
        tok = (i, self.dval[i])
        for x in ins:
            s = self.st.setdefault(self._key(x), [None, {}])
            s[1]["d%d" % i] = tok
        for x in outs:
            s = self.st.setdefault(self._key(x), [None, {}])
            s[0] = tok
            s[1] = {}
        return tok

    def barrier(self):
        toks = [(e, self.cnt[e]) for e in self.E if self.cnt[e]]
        toks += [(i, self.dval[i]) for i in range(self.NDMA) if self.dval[i]]
        for e in self.E:
            for t in toks:
                if t[0] != e:
                    self.wait(e, t)
        self.st = {}

    def finish(self):
        for i in range(self.NDMA):
            if self.dval[i]:
                self.wait("sp", (i, self.dval[i]))
        for e in self.E:
            if e != "sp" and self.cnt[e]:
                self.wait("sp", (e, self.cnt[e]))


def V(ap, *dims):
    return bass.AP(ap.tensor, ap.offset, [list(ap.ap[0])] + [list(d) for d in dims])


C_ID, C_IOTA, C_EPS, C_DSELB, C_ONES, C_NMLS, C_NMUS, C_NMLI, C_NMUI, C_NMP, C_NMN, C_NCOL = 0, 128, 256, 264, 272, 400, 464, 528, 592, 656, 784, 912


def host_consts():
    c = np.zeros((128, C_NCOL), np.float32)
    c[:, C_ID:C_ID + 128] = np.eye(128, dtype=np.float32)
    c[:, C_IOTA:C_IOTA + 128] = np.arange(128, dtype=np.float32)[None, :]
    c[:, C_EPS] = EPS
    c[:, C_EPS + 1] = -0.5
    c[:, C_EPS + 2] = np.e
    c[8:16, C_DSELB] = 1.0
    c[:, C_ONES:C_ONES + 128] = 1.0
    p = np.arange(128)[:, None]
    f = np.arange(64)[None, :]
    NEG = -1e4
    c[:, C_NMLS:C_NMLS + 64] = np.where(p > f, 0.0, NEG)
    c[:, C_NMUS:C_NMUS + 64] = np.where(f > p, 0.0, NEG)
    c[:, C_NMLI:C_NMLI + 64] = np.where(p >= f, 0.0, NEG)
    c[:, C_NMUI:C_NMUI + 64] = np.where(f >= p, 0.0, NEG)
    f2 = np.arange(128)[None, :]
    c[:, C_NMP:C_NMP + 128] = np.where(p >= f2, 0.0, NEG)
    c[:, C_NMN:C_NMN + 128] = np.where(p <= f2, 0.0, NEG)
    return c


class Ctx:
    pass


def load_consts(k, g):
    g.cst = k.sb([128, C_NCOL], F32, "cst_sb")
    k.dma("sp", g.cst[:], g.d_cst, [g.cst[:]], [])
    g.ident = g.cst[:, C_ID:C_ID + 128]
    g.iota = g.cst[:, C_IOTA:C_IOTA + 128]
    g.epsT = g.cst[:, C_EPS:C_EPS + 1]
    g.ones = g.cst[:, C_ONES:C_ONES + 128]
    g.psb = [k.nc.alloc_psum_tensor("psb%d" % i, [128, 512], F32) for i in range(8)]


def bcast_row_load(k, dst, row_ap, n):
    P = dst.shape[0]
    src = bass.AP(row_ap.tensor, row_ap.offset, [[0, P], [1, n]])
    k.dma("sp", dst, src, [dst], [])


def peer_prepass_early(k, g, layers):
    nc = k.nc
    for li in layers:
        sem = nc.alloc_semaphore("s_pp%d" % li)
        k.dsem.append(sem)
        k.dval.append(0)
        si = len(k.dsem) - 1
        n = 0
        for src, dst in ((g.d_ut[li], g.d_utb[li]), (g.d_v[li], g.d_vb[li])):
            sv = src.rearrange("c p f -> (c p) f").rearrange("(a b) f -> a (b f)", b=8)
            dv = dst.rearrange("c p f -> (c p) f").rearrange("(a b) f -> a (b f)", b=8)
            for r0 in range(0, 2048, 128):
                nc.gpsimd.dma_start(out=dv[r0:r0 + 128, :], in_=sv[r0:r0 + 128, :]).then_inc(sem, 16)
                n += 1
        for blk in range(4):
            nc.gpsimd.dma_start(out=g.d_wqb[li][blk], in_=g.d_wq[li][:, blk * 512:(blk + 1) * 512].rearrange("(k p) f -> p k f", p=128)
                                ).then_inc(sem, 16)
            n += 1
        k.dval[si] = 16 * n
        k.st[("wb", li)] = [(si, 16 * n), {}]
        k.pp_tok = getattr(k, "pp_tok", {})
        k.pp_tok[li] = (si, 16 * n)


def peer_prepass(k, g, li):
    m = k.mark()
    CB = 8
    bufs = [k.sb([128, CB, 1024], BF16, "ppb%d" % i) for i in range(3)]
    n = 0
    for src, dst in ((g.d_ut[li], g.d_utb), (g.d_v[li], g.d_vb)):
        for c0 in range(0, 128, CB):
            b = bufs[n % 3]
            n += 1
            k.dma("pool", b[:], src[c0:c0 + CB].rearrange("c p f -> p c f"), [b[:]], [])
            for c1 in range(0, CB, 2):
                k.dma("pool", dst[(c0 + c1) // 2], b[:, c1:c1 + 2, :], [("x", ("wb", li))], [b[:]])
    for blk in range(4):
        b = bufs[n % 3]
        n += 1
        bv = b[:].rearrange("p c (a f) -> p (c a) f", a=2)[:, 0:8, :]
        for kk in range(8):
            k.dma("pool", bv[:, kk, :], g.d_wq[li, kk * 128:(kk + 1) * 128, blk * 512:(blk + 1) * 512], [b[:]], [])
        k.dma("pool", g.d_wqb[blk], bv, [("x", ("wb", li))], [b[:]])
    k.release(m)


def peer_layer(k, g, li, ntiles, final):
    nc = k.nc
    m0 = k.mark()
    TG = 2
    NTOKG = TG * 128
    vec, act, pe, pool = nc.vector, nc.scalar, nc.tensor, nc.gpsimd
    wqb = [k.sb([128, 8, 512], BF16, "wqb%d" % i) for i in range(2)]
    wk = k.sb([128, 16, 128], F32, "wk")
    skn = wk
    k.dma("sp", skn[:], g.d_sk[li].rearrange("h p k j -> k (h p) j"), [skn[:]], [])
    skt = k.sb([128, 16, 128], F32, "skt")
    for hp in range(16):
        ps = g.psb[hp % 2]
        k.op("pe", lambda: pe.transpose(ps[:, 0:128], skn[:, hp, :], g.ident), [ps[:]], [skn[:], g.cst[:]])
        k.op("act", lambda: act.copy(out=skt[:, hp, :], in_=ps[:, 0:128]), [skt[:]], [ps[:]])
    GS = k.sb([128, 1024], F32, "modgs")
    SH = k.sb([128, 1024], F32, "modsh")
    GT = k.sb([128, 1024], F32, "modgt")
    cur_a, cur_e = [-1], [-1]

    def set_mods_a(r):
        if cur_a[0] != r:
            cur_a[0] = r
            bcast_row_load(k, GS[:], g.d_mod[li, r, 3], 1024)
            bcast_row_load(k, SH[:], g.d_mod[li, r, 4], 1024)

    def set_mods_e(r):
        if cur_e[0] != r:
            cur_e[0] = r
            bcast_row_load(k, GT[:], g.d_mod[li, r, 5], 1024)
    if final:
        fg = k.sb([128, 1024], F32, "fing")
        bcast_row_load(k, fg[:], g.d_fg, 1024)
    gbuf = k.sb([128, NTOKG, 128], BF16, "gbuf")
    h2T = [k.sb([128, 8, NTOKG], BF16, "h2T%d" % i) for i in range(2)]
    xt = [[k.sb([128, 1024], F32, "xt%d_%d" % (p, i)) for i in range(TG)] for p in range(2)]
    selT = [[k.sb([128, 3, 128], F32, "selT%d_%d" % (p, i)) for i in range(TG)] for p in range(2)]
    hh = k.sb([128, 1024], F32, "hh")
    ss = k.sb([128, 4], F32, "ss")
    ss2 = k.sb([128, 4], F32, "ss2")
    qT = k.sb([128, 2, 256], F32, "qT")
    top2 = [k.sb([128, 16, 16], F32, "top%d" % i) for i in range(TG)]
    idx2 = [k.sb([128, 16, 16], U32, "idx%d" % i) for i in range(TG)]
    idxf = k.sb([128, 16, 16], F32, "idxf")
    cand = k.sb([128, 8, 256], F32, "cand")
    wk2 = wk[:].rearrange("p (a b) k -> p a (b k)", b=2)
    ctop = k.sb([128, 8, 16], F32, "ctop")
    pos = k.sb([128, 8, 16], U32, "pos")
    posi = k.sb([128, 2, 128], U32, "posi")
    posf = k.sb([128, 2, 128], F32, "posf")
    ee = k.sb([128, 8, 16], F32, "ee")
    zz = k.sb([128, 16], F32, "zz")
    eq = wk[:].rearrange("p a (b i) -> p (a b) i", i=16)
    sel = k.sb([128, 3, 128], F32, "sel")
    TB = 8
    ohA = [k.sb([128, TB, 128], BF16, "ohA%d" % i) for i in range(2)]
    ohB = [k.sb([128, TB, 128], BF16, "ohB%d" % i) for i in range(2)]
    iotab = k.sb([128, 128], BF16, "iotab")
    k.op("dve", lambda: vec.tensor_copy(out=iotab[:], in_=g.iota), [iotab[:]], [g.cst[:]])
    CB = 2
    NWB = 3
    utb = [k.sb([128, CB, 1024], BF16, "utb%d" % i) for i in range(NWB)]
    vbb = [k.sb([128, CB, 1024], BF16, "vbb%d" % i) for i in range(NWB)]
    actS = [k.sb([128, NTOKG], F32, "actS%d" % i) for i in range(2)]
    ga = [k.sb([128, NTOKG], BF16, "ga%d" % i) for i in range(2)]
    osb = hh

    psO = g.psb[0:4]
    psA = g.psb[4:6]
    psX = g.psb[6]
    psS = g.psb[7]

    ngroups = (ntiles + TG - 1) // TG

    def group_tiles(gi):
        return list(range(gi * TG, min(ntiles, (gi + 1) * TG)))

    def A1(gi):
        par = gi % 2
        tiles = group_tiles(gi)
        ntk = len(tiles) * 128
        set_mods_a(0 if tiles[0] < L // 128 else 1)
        hT_ = h2T[par]
        for ti, t in enumerate(tiles):
            x = xt[par][ti]
            k.dma("sp", x[:], g.d_xs[t * 128:(t + 1) * 128, :], [x[:]], [("x", ("xs", t))])
            norm_mod(k, g, x, hh, ss, GS, SH)
            yield
            for hf in range(2):
                for kk in range(4):
                    k.op("pe", lambda: pe.transpose(psX[:, kk * 128:(kk + 1) * 128], hh[:, (hf * 4 + kk) * 128:(hf * 4 + kk + 1) * 128],
                                                    g.ident), [psX[:]], [hh[:], g.cst[:]], inc=(kk == 3))
                k.op("act", lambda: act.copy(out=hT_[:, hf * 4:hf * 4 + 4, ti * 128:(ti + 1) * 128],
                                             in_=psX[:].rearrange("p (a b) -> p a b", a=4)), [hT_[:]], [psX[:]])
                yield
        k.dma("sp", wqb[0][:], g.d_wqb[li][0], [wqb[0][:]], [("x", ("wb", li))])
        for blk in range(4):
            wq = wqb[blk % 2]
            if blk + 1 < 4:
                k.dma("sp", wqb[(blk + 1) % 2][:], g.d_wqb[li][blk + 1], [wqb[(blk + 1) % 2][:]], [("x", ("wb", li))])
            for half in range(2):
                for h2 in range(2):
                    h4 = half * 2 + h2
                    for kk in range(8):
                        k.op("pe", lambda: pe.matmul(psX[:, h2 * 256:h2 * 256 + ntk], lhsT=wq[:, kk, h4 * 128:(h4 + 1) * 128],
                                                     rhs=hT_[:, kk, 0:ntk], start=(kk == 0), stop=(kk == 7)),
                             [psX[:]], [wq[:], hT_[:]], inc=(kk == 7 and h2 == 1))
                    yield
                k.op("act", lambda: act.copy(out=qT[:], in_=psX[:].rearrange("p (a b) -> p a b", a=2)), [qT[:]], [psX[:]])
                for ti in range(len(tiles)):
                    for h2 in range(2):
                        k.op("pe", lambda: pe.matmul(psS[:, (ti * 2 + h2) * 128:(ti * 2 + h2 + 1) * 128],
                                                     lhsT=qT[:, h2, ti * 128:(ti + 1) * 128], rhs=skt[:, blk * 4 + half * 2 + h2, :],
                                                     start=True, stop=True), [psS[:]], [qT[:], skt[:]],
                             inc=(h2 == 1 and ti == len(tiles) - 1))
                yield
                for ti in range(len(tiles)):
                    tp, ix = top2[ti], idx2[ti]
                    hps = [(blk * 4 + half * 2 + h2, (ti * 2 + h2)) for h2 in range(2)]

                    def S(c_):
                        return psS[:, c_ * 128:(c_ + 1) * 128]

                    for hp, c_ in hps:
                        k.op("dve", lambda: vec.max(out=tp[:, hp, 0:8], in_=S(c_)), [("x", ("topa", ti))], [psS[:]])
                    for hp, c_ in hps:
                        k.op("dve", lambda: vec.match_replace(out=wk[:, hp, :], in_to_replace=tp[:, hp, 0:8], in_values=S(c_),
                                                              imm_value=-1e30), [wk[:]], [("x", ("topa", ti)), psS[:]])
                    yield
                    for hp, c_ in hps:
                        k.op("dve", lambda: vec.max(out=tp[:, hp, 8:16], in_=wk[:, hp, :]), [("x", ("topb", ti))], [wk[:]])
                    for hp, c_ in hps:
                        k.op("dve", lambda: vec.max_index(out=ix[:, hp, 0:8], in_max=tp[:, hp, 0:8], in_values=S(c_)),
                             [("x", ("idxa", ti))], [("x", ("topa", ti)), psS[:]])
                    yield
                    for hp, c_ in hps:
                        k.op("dve", lambda: vec.max_index(out=ix[:, hp, 8:16], in_max=tp[:, hp, 8:16], in_values=S(c_)),
                             [("x", ("idxb", ti))], [("x", ("topb", ti)), psS[:]])
                    yield
        for ti, t in enumerate(tiles):
            top, idx = top2[ti], idx2[ti]
            k.op("dve", lambda: vec.tensor_copy(out=idxf[:], in_=idx[:]), [idxf[:]], [("x", ("idxa", ti)), ("x", ("idxb", ti))])
            t1 = V(top[:, 0, :], (32, 8), (1, 16), (0, 16))
            t2 = V(top[:, 1, :], (32, 8), (0, 16), (1, 16))
            k.op("dve", lambda: vec.tensor_tensor(out=cand[:].rearrange("p h (i j) -> p h i j", i=16), in0=t1, in1=t2,
                                                  op=ALU.add), [cand[:]], [("x", ("topa", ti)), ("x", ("topb", ti))])
            yield
            for h in range(8):
                k.op("dve", lambda: vec.max(out=ctop[:, h, 0:8], in_=cand[:, h, :]), [("x", "ctopa")], [cand[:]])
            yield
            for h in range(8):
                k.op("dve", lambda: vec.match_replace(out=wk2[:, h, :], in_to_replace=ctop[:, h, 0:8], in_values=cand[:, h, :],
                                                      imm_value=-1e30), [wk[:]], [("x", "ctopa"), cand[:]])
            yield
            for h in range(8):
                k.op("dve", lambda: vec.max(out=ctop[:, h, 8:16], in_=wk2[:, h, :]), [("x", "ctopb")], [wk[:]])
            yield
            for h in range(8):
                k.op("dve", lambda: vec.max_index(out=pos[:, h, 0:8], in_max=ctop[:, h, 0:8], in_values=cand[:, h, :]),
                     [("x", "posa")], [("x", "ctopa"), cand[:]])
            yield
            for h in range(8):
                k.op("dve", lambda: vec.max_index(out=pos[:, h, 8:16], in_max=ctop[:, h, 8:16], in_values=cand[:, h, :]),
                     [("x", "posb")], [("x", "ctopb"), cand[:]])
            yield
            k.op("dve", lambda: vec.tensor_tensor(out=ee[:], in0=ctop[:], in1=V(ctop[:, 0, 0:1], (16, 8), (0, 16)),
                                                  op=ALU.subtract), [ee[:]], [("x", "ctopa"), ("x", "ctopb")])
            k.op("act", lambda: act.activation(out=ee[:], in_=ee[:], func=AF.Exp), [ee[:]], [ee[:]])
            k.op("dve", lambda: vec.tensor_reduce(out=zz[:, 0:8], in_=ee[:], axis=AX.X, op=ALU.add), [("x", "zz0")], [ee[:]])
            k.op("dve", lambda: vec.reciprocal(out=zz[:, 8:16], in_=zz[:, 0:8]), [("x", "zz1")], [("x", "zz0")])
            k.op("dve", lambda: vec.tensor_tensor(out=sel[:, 2, :].rearrange("p (h r) -> p h r", h=8), in0=ee[:],
                                                  in1=V(zz[:, 8:9], (1, 8), (0, 16)), op=ALU.mult),
                 [("x", "sel2")], [ee[:], ("x", "zz1")])
            yield
            posv = pos[:].rearrange("p h r -> p (h r)")
            k.op("dve", lambda: vec.tensor_single_scalar(out=posi[:, 0, :], in_=posv, scalar=4, op=ALU.logical_shift_right),
                 [("x", "posi0")], [("x", "posa"), ("x", "posb")])
            k.op("dve", lambda: vec.tensor_single_scalar(out=posi[:, 1, :], in_=posv, scalar=15, op=ALU.bitwise_and),
                 [("x", "posi1")], [("x", "posa"), ("x", "posb")])
            k.op("dve", lambda: vec.tensor_copy(out=posf[:], in_=posi[:]), [posf[:]], [("x", "posi0"), ("x", "posi1")])
            yield
            for p_ in range(2):
                k.op("dve", lambda: vec.tensor_tensor(out=eq, in0=V(posf[:, p_, :], (1, 128), (0, 16)),
                                                      in1=V(g.iota[:, 0:16], (0, 128), (1, 16)), op=ALU.is_equal),
                     [wk[:]], [posf[:], g.cst[:]])
                yield
                k.op("pool", lambda: pool.tensor_tensor(out=eq.rearrange("p (h r) i -> p h r i", h=8),
                                                        in0=eq.rearrange("p (h r) i -> p h r i", h=8),
                                                        in1=V(idxf[:, p_, :], (32, 8), (0, 16), (1, 16)), op=ALU.mult),
                     [wk[:]], [wk[:], idxf[:]])
                k.op("dve", lambda: vec.tensor_reduce(out=sel[:, p_, :], in_=eq, axis=AX.X, op=ALU.add),
                     [("x", ("sel", p_))], [wk[:]])
                yield
            for q in range(3):
                k.op("pe", lambda: pe.transpose(psX[:, q * 128:(q + 1) * 128], sel[:, q, :], g.ident), [psX[:]],
                     [("x", ("sel", 0)), ("x", ("sel", 1)), ("x", "sel2"), g.cst[:]], inc=(q == 2))
            sT = selT[par][ti]
            k.op("act", lambda: act.copy(out=sT[:], in_=psX[:, 0:384].rearrange("p (a b) -> p a b", a=3)), [sT[:]], [psX[:]])
            yield

    def A2(gi):
        par = gi % 2
        tiles = group_tiles(gi)
        nq = 0
        for ti, t in enumerate(tiles):
            sT = selT[par][ti]
            for b in range(128 // TB):
                A = ohA[b % 2]
                B = ohB[b % 2]
                t0 = b * TB
                for tt in range(TB):
                    k.op("dve", lambda: vec.tensor_scalar(out=A[:, tt, :], in0=iotab[:], scalar1=sT[:, 0, t0 + tt:t0 + tt + 1],
                                                          scalar2=sT[:, 2, t0 + tt:t0 + tt + 1], op0=ALU.is_equal, op1=ALU.mult),
                         [A[:]], [sT[:], iotab[:]])
                    if False:
                        k.op("pool", lambda: pool.tensor_scalar(out=B[:, tt, :], in0=iotab[:], scalar1=sT[:, 1, t0 + tt:t0 + tt + 1],
                                                                scalar2=None, op0=ALU.is_equal), [("x", ("ohB", b % 2, "p"))],
                             [sT[:], iotab[:], ("x", ("ohBr", b % 2))])
                    else:
                        k.op("dve", lambda: vec.tensor_scalar(out=B[:, tt, :], in0=iotab[:], scalar1=sT[:, 1, t0 + tt:t0 + tt + 1],
                                                              scalar2=None, op0=ALU.is_equal), [("x", ("ohB", b % 2, "d"))],
                             [sT[:], iotab[:], ("x", ("ohBr", b % 2))])
                for q in range(TB // 4):
                    ps = psA[nq % 2]
                    nq += 1
                    for u in range(4):
                        tt = q * 4 + u
                        k.op("pe", lambda: pe.matmul(ps[:, u * 128:(u + 1) * 128], lhsT=B[:, tt, :], rhs=A[:, tt, :], start=True,
                                                     stop=True), [ps[:], ("x", ("ohBr", b % 2))],
                             [A[:], ("x", ("ohB", b % 2, "p")), ("x", ("ohB", b % 2, "d"))], inc=(u == 3))
                    tg0 = ti * 128 + t0 + q * 4
                    k.op("act", lambda: act.copy(out=gbuf[:, tg0:tg0 + 4, :], in_=ps[:].rearrange("p (a b) -> p a b", a=4)),
                         [("x", "gbufW")], [ps[:], ("x", "gbufR"), ("x", "gbufR2")])

    def B(gi, nxt):
        par = gi % 2
        tiles = group_tiles(gi)
        ntg = len(tiles)
        ntok = ntg * 128
        hT_ = h2T[par]

        def load_w(cb):
            b = (cb // CB) % NWB
            k.dma("sp", utb[b][:], g.d_utb[li][cb:cb + CB].rearrange("c p f -> p c f"), [utb[b][:]], [("x", ("wb", li))])
            k.dma("sp", vbb[b][:], g.d_vb[li][cb:cb + CB].rearrange("c p f -> p c f"), [vbb[b][:]], [("x", ("wb", li))])

        def umm(c):
            b = (c // CB) % NWB
            ps = psA[c % 2]
            for kk in range(8):
                k.op("pe", lambda: pe.matmul(ps[:, 0:ntok], lhsT=utb[b][:, c % CB, kk * 128:(kk + 1) * 128],
                                             rhs=hT_[:, kk, 0:ntok], start=(kk == 0), stop=(kk == 7)),
                     [ps[:]], [utb[b][:], hT_[:]], inc=(kk == 7))

        load_w(0)
        load_w(CB)
        umm(0)
        for c in range(128):
            if c % CB == 0 and c + 2 * CB < 128:
                load_w(c + 2 * CB)
            if c + 1 < 128:
                umm(c + 1)
            b = (c // CB) % NWB
            ps = psA[c % 2]
            a_ = actS[c % 2]
            g_ = ga[c % 2]
            k.op("act", lambda: act.activation(out=a_[:, 0:ntok], in_=ps[:, 0:ntok], func=AF.Gelu), [a_[:]], [ps[:]])
            if c % 2 == 0:
                k.op("dve", lambda: vec.tensor_tensor(out=g_[:, 0:ntok], in0=a_[:, 0:ntok], in1=V(gbuf[:, 0, c:c + 1], (128, ntok)),
                                                      op=ALU.mult), [g_[:], ("x", "gbufR")], [a_[:], ("x", "gbufW")])
            else:
                k.op("pool", lambda: pool.tensor_tensor(out=g_[:, 0:ntok], in0=a_[:, 0:ntok], in1=V(gbuf[:, 0, c:c + 1], (128, ntok)),
                                                        op=ALU.mult), [g_[:], ("x", "gbufR2")], [a_[:], ("x", "gbufW")])
            for ti in range(ntg):
                for hf in range(2):
                    k.op("pe", lambda: pe.matmul(psO[ti * 2 + hf][:], lhsT=g_[:, ti * 128:(ti + 1) * 128],
                                                 rhs=vbb[b][:, c % CB, hf * 512:(hf + 1) * 512], start=(c == 0), stop=(c == 127)),
                         [psO[ti * 2 + hf][:]], [g_[:], vbb[b][:]], inc=(ti == ntg - 1 and hf == 1))
            if nxt is not None and c >= 2:
                for _ in range(STEPS):
                    next(nxt, None)
        if nxt is not None:
            for _ in nxt:
                pass
        set_mods_e(0 if tiles[0] < L // 128 else 1)
        for ti, t in enumerate(tiles):
            x = xt[par][ti]
            for hf in range(2):
                sl = slice(hf * 512, (hf + 1) * 512)
                k.op("dve", lambda: vec.tensor_tensor(out=osb[:, sl], in0=psO[ti * 2 + hf][:], in1=GT[:, sl], op=ALU.mult),
                     [osb[:]], [psO[ti * 2 + hf][:], GT[:]])
            k.op("pool", lambda: pool.tensor_tensor(out=osb[:], in0=osb[:], in1=x[:], op=ALU.add), [osb[:]], [osb[:], x[:]])
            if not final:
                k.dma("pool", g.d_xs[t * 128:(t + 1) * 128, :], osb[:], [("x", ("xs", t))], [osb[:]])
            else:
                norm_mod_final(k, g, osb, x, ss2, fg)
                k.dma("pool", g.d_out[t * 128:(t + 1) * 128, :], x[:], [("x", ("out", t))], [x[:]])

    STEPS = 1
    n_a1 = 6 + 24 + 32 * TG * 3 // 3 + 18 * TG
    STEPS = 1
    for _ in A1(0):
        pass
    for gi in range(ngroups):
        A2(gi)
        nxt = A1(gi + 1) if gi + 1 < ngroups else None
        B(gi, nxt)
    k.release(m0)


def norm_mod_final(k, g, x, out, ss, fg):
    nc = k.nc
    vec, act = nc.vector, nc.scalar
    k.op("dve", lambda: vec.scalar_tensor_tensor(out=out[:], in0=x[:], scalar=1.0, in1=x[:], op0=ALU.mult, op1=ALU.mult,
                                                accum_out=ss[:, 0:1]), [out[:], ss[:]], [x[:]])
    k.op("act", lambda: act.activation(out=ss[:, 1:2], in_=ss[:, 0:1], func=AF.Sqrt, scale=1.0 / D, bias=g.epsT), [("x", "fss1")],
         [ss[:]])
    k.op("dve", lambda: vec.reciprocal(out=ss[:, 2:3], in_=ss[:, 1:2]), [("x", "fss2")], [("x", "fss1")])
    k.op("dve", lambda: vec.scalar_tensor_tensor(out=out[:], in0=x[:], scalar=ss[:, 2:3], in1=fg[:], op0=ALU.mult, op1=ALU.mult),
         [out[:]], [x[:], ("x", "fss2"), fg[:]])


def EPS_AP(g):
    return g.epsT[:, 0:1]


def build(phases=("all",), dev=None):
    dev = dev or {}
    k = K()
    nc = k.nc
    g = Ctx()

    k.ext_inputs = []

    def din(name, shape, dt=F32):
        k.ext_inputs.append(name)
        return nc.dram_tensor(name, list(shape), dt, kind="ExternalInput").ap()

    def dint(name, shape, dt=F32):
        return nc.dram_tensor(name, list(shape), dt, kind="Internal").ap()

    g.d_x = din("x", [L, D])
    g.d_ctx = din("ctx", [CTX, D])
    g.d_cst = din("cst", [128, C_NCOL])
    g.d_wq = din("peer_wq", [2, D, 2048])
    g.d_sk = din("peer_sk", [2, 8, 2, 128, 128])
    g.d_ut = din("peer_ut", [2, 128, 128, 1024])
    g.d_v = din("peer_v", [2, 128, 128, 1024])
    g.d_fg = din("final_g", [D])
    g.d_cT = din("cT", [128, 8, 2])
    g.d_adaw = din("ada_w", [2, D, 6144])
    g.d_adab = din("ada_b", [2, 6144])
    g.d_n1g = din("norm1_g", [2, D])
    g.d_n2g = din("norm2_g", [2, D])
    g.d_cst2 = din("cst2", [16, NTOK])
    g.d_gwin = din("gdn_w_in", [D, 4128])
    g.d_gconv = din("gdn_conv", [24, 128, 5])
    g.d_galog = din("gdn_a_log", [2, 8])
    g.d_gdtb = din("gdn_dt_bias", [2, 8])
    g.d_gng = din("gdn_norm_g", [128])
    g.d_gwout = din("gdn_w_out", [D, D])
    g.d_swin = din("swa_w_in", [D, 1536])
    g.d_swout = din("swa_w_out", [D, D])
    g.d_ssink = din("swa_sinks", [16])
    g.d_rope = din("rope", [2, L, 64])
    g.d_hT = dint("hT", [NT, 128, 8, 128], BF16)
    g.d_qkv = dint("qkv", [24, 128, NTOK], BF16)
    g.d_rows = dint("rows", [2, 16, NTOK])
    g.d_cd = dint("cd", [16, NCH])
    g.d_o = dint("gdn_o", [2, NTOK, D])
    if dev.get("mod_ext"):
        g.d_mod = din("mod", [2, 2, 6, D])
    else:
        g.d_mod = dint("mod", [2, 2, 6, D])
    g.d_xs = dint("xs", [NTOK, D])
    g.d_utb = dint("utb", [2, 128, 128, 1024], BF16)
    g.d_vb = dint("vb", [2, 128, 128, 1024], BF16)
    g.d_wqb = dint("wqb", [2, 4, 128, 8, 512], BF16)
    g.d_out = nc.dram_tensor("out", [L, D], F32, kind="ExternalOutput").ap()
    if dev.get("xs_out"):
        g.d_xso = nc.dram_tensor("xs_out", [NTOK, D], F32, kind="ExternalOutput").ap()

    load_consts(k, g)
    peer_prepass_early(k, g, [ph[1] for ph in phases if ph[0] == "peer"])
    for r0 in range(0, L, 512):
        k.dma("sp", g.d_xs[r0:r0 + 512, :], g.d_x[r0:r0 + 512, :], [("x", ("xs", r0 // 128 + i)) for i in range(4)], [])
    k.dma("sp", g.d_xs[L:NTOK, :], g.d_ctx, [("x", ("xs", 32)), ("x", ("xs", 33))], [])

    for ph in phases:
        if ph[0] == "adaln":
            adaln_phase(k, g)
        elif ph[0] == "gdn":
            m_ = k.mark()
            build_hT(k, g, 0, NT)
            gdn_rows(k, g)
            gdn_qkv(k, g)
            gdn_scan(k, g)
            k.release(m_)
            gdn_out(k, g, 0, NT)
        elif ph[0] == "swa":
            build_hT(k, g, 1, NT)
            swa_phase(k, g, 1)
        elif ph[0] == "peer":
            _, li, ntiles, final = ph
            k.st[("wb", li)] = [k.pp_tok[li], {}]
            peer_layer(k, g, li, ntiles, final)
    if dev.get("mod_out"):
        k.barrier()
        d_mo = nc.dram_tensor("mod_out", [2, 2, 6, D], F32, kind="ExternalOutput").ap()
        k.dma("sp", d_mo.rearrange("a b c d -> (a b c) d"), g.d_mod.rearrange("a b c d -> (a b c) d"), [("x", "modo")], [])
    if dev.get("xs_out"):
        k.barrier()
        for r0 in range(0, NTOK, 256):
            k.dma("sp", g.d_xso[r0:r0 + 256, :], g.d_xs[r0:r0 + 256, :], [("x", ("xso", r0))], [])
    k.finish()
    return k


def adaln_phase(k, g):
    nc = k.nc
    m = k.mark()
    cT = k.sb([128, 8, 2], F32, "cT")
    k.dma("sp", cT[:], g.d_cT, [cT[:]], [])
    sc = k.sb([128, 8, 2], F32, "scT")
    k.op("act", lambda: nc.scalar.activation(out=sc[:], in_=cT[:], func=AF.Silu), [sc[:]], [cT[:]])
    bb = k.sb([2, 6144], F32, "adab")
    msb = k.sb([2, 6144], F32, "adam")
    n1 = k.sb([2, 1024], F32, "n1g")
    n2 = k.sb([2, 1024], F32, "n2g")
    o1 = k.sb([2, 1024], F32, "ado1")
    o2 = k.sb([2, 1024], F32, "ado2")
    wb = [k.sb([128, 8, 512], F32, "adaw%d" % i) for i in range(2)]
    for li in range(2):
        bcast_row_load(k, bb[:], g.d_adab[li], 6144)
        bcast_row_load(k, n1[:], g.d_n1g[li], 1024)
        bcast_row_load(k, n2[:], g.d_n2g[li], 1024)
        for n in range(12):
            w = wb[n % 2]
            k.dma("sp", w[:], g.d_adaw[li][:, n * 512:(n + 1) * 512].rearrange("(k p) f -> p k f", p=128), [w[:]], [])
            ps = g.psb[n % 2]
            for kk in range(8):
                k.op("pe", lambda: nc.tensor.matmul(ps[0:2, :], lhsT=sc[:, kk, :], rhs=w[:, kk, :], start=(kk == 0), stop=(kk == 7)),
                     [ps[:]], [sc[:], w[:]], inc=(kk == 7))
            k.op("dve", lambda: nc.vector.tensor_tensor(out=msb[:, n * 512:(n + 1) * 512], in0=ps[0:2, :],
                                                        in1=bb[:, n * 512:(n + 1) * 512], op=ALU.add), [msb[:]], [ps[:], bb[:]])
        k.op("dve", lambda: nc.vector.scalar_tensor_tensor(out=o1[:], in0=msb[:, 1024:2048], scalar=1.0, in1=n1[:], op0=ALU.add,
                                                          op1=ALU.mult), [o1[:]], [msb[:], n1[:]])
        k.op("dve", lambda: nc.vector.scalar_tensor_tensor(out=o2[:], in0=msb[:, 4096:5120], scalar=1.0, in1=n2[:], op0=ALU.add,
                                                          op1=ALU.mult), [o2[:]], [msb[:], n2[:]])
        srcs = [o1[:], msb[:, 0:1024], msb[:, 2048:3072], o2[:], msb[:, 3072:4096], msb[:, 5120:6144]]
        for j in range(6):
            k.dma("sp", g.d_mod[li, :, j, :], srcs[j], [("x", ("mod", li))], [o1[:], o2[:], msb[:]])
    k.release(m)


def norm_mod(k, g, x, hh, ss, GS, SH):
    nc = k.nc
    vec, act, pool = nc.vector, nc.scalar, nc.gpsimd
    k.op("dve", lambda: vec.scalar_tensor_tensor(out=hh[:], in0=x[:], scalar=1.0, in1=x[:], op0=ALU.mult, op1=ALU.mult,
                                                accum_out=ss[:, 0:1]), [hh[:], ss[:]], [x[:]])
    k.op("dve", lambda: vec.tensor_scalar(out=ss[:, 1:2], in0=ss[:, 0:1], scalar1=1.0 / D, scalar2=EPS, op0=ALU.mult, op1=ALU.add),
         [("x", "ss1")], [ss[:]])
    k.op("pool", lambda: pool.tensor_tensor(out=ss[:, 2:3], in0=ss[:, 1:2], in1=g.cst[:, C_EPS + 1:C_EPS + 2], op=ALU.pow),
         [("x", "ss2")], [("x", "ss1"), g.cst[:]])
    k.op("dve", lambda: vec.scalar_tensor_tensor(out=hh[:], in0=x[:], scalar=ss[:, 2:3], in1=GS[:], op0=ALU.mult, op1=ALU.mult),
         [hh[:]], [x[:], ("x", "ss2"), GS[:]])
    k.op("pool", lambda: pool.tensor_tensor(out=hh[:], in0=hh[:], in1=SH[:], op=ALU.add), [hh[:]], [hh[:], SH[:]])


def build_hT(k, g, li, ntiles):
    nc = k.nc
    m = k.mark()
    GS = k.sb([128, 1024], F32, "hgs")
    SH = k.sb([128, 1024], F32, "hsh")
    xt = [k.sb([128, 1024], F32, "hxt%d" % i) for i in range(2)]
    hh = k.sb([128, 1024], F32, "hhh")
    ss = k.sb([128, 4], F32, "hss")
    hTt = [k.sb([128, 8, 128], BF16, "hTt%d" % i) for i in range(3)]
    cur = -1
    pend = None
    for t in range(ntiles):
        r = 0 if t < L // 128 else 1
        if r != cur:
            cur = r
            bcast_row_load(k, GS[:], g.d_mod[li, r, 0], 1024)
            bcast_row_load(k, SH[:], g.d_mod[li, r, 1], 1024)
        x = xt[t % 2]
        k.dma("sp", x[:], g.d_xs[t * 128:(t + 1) * 128, :], [x[:]], [("x", ("xs", t))])
        norm_mod(k, g, x, hh, ss, GS, SH)
        o = hTt[t % 3]
        for kk in range(8):
            ps = g.psb[kk // 4]
            k.op("pe", lambda: nc.tensor.transpose(ps[:, (kk % 4) * 128:(kk % 4 + 1) * 128], hh[:, kk * 128:(kk + 1) * 128], g.ident),
                 [ps[:]], [hh[:], g.cst[:]], inc=(kk % 4 == 3))
            if kk % 4 == 3:
                h0 = kk // 4
                if h0 == 0:
                    k.op("act", lambda: nc.scalar.copy(out=o[:, 0:4, :], in_=ps[:].rearrange("p (a b) -> p a b", a=4)), [o[:]], [ps[:]])
                else:
                    k.op("dve", lambda: nc.vector.tensor_copy(out=o[:, 4:8, :], in_=ps[:].rearrange("p (a b) -> p a b", a=4)),
                         [o[:]], [ps[:]])
        if pend is not None:
            k.dma("pool", g.d_hT[pend[0]], pend[1][:], [("x", ("hT", pend[0]))], [pend[1][:]])
        pend = (t, o)
    k.dma("pool", g.d_hT[pend[0]], pend[1][:], [("x", ("hT", pend[0]))], [pend[1][:]])
    k.release(m)


NCH = NTOK // 64


def gdn_rows(k, g):
    nc = k.nc
    vec, act, pe = nc.vector, nc.scalar, nc.tensor
    g.TM = k.sb([64, NCH, 4, 16], F32, "gdnTM")
    m = k.mark()
    wab = k.sb([128, 8, 32], BF16, "wab")
    for kk in range(8):
        k.dma("pool", wab[:, kk, :], g.d_gwin[kk * 128:(kk + 1) * 128, 4096:4128], [wab[:]], [])
    alog = k.sb([16, 4], F32, "alog")
    k.dma("sp", alog[:, 0:1], g.d_galog.rearrange("d (h o) -> (d h) o", o=1), [alog[:]], [])
    k.dma("sp", alog[:, 1:2], g.d_gdtb.rearrange("d (h o) -> (d h) o", o=1), [alog[:]], [])
    k.op("act", lambda: act.activation(out=alog[:, 2:3], in_=alog[:, 0:1], func=AF.Exp), [("x", "alog2")], [alog[:]])
    k.op("dve", lambda: vec.tensor_scalar(out=alog[:, 3:4], in0=alog[:, 2:3], scalar1=-1.0, scalar2=None, op0=ALU.mult),
         [("x", "alog3")], [("x", "alog2")])
    rows = {nm: k.sb([16, NTOK], F32, "row_" + nm) for nm in ("g", "beta", "Gf", "Gb", "eG", "kdf")}
    cm = k.sb([16, NTOK], F32, "row_cm")
    k.dma("sp", cm[:], g.d_cst2, [cm[:]], [])
    hb = [k.sb([128, 4, 8, 128], BF16, "rhb%d" % i) for i in range(2)]
    for blk in range((NT + 3) // 4):
        t0 = blk * 4
        nt = min(4, NT - t0)
        h = hb[blk % 2]
        k.dma("sp", h[:, 0:nt], g.d_hT[t0:t0 + nt].rearrange("t p k n -> p t k n"), [h[:]], [("x", ("hT", t0 + i)) for i in range(nt)])
        psa, psb_ = g.psb[(blk % 2) * 2], g.psb[(blk % 2) * 2 + 1]
        for half, ps in ((0, psa), (1, psb_)):
            for t in range(nt):
                for kk in range(8):
                    k.op("pe", lambda: pe.matmul(ps[0:16, t * 128:(t + 1) * 128], lhsT=wab[:, kk, half * 16:(half + 1) * 16],
                                                 rhs=h[:, t, kk, :], start=(kk == 0), stop=(kk == 7)), [ps[:]], [wab[:], h[:]],
                         inc=(kk == 7 and t == nt - 1))
        sl = slice(t0 * 128, (t0 + nt) * 128)
        w = nt * 128
        k.op("act", lambda: act.activation(out=rows["g"][:, sl], in_=psa[0:16, 0:w], func=AF.Exp, bias=alog[:, 1:2]),
             [rows["g"][:]], [psa[:], alog[:]])
        k.op("act", lambda: act.activation(out=rows["g"][:, sl], in_=rows["g"][:, sl], func=AF.Ln, bias=1.0), [rows["g"][:]],
             [rows["g"][:]])
        k.op("dve", lambda: vec.tensor_scalar(out=rows["g"][:, sl], in0=rows["g"][:, sl], scalar1=alog[:, 3:4], scalar2=None,
                                              op0=ALU.mult), [rows["g"][:]], [rows["g"][:], ("x", "alog3")])
        k.op("act", lambda: act.activation(out=rows["beta"][:, sl], in_=psb_[0:16, 0:w], func=AF.Sigmoid), [rows["beta"][:]],
             [psb_[:]])
    G, Gf, Gb, B, eG, kdf = rows["g"], rows["Gf"], rows["Gb"], rows["beta"], rows["eG"], rows["kdf"]
    k.op("dve", lambda: vec.tensor_tensor_scan(out=Gf[:], data0=cm[:], data1=G[:], initial=0.0, op0=ALU.mult, op1=ALU.add),
         [Gf[:]], [cm[:], G[:]])
    tot = V(Gf[:, 63:64], (64, NCH), (0, 64))
    v3 = lambda t: t[:].rearrange("p (c j) -> p c j", j=64)
    k.op("dve", lambda: vec.tensor_tensor(out=v3(Gb), in0=tot, in1=v3(Gf), op=ALU.subtract), [Gb[:]], [Gf[:]])
    k.op("dve", lambda: vec.tensor_tensor(out=Gb[:], in0=Gb[:], in1=G[:], op=ALU.add), [Gb[:]], [Gb[:], G[:]])
    k.op("dve", lambda: vec.tensor_tensor(out=Gb[:], in0=Gb[:], in1=Gf[:], op=ALU.subtract), [Gb[:]], [Gb[:], Gf[:]])
    k.op("dve", lambda: vec.scalar_tensor_tensor(out=Gb[:], in0=Gb[:], scalar=g.cst[0:16, C_DSELB:C_DSELB + 1], in1=Gf[:],
                                                op0=ALU.mult, op1=ALU.add), [Gb[:]], [Gb[:], Gf[:], g.cst[:]])
    GD = Gb
    k.op("act", lambda: act.activation(out=eG[:], in_=GD[:], func=AF.Exp), [eG[:]], [GD[:]])
    k.op("dve", lambda: vec.tensor_tensor(out=v3(kdf), in0=tot, in1=v3(GD), op=ALU.subtract), [kdf[:]], [Gf[:], GD[:]])
    k.op("act", lambda: act.activation(out=kdf[:], in_=kdf[:], func=AF.Exp), [kdf[:]], [kdf[:]])
    cd = k.sb([16, NCH], F32, "row_cd")
    k.op("act", lambda: act.activation(out=cd[:], in_=V(Gf[:, 63:64], (64, NCH)), func=AF.Exp), [cd[:]], [Gf[:]])
    k.dma("sp", g.d_rows[0], GD[:], [("x", "rows")], [GD[:]])
    k.dma("sp", g.d_rows[1], B[:], [("x", "rows")], [B[:]])
    k.dma("sp", g.d_cd, cd[:], [("x", "rows")], [cd[:]])
    nB = rows["Gf"]
    k.op("dve", lambda: vec.tensor_scalar(out=nB[:], in0=B[:], scalar1=-1.0, scalar2=None, op0=ALU.mult), [nB[:]], [B[:], cd[:]])
    for c in range(NCH):
        ps = g.psb[4 + (c // 8) % 2]
        for q, src in enumerate((GD, nB, eG, kdf)):
            col = (c % 8) * 64 + q * 16
            k.op("pe", lambda: pe.transpose(ps[0:64, col:col + 16], src[:, c * 64:(c + 1) * 64], g.ident[0:16, 0:16]), [ps[:]],
                 [src[:], g.cst[:]], inc=(q == 3 and (c % 8 == 7 or c == NCH - 1)))
        if c % 8 == 7 or c == NCH - 1:
            c0 = (c // 8) * 8
            n = c - c0 + 1
            k.op("act", lambda: act.copy(out=g.TM[:, c0:c0 + n].rearrange("p c q d -> p (c q d)"), in_=ps[0:64, 0:n * 64]),
                 [g.TM[:]], [ps[:]])
    k.release(m)


def gdn_qkv(k, g):
    nc = k.nc
    vec, act, pe, pool = nc.vector, nc.scalar, nc.tensor, nc.gpsimd
    m = k.mark()
    PW = NTOK + 8
    Ps = [k.sb([128, PW], F32, "gP%d" % i) for i in range(2)]
    for P in Ps:
        k.op("pool", lambda: pool.memset(P[:], 0.0), [P[:]], [])
    R = [k.sb([128, NTOK], F32, "gR%d" % i) for i in range(2)]
    RB = [k.sb([128, NTOK], BF16, "gRB%d" % i) for i in range(2)]
    cw = k.sb([128, 24, 5], F32, "gcw")
    k.dma("sp", cw[:], g.d_gconv.rearrange("f p j -> p f j"), [cw[:]], [])
    wf = [k.sb([128, 8, 128], BF16, "gwf%d" % i) for i in range(2)]
    hb = [k.sb([128, 4, 8, 128], BF16, "ghb%d" % i) for i in range(2)]
    sq = [k.sb([128, 512], F32, "gsq%d" % i) for i in range(2)]
    rn = [k.sb([128, 512], F32, "grn%d" % i) for i in range(2)]
    nblk = (NT + 3) // 4
    def load_w(fc_):
        w_ = wf[fc_ % 2]
        for kk in range(8):
            k.dma("pool", w_[:, kk, :], g.d_gwin[kk * 128:(kk + 1) * 128, fc_ * 128:(fc_ + 1) * 128], [w_[:]], [])

    load_w(0)
    pending_store = None
    for fc in range(24):
        w = wf[fc % 2]
        P = Ps[fc % 2]
        for blk in range(nblk):
            t0 = blk * 4
            nt = min(4, NT - t0)
            h = hb[blk % 2]
            k.dma("sp", h[:, 0:nt], g.d_hT[t0:t0 + nt].rearrange("t p k n -> p t k n"), [h[:]],
                  [("x", ("hT", t0 + i)) for i in range(nt)])
            ps = g.psb[blk % 2]
            for t in range(nt):
                for kk in range(8):
                    k.op("pe", lambda: pe.matmul(ps[:, t * 128:(t + 1) * 128], lhsT=w[:, kk, :], rhs=h[:, t, kk, :], start=(kk == 0),
                                                 stop=(kk == 7)), [ps[:]], [w[:], h[:]], inc=(kk == 7 and t == nt - 1))
            off = t0 * 128 + (2 if t0 < 32 else 6)
            k.op("act", lambda: act.copy(out=P[:, off:off + nt * 128], in_=ps[:, 0:nt * 128]), [P[:]], [ps[:]])
        if fc + 1 < 24:
            load_w(fc + 1)
        if pending_store is not None:
            pf, prb = pending_store
            k.dma("pool", g.d_qkv[pf], prb[:], [("x", ("qkv", pf))], [prb[:]])
            pending_store = None
        r = R[fc % 2]
        for (o0, n, p0) in ((0, L, 0), (L, CTX, L + 4)):
            k.op("dve", lambda: vec.tensor_scalar(out=r[:, o0:o0 + n], in0=P[:, p0:p0 + n], scalar1=cw[:, fc, 0:1], scalar2=None,
                                                  op0=ALU.mult), [r[:]], [P[:], cw[:]])
            for j in range(1, 5):
                k.op("dve", lambda: vec.scalar_tensor_tensor(out=r[:, o0:o0 + n], in0=P[:, p0 + j:p0 + j + n], scalar=cw[:, fc, j:j + 1],
                                                            in1=r[:, o0:o0 + n], op0=ALU.mult, op1=ALU.add), [r[:]], [P[:], cw[:], r[:]])
        rb = RB[fc % 2]
        if fc >= 16:
            k.op("act", lambda: act.activation(out=rb[:], in_=r[:], func=AF.Silu), [rb[:]], [r[:]])
        else:
            k.op("act", lambda: act.activation(out=r[:], in_=r[:], func=AF.Silu), [r[:]], [r[:]])
        if fc < 16:
            scl = (128.0 ** -0.5) if fc < 8 else 1.0
            for b in range((NTOK + 511) // 512):
                c0 = b * 512
                n = min(512, NTOK - c0)
                s_, r_ = sq[b % 2], rn[b % 2]
                ps = g.psb[2 + b % 2]
                k.op("pool", lambda: pool.tensor_tensor(out=s_[:, 0:n], in0=r[:, c0:c0 + n], in1=r[:, c0:c0 + n], op=ALU.mult), [s_[:]],
                     [r[:]])
                k.op("pe", lambda: pe.matmul(ps[:, 0:n], lhsT=g.ones, rhs=s_[:, 0:n], start=True, stop=True), [ps[:]], [s_[:], g.cst[:]])
                k.op("act", lambda: act.activation(out=r_[:, 0:n], in_=ps[:, 0:n], func=AF.Sqrt, bias=g.epsT), [r_[:]], [ps[:], g.cst[:]])
                k.op("dve", lambda: vec.reciprocal(out=r_[:, 0:n], in_=r_[:, 0:n]), [r_[:]], [r_[:]])
                k.op("dve", lambda: vec.scalar_tensor_tensor(out=rb[:, c0:c0 + n], in0=r[:, c0:c0 + n], scalar=scl, in1=r_[:, 0:n],
                                                            op0=ALU.mult, op1=ALU.mult), [rb[:]], [r[:], r_[:]])
        pending_store = (fc, rb)
    pf, prb = pending_store
    k.dma("pool", g.d_qkv[pf], prb[:], [("x", ("qkv", pf))], [prb[:]])
    k.release(m)


def gdn_scan(k, g):
    nc = k.nc
    vec, act, pe, pool = nc.vector, nc.scalar, nc.tensor, nc.gpsimd
    m = k.mark()
    BC = 8
    BW = BC * 64
    id64 = g.cst[0:64, C_ID:C_ID + 64]
    TM = g.TM
    masks = {nm: g.cst[0:64, c0:c0 + 64] for nm, c0 in (("ls", C_NMLS), ("us", C_NMUS), ("li", C_NMLI), ("ui", C_NMUI))}
    idb = k.sb([128, 128], BF16, "gidb")
    k.op("dve", lambda: vec.tensor_copy(out=idb[:], in_=g.ident), [idb[:]], [g.cst[:]])

    class BS:
        pass

    sets = []
    for d in range(4):
        b_ = BS()
        n_ = "d%d" % d
        b_.qTb = k.sb([128, BW], BF16, "qTb" + n_)
        b_.kTb = k.sb([128, BW], BF16, "kTb" + n_)
        b_.vTb = k.sb([128, BW], BF16, "vTb" + n_)
        b_.grow = k.sb([64, BC, 64], F32, "grow" + n_)
        b_.brow = k.sb([64, BC, 64], F32, "brow" + n_)
        b_.cdb = k.sb([128, BC], F32, "cdb" + n_)
        b_.ktm = k.sb([64, BC, 128], BF16, "ktm" + n_)
        b_.vtm = k.sb([64, BC, 128], BF16, "vtm" + n_)
        b_.kdec = k.sb([64, BC, 128], BF16, "kdec" + n_)
        b_.dif = k.sb([64, BC, 64], F32, "dif" + n_)
        b_.E = [k.sb([64, BC, 64], F32, "gE%d%s" % (i, n_)) for i in range(3)]
        b_.Y = [k.sb([64, BC, 64], BF16, "gY%d%s" % (i, n_)) for i in range(2)]
        b_.YT = [k.sb([64, BC, 64], BF16, "gYT%d%s" % (i, n_)) for i in range(2)]
        b_.PT = [k.sb([64, BC, 64], BF16, "gPT%d%s" % (i, n_)) for i in range(2)]
        b_.AQ = k.sb([64, BC, 64], BF16, "gAQ" + n_)
        b_.ob = k.sb([64, BC, 128], F32, "gob" + n_)
        b_.S = k.sb([128, 128], F32, "gS" + n_)
        b_.Sb = k.sb([128, 128], BF16, "gSb" + n_)
        b_.t2a = k.sb([64, 128], F32, "gt2a" + n_)
        b_.t2b = k.sb([64, 128], BF16, "gt2b" + n_)
        b_.vnew = k.sb([64, 128], BF16, "gvnew" + n_)
        b_.t3 = k.sb([64, 128], F32, "gt3" + n_)
        b_.pb = [g.psb[2 * d], g.psb[2 * d + 1], g.psb[2 * d], g.psb[2 * d + 1]]
        sets.append(b_)

    def evac(i, out, in_):
        if True:
            k.op("act", lambda: act.copy(out=out, in_=in_), [out], [in_])
        else:
            k.op("dve", lambda: vec.tensor_copy(out=out, in_=in_), [out], [in_])

    def run(h, d, si):
        B_ = sets[si]
        qTb, kTb, vTb, grow, brow, cdb, ktm, vtm, kdec, dif = (B_.qTb, B_.kTb, B_.vTb, B_.grow, B_.brow, B_.cdb, B_.ktm, B_.vtm,
                                                               B_.kdec, B_.dif)
        E, Y, YT, PT, AQ, ob, S, Sb, t2a, t2b, vnew, t3, pb = (B_.E, B_.Y, B_.YT, B_.PT, B_.AQ, B_.ob, B_.S, B_.Sb, B_.t2a, B_.t2b,
                                                              B_.vnew, B_.t3, B_.pb)
        dh = d * 8 + h
        lat = [(c0, 8) for c0 in range(0, 64, 8)]
        batches = [(64, 4)] + (lat if d == 0 else lat[::-1])
        k.op("pool", lambda: pool.memset(S[:], 0.0), [S[:]], [])
        k.op("pool", lambda: pool.memset(Sb[:], 0.0), [Sb[:]], [])

        def rk(r):
            return pb[3][:]

        for (c0, nch) in batches:
            tok0 = c0 * 64
            W = nch * 64
            k.dma("sp", qTb[:, 0:W], g.d_qkv[h][:, tok0:tok0 + W], [qTb[:]], [("x", ("qkv", h))])
            k.dma("sp", kTb[:, 0:W], g.d_qkv[8 + h][:, tok0:tok0 + W], [kTb[:]], [("x", ("qkv", 8 + h))])
            k.dma("sp", vTb[:, 0:W], g.d_qkv[16 + h][:, tok0:tok0 + W], [vTb[:]], [("x", ("qkv", 16 + h))])
            for dst, ri in ((grow, 0), (brow, 1)):
                src = g.d_rows[ri, dh, tok0:tok0 + W]
                k.dma("sp", dst[:, 0:nch].rearrange("p c j -> p (c j)"), bass.AP(src.tensor, src.offset, [[0, 64], [1, W]]),
                      [dst[:]], [("x", "rows")])
            srcc = g.d_cd[dh, c0:c0 + nch]
            k.dma("sp", cdb[:, 0:nch], bass.AP(srcc.tensor, srcc.offset, [[0, 128], [1, nch]]), [cdb[:]], [("x", "rows")])
            yield

            def tmc(q, inner):
                return V(TM[:, c0, q, dh:dh + 1], (64, nch), (0, inner))

            for si, (src, dst) in enumerate(((kTb, ktm), (vTb, vtm))):
                psv = pb[si][:].bitcast(BF16)
                for c in range(nch):
                    k.op("pe", lambda: pe.transpose(psv[0:64, c * 128:(c + 1) * 128], src[:, c * 64:(c + 1) * 64], idb[:]), [pb[si][:]],
                         [src[:], idb[:]], inc=(c == nch - 1))
                evac(si, dst[:, 0:nch].rearrange("p c f -> p (c f)"), psv[0:64, 0:nch * 128])
                yield
            k.op("pool", lambda: pool.tensor_tensor(out=kdec[:, 0:nch], in0=ktm[:, 0:nch], in1=tmc(3, 128), op=ALU.mult), [kdec[:]],
                 [ktm[:], TM[:]])
            yield
            for c in range(nch):
                k.op("pe", lambda: pe.matmul(pb[1][0:64, c * 64:(c + 1) * 64], lhsT=kTb[:, c * 64:(c + 1) * 64],
                                             rhs=kTb[:, c * 64:(c + 1) * 64], start=True, stop=True), [pb[1][:]], [kTb[:]],
                     inc=(c == nch - 1))
            for c in range(nch):
                k.op("pe", lambda: pe.matmul(pb[2][0:64, c * 64:(c + 1) * 64], lhsT=kTb[:, c * 64:(c + 1) * 64],
                                             rhs=qTb[:, c * 64:(c + 1) * 64], start=True, stop=True), [pb[2][:]],
                     [kTb[:], qTb[:]], inc=(c == nch - 1))
            yield
            k.op("pool", lambda: pool.tensor_tensor(out=dif[:, 0:nch], in0=grow[:, 0:nch], in1=tmc(0, 64), op=ALU.subtract), [dif[:]],
                 [grow[:], TM[:]])
            mk = ("ls", "us", "ui") if d == 0 else ("us", "ls", "li")
            for i in range(3):
                mv = V(masks[mk[i]], (0, nch), (1, 64))
                k.op("pool", lambda: pool.tensor_tensor(out=E[i][:, 0:nch], in0=dif[:, 0:nch], in1=mv,
                                                        op=(ALU.subtract if i == 0 else ALU.add)), [E[i][:]], [dif[:], g.cst[:]])
                k.op("act", lambda: act.activation(out=E[i][:, 0:nch], in_=E[i][:, 0:nch], func=AF.Exp,
                                                   scale=(-1.0 if i == 0 else 1.0)), [E[i][:]], [E[i][:]])
                yield
            cs = slice(0, nch)
            kk3 = pb[1][0:64, 0:nch * 64].rearrange("p (c j) -> p c j", j=64)
            qk3 = pb[2][0:64, 0:nch * 64].rearrange("p (c j) -> p c j", j=64)
            k.op("dve", lambda: vec.tensor_tensor(out=E[0][:, cs], in0=kk3, in1=E[0][:, cs], op=ALU.mult), [E[0][:]],
                 [pb[1][:], E[0][:]])
            k.op("dve", lambda: vec.scalar_tensor_tensor(out=Y[0][:, cs], in0=E[0][:, cs], scalar=1.0, in1=tmc(1, 64),
                                                        op0=ALU.mult, op1=ALU.mult), [Y[0][:]], [E[0][:], TM[:]])
            yield
            k.op("dve", lambda: vec.tensor_tensor(out=E[1][:, cs], in0=kk3, in1=E[1][:, cs], op=ALU.mult), [E[1][:]],
                 [pb[1][:], E[1][:]])
            k.op("dve", lambda: vec.scalar_tensor_tensor(out=YT[0][:, cs], in0=E[1][:, cs], scalar=-1.0, in1=brow[:, cs],
                                                        op0=ALU.mult, op1=ALU.mult), [YT[0][:]], [E[1][:], brow[:]])
            yield
            k.op("dve", lambda: vec.tensor_tensor(out=AQ[:, cs], in0=qk3, in1=E[2][:, cs], op=ALU.mult), [AQ[:]],
                 [pb[2][:], E[2][:]])
            k.op("pool", lambda: pool.tensor_tensor(out=PT[0][:, 0:nch], in0=YT[0][:, 0:nch], in1=V(id64, (0, nch), (1, 64)),
                                                    op=ALU.add), [PT[0][:]], [YT[0][:], g.cst[:]])
            yield
            cy, cp = 0, 0
            for lvl in range(5):
                ny = 1 - cy
                for c in range(nch):
                    k.op("pe", lambda: pe.matmul(pb[0][0:64, c * 64:(c + 1) * 64], lhsT=YT[cy][:, c, :], rhs=Y[cy][:, c, :],
                                                 start=True, stop=True), [pb[0][:]], [YT[cy][:], Y[cy][:]], inc=(c == nch - 1))
                evac(0, Y[ny][:, 0:nch].rearrange("p c j -> p (c j)"), pb[0][0:64, 0:nch * 64])
                yield
                if lvl < 4:
                    for c in range(nch):
                        k.op("pe", lambda: pe.matmul(pb[1][0:64, c * 64:(c + 1) * 64], lhsT=Y[cy][:, c, :], rhs=YT[cy][:, c, :],
                                                     start=True, stop=True), [pb[1][:]], [YT[cy][:], Y[cy][:]], inc=(c == nch - 1))
                    evac(1, YT[ny][:, 0:nch].rearrange("p c j -> p (c j)"), pb[1][0:64, 0:nch * 64])
                    yield
                np_ = 1 - cp
                for c in range(nch):
                    k.op("pe", lambda: pe.matmul(pb[2][0:64, c * 64:(c + 1) * 64], lhsT=Y[ny][:, c, :], rhs=PT[cp][:, c, :],
                                                 start=True, stop=True), [pb[2][:]], [Y[ny][:], PT[cp][:]], inc=(c == nch - 1))
                k.op("dve", lambda: vec.tensor_tensor(out=PT[np_][:, 0:nch].rearrange("p c j -> p (c j)"), in0=pb[2][0:64, 0:nch * 64],
                                                      in1=PT[cp][:, 0:nch].rearrange("p c j -> p (c j)"), op=ALU.add),
                     [PT[np_][:]], [pb[2][:], PT[cp][:]])
                yield
                cy, cp = ny, np_
            TT = PT[cp]
            order = range(nch) if d == 0 else range(nch - 1, -1, -1)
            R = pb[3]
            for c in order:
                gc = c0 + c
                eGc = TM[:, gc, 2, dh:dh + 1]
                bc_ = TM[:, gc, 1, dh:dh + 1]
                k.op("pe", lambda: pe.matmul(R[0:64, 0:128], lhsT=kTb[:, c * 64:(c + 1) * 64], rhs=Sb[:], start=True, stop=True),
                     [rk(0)], [kTb[:], Sb[:]])
                k.op("pe", lambda: pe.matmul(R[0:64, 384:512], lhsT=qTb[:, c * 64:(c + 1) * 64], rhs=Sb[:], start=True, stop=True),
                     [rk(3)], [qTb[:], Sb[:]])
                k.op("dve", lambda: vec.scalar_tensor_tensor(out=t2a[:], in0=R[0:64, 0:128], scalar=eGc, in1=vtm[:, c, :],
                                                            op0=ALU.mult, op1=ALU.subtract), [t2a[:]], [rk(0), TM[:], vtm[:]])
                k.op("act", lambda: act.activation(out=t2b[:], in_=t2a[:], func=AF.Identity, scale=bc_), [t2b[:]], [t2a[:], TM[:]])
                yield
                k.op("pe", lambda: pe.matmul(R[0:64, 128:256], lhsT=TT[:, c, :], rhs=t2b[:], start=True, stop=True), [rk(1)],
                     [TT[:], t2b[:]])
                k.op("act", lambda: act.copy(out=vnew[:], in_=R[0:64, 128:256]), [vnew[:]], [rk(1)])
                yield
                k.op("pe", lambda: pe.matmul(R[:, 256:384], lhsT=kdec[:, c, :], rhs=vnew[:], start=True, stop=True), [rk(2)],
                     [kdec[:], vnew[:]])
                k.op("pe", lambda: pe.matmul(pb[2][0:64, 0:128], lhsT=AQ[:, c, :], rhs=vnew[:], start=True, stop=True), [pb[2][:]],
                     [AQ[:], vnew[:]])
                k.op("dve", lambda: vec.scalar_tensor_tensor(out=S[:], in0=S[:], scalar=cdb[:, c:c + 1], in1=R[:, 256:384],
                                                            op0=ALU.mult, op1=ALU.add), [S[:]], [S[:], cdb[:], rk(2)])
                k.op("act", lambda: act.copy(out=Sb[:], in_=S[:]), [Sb[:]], [S[:]])
                k.op("act", lambda: act.activation(out=t3[:], in_=R[0:64, 384:512], func=AF.Identity, scale=eGc), [t3[:]],
                     [rk(3), TM[:]])
                k.op("dve", lambda: vec.tensor_tensor(out=ob[:, c, :], in0=pb[2][0:64, 0:128], in1=t3[:], op=ALU.add), [ob[:]],
                     [pb[2][:], t3[:]])
                yield
            dsto = g.d_o[d][tok0:tok0 + W, h * 128:(h + 1) * 128].rearrange("(c p) f -> p c f", p=64)
            k.dma("pool", dsto, ob[:, 0:nch], [("x", ("o", d, c0))], [ob[:]])
            yield

    for h2 in range(4):
        gens = [run(2 * h2, 0, 0), run(2 * h2, 1, 1), run(2 * h2 + 1, 0, 2), run(2 * h2 + 1, 1, 3)]
        alive = [True] * 4
        while any(alive):
            for i in range(4):
                if alive[i]:
                    try:
                        next(gens[i])
                    except StopIteration:
                        alive[i] = False
    k.release(m)


def gdn_out(k, g, li, ntiles):
    nc = k.nc
    vec, act, pe, pool = nc.vector, nc.scalar, nc.tensor, nc.gpsimd
    m = k.mark()
    wz = k.sb([128, 8, 1024], BF16, "wz")
    wo = k.sb([128, 8, 1024], BF16, "wo")
    for kk in range(8):
        k.dma("pool", wz[:, kk, :], g.d_gwin[kk * 128:(kk + 1) * 128, 3072:4096], [wz[:]], [])
        k.dma("pool", wo[:, kk, :], g.d_gwout[kk * 128:(kk + 1) * 128, :], [wo[:]], [])
    ng = k.sb([128, 128], F32, "gng")
    bcast_row_load(k, ng[:], g.d_gng, 128)
    GT = k.sb([128, 1024], F32, "ggt")
    of = [k.sb([128, 1024], F32, "gof%d" % i) for i in range(2)]
    obw = [k.sb([128, 1024], F32, "gobw%d" % i) for i in range(2)]
    xt = [k.sb([128, 1024], F32, "gxt%d" % i) for i in range(2)]
    hT = [k.sb([128, 8, 128], BF16, "ghT%d" % i) for i in range(2)]
    sq = k.sb([128, 1024], F32, "gsq")
    zs = k.sb([128, 1024], F32, "gzs")
    st = k.sb([128, 24], F32, "gst")
    ogT = k.sb([128, 8, 128], BF16, "gogT")
    cur = -1
    for t in range(ntiles):
        r = 0 if t < L // 128 else 1
        if r != cur:
            cur = r
            bcast_row_load(k, GT[:], g.d_mod[li, r, 2], 1024)
        o, o2, x, h = of[t % 2], obw[t % 2], xt[t % 2], hT[t % 2]
        k.dma("sp", o[:], g.d_o[0][t * 128:(t + 1) * 128, :], [o[:]], [("x", ("o", 0, c)) for c in range(0, 72, 8)])
        k.dma("sp", o2[:], g.d_o[1][t * 128:(t + 1) * 128, :], [o2[:]], [("x", ("o", 1, c)) for c in range(0, 72, 8)])
        k.dma("sp", x[:], g.d_xs[t * 128:(t + 1) * 128, :], [x[:]], [("x", ("xs", t))])
        k.dma("sp", h[:], g.d_hT[t], [h[:]], [("x", ("hT", t))])
        for hf in range(2):
            ps = g.psb[hf]
            for kk in range(8):
                k.op("pe", lambda: pe.matmul(ps[:], lhsT=h[:, kk, :], rhs=wz[:, kk, hf * 512:(hf + 1) * 512], start=(kk == 0),
                                             stop=(kk == 7)), [ps[:]], [h[:], wz[:]], inc=(kk == 7))
            k.op("act", lambda: act.activation(out=zs[:, hf * 512:(hf + 1) * 512], in_=ps[:], func=AF.Silu), [zs[:]], [ps[:]])
        k.op("pool", lambda: pool.tensor_tensor(out=o[:], in0=o[:], in1=o2[:], op=ALU.add), [o[:]], [o[:], o2[:]])
        k.op("pool", lambda: pool.tensor_tensor(out=sq[:], in0=o[:], in1=o[:], op=ALU.mult), [sq[:]], [o[:]])
        k.op("dve", lambda: vec.tensor_reduce(out=st[:, 0:8], in_=sq[:].rearrange("p (h f) -> p h f", f=128), axis=AX.X, op=ALU.add),
             [("x", "gst0")], [sq[:]])
        k.op("act", lambda: act.activation(out=st[:, 8:16], in_=st[:, 0:8], func=AF.Sqrt, scale=1.0 / 128, bias=g.epsT), [("x", "gst1")],
             [("x", "gst0"), g.cst[:]])
        k.op("dve", lambda: vec.reciprocal(out=st[:, 16:24], in_=st[:, 8:16]), [("x", "gst2")], [("x", "gst1")])
        o3 = o[:].rearrange("p (h f) -> p h f", f=128)
        k.op("dve", lambda: vec.tensor_tensor(out=o3, in0=o3, in1=V(st[:, 16:17], (1, 8), (0, 128)), op=ALU.mult), [o[:]],
             [o[:], ("x", "gst2")])
        k.op("pool", lambda: pool.tensor_tensor(out=o3, in0=o3, in1=V(ng[:, 0:128], (0, 8), (1, 128)), op=ALU.mult), [o[:]], [o[:], ng[:]])
        k.op("dve", lambda: vec.tensor_tensor(out=o[:], in0=o[:], in1=zs[:], op=ALU.mult), [o[:]], [o[:], zs[:]])
        for kk in range(8):
            ps = g.psb[2 + kk // 4]
            k.op("pe", lambda: pe.transpose(ps[:, (kk % 4) * 128:(kk % 4 + 1) * 128], o[:, kk * 128:(kk + 1) * 128], g.ident), [ps[:]],
                 [o[:], g.cst[:]], inc=(kk % 4 == 3))
            if kk % 4 == 3:
                h0 = kk // 4
                k.op("act", lambda: act.copy(out=ogT[:, h0 * 4:h0 * 4 + 4, :], in_=ps[:].rearrange("p (a b) -> p a b", a=4)), [ogT[:]],
                     [ps[:]])
        for hf in range(2):
            ps = g.psb[4 + hf]
            for kk in range(8):
                k.op("pe", lambda: pe.matmul(ps[:], lhsT=ogT[:, kk, :], rhs=wo[:, kk, hf * 512:(hf + 1) * 512], start=(kk == 0),
                                             stop=(kk == 7)), [ps[:]], [ogT[:], wo[:]], inc=(kk == 7))
            sl = slice(hf * 512, (hf + 1) * 512)
            k.op("dve", lambda: vec.tensor_tensor(out=o2[:, sl], in0=ps[:], in1=GT[:, sl], op=ALU.mult), [o2[:]], [ps[:], GT[:]])
        k.op("pool", lambda: pool.tensor_tensor(out=o2[:], in0=o2[:], in1=x[:], op=ALU.add), [o2[:]], [o2[:], x[:]])
        k.dma("pool", g.d_xs[t * 128:(t + 1) * 128, :], o2[:], [("x", ("xs", t))], [o2[:]])
    k.release(m)


def host_cst2():
    r = np.ones((16, NTOK), np.float32)
    r[:, ::64] = 0.0
    return r


def prep_shared(inp):
    sh = {}
    sh["cst"] = host_consts()
    sh["cst2"] = host_cst2()
    sh["peer_wq"] = np.ascontiguousarray(inp["peer_w_query"], dtype=np.float32)
    sh["peer_sk"] = np.ascontiguousarray(inp["peer_sub_keys"], dtype=np.float32)
    u = np.asarray(inp["peer_u"], dtype=np.float32).reshape(2, 128, 128, 8, 128)
    sh["peer_ut"] = np.ascontiguousarray(u.transpose(0, 1, 4, 3, 2)).reshape(2, 128, 128, 1024)
    sh["peer_v"] = np.ascontiguousarray(inp["peer_v"], dtype=np.float32).reshape(2, 128, 128, 1024)
    sh["final_g"] = np.ascontiguousarray(inp["final_g"], dtype=np.float32)
    sh["ada_w"] = np.ascontiguousarray(inp["ada_w"], dtype=np.float32)
    sh["ada_b"] = np.ascontiguousarray(inp["ada_b"], dtype=np.float32)
    sh["norm1_g"] = np.ascontiguousarray(inp["norm1_g"], dtype=np.float32)
    sh["norm2_g"] = np.ascontiguousarray(inp["norm2_g"], dtype=np.float32)
    sh["gdn_w_in"] = np.ascontiguousarray(inp["gdn_w_in"][0], dtype=np.float32)
    cw = np.asarray(inp["gdn_conv_w"][0], dtype=np.float32)
    sh["gdn_conv"] = np.ascontiguousarray(cw.T.reshape(24, 128, 5))
    sh["gdn_a_log"] = np.ascontiguousarray(inp["gdn_a_log"][0], dtype=np.float32)
    sh["gdn_dt_bias"] = np.ascontiguousarray(inp["gdn_dt_bias"][0], dtype=np.float32)
    sh["gdn_norm_g"] = np.ascontiguousarray(inp["gdn_norm_g"][0], dtype=np.float32)
    sh["gdn_w_out"] = np.ascontiguousarray(inp["gdn_w_out"][0], dtype=np.float32)
    sh["swa_w_in"] = np.ascontiguousarray(inp["swa_w_in"][0], dtype=np.float32)
    sh["swa_w_out"] = np.ascontiguousarray(inp["swa_w_out"][0], dtype=np.float32)
    sh["swa_sinks"] = np.ascontiguousarray(inp["swa_sinks"][0], dtype=np.float32)
    sh["rope"] = host_rope()
    return sh


def prep_core(inp, b):
    pc = {}
    pc["x"] = np.ascontiguousarray(inp["x"][b], dtype=np.float32)
    pc["ctx"] = np.ascontiguousarray(inp["ctx"][b], dtype=np.float32)
    cc = np.stack([np.asarray(inp["c"][b], np.float32), np.asarray(inp["c_ctx"], np.float32)], axis=-1)
    pc["cT"] = np.ascontiguousarray(cc.reshape(8, 128, 2).transpose(1, 0, 2))
    return pc


def host_rope():
    rows = L // 64
    row = np.repeat(np.arange(rows), 64).astype(np.float32)
    col = np.tile(np.arange(64), rows).astype(np.float32)
    inv = (10000.0 ** (-np.arange(0, 32, 2, dtype=np.float32) / 32.0)).astype(np.float32)
    ar = row[:, None] * inv
    ac = col[:, None] * inv
    cos = np.concatenate([np.cos(ar), np.cos(ar), np.cos(ac), np.cos(ac)], axis=1)
    sin = np.concatenate([-np.sin(ar), np.sin(ar), -np.sin(ac), np.sin(ac)], axis=1)
    return np.ascontiguousarray(np.stack([cos, sin]).astype(np.float32))


def swa_phase(k, g, li):
    nc = k.nc
    vec, act, pe, pool = nc.vector, nc.scalar, nc.tensor, nc.gpsimd
    m = k.mark()
    NLT = L // 128
    win = k.sb([128, 8, 1536], BF16, "swin")
    wo = k.sb([128, 8, 1024], BF16, "swo")
    for kk in range(8):
        k.dma("pool", win[:, kk, :], g.d_swin[kk * 128:(kk + 1) * 128, :], [win[:]], [])
        k.dma("pool", wo[:, kk, :], g.d_swout[kk * 128:(kk + 1) * 128, :], [wo[:]], [])
    QT = k.sb([128, 8, L], BF16, "sQT")
    KT = k.sb([128, 4, NTOK], BF16, "sKT")
    V1 = k.sb([128, NT, 4, 65], BF16, "sV1")
    k.op("pool", lambda: pool.memset(V1[:], 1.0), [V1[:]], [])
    idb = k.sb([128, 128], BF16, "sidb")
    nmp = k.sb([128, 128], BF16, "snmp")
    nmn = k.sb([128, 128], BF16, "snmn")
    k.op("dve", lambda: vec.tensor_copy(out=idb[:], in_=g.ident), [idb[:]], [g.cst[:]])
    k.op("dve", lambda: vec.tensor_copy(out=nmp[:], in_=g.cst[:, C_NMP:C_NMP + 128]), [nmp[:]], [g.cst[:]])
    k.op("dve", lambda: vec.tensor_copy(out=nmn[:], in_=g.cst[:, C_NMN:C_NMN + 128]), [nmn[:]], [g.cst[:]])
    esink = k.sb([128, 16], F32, "sesink")
    bcast_row_load(k, esink[:], g.d_ssink, 16)
    k.op("act", lambda: act.activation(out=esink[:], in_=esink[:], func=AF.Exp), [esink[:]], [esink[:]])
    GT = k.sb([128, 1024], F32, "sgt")
    bcast_row_load(k, GT[:], g.d_mod[li, 0, 2], 1024)
    hT = [k.sb([128, 8, 128], BF16, "shT%d" % i) for i in range(2)]
    cs = [k.sb([128, 2, 64], F32, "scs%d" % i) for i in range(2)]
    tmps = [k.sb([128, 512], F32, "stmp%d" % i) for i in range(3)]
    tmp2s = [k.sb([128, 512], F32, "stmp2%d" % i) for i in range(3)]
    qrs = [k.sb([128, 1024], BF16, "sqr%d" % i) for i in range(2)]
    krs = [k.sb([128, 4, 2, 64], BF16, "skr%d" % i) for i in range(2)]
    pb = g.psb
    for t in range(NT):
        lat = t < NLT
        h = hT[t % 2]
        k.dma("sp", h[:], g.d_hT[t], [h[:]], [("x", ("hT", t))])
        if lat:
            c_ = cs[t % 2]
            k.dma("sp", c_[:], g.d_rope[:, t * 128:(t + 1) * 128, :].rearrange("a p e -> p a e"), [c_[:]], [])
        qr, kr = qrs[t % 2], krs[t % 2]
        banks = [0, 1, 2] if lat else [2]
        for b in banks:
            for kk in range(8):
                k.op("pe", lambda: pe.matmul(pb[b][:], lhsT=h[:, kk, :], rhs=win[:, kk, b * 512:(b + 1) * 512], start=(kk == 0),
                                             stop=(kk == 7)), [pb[b][:]], [h[:], win[:]], inc=(kk == 7))
        if lat:
            for b, H, dst in ((0, 8, qr[:, 0:512]), (1, 8, qr[:, 512:1024]), (2, 4, None)):
                W = H * 64
                tmp, tmp2 = tmps[b], tmp2s[b]
                src = pb[b][:, 0:W]
                for hf in range(2):
                    pv = V(pb[b][:, (1 - hf) * 16:(1 - hf) * 16 + 1], (32, 2 * H), (1, 16))
                    sv = V(c_[:, 1, hf * 16:hf * 16 + 1], (0, H), (32, 2), (1, 16))
                    ov = V(tmp[:, hf * 16:hf * 16 + 1], (32, 2 * H), (1, 16))
                    pv4 = V(pb[b][:, (1 - hf) * 16:(1 - hf) * 16 + 1], (64, H), (32, 2), (1, 16))
                    ov4 = V(tmp[:, hf * 16:hf * 16 + 1], (64, H), (32, 2), (1, 16))
                    k.op("dve", lambda: vec.tensor_tensor(out=ov4, in0=pv4, in1=sv, op=ALU.mult), [tmp[:]], [pb[b][:], c_[:]])
                k.op("dve", lambda: vec.tensor_tensor(out=tmp2[:, 0:W].rearrange("p (h e) -> p h e", e=64),
                                                      in0=src.rearrange("p (h e) -> p h e", e=64), in1=V(c_[:, 0, 0:1], (0, H), (1, 64)),
                                                      op=ALU.mult), [tmp2[:]], [pb[b][:], c_[:]])
                if b < 2:
                    k.op("pool", lambda: pool.tensor_tensor(out=dst, in0=tmp[:, 0:W], in1=tmp2[:, 0:W], op=ALU.add), [qr[:]],
                         [tmp[:], tmp2[:]])
                else:
                    for dup in range(2):
                        k.op("pool", lambda: pool.tensor_tensor(out=kr[:, :, dup, :], in0=tmp[:, 0:W].rearrange("p (h e) -> p h e", e=64),
                                                                in1=tmp2[:, 0:W].rearrange("p (h e) -> p h e", e=64), op=ALU.add),
                             [kr[:]], [tmp[:], tmp2[:]])
            psq = pb[3][:].bitcast(BF16)
            for pr in range(8):
                k.op("pe", lambda: pe.transpose(psq[:, pr * 128:(pr + 1) * 128], qr[:, pr * 128:(pr + 1) * 128], idb[:]), [pb[3][:]],
                     [qr[:], idb[:]], inc=(pr == 7))
            k.op("act", lambda: act.copy(out=QT[:, :, t * 128:(t + 1) * 128], in_=psq[:, 0:1024].rearrange("p (a b) -> p a b", a=8)),
                 [QT[:]], [pb[3][:]])
        else:
            for dup in range(2):
                k.op("act", lambda: act.copy(out=kr[:, :, dup, :], in_=pb[2][:, 0:256].rearrange("p (h e) -> p h e", e=64)), [kr[:]],
                     [pb[2][:]])
        psk = pb[4][:].bitcast(BF16)
        for j in range(4):
            k.op("pe", lambda: pe.transpose(psk[:, j * 128:(j + 1) * 128], kr[:, j].rearrange("p a e -> p (a e)"), idb[:]), [pb[4][:]],
                 [kr[:], idb[:]], inc=(j == 3))
        k.op("dve", lambda: vec.tensor_copy(out=KT[:, :, t * 128:(t + 1) * 128], in_=psk[:, 0:512].rearrange("p (a b) -> p a b", a=4)),
             [KT[:]], [pb[4][:]])
        k.op("act", lambda: act.copy(out=V1[:, t, :, 0:64], in_=pb[2][:, 256:512].rearrange("p (a b) -> p a b", a=4)), [V1[:]], [pb[2][:]])
    PT = [k.sb([128, 640], BF16, "sPT%d" % i) for i in range(2)]
    otm = k.sb([128, 1024], BF16, "sotm")
    den = k.sb([128, 8], F32, "sden")
    ogT = k.sb([128, 8, 128], BF16, "sogT")
    xt = [k.sb([128, 1024], F32, "sxt%d" % i) for i in range(2)]
    ysb = k.sb([128, 1024], F32, "sysb")
    psqT = pb[6][:].bitcast(BF16)
    n = 0
    for t in range(NLT):
        x = xt[t % 2]
        k.dma("sp", x[:], g.d_xs[t * 128:(t + 1) * 128, :], [x[:]], [("x", ("xs", t))])
        kbs = ([(t - 1, "p")] if t > 0 else []) + [(t, "c")] + ([(t + 1, "n")] if t < NLT - 1 else []) + [(NLT, "c"), (NLT + 1, "c")]
        nkb = len(kbs)
        for j in range(4):
            pso = pb[4 + j % 2]
            for hh in range(4):
                hq = 4 * j + hh
                pair, u = hq // 2, hq % 2
                sl = slice(u * 64, (u + 1) * 64)
                bs = (n % 2) * 2
                n += 1
                for i, (kt, kind) in enumerate(kbs):
                    dst = pb[bs + i // 4][:, (i % 4) * 128:(i % 4 + 1) * 128]
                    last = kind == "c"
                    k.op("pe", lambda: pe.matmul(dst, lhsT=KT[sl, j, kt * 128:(kt + 1) * 128], rhs=QT[sl, pair, t * 128:(t + 1) * 128],
                                                 start=True, stop=last), [pb[bs + i // 4][:]], [KT[:], QT[:]],
                         inc=(last and (i == nkb - 1 or i == 3)))
                    if not last:
                        mk_ = nmp if kind == "p" else nmn
                        k.op("pe", lambda: pe.matmul(dst, lhsT=idb[:], rhs=mk_[:], start=False, stop=True), [pb[bs + i // 4][:]],
                             [idb[:], mk_[:]], inc=(i == nkb - 1 or i == 3))
                P = PT[n % 2]
                n0 = min(nkb, 4) * 128
                k.op("act", lambda: act.activation(out=P[:, 0:n0], in_=pb[bs][:, 0:n0], func=AF.Exp, scale=0.125), [P[:]], [pb[bs][:]])
                if nkb > 4:
                    k.op("act", lambda: act.activation(out=P[:, 512:640], in_=pb[bs + 1][:, 0:128], func=AF.Exp, scale=0.125), [P[:]],
                         [pb[bs + 1][:]])
                for i, (kt, kind) in enumerate(kbs):
                    k.op("pe", lambda: pe.matmul(pso[:, hh * 128:hh * 128 + 65], lhsT=P[:, i * 128:(i + 1) * 128], rhs=V1[:, kt, j, :],
                                                 start=(i == 0), stop=(i == nkb - 1)), [pso[:]], [P[:], V1[:]],
                         inc=(i == nkb - 1 and hh == 3))
            k.op("dve", lambda: vec.tensor_tensor(out=den[:, 0:4], in0=V(pso[:, 64:65], (128, 4)), in1=esink[:, 4 * j:4 * j + 4], op=ALU.add),
                 [("x", "sden0")], [pso[:], esink[:]])
            k.op("dve", lambda: vec.reciprocal(out=den[:, 4:8], in_=den[:, 0:4]), [("x", "sden1")], [("x", "sden0")])
            k.op("dve", lambda: vec.tensor_tensor(out=otm[:, j * 256:(j + 1) * 256].rearrange("p (a e) -> p a e", e=64),
                                                  in0=V(pso[:, 0:1], (128, 4), (1, 64)), in1=V(den[:, 4:5], (1, 4), (0, 64)), op=ALU.mult),
                 [otm[:]], [pso[:], ("x", "sden1")])
        for kk in range(8):
            k.op("pe", lambda: pe.transpose(psqT[:, kk * 128:(kk + 1) * 128], otm[:, kk * 128:(kk + 1) * 128], idb[:]), [pb[6][:]],
                 [otm[:], idb[:]], inc=(kk == 7))
        k.op("act", lambda: act.copy(out=ogT[:], in_=psqT[:, 0:1024].rearrange("p (a b) -> p a b", a=8)), [ogT[:]], [pb[6][:]])
        for hf in range(2):
            ps = pb[7 - hf]
            for kk in range(8):
                k.op("pe", lambda: pe.matmul(ps[:], lhsT=ogT[:, kk, :], rhs=wo[:, kk, hf * 512:(hf + 1) * 512], start=(kk == 0), stop=(kk == 7)),
                     [ps[:]], [ogT[:], wo[:]], inc=(kk == 7))
            sl2 = slice(hf * 512, (hf + 1) * 512)
            k.op("dve", lambda: vec.tensor_tensor(out=ysb[:, sl2], in0=ps[:], in1=GT[:, sl2], op=ALU.mult), [ysb[:]], [ps[:], GT[:]])
        k.op("pool", lambda: pool.tensor_tensor(out=ysb[:], in0=ysb[:], in1=x[:], op=ALU.add), [ysb[:]], [ysb[:], x[:]])
        k.dma("pool", g.d_xs[t * 128:(t + 1) * 128, :], ysb[:], [("x", ("xs", t))], [ysb[:]])
    k.release(m)


FULL_PHASES = (("adaln",), ("gdn",), ("peer", 0, NT, False), ("swa",), ("peer", 1, L // 128, True))


def kernel(**inputs):
    k = build(phases=FULL_PHASES)
    sh = prep_shared(inputs)
    names = set(k.ext_inputs)
    in_maps = []
    for b in range(8):
        d_ = dict(sh)
        d_.update(prep_core(inputs, b))
        in_maps.append({a: v for a, v in d_.items() if a in names})
    res = run_bass_kernel_spmd(k.nc, in_maps, core_ids=list(range(8)))
    return np.stack([np.asarray(r["out"], dtype=np.float32) for r in res.results], axis=0)
```

```python
import numpy as np
import concourse.bass as bass
import concourse.mybir as mybir
from concourse.bass_utils import run_bass_kernel_spmd

F32 = mybir.dt.float32
BF16 = mybir.dt.bfloat16
U32 = mybir.dt.uint32
AF = mybir.ActivationFunctionType
ALU = mybir.AluOpType
AX = mybir.AxisListType

D = 1024
L = 4096
CTX = 256
NTOK = L + CTX
NT = NTOK // 128
EPS = 1e-6
NEXP_T = 128


class K:
    def __init__(self):
        self.nc = bass.Bass("TRN2", target_bir_lowering=False)
        nc = self.nc
        self.E = {"pe": nc.tensor, "act": nc.scalar, "dve": nc.vector, "pool": nc.gpsimd, "sp": nc.sync}
        self.sem = {e: nc.alloc_semaphore("s_" + e) for e in self.E}
        self.cnt = {e: 0 for e in self.E}
        self.seen = {e: {} for e in self.E}
        self.st = {}
        self.NDMA = 24
        self.dsem = [nc.alloc_semaphore("s_dma%d" % i) for i in range(self.NDMA)]
        self.dval = [0] * self.NDMA
        self.dnext = 0
        self.nsb = 0
        self.stack = []

    def sb(self, shape, dtype=F32, name=None):
        self.nsb += 1
        name = "sb%d_%s" % (self.nsb, name or "t")
        cm = self.nc.sbuf_tensor(name, list(shape), dtype)
        t = cm.__enter__()
        self.stack.append(cm)
        return t

    def mark(self):
        return len(self.stack)

    def release(self, mark):
        self.barrier()
        while len(self.stack) > mark:
            self.stack.pop().__exit__(None, None, None)

    def _semh(self, sk):
        return self.sem[sk] if isinstance(sk, str) else self.dsem[sk]

    def wait(self, e, tok):
        sk, v = tok
        if sk == e and e == "pe":
            return
        if self.seen[e].get(sk, 0) >= v:
            return
        self.E[e].wait_ge(self._semh(sk), v)
        self.seen[e][sk] = v

    @staticmethod
    def _key(x):
        if isinstance(x, tuple):
            return x[1]
        if isinstance(x, str):
            return x
        return x.tensor.name

    def _deps(self, e, outs, ins):
        deps = []
        for x in ins:
            s = self.st.get(self._key(x))
            if s and s[0]:
                deps.append(s[0])
        for x in outs:
            s = self.st.get(self._key(x))
            if s:
                if s[0] and s[0][0] != e:
                    deps.append(s[0])
                for re_, rt in s[1].items():
                    if re_ != e:
                        deps.append(rt)
        for t in deps:
            self.wait(e, t)

    def _commit(self, tok, e, outs, ins):
        for x in ins:
            s = self.st.setdefault(self._key(x), [None, {}])
            s[1][e] = tok
        for x in outs:
            s = self.st.setdefault(self._key(x), [None, {}])
            s[0] = tok
            s[1] = {}

    def op(self, e, fn, outs, ins, inc=True):
        self._deps(e, outs, ins)
        inst = fn()
        tok = (e, self.cnt[e] + 1)
        if inc:
            inst.then_inc(self.sem[e], 1)
            self.cnt[e] += 1
        self._commit(tok, e, outs, ins)
        return inst

    def dma(self, q, out, in_, outs, ins, **kw):
        self._deps(q, outs, ins)
        i = self.dnext
        self.dnext = (self.dnext + 1) % self.NDMA
        if self.dval[i]:
            self.wait(q, (i, self.dval[i]))
        self.dval[i] += 16
        self.E[q].dma_start(out=out, in_=in_, **kw).then_inc(self.dsem[i], 16)
        tok = (i, self.dval[i])
        for x in ins:
            s = self.st.setdefault(self._key(x), [None, {}])
            s[1]["d%d" % i] = tok
        for x in outs:
            s = self.st.setdefault(self._key(x), [None, {}])
            s[0] = tok
            s[1] = {}
        return tok

    def barrier(self):
        toks = [(e, self.cnt[e]) for e in self.E if self.cnt[e]]
        toks += [(i, self.dval[i]) for i in range(self.NDMA) if self.dval[i]]
        for e in self.E:
            for t in toks:
                if t[0] != e:
                    self.wait(e, t)
        self.st = {}

    def finish(self):
        for i in range(self.NDMA):
            if self.dval[i]:
                self.wait("sp", (i, self.dval[i]))
        for e in self.E:
            if e != "sp" and self.cnt[e]:
                self.wait("sp", (e, self.cnt[e]))


def V(ap, *dims):
    return bass.AP(ap.tensor, ap.offset, [list(ap.ap[0])] + [list(d) for d in dims])


C_ID, C_IOTA, C_EPS, C_DSELB, C_ONES, C_NMLS, C_NMUS, C_NMLI, C_NMUI, C_NMP, C_NMN, C_NCOL = 0, 128, 256, 264, 272, 400, 464, 528, 592, 656, 784, 912


def host_consts():
    c = np.zeros((128, C_NCOL), np.float32)
    c[:, C_ID:C_ID + 128] = np.eye(128, dtype=np.float32)
    c[:, C_IOTA:C_IOTA + 128] = np.arange(128, dtype=np.float32)[None, :]
    c[:, C_EPS] = EPS
    c[:, C_EPS + 1] = -0.5
    c[:, C_EPS + 2] = np.e
    c[8:16, C_DSELB] = 1.0
    c[:, C_ONES:C_ONES + 128] = 1.0
    p = np.arange(128)[:, None]
    f = np.arange(64)[None, :]
    NEG = -1e4
    c[:, C_NMLS:C_NMLS + 64] = np.where(p > f, 0.0, NEG)
    c[:, C_NMUS:C_NMUS + 64] = np.where(f > p, 0.0, NEG)
    c[:, C_NMLI:C_NMLI + 64] = np.where(p >= f, 0.0, NEG)
    c[:, C_NMUI:C_NMUI + 64] = np.where(f >= p, 0.0, NEG)
    f2 = np.arange(128)[None, :]
    c[:, C_NMP:C_NMP + 128] = np.where(p >= f2, 0.0, NEG)
    c[:, C_NMN:C_NMN + 128] = np.where(p <= f2, 0.0, NEG)
    return c


class Ctx:
    pass


def load_consts(k, g):
    g.cst = k.sb([128, C_NCOL], F32, "cst_sb")
    k.dma("sp", g.cst[:], g.d_cst, [g.cst[:]], [])
    g.ident = g.cst[:, C_ID:C_ID + 128]
    g.iota = g.cst[:, C_IOTA:C_IOTA + 128]
    g.epsT = g.cst[:, C_EPS:C_EPS + 1]
    g.ones = g.cst[:, C_ONES:C_ONES + 128]
    g.psb = [k.nc.alloc_psum_tensor("psb%d" % i, [128, 512], F32) for i in range(8)]


def bcast_row_load(k, dst, row_ap, n):
    P = dst.shape[0]
    src = bass.AP(row_ap.tensor, row_ap.offset, [[0, P], [1, n]])
    k.dma("sp", dst, src, [dst], [])


def peer_prepass_early(k, g, layers):
    nc = k.nc
    for li in layers:
        sem = nc.alloc_semaphore("s_pp%d" % li)
        k.dsem.append(sem)
        k.dval.append(0)
        si = len(k.dsem) - 1
        n = 0
        for src, dst in ((g.d_ut[li], g.d_utb[li]), (g.d_v[li], g.d_vb[li])):
            sv = src.rearrange("c p f -> (c p) f").rearrange("(a b) f -> a (b f)", b=8)
            dv = dst.rearrange("c p f -> (c p) f").rearrange("(a b) f -> a (b f)", b=8)
            for r0 in range(0, 2048, 128):
                nc.gpsimd.dma_start(out=dv[r0:r0 + 128, :], in_=sv[r0:r0 + 128, :]).then_inc(sem, 16)
                n += 1
        for blk in range(4):
            nc.gpsimd.dma_start(out=g.d_wqb[li][blk], in_=g.d_wq[li][:, blk * 512:(blk + 1) * 512].rearrange("(k p) f -> p k f", p=128)
                                ).then_inc(sem, 16)
            n += 1
        k.dval[si] = 16 * n
        k.st[("wb", li)] = [(si, 16 * n), {}]
        k.pp_tok = getattr(k, "pp_tok", {})
        k.pp_tok[li] = (si, 16 * n)


def peer_prepass(k, g, li):
    m = k.mark()
    CB = 8
    bufs = [k.sb([128, CB, 1024], BF16, "ppb%d" % i) for i in range(3)]
    n = 0
    for src, dst in ((g.d_ut[li], g.d_utb), (g.d_v[li], g.d_vb)):
        for c0 in range(0, 128, CB):
            b = bufs[n % 3]
            n += 1
            k.dma("pool", b[:], src[c0:c0 + CB].rearrange("c p f -> p c f"), [b[:]], [])
            for c1 in range(0, CB, 2):
                k.dma("pool", dst[(c0 + c1) // 2], b[:, c1:c1 + 2, :], [("x", ("wb", li))], [b[:]])
    for blk in range(4):
        b = bufs[n % 3]
        n += 1
        bv = b[:].rearrange("p c (a f) -> p (c a) f", a=2)[:, 0:8, :]
        for kk in range(8):
            k.dma("pool", bv[:, kk, :], g.d_wq[li, kk * 128:(kk + 1) * 128, blk * 512:(blk + 1) * 512], [b[:]], [])
        k.dma("pool", g.d_wqb[blk], bv, [("x", ("wb", li))], [b[:]])
    k.release(m)


def peer_layer(k, g, li, ntiles, final):
    nc = k.nc
    m0 = k.mark()
    TG = 2
    NTOKG = TG * 128
    vec, act, pe, pool = nc.vector, nc.scalar, nc.tensor, nc.gpsimd
    wqb = [k.sb([128, 8, 512], BF16, "wqb%d" % i) for i in range(2)]
    wk = k.sb([128, 16, 128], F32, "wk")
    skn = wk
    k.dma("sp", skn[:], g.d_sk[li].rearrange("h p k j -> k (h p) j"), [skn[:]], [])
    skt = k.sb([128, 16, 128], F32, "skt")
    for hp in range(16):
        ps = g.psb[hp % 2]
        k.op("pe", lambda: pe.transpose(ps[:, 0:128], skn[:, hp, :], g.ident), [ps[:]], [skn[:], g.cst[:]])
        k.op("act", lambda: act.copy(out=skt[:, hp, :], in_=ps[:, 0:128]), [skt[:]], [ps[:]])
    GS = k.sb([128, 1024], F32, "modgs")
    SH = k.sb([128, 1024], F32, "modsh")
    GT = k.sb([128, 1024], F32, "modgt")
    cur_a, cur_e = [-1], [-1]

    def set_mods_a(r):
        if cur_a[0] != r:
            cur_a[0] = r
            bcast_row_load(k, GS[:], g.d_mod[li, r, 3], 1024)
            bcast_row_load(k, SH[:], g.d_mod[li, r, 4], 1024)

    def set_mods_e(r):
        if cur_e[0] != r:
            cur_e[0] = r
            bcast_row_load(k, GT[:], g.d_mod[li, r, 5], 1024)
    if final:
        fg = k.sb([128, 1024], F32, "fing")
        bcast_row_load(k, fg[:], g.d_fg, 1024)
    gbuf = k.sb([128, NTOKG, 128], BF16, "gbuf")
    h2T = [k.sb([128, 8, NTOKG], BF16, "h2T%d" % i) for i in range(2)]
    xt = [[k.sb([128, 1024], F32, "xt%d_%d" % (p, i)) for i in range(TG)] for p in range(2)]
    selT = [[k.sb([128, 3, 128], F32, "selT%d_%d" % (p, i)) for i in range(TG)] for p in range(2)]
    hh = k.sb([128, 1024], F32, "hh")
    ss = k.sb([128, 4], F32, "ss")
    ss2 = k.sb([128, 4], F32, "ss2")
    qT = k.sb([128, 2, 256], F32, "qT")
    top2 = [k.sb([128, 16, 16], F32, "top%d" % i) for i in range(TG)]
    idx2 = [k.sb([128, 16, 16], U32, "idx%d" % i) for i in range(TG)]
    idxf = k.sb([128, 16, 16], F32, "idxf")
    cand = k.sb([128, 8, 256], F32, "cand")
    wk2 = wk[:].rearrange("p (a b) k -> p a (b k)", b=2)
    ctop = k.sb([128, 8, 16], F32, "ctop")
    pos = k.sb([128, 8, 16], U32, "pos")
    posi = k.sb([128, 2, 128], U32, "posi")
    posf = k.sb([128, 2, 128], F32, "posf")
    ee = k.sb([128, 8, 16], F32, "ee")
    zz = k.sb([128, 16], F32, "zz")
    eq = wk[:].rearrange("p a (b i) -> p (a b) i", i=16)
    sel = k.sb([128, 3, 128], F32, "sel")
    TB = 8
    ohA = [k.sb([128, TB, 128], BF16, "ohA%d" % i) for i in range(2)]
    ohB = [k.sb([128, TB, 128], BF16, "ohB%d" % i) for i in range(2)]
    iotab = k.sb([128, 128], BF16, "iotab")
    k.op("dve", lambda: vec.tensor_copy(out=iotab[:], in_=g.iota), [iotab[:]], [g.cst[:]])
    CB = 2
    NWB = 3
    utb = [k.sb([128, CB, 1024], BF16, "utb%d" % i) for i in range(NWB)]
    vbb = [k.sb([128, CB, 1024], BF16, "vbb%d" % i) for i in range(NWB)]
    actS = [k.sb([128, NTOKG], F32, "actS%d" % i) for i in range(2)]
    ga = [k.sb([128, NTOKG], BF16, "ga%d" % i) for i in range(2)]
    osb = hh

    psO = g.psb[0:4]
    psA = g.psb[4:6]
    psX = g.psb[6]
    psS = g.psb[7]

    ngroups = (ntiles + TG - 1) // TG

    def group_tiles(gi):
        return list(range(gi * TG, min(ntiles, (gi + 1) * TG)))

    def A1(gi):
        par = gi % 2
        tiles = group_tiles(gi)
        ntk = len(tiles) * 128
        set_mods_a(0 if tiles[0] < L // 128 else 1)
        hT_ = h2T[par]
        for ti, t in enumerate(tiles):
            x = xt[par][ti]
            k.dma("sp", x[:], g.d_xs[t * 128:(t + 1) * 128, :], [x[:]], [("x", ("xs", t))])
            norm_mod(k, g, x, hh, ss, GS, SH)
            yield
            for hf in range(2):
                for kk in range(4):
                    k.op("pe", lambda: pe.transpose(psX[:, kk * 128:(kk + 1) * 128], hh[:, (hf * 4 + kk) * 128:(hf * 4 + kk + 1) * 128],
                                                    g.ident), [psX[:]], [hh[:], g.cst[:]], inc=(kk == 3))
                k.op("act", lambda: act.copy(out=hT_[:, hf * 4:hf * 4 + 4, ti * 128:(ti + 1) * 128],
                                             in_=psX[:].rearrange("p (a b) -> p a b", a=4)), [hT_[:]], [psX[:]])
                yield
        k.dma("sp", wqb[0][:], g.d_wqb[li][0], [wqb[0][:]], [("x", ("wb", li))])
        for blk in range(4):
            wq = wqb[blk % 2]
            if blk + 1 < 4:
                k.dma("sp", wqb[(blk + 1) % 2][:], g.d_wqb[li][blk + 1], [wqb[(blk + 1) % 2][:]], [("x", ("wb", li))])
            for half in range(2):
                for h2 in range(2):
                    h4 = half * 2 + h2
                    for kk in range(8):
                        k.op("pe", lambda: pe.matmul(psX[:, h2 * 256:h2 * 256 + ntk], lhsT=wq[:, kk, h4 * 128:(h4 + 1) * 128],
                                                     rhs=hT_[:, kk, 0:ntk], start=(kk == 0), stop=(kk == 7)),
                             [psX[:]], [wq[:], hT_[:]], inc=(kk == 7 and h2 == 1))
                    yield
                k.op("act", lambda: act.copy(out=qT[:], in_=psX[:].rearrange("p (a b) -> p a b", a=2)), [qT[:]], [psX[:]])
                for ti in range(len(tiles)):
                    for h2 in range(2):
                        k.op("pe", lambda: pe.matmul(psS[:, (ti * 2 + h2) * 128:(ti * 2 + h2 + 1) * 128],
                                                     lhsT=qT[:, h2, ti * 128:(ti + 1) * 128], rhs=skt[:, blk * 4 + half * 2 + h2, :],
                                                     start=True, stop=True), [psS[:]], [qT[:], skt[:]],
                             inc=(h2 == 1 and ti == len(tiles) - 1))
                yield
                for ti in range(len(tiles)):
                    tp, ix = top2[ti], idx2[ti]
                    hps = [(blk * 4 + half * 2 + h2, (ti * 2 + h2)) for h2 in range(2)]

                    def S(c_):
                        return psS[:, c_ * 128:(c_ + 1) * 128]

                    for hp, c_ in hps:
                        k.op("dve", lambda: vec.max(out=tp[:, hp, 0:8], in_=S(c_)), [("x", ("topa", ti))], [psS[:]])
                    for hp, c_ in hps:
                        k.op("dve", lambda: vec.match_replace(out=wk[:, hp, :], in_to_replace=tp[:, hp, 0:8], in_values=S(c_),
                                                              imm_value=-1e30), [wk[:]], [("x", ("topa", ti)), psS[:]])
                    yield
                    for hp, c_ in hps:
                        k.op("dve", lambda: vec.max(out=tp[:, hp, 8:16], in_=wk[:, hp, :]), [("x", ("topb", ti))], [wk[:]])
                    for hp, c_ in hps:
                        k.op("dve", lambda: vec.max_index(out=ix[:, hp, 0:8], in_max=tp[:, hp, 0:8], in_values=S(c_)),
                             [("x", ("idxa", ti))], [("x", ("topa", ti)), psS[:]])
                    yield
                    for hp, c_ in hps:
                        k.op("dve", lambda: vec.max_index(out=ix[:, hp, 8:16], in_max=tp[:, hp, 8:16], in_values=S(c_)),
                             [("x", ("idxb", ti))], [("x", ("topb", ti)), psS[:]])
                    yield
        for ti, t in enumerate(tiles):
            top, idx = top2[ti], idx2[ti]
            k.op("dve", lambda: vec.tensor_copy(out=idxf[:], in_=idx[:]), [idxf[:]], [("x", ("idxa", ti)), ("x", ("idxb", ti))])
            t1 = V(top[:, 0, :], (32, 8), (1, 16), (0, 16))
            t2 = V(top[:, 1, :], (32, 8), (0, 16), (1, 16))
            k.op("dve", lambda: vec.tensor_tensor(out=cand[:].rearrange("p h (i j) -> p h i j", i=16), in0=t1, in1=t2,
                                                  op=ALU.add), [cand[:]], [("x", ("topa", ti)), ("x", ("topb", ti))])
            yield
            for h in range(8):
                k.op("dve", lambda: vec.max(out=ctop[:, h, 0:8], in_=cand[:, h, :]), [("x", "ctopa")], [cand[:]])
            yield
            for h in range(8):
                k.op("dve", lambda: vec.match_replace(out=wk2[:, h, :], in_to_replace=ctop[:, h, 0:8], in_values=cand[:, h, :],
                                                      imm_value=-1e30), [wk[:]], [("x", "ctopa"), cand[:]])
            yield
            for h in range(8):
                k.op("dve", lambda: vec.max(out=ctop[:, h, 8:16], in_=wk2[:, h, :]), [("x", "ctopb")], [wk[:]])
            yield
            for h in range(8):
                k.op("dve", lambda: vec.max_index(out=pos[:, h, 0:8], in_max=ctop[:, h, 0:8], in_values=cand[:, h, :]),
                     [("x", "posa")], [("x", "ctopa"), cand[:]])
            yield
            for h in range(8):
                k.op("dve", lambda: vec.max_index(out=pos[:, h, 8:16], in_max=ctop[:, h, 8:16], in_values=cand[:, h, :]),
                     [("x", "posb")], [("x", "ctopb"), cand[:]])
            yield
            k.op("dve", lambda: vec.tensor_tensor(out=ee[:], in0=ctop[:], in1=V(ctop[:, 0, 0:1], (16, 8), (0, 16)),
                                                  op=ALU.subtract), [ee[:]], [("x", "ctopa"), ("x", "ctopb")])
            k.op("act", lambda: act.activation(out=ee[:], in_=ee[:], func=AF.Exp), [ee[:]], [ee[:]])
            k.op("dve", lambda: vec.tensor_reduce(out=zz[:, 0:8], in_=ee[:], axis=AX.X, op=ALU.add), [("x", "zz0")], [ee[:]])
            k.op("dve", lambda: vec.reciprocal(out=zz[:, 8:16], in_=zz[:, 0:8]), [("x", "zz1")], [("x", "zz0")])
            k.op("dve", lambda: vec.tensor_tensor(out=sel[:, 2, :].rearrange("p (h r) -> p h r", h=8), in0=ee[:],
                                                  in1=V(zz[:, 8:9], (1, 8), (0, 16)), op=ALU.mult),
                 [("x", "sel2")], [ee[:], ("x", "zz1")])
            yield
            posv = pos[:].rearrange("p h r -> p (h r)")
            k.op("dve", lambda: vec.tensor_single_scalar(out=posi[:, 0, :], in_=posv, scalar=4, op=ALU.logical_shift_right),
                 [("x", "posi0")], [("x", "posa"), ("x", "posb")])
            k.op("dve", lambda: vec.tensor_single_scalar(out=posi[:, 1, :], in_=posv, scalar=15, op=ALU.bitwise_and),
                 [("x", "posi1")], [("x", "posa"), ("x", "posb")])
            k.op("dve", lambda: vec.tensor_copy(out=posf[:], in_=posi[:]), [posf[:]], [("x", "posi0"), ("x", "posi1")])
            yield
            for p_ in range(2):
                k.op("dve", lambda: vec.tensor_tensor(out=eq, in0=V(posf[:, p_, :], (1, 128), (0, 16)),
                                                      in1=V(g.iota[:, 0:16], (0, 128), (1, 16)), op=ALU.is_equal),
                     [wk[:]], [posf[:], g.cst[:]])
                yield
                k.op("pool", lambda: pool.tensor_tensor(out=eq.rearrange("p (h r) i -> p h r i", h=8),
                                                        in0=eq.rearrange("p (h r) i -> p h r i", h=8),
                                                        in1=V(idxf[:, p_, :], (32, 8), (0, 16), (1, 16)), op=ALU.mult),
                     [wk[:]], [wk[:], idxf[:]])
                k.op("dve", lambda: vec.tensor_reduce(out=sel[:, p_, :], in_=eq, axis=AX.X, op=ALU.add),
                     [("x", ("sel", p_))], [wk[:]])
                yield
            for q in range(3):
                k.op("pe", lambda: pe.transpose(psX[:, q * 128:(q + 1) * 128], sel[:, q, :], g.ident), [psX[:]],
                     [("x", ("sel", 0)), ("x", ("sel", 1)), ("x", "sel2"), g.cst[:]], inc=(q == 2))
            sT = selT[par][ti]
            k.op("act", lambda: act.copy(out=sT[:], in_=psX[:, 0:384].rearrange("p (a b) -> p a b", a=3)), [sT[:]], [psX[:]])
            yield

    def A2(gi):
        par = gi % 2
        tiles = group_tiles(gi)
        nq = 0
        for ti, t in enumerate(tiles):
            sT = selT[par][ti]
            for b in range(128 // TB):
                A = ohA[b % 2]
                B = ohB[b % 2]
                t0 = b * TB
                for tt in range(TB):
                    k.op("dve", lambda: vec.tensor_scalar(out=A[:, tt, :], in0=iotab[:], scalar1=sT[:, 0, t0 + tt:t0 + tt + 1],
                                                          scalar2=sT[:, 2, t0 + tt:t0 + tt + 1], op0=ALU.is_equal, op1=ALU.mult),
                         [A[:]], [sT[:], iotab[:]])
                    if False:
                        k.op("pool", lambda: pool.tensor_scalar(out=B[:, tt, :], in0=iotab[:], scalar1=sT[:, 1, t0 + tt:t0 + tt + 1],
                                                                scalar2=None, op0=ALU.is_equal), [("x", ("ohB", b % 2, "p"))],
                             [sT[:], iotab[:], ("x", ("ohBr", b % 2))])
                    else:
                        k.op("dve", lambda: vec.tensor_scalar(out=B[:, tt, :], in0=iotab[:], scalar1=sT[:, 1, t0 + tt:t0 + tt + 1],
                                                              scalar2=None, op0=ALU.is_equal), [("x", ("ohB", b % 2, "d"))],
                             [sT[:], iotab[:], ("x", ("ohBr", b % 2))])
                for q in range(TB // 4):
                    ps = psA[nq % 2]
                    nq += 1
                    for u in range(4):
                        tt = q * 4 + u
                        k.op("pe", lambda: pe.matmul(ps[:, u * 128:(u + 1) * 128], lhsT=B[:, tt, :], rhs=A[:, tt, :], start=True,
                                                     stop=True), [ps[:], ("x", ("ohBr", b % 2))],
                             [A[:], ("x", ("ohB", b % 2, "p")), ("x", ("ohB", b % 2, "d"))], inc=(u == 3))
                    tg0 = ti * 128 + t0 + q * 4
                    k.op("act", lambda: act.copy(out=gbuf[:, tg0:tg0 + 4, :], in_=ps[:].rearrange("p (a b) -> p a b", a=4)),
                         [("x", "gbufW")], [ps[:], ("x", "gbufR"), ("x", "gbufR2")])

    def B(gi, nxt):
        par = gi % 2
        tiles = group_tiles(gi)
        ntg = len(tiles)
        ntok = ntg * 128
        hT_ = h2T[par]

        def load_w(cb):
            b = (cb // CB) % NWB
            k.dma("sp", utb[b][:], g.d_utb[li][cb:cb + CB].rearrange("c p f -> p c f"), [utb[b][:]], [("x", ("wb", li))])
            k.dma("sp", vbb[b][:], g.d_vb[li][cb:cb + CB].rearrange("c p f -> p c f"), [vbb[b][:]], [("x", ("wb", li))])

        def umm(c):
            b = (c // CB) % NWB
            ps = psA[c % 2]
            for kk in range(8):
                k.op("pe", lambda: pe.matmul(ps[:, 0:ntok], lhsT=utb[b][:, c % CB, kk * 128:(kk + 1) * 128],
                                             rhs=hT_[:, kk, 0:ntok], start=(kk == 0), stop=(kk == 7)),
                     [ps[:]], [utb[b][:], hT_[:]], inc=(kk == 7))

        load_w(0)
        load_w(CB)
        umm(0)
        for c in range(128):
            if c % CB == 0 and c + 2 * CB < 128:
                load_w(c + 2 * CB)
            if c + 1 < 128:
                umm(c + 1)
            b = (c // CB) % NWB
            ps = psA[c % 2]
            a_ = actS[c % 2]
            g_ = ga[c % 2]
            k.op("act", lambda: act.activation(out=a_[:, 0:ntok], in_=ps[:, 0:ntok], func=AF.Gelu), [a_[:]], [ps[:]])
            if c % 2 == 0:
                k.op("dve", lambda: vec.tensor_tensor(out=g_[:, 0:ntok], in0=a_[:, 0:ntok], in1=V(gbuf[:, 0, c:c + 1], (128, ntok)),
                                                      op=ALU.mult), [g_[:], ("x", "gbufR")], [a_[:], ("x", "gbufW")])
            else:
                k.op("pool", lambda: pool.tensor_tensor(out=g_[:, 0:ntok], in0=a_[:, 0:ntok], in1=V(gbuf[:, 0, c:c + 1], (128, ntok)),
                                                        op=ALU.mult), [g_[:], ("x", "gbufR2")], [a_[:], ("x", "gbufW")])
            for ti in range(ntg):
                for hf in range(2):
                    k.op("pe", lambda: pe.matmul(psO[ti * 2 + hf][:], lhsT=g_[:, ti * 128:(ti + 1) * 128],
                                                 rhs=vbb[b][:, c % CB, hf * 512:(hf + 1) * 512], start=(c == 0), stop=(c == 127)),
                         [psO[ti * 2 + hf][:]], [g_[:], vbb[b][:]], inc=(ti == ntg - 1 and hf == 1))
            if nxt is not None and c >= 2:
                for _ in range(STEPS):
                    next(nxt, None)
        if nxt is not None:
            for _ in nxt:
                pass
        set_mods_e(0 if tiles[0] < L // 128 else 1)
        for ti, t in enumerate(tiles):
            x = xt[par][ti]
            for hf in range(2):
                sl = slice(hf * 512, (hf + 1) * 512)
                k.op("dve", lambda: vec.tensor_tensor(out=osb[:, sl], in0=psO[ti * 2 + hf][:], in1=GT[:, sl], op=ALU.mult),
                     [osb[:]], [psO[ti * 2 + hf][:], GT[:]])
            k.op("pool", lambda: pool.tensor_tensor(out=osb[:], in0=osb[:], in1=x[:], op=ALU.add), [osb[:]], [osb[:], x[:]])
            if not final:
                k.dma("pool", g.d_xs[t * 128:(t + 1) * 128, :], osb[:], [("x", ("xs", t))], [osb[:]])
            else:
                norm_mod_final(k, g, osb, x, ss2, fg)
                k.dma("pool", g.d_out[t * 128:(t + 1) * 128, :], x[:], [("x", ("out", t))], [x[:]])

    STEPS = 1
    n_a1 = 6 + 24 + 32 * TG * 3 // 3 + 18 * TG
    STEPS = 1
    for _ in A1(0):
        pass
    for gi in range(ngroups):
        A2(gi)
        nxt = A1(gi + 1) if gi + 1 < ngroups else None
        B(gi, nxt)
    k.release(m0)


def norm_mod_final(k, g, x, out, ss, fg):
    nc = k.nc
    vec, act = nc.vector, nc.scalar
    k.op("dve", lambda: vec.scalar_tensor_tensor(out=out[:], in0=x[:], scalar=1.0, in1=x[:], op0=ALU.mult, op1=ALU.mult,
                                                accum_out=ss[:, 0:1]), [out[:], ss[:]], [x[:]])
    k.op("act", lambda: act.activation(out=ss[:, 1:2], in_=ss[:, 0:1], func=AF.Sqrt, scale=1.0 / D, bias=g.epsT), [("x", "fss1")],
         [ss[:]])
    k.op("dve", lambda: vec.reciprocal(out=ss[:, 2:3], in_=ss[:, 1:2]), [("x", "fss2")], [("x", "fss1")])
    k.op("dve", lambda: vec.scalar_tensor_tensor(out=out[:], in0=x[:], scalar=ss[:, 2:3], in1=fg[:], op0=ALU.mult, op1=ALU.mult),
         [out[:]], [x[:], ("x", "fss2"), fg[:]])


def EPS_AP(g):
    return g.epsT[:, 0:1]


def build(phases=("all",), dev=None):
    dev = dev or {}
    k = K()
    nc = k.nc
    g = Ctx()

    k.ext_inputs = []

    def din(name, shape, dt=F32):
        k.ext_inputs.append(name)
        return nc.dram_tensor(name, list(shape), dt, kind="ExternalInput").ap()

    def dint(name, shape, dt=F32):
        return nc.dram_tensor(name, list(shape), dt, kind="Internal").ap()

    g.d_x = din("x", [L, D])
    g.d_ctx = din("ctx", [CTX, D])
    g.d_cst = din("cst", [128, C_NCOL])
    g.d_wq = din("peer_wq", [2, D, 2048])
    g.d_sk = din("peer_sk", [2, 8, 2, 128, 128])
    g.d_ut = din("peer_ut", [2, 128, 128, 1024])
    g.d_v = din("peer_v", [2, 128, 128, 1024])
    g.d_fg = din("final_g", [D])
    g.d_cT = din("cT", [128, 8, 2])
    g.d_adaw = din("ada_w", [2, D, 6144])
    g.d_adab = din("ada_b", [2, 6144])
    g.d_n1g = din("norm1_g", [2, D])
    g.d_n2g = din("norm2_g", [2, D])
    g.d_cst2 = din("cst2", [16, NTOK])
    g.d_gwin = din("gdn_w_in", [D, 4128])
    g.d_gconv = din("gdn_conv", [24, 128, 5])
    g.d_galog = din("gdn_a_log", [2, 8])
    g.d_gdtb = din("gdn_dt_bias", [2, 8])
    g.d_gng = din("gdn_norm_g", [128])
    g.d_gwout = din("gdn_w_out", [D, D])
    g.d_swin = din("swa_w_in", [D, 1536])
    g.d_swout = din("swa_w_out", [D, D])
    g.d_ssink = din("swa_sinks", [16])
    g.d_rope = din("rope", [2, L, 64])
    g.d_hT = dint("hT", [NT, 128, 8, 128], BF16)
    g.d_qkv = dint("qkv", [24, 128, NTOK], BF16)
    g.d_rows = dint("rows", [2, 16, NTOK])
    g.d_cd = dint("cd", [16, NCH])
    g.d_o = dint("gdn_o", [2, NTOK, D])
    if dev.get("mod_ext"):
        g.d_mod = din("mod", [2, 2, 6, D])
    else:
        g.d_mod = dint("mod", [2, 2, 6, D])
    g.d_xs = dint("xs", [NTOK, D])
    g.d_utb = dint("utb", [2, 128, 128, 1024], BF16)
    g.d_vb = dint("vb", [2, 128, 128, 1024], BF16)
    g.d_wqb = dint("wqb", [2, 4, 128, 8, 512], BF16)
    g.d_out = nc.dram_tensor("out", [L, D], F32, kind="ExternalOutput").ap()
    if dev.get("xs_out"):
        g.d_xso = nc.dram_tensor("xs_out", [NTOK, D], F32, kind="ExternalOutput").ap()

    load_consts(k, g)
    pp_layers = [ph[1] for ph in phases if ph[0] == "peer"]
    if not (phases and phases[0][0] == "adaln"):
        peer_prepass_early(k, g, pp_layers)
    for r0 in range(0, L, 512):
        k.dma("sp", g.d_xs[r0:r0 + 512, :], g.d_x[r0:r0 + 512, :], [("x", ("xs", r0 // 128 + i)) for i in range(4)], [])
    k.dma("sp", g.d_xs[L:NTOK, :], g.d_ctx, [("x", ("xs", 32)), ("x", ("xs", 33))], [])

    for ph in phases:
        if ph[0] == "adaln":
            adaln_phase(k, g)
            peer_prepass_early(k, g, pp_layers)
        elif ph[0] == "gdn":
            m_ = k.mark()
            build_hT(k, g, 0, NT)
            gdn_rows(k, g)
            gdn_qkv(k, g)
            gdn_scan(k, g)
            k.release(m_)
            gdn_out(k, g, 0, NT)
        elif ph[0] == "swa":
            build_hT(k, g, 1, NT)
            swa_phase(k, g, 1)
        elif ph[0] == "peer":
            _, li, ntiles, final = ph
            k.st[("wb", li)] = [k.pp_tok[li], {}]
            peer_layer(k, g, li, ntiles, final)
    if dev.get("mod_out"):
        k.barrier()
        d_mo = nc.dram_tensor("mod_out", [2, 2, 6, D], F32, kind="ExternalOutput").ap()
        k.dma("sp", d_mo.rearrange("a b c d -> (a b c) d"), g.d_mod.rearrange("a b c d -> (a b c) d"), [("x", "modo")], [])
    if dev.get("xs_out"):
        k.barrier()
        for r0 in range(0, NTOK, 256):
            k.dma("sp", g.d_xso[r0:r0 + 256, :], g.d_xs[r0:r0 + 256, :], [("x", ("xso", r0))], [])
    k.finish()
    return k


def adaln_phase(k, g):
    nc = k.nc
    m = k.mark()
    cT = k.sb([128, 8, 2], F32, "cT")
    k.dma("sp", cT[:], g.d_cT, [cT[:]], [])
    sc = k.sb([128, 8, 2], F32, "scT")
    k.op("act", lambda: nc.scalar.activation(out=sc[:], in_=cT[:], func=AF.Silu), [sc[:]], [cT[:]])
    bb = k.sb([2, 6144], F32, "adab")
    msb = k.sb([2, 6144], F32, "adam")
    n1 = k.sb([2, 1024], F32, "n1g")
    n2 = k.sb([2, 1024], F32, "n2g")
    o1 = k.sb([2, 1024], F32, "ado1")
    o2 = k.sb([2, 1024], F32, "ado2")
    wb = [k.sb([128, 8, 512], F32, "adaw%d" % i) for i in range(2)]
    for li in range(2):
        bcast_row_load(k, bb[:], g.d_adab[li], 6144)
        bcast_row_load(k, n1[:], g.d_n1g[li], 1024)
        bcast_row_load(k, n2[:], g.d_n2g[li], 1024)
        for n in range(12):
            w = wb[n % 2]
            k.dma("pool", w[:], g.d_adaw[li][:, n * 512:(n + 1) * 512].rearrange("(k p) f -> p k f", p=128), [w[:]], [])
            ps = g.psb[n % 2]
            for kk in range(8):
                k.op("pe", lambda: nc.tensor.matmul(ps[0:2, :], lhsT=sc[:, kk, :], rhs=w[:, kk, :], start=(kk == 0), stop=(kk == 7)),
                     [ps[:]], [sc[:], w[:]], inc=(kk == 7))
            k.op("dve", lambda: nc.vector.tensor_tensor(out=msb[:, n * 512:(n + 1) * 512], in0=ps[0:2, :],
                                                        in1=bb[:, n * 512:(n + 1) * 512], op=ALU.add), [msb[:]], [ps[:], bb[:]])
        k.op("dve", lambda: nc.vector.scalar_tensor_tensor(out=o1[:], in0=msb[:, 1024:2048], scalar=1.0, in1=n1[:], op0=ALU.add,
                                                          op1=ALU.mult), [o1[:]], [msb[:], n1[:]])
        k.op("dve", lambda: nc.vector.scalar_tensor_tensor(out=o2[:], in0=msb[:, 4096:5120], scalar=1.0, in1=n2[:], op0=ALU.add,
                                                          op1=ALU.mult), [o2[:]], [msb[:], n2[:]])
        srcs = [o1[:], msb[:, 0:1024], msb[:, 2048:3072], o2[:], msb[:, 3072:4096], msb[:, 5120:6144]]
        for j in range(6):
            k.dma("sp", g.d_mod[li, :, j, :], srcs[j], [("x", ("mod", li))], [o1[:], o2[:], msb[:]])
    k.release(m)


def norm_mod(k, g, x, hh, ss, GS, SH):
    nc = k.nc
    vec, act, pool = nc.vector, nc.scalar, nc.gpsimd
    k.op("dve", lambda: vec.scalar_tensor_tensor(out=hh[:], in0=x[:], scalar=1.0, in1=x[:], op0=ALU.mult, op1=ALU.mult,
                                                accum_out=ss[:, 0:1]), [hh[:], ss[:]], [x[:]])
    k.op("dve", lambda: vec.tensor_scalar(out=ss[:, 1:2], in0=ss[:, 0:1], scalar1=1.0 / D, scalar2=EPS, op0=ALU.mult, op1=ALU.add),
         [("x", "ss1")], [ss[:]])
    k.op("pool", lambda: pool.tensor_tensor(out=ss[:, 2:3], in0=ss[:, 1:2], in1=g.cst[:, C_EPS + 1:C_EPS + 2], op=ALU.pow),
         [("x", "ss2")], [("x", "ss1"), g.cst[:]])
    k.op("dve", lambda: vec.scalar_tensor_tensor(out=hh[:], in0=x[:], scalar=ss[:, 2:3], in1=GS[:], op0=ALU.mult, op1=ALU.mult),
         [hh[:]], [x[:], ("x", "ss2"), GS[:]])
    k.op("pool", lambda: pool.tensor_tensor(out=hh[:], in0=hh[:], in1=SH[:], op=ALU.add), [hh[:]], [hh[:], SH[:]])


def build_hT(k, g, li, ntiles):
    nc = k.nc
    m = k.mark()
    GS = k.sb([128, 1024], F32, "hgs")
    SH = k.sb([128, 1024], F32, "hsh")
    xt = [k.sb([128, 1024], F32, "hxt%d" % i) for i in range(2)]
    hh = k.sb([128, 1024], F32, "hhh")
    ss = k.sb([128, 4], F32, "hss")
    hTt = [k.sb([128, 8, 128], BF16, "hTt%d" % i) for i in range(3)]
    cur = -1
    pend = None
    for t in range(ntiles):
        r = 0 if t < L // 128 else 1
        if r != cur:
            cur = r
            bcast_row_load(k, GS[:], g.d_mod[li, r, 0], 1024)
            bcast_row_load(k, SH[:], g.d_mod[li, r, 1], 1024)
        x = xt[t % 2]
        k.dma("sp", x[:], g.d_xs[t * 128:(t + 1) * 128, :], [x[:]], [("x", ("xs", t))])
        norm_mod(k, g, x, hh, ss, GS, SH)
        o = hTt[t % 3]
        for kk in range(8):
            ps = g.psb[kk // 4]
            k.op("pe", lambda: nc.tensor.transpose(ps[:, (kk % 4) * 128:(kk % 4 + 1) * 128], hh[:, kk * 128:(kk + 1) * 128], g.ident),
                 [ps[:]], [hh[:], g.cst[:]], inc=(kk % 4 == 3))
            if kk % 4 == 3:
                h0 = kk // 4
                if h0 == 0:
                    k.op("act", lambda: nc.scalar.copy(out=o[:, 0:4, :], in_=ps[:].rearrange("p (a b) -> p a b", a=4)), [o[:]], [ps[:]])
                else:
                    k.op("dve", lambda: nc.vector.tensor_copy(out=o[:, 4:8, :], in_=ps[:].rearrange("p (a b) -> p a b", a=4)),
                         [o[:]], [ps[:]])
        if pend is not None:
            k.dma("pool", g.d_hT[pend[0]], pend[1][:], [("x", ("hT", pend[0]))], [pend[1][:]])
        pend = (t, o)
    k.dma("pool", g.d_hT[pend[0]], pend[1][:], [("x", ("hT", pend[0]))], [pend[1][:]])
    k.release(m)


NCH = NTOK // 64


def gdn_rows(k, g):
    nc = k.nc
    vec, act, pe = nc.vector, nc.scalar, nc.tensor
    g.TM = k.sb([64, NCH, 4, 16], F32, "gdnTM")
    m = k.mark()
    wab = k.sb([128, 8, 32], BF16, "wab")
    for kk in range(8):
        k.dma("pool", wab[:, kk, :], g.d_gwin[kk * 128:(kk + 1) * 128, 4096:4128], [wab[:]], [])
    alog = k.sb([16, 4], F32, "alog")
    k.dma("sp", alog[:, 0:1], g.d_galog.rearrange("d (h o) -> (d h) o", o=1), [alog[:]], [])
    k.dma("sp", alog[:, 1:2], g.d_gdtb.rearrange("d (h o) -> (d h) o", o=1), [alog[:]], [])
    k.op("act", lambda: act.activation(out=alog[:, 2:3], in_=alog[:, 0:1], func=AF.Exp), [("x", "alog2")], [alog[:]])
    k.op("dve", lambda: vec.tensor_scalar(out=alog[:, 3:4], in0=alog[:, 2:3], scalar1=-1.0, scalar2=None, op0=ALU.mult),
         [("x", "alog3")], [("x", "alog2")])
    rows = {nm: k.sb([16, NTOK], F32, "row_" + nm) for nm in ("g", "beta", "Gf", "Gb", "eG", "kdf")}
    cm = k.sb([16, NTOK], F32, "row_cm")
    k.dma("sp", cm[:], g.d_cst2, [cm[:]], [])
    hb = [k.sb([128, 4, 8, 128], BF16, "rhb%d" % i) for i in range(2)]
    for blk in range((NT + 3) // 4):
        t0 = blk * 4
        nt = min(4, NT - t0)
        h = hb[blk % 2]
        k.dma("sp", h[:, 0:nt], g.d_hT[t0:t0 + nt].rearrange("t p k n -> p t k n"), [h[:]], [("x", ("hT", t0 + i)) for i in range(nt)])
        psa, psb_ = g.psb[(blk % 2) * 2], g.psb[(blk % 2) * 2 + 1]
        for half, ps in ((0, psa), (1, psb_)):
            for t in range(nt):
                for kk in range(8):
                    k.op("pe", lambda: pe.matmul(ps[0:16, t * 128:(t + 1) * 128], lhsT=wab[:, kk, half * 16:(half + 1) * 16],
                                                 rhs=h[:, t, kk, :], start=(kk == 0), stop=(kk == 7)), [ps[:]], [wab[:], h[:]],
                         inc=(kk == 7 and t == nt - 1))
        sl = slice(t0 * 128, (t0 + nt) * 128)
        w = nt * 128
        k.op("act", lambda: act.activation(out=rows["g"][:, sl], in_=psa[0:16, 0:w], func=AF.Exp, bias=alog[:, 1:2]),
             [rows["g"][:]], [psa[:], alog[:]])
        k.op("act", lambda: act.activation(out=rows["g"][:, sl], in_=rows["g"][:, sl], func=AF.Ln, bias=1.0), [rows["g"][:]],
             [rows["g"][:]])
        k.op("dve", lambda: vec.tensor_scalar(out=rows["g"][:, sl], in0=rows["g"][:, sl], scalar1=alog[:, 3:4], scalar2=None,
                                              op0=ALU.mult), [rows["g"][:]], [rows["g"][:], ("x", "alog3")])
        k.op("act", lambda: act.activation(out=rows["beta"][:, sl], in_=psb_[0:16, 0:w], func=AF.Sigmoid), [rows["beta"][:]],
             [psb_[:]])
    G, Gf, Gb, B, eG, kdf = rows["g"], rows["Gf"], rows["Gb"], rows["beta"], rows["eG"], rows["kdf"]
    k.op("dve", lambda: vec.tensor_tensor_scan(out=Gf[:], data0=cm[:], data1=G[:], initial=0.0, op0=ALU.mult, op1=ALU.add),
         [Gf[:]], [cm[:], G[:]])
    tot = V(Gf[:, 63:64], (64, NCH), (0, 64))
    v3 = lambda t: t[:].rearrange("p (c j) -> p c j", j=64)
    k.op("dve", lambda: vec.tensor_tensor(out=v3(Gb), in0=tot, in1=v3(Gf), op=ALU.subtract), [Gb[:]], [Gf[:]])
    k.op("dve", lambda: vec.tensor_tensor(out=Gb[:], in0=Gb[:], in1=G[:], op=ALU.add), [Gb[:]], [Gb[:], G[:]])
    k.op("dve", lambda: vec.tensor_tensor(out=Gb[:], in0=Gb[:], in1=Gf[:], op=ALU.subtract), [Gb[:]], [Gb[:], Gf[:]])
    k.op("dve", lambda: vec.scalar_tensor_tensor(out=Gb[:], in0=Gb[:], scalar=g.cst[0:16, C_DSELB:C_DSELB + 1], in1=Gf[:],
                                                op0=ALU.mult, op1=ALU.add), [Gb[:]], [Gb[:], Gf[:], g.cst[:]])
    GD = Gb
    k.op("act", lambda: act.activation(out=eG[:], in_=GD[:], func=AF.Exp), [eG[:]], [GD[:]])
    k.op("dve", lambda: vec.tensor_tensor(out=v3(kdf), in0=tot, in1=v3(GD), op=ALU.subtract), [kdf[:]], [Gf[:], GD[:]])
    k.op("act", lambda: act.activation(out=kdf[:], in_=kdf[:], func=AF.Exp), [kdf[:]], [kdf[:]])
    cd = k.sb([16, NCH], F32, "row_cd")
    k.op("act", lambda: act.activation(out=cd[:], in_=V(Gf[:, 63:64], (64, NCH)), func=AF.Exp), [cd[:]], [Gf[:]])
    k.dma("sp", g.d_rows[0], GD[:], [("x", "rows")], [GD[:]])
    k.dma("sp", g.d_rows[1], B[:], [("x", "rows")], [B[:]])
    k.dma("sp", g.d_cd, cd[:], [("x", "rows")], [cd[:]])
    nB = rows["Gf"]
    k.op("dve", lambda: vec.tensor_scalar(out=nB[:], in0=B[:], scalar1=-1.0, scalar2=None, op0=ALU.mult), [nB[:]], [B[:], cd[:]])
    for c in range(NCH):
        ps = g.psb[4 + (c // 8) % 2]
        for q, src in enumerate((GD, nB, eG, kdf)):
            col = (c % 8) * 64 + q * 16
            k.op("pe", lambda: pe.transpose(ps[0:64, col:col + 16], src[:, c * 64:(c + 1) * 64], g.ident[0:16, 0:16]), [ps[:]],
                 [src[:], g.cst[:]], inc=(q == 3 and (c % 8 == 7 or c == NCH - 1)))
        if c % 8 == 7 or c == NCH - 1:
            c0 = (c // 8) * 8
            n = c - c0 + 1
            k.op("act", lambda: act.copy(out=g.TM[:, c0:c0 + n].rearrange("p c q d -> p (c q d)"), in_=ps[0:64, 0:n * 64]),
                 [g.TM[:]], [ps[:]])
    k.release(m)


def gdn_qkv(k, g):
    nc = k.nc
    vec, act, pe, pool = nc.vector, nc.scalar, nc.tensor, nc.gpsimd
    m = k.mark()
    PW = NTOK + 8
    Ps = [k.sb([128, PW], F32, "gP%d" % i) for i in range(2)]
    for P in Ps:
        k.op("pool", lambda: pool.memset(P[:], 0.0), [P[:]], [])
    R = [k.sb([128, NTOK], F32, "gR%d" % i) for i in range(2)]
    RB = [k.sb([128, NTOK], BF16, "gRB%d" % i) for i in range(2)]
    cw = k.sb([128, 24, 5], F32, "gcw")
    k.dma("sp", cw[:], g.d_gconv.rearrange("f p j -> p f j"), [cw[:]], [])
    wf = [k.sb([128, 8, 128], BF16, "gwf%d" % i) for i in range(2)]
    hb = [k.sb([128, 4, 8, 128], BF16, "ghb%d" % i) for i in range(2)]
    sq = [k.sb([128, 512], F32, "gsq%d" % i) for i in range(2)]
    rn = [k.sb([128, 512], F32, "grn%d" % i) for i in range(2)]
    nblk = (NT + 3) // 4
    def load_w(fc_):
        w_ = wf[fc_ % 2]
        for kk in range(8):
            k.dma("pool", w_[:, kk, :], g.d_gwin[kk * 128:(kk + 1) * 128, fc_ * 128:(fc_ + 1) * 128], [w_[:]], [])

    load_w(0)
    pending_store = None
    for fc in range(24):
        w = wf[fc % 2]
        P = Ps[fc % 2]
        for blk in range(nblk):
            t0 = blk * 4
            nt = min(4, NT - t0)
            h = hb[blk % 2]
            k.dma("sp", h[:, 0:nt], g.d_hT[t0:t0 + nt].rearrange("t p k n -> p t k n"), [h[:]],
                  [("x", ("hT", t0 + i)) for i in range(nt)])
            ps = g.psb[blk % 2]
            for t in range(nt):
                for kk in range(8):
                    k.op("pe", lambda: pe.matmul(ps[:, t * 128:(t + 1) * 128], lhsT=w[:, kk, :], rhs=h[:, t, kk, :], start=(kk == 0),
                                                 stop=(kk == 7)), [ps[:]], [w[:], h[:]], inc=(kk == 7 and t == nt - 1))
            off = t0 * 128 + (2 if t0 < 32 else 6)
            k.op("act", lambda: act.copy(out=P[:, off:off + nt * 128], in_=ps[:, 0:nt * 128]), [P[:]], [ps[:]])
        if fc + 1 < 24:
            load_w(fc + 1)
        if pending_store is not None:
            pf, prb = pending_store
            k.dma("pool", g.d_qkv[pf], prb[:], [("x", ("qkv", pf))], [prb[:]])
            pending_store = None
        r = R[fc % 2]
        for (o0, n, p0) in ((0, L, 0), (L, CTX, L + 4)):
            k.op("dve", lambda: vec.tensor_scalar(out=r[:, o0:o0 + n], in0=P[:, p0:p0 + n], scalar1=cw[:, fc, 0:1], scalar2=None,
                                                  op0=ALU.mult), [r[:]], [P[:], cw[:]])
            for j in range(1, 5):
                k.op("dve", lambda: vec.scalar_tensor_tensor(out=r[:, o0:o0 + n], in0=P[:, p0 + j:p0 + j + n], scalar=cw[:, fc, j:j + 1],
                                                            in1=r[:, o0:o0 + n], op0=ALU.mult, op1=ALU.add), [r[:]], [P[:], cw[:], r[:]])
        rb = RB[fc % 2]
        if fc >= 16:
            k.op("act", lambda: act.activation(out=rb[:], in_=r[:], func=AF.Silu), [rb[:]], [r[:]])
        else:
            k.op("act", lambda: act.activation(out=r[:], in_=r[:], func=AF.Silu), [r[:]], [r[:]])
        if fc < 16:
            scl = (128.0 ** -0.5) if fc < 8 else 1.0
            for b in range((NTOK + 511) // 512):
                c0 = b * 512
                n = min(512, NTOK - c0)
                s_, r_ = sq[b % 2], rn[b % 2]
                ps = g.psb[2 + b % 2]
                k.op("pool", lambda: pool.tensor_tensor(out=s_[:, 0:n], in0=r[:, c0:c0 + n], in1=r[:, c0:c0 + n], op=ALU.mult), [s_[:]],
                     [r[:]])
                k.op("pe", lambda: pe.matmul(ps[:, 0:n], lhsT=g.ones, rhs=s_[:, 0:n], start=True, stop=True), [ps[:]], [s_[:], g.cst[:]])
                k.op("act", lambda: act.activation(out=r_[:, 0:n], in_=ps[:, 0:n], func=AF.Sqrt, bias=g.epsT), [r_[:]], [ps[:], g.cst[:]])
                k.op("dve", lambda: vec.reciprocal(out=r_[:, 0:n], in_=r_[:, 0:n]), [r_[:]], [r_[:]])
                k.op("dve", lambda: vec.scalar_tensor_tensor(out=rb[:, c0:c0 + n], in0=r[:, c0:c0 + n], scalar=scl, in1=r_[:, 0:n],
                                                            op0=ALU.mult, op1=ALU.mult), [rb[:]], [r[:], r_[:]])
        pending_store = (fc, rb)
    pf, prb = pending_store
    k.dma("pool", g.d_qkv[pf], prb[:], [("x", ("qkv", pf))], [prb[:]])
    k.release(m)


def gdn_scan(k, g):
    nc = k.nc
    vec, act, pe, pool = nc.vector, nc.scalar, nc.tensor, nc.gpsimd
    m = k.mark()
    BC = 8
    BW = BC * 64
    id64 = g.cst[0:64, C_ID:C_ID + 64]
    TM = g.TM
    masks = {nm: g.cst[0:64, c0:c0 + 64] for nm, c0 in (("ls", C_NMLS), ("us", C_NMUS), ("li", C_NMLI), ("ui", C_NMUI))}
    idb = k.sb([128, 128], BF16, "gidb")
    k.op("dve", lambda: vec.tensor_copy(out=idb[:], in_=g.ident), [idb[:]], [g.cst[:]])

    class BS:
        pass

    sets = []
    for d in range(4):
        b_ = BS()
        n_ = "d%d" % d
        b_.qTb = k.sb([128, BW], BF16, "qTb" + n_)
        b_.kTb = k.sb([128, BW], BF16, "kTb" + n_)
        b_.vTb = k.sb([128, BW], BF16, "vTb" + n_)
        b_.grow = k.sb([64, BC, 64], F32, "grow" + n_)
        b_.brow = k.sb([64, BC, 64], F32, "brow" + n_)
        b_.cdb = k.sb([128, BC], F32, "cdb" + n_)
        b_.ktm = k.sb([64, BC, 128], BF16, "ktm" + n_)
        b_.vtm = k.sb([64, BC, 128], BF16, "vtm" + n_)
        b_.kdec = k.sb([64, BC, 128], BF16, "kdec" + n_)
        b_.dif = k.sb([64, BC, 64], F32, "dif" + n_)
        b_.E = [k.sb([64, BC, 64], F32, "gE%d%s" % (i, n_)) for i in range(3)]
        b_.Y = [k.sb([64, BC, 64], BF16, "gY%d%s" % (i, n_)) for i in range(2)]
        b_.YT = [k.sb([64, BC, 64], BF16, "gYT%d%s" % (i, n_)) for i in range(2)]
        b_.PT = [k.sb([64, BC, 64], BF16, "gPT%d%s" % (i, n_)) for i in range(2)]
        b_.AQ = k.sb([64, BC, 64], BF16, "gAQ" + n_)
        b_.ob = k.sb([64, BC, 128], F32, "gob" + n_)
        b_.S = k.sb([128, 128], F32, "gS" + n_)
        b_.Sb = k.sb([128, 128], BF16, "gSb" + n_)
        b_.t2a = k.sb([64, 128], F32, "gt2a" + n_)
        b_.t2b = k.sb([64, 128], BF16, "gt2b" + n_)
        b_.vnew = k.sb([64, 128], BF16, "gvnew" + n_)
        b_.t3 = k.sb([64, 128], F32, "gt3" + n_)
        b_.pb = [g.psb[2 * d], g.psb[2 * d + 1], g.psb[2 * d], g.psb[2 * d + 1]]
        sets.append(b_)

    def evac(i, out, in_):
        if True:
            k.op("act", lambda: act.copy(out=out, in_=in_), [out], [in_])
        else:
            k.op("dve", lambda: vec.tensor_copy(out=out, in_=in_), [out], [in_])

    def run(h, d, si):
        B_ = sets[si]
        qTb, kTb, vTb, grow, brow, cdb, ktm, vtm, kdec, dif = (B_.qTb, B_.kTb, B_.vTb, B_.grow, B_.brow, B_.cdb, B_.ktm, B_.vtm,
                                                               B_.kdec, B_.dif)
        E, Y, YT, PT, AQ, ob, S, Sb, t2a, t2b, vnew, t3, pb = (B_.E, B_.Y, B_.YT, B_.PT, B_.AQ, B_.ob, B_.S, B_.Sb, B_.t2a, B_.t2b,
                                                              B_.vnew, B_.t3, B_.pb)
        dh = d * 8 + h
        lat = [(c0, 8) for c0 in range(0, 64, 8)]
        batches = [(64, 4)] + (lat if d == 0 else lat[::-1])
        k.op("pool", lambda: pool.memset(S[:], 0.0), [S[:]], [])
        k.op("pool", lambda: pool.memset(Sb[:], 0.0), [Sb[:]], [])

        def rk(r):
            return pb[3][:]

        for (c0, nch) in batches:
            tok0 = c0 * 64
            W = nch * 64
            k.dma("sp", qTb[:, 0:W], g.d_qkv[h][:, tok0:tok0 + W], [qTb[:]], [("x", ("qkv", h))])
            k.dma("sp", kTb[:, 0:W], g.d_qkv[8 + h][:, tok0:tok0 + W], [kTb[:]], [("x", ("qkv", 8 + h))])
            k.dma("sp", vTb[:, 0:W], g.d_qkv[16 + h][:, tok0:tok0 + W], [vTb[:]], [("x", ("qkv", 16 + h))])
            for dst, ri in ((grow, 0), (brow, 1)):
                src = g.d_rows[ri, dh, tok0:tok0 + W]
                k.dma("sp", dst[:, 0:nch].rearrange("p c j -> p (c j)"), bass.AP(src.tensor, src.offset, [[0, 64], [1, W]]),
                      [dst[:]], [("x", "rows")])
            srcc = g.d_cd[dh, c0:c0 + nch]
            k.dma("sp", cdb[:, 0:nch], bass.AP(srcc.tensor, srcc.offset, [[0, 128], [1, nch]]), [cdb[:]], [("x", "rows")])
            yield

            def tmc(q, inner):
                return V(TM[:, c0, q, dh:dh + 1], (64, nch), (0, inner))

            for si, (src, dst) in enumerate(((kTb, ktm), (vTb, vtm))):
                psv = pb[si][:].bitcast(BF16)
                for c in range(nch):
                    k.op("pe", lambda: pe.transpose(psv[0:64, c * 128:(c + 1) * 128], src[:, c * 64:(c + 1) * 64], idb[:]), [pb[si][:]],
                         [src[:], idb[:]], inc=(c == nch - 1))
                evac(si, dst[:, 0:nch].rearrange("p c f -> p (c f)"), psv[0:64, 0:nch * 128])
                yield
            k.op("pool", lambda: pool.tensor_tensor(out=kdec[:, 0:nch], in0=ktm[:, 0:nch], in1=tmc(3, 128), op=ALU.mult), [kdec[:]],
                 [ktm[:], TM[:]])
            yield
            for c in range(nch):
                k.op("pe", lambda: pe.matmul(pb[1][0:64, c * 64:(c + 1) * 64], lhsT=kTb[:, c * 64:(c + 1) * 64],
                                             rhs=kTb[:, c * 64:(c + 1) * 64], start=True, stop=True), [pb[1][:]], [kTb[:]],
                     inc=(c == nch - 1))
            for c in range(nch):
                k.op("pe", lambda: pe.matmul(pb[2][0:64, c * 64:(c + 1) * 64], lhsT=kTb[:, c * 64:(c + 1) * 64],
                                             rhs=qTb[:, c * 64:(c + 1) * 64], start=True, stop=True), [pb[2][:]],
                     [kTb[:], qTb[:]], inc=(c == nch - 1))
            yield
            k.op("pool", lambda: pool.tensor_tensor(out=dif[:, 0:nch], in0=grow[:, 0:nch], in1=tmc(0, 64), op=ALU.subtract), [dif[:]],
                 [grow[:], TM[:]])
            mk = ("ls", "us", "ui") if d == 0 else ("us", "ls", "li")
            for i in range(3):
                mv = V(masks[mk[i]], (0, nch), (1, 64))
                k.op("pool", lambda: pool.tensor_tensor(out=E[i][:, 0:nch], in0=dif[:, 0:nch], in1=mv,
                                                        op=(ALU.subtract if i == 0 else ALU.add)), [E[i][:]], [dif[:], g.cst[:]])
                k.op("act", lambda: act.activation(out=E[i][:, 0:nch], in_=E[i][:, 0:nch], func=AF.Exp,
                                                   scale=(-1.0 if i == 0 else 1.0)), [E[i][:]], [E[i][:]])
                yield
            cs = slice(0, nch)
            kk3 = pb[1][0:64, 0:nch * 64].rearrange("p (c j) -> p c j", j=64)
            qk3 = pb[2][0:64, 0:nch * 64].rearrange("p (c j) -> p c j", j=64)
            k.op("dve", lambda: vec.tensor_tensor(out=E[0][:, cs], in0=kk3, in1=E[0][:, cs], op=ALU.mult), [E[0][:]],
                 [pb[1][:], E[0][:]])
            k.op("dve", lambda: vec.scalar_tensor_tensor(out=Y[0][:, cs], in0=E[0][:, cs], scalar=1.0, in1=tmc(1, 64),
                                                        op0=ALU.mult, op1=ALU.mult), [Y[0][:]], [E[0][:], TM[:]])
            yield
            k.op("dve", lambda: vec.tensor_tensor(out=E[1][:, cs], in0=kk3, in1=E[1][:, cs], op=ALU.mult), [E[1][:]],
                 [pb[1][:], E[1][:]])
            k.op("dve", lambda: vec.scalar_tensor_tensor(out=YT[0][:, cs], in0=E[1][:, cs], scalar=-1.0, in1=brow[:, cs],
                                                        op0=ALU.mult, op1=ALU.mult), [YT[0][:]], [E[1][:], brow[:]])
            yield
            k.op("dve", lambda: vec.tensor_tensor(out=AQ[:, cs], in0=qk3, in1=E[2][:, cs], op=ALU.mult), [AQ[:]],
                 [pb[2][:], E[2][:]])
            k.op("pool", lambda: pool.tensor_tensor(out=PT[0][:, 0:nch], in0=YT[0][:, 0:nch], in1=V(id64, (0, nch), (1, 64)),
                                                    op=ALU.add), [PT[0][:]], [YT[0][:], g.cst[:]])
            yield
            cy, cp = 0, 0
            for lvl in range(5):
                ny = 1 - cy
                for c in range(nch):
                    k.op("pe", lambda: pe.matmul(pb[0][0:64, c * 64:(c + 1) * 64], lhsT=YT[cy][:, c, :], rhs=Y[cy][:, c, :],
                                                 start=True, stop=True), [pb[0][:]], [YT[cy][:], Y[cy][:]], inc=(c == nch - 1))
                evac(0, Y[ny][:, 0:nch].rearrange("p c j -> p (c j)"), pb[0][0:64, 0:nch * 64])
                yield
                if lvl < 4:
                    for c in range(nch):
                        k.op("pe", lambda: pe.matmul(pb[1][0:64, c * 64:(c + 1) * 64], lhsT=Y[cy][:, c, :], rhs=YT[cy][:, c, :],
                                                     start=True, stop=True), [pb[1][:]], [YT[cy][:], Y[cy][:]], inc=(c == nch - 1))
                    evac(1, YT[ny][:, 0:nch].rearrange("p c j -> p (c j)"), pb[1][0:64, 0:nch * 64])
                    yield
                np_ = 1 - cp
                for c in range(nch):
                    k.op("pe", lambda: pe.matmul(pb[2][0:64, c * 64:(c + 1) * 64], lhsT=Y[ny][:, c, :], rhs=PT[cp][:, c, :],
                                                 start=True, stop=True), [pb[2][:]], [Y[ny][:], PT[cp][:]], inc=(c == nch - 1))
                k.op("dve", lambda: vec.tensor_tensor(out=PT[np_][:, 0:nch].rearrange("p c j -> p (c j)"), in0=pb[2][0:64, 0:nch * 64],
                                                      in1=PT[cp][:, 0:nch].rearrange("p c j -> p (c j)"), op=ALU.add),
                     [PT[np_][:]], [pb[2][:], PT[cp][:]])
                yield
                cy, cp = ny, np_
            TT = PT[cp]
            order = range(nch) if d == 0 else range(nch - 1, -1, -1)
            R = pb[3]
            for c in order:
                gc = c0 + c
                eGc = TM[:, gc, 2, dh:dh + 1]
                bc_ = TM[:, gc, 1, dh:dh + 1]
                k.op("pe", lambda: pe.matmul(R[0:64, 0:128], lhsT=kTb[:, c * 64:(c + 1) * 64], rhs=Sb[:], start=True, stop=True),
                     [rk(0)], [kTb[:], Sb[:]])
                k.op("pe", lambda: pe.matmul(R[0:64, 384:512], lhsT=qTb[:, c * 64:(c + 1) * 64], rhs=Sb[:], start=True, stop=True),
                     [rk(3)], [qTb[:], Sb[:]])
                k.op("dve", lambda: vec.scalar_tensor_tensor(out=t2a[:], in0=R[0:64, 0:128], scalar=eGc, in1=vtm[:, c, :],
                                                            op0=ALU.mult, op1=ALU.subtract), [t2a[:]], [rk(0), TM[:], vtm[:]])
                k.op("act", lambda: act.activation(out=t2b[:], in_=t2a[:], func=AF.Identity, scale=bc_), [t2b[:]], [t2a[:], TM[:]])
                yield
                k.op("pe", lambda: pe.matmul(R[0:64, 128:256], lhsT=TT[:, c, :], rhs=t2b[:], start=True, stop=True), [rk(1)],
                     [TT[:], t2b[:]])
                k.op("act", lambda: act.copy(out=vnew[:], in_=R[0:64, 128:256]), [vnew[:]], [rk(1)])
                yield
                k.op("pe", lambda: pe.matmul(R[:, 256:384], lhsT=kdec[:, c, :], rhs=vnew[:], start=True, stop=True), [rk(2)],
                     [kdec[:], vnew[:]])
                k.op("pe", lambda: pe.matmul(pb[2][0:64, 0:128], lhsT=AQ[:, c, :], rhs=vnew[:], start=True, stop=True), [pb[2][:]],
                     [AQ[:], vnew[:]])
                k.op("dve", lambda: vec.scalar_tensor_tensor(out=S[:], in0=S[:], scalar=cdb[:, c:c + 1], in1=R[:, 256:384],
                                                            op0=ALU.mult, op1=ALU.add), [S[:]], [S[:], cdb[:], rk(2)])
                k.op("act", lambda: act.copy(out=Sb[:], in_=S[:]), [Sb[:]], [S[:]])
                k.op("act", lambda: act.activation(out=t3[:], in_=R[0:64, 384:512], func=AF.Identity, scale=eGc), [t3[:]],
                     [rk(3), TM[:]])
                k.op("dve", lambda: vec.tensor_tensor(out=ob[:, c, :], in0=pb[2][0:64, 0:128], in1=t3[:], op=ALU.add), [ob[:]],
                     [pb[2][:], t3[:]])
                yield
            dsto = g.d_o[d][tok0:tok0 + W, h * 128:(h + 1) * 128].rearrange("(c p) f -> p c f", p=64)
            k.dma("pool", dsto, ob[:, 0:nch], [("x", ("o", d, c0))], [ob[:]])
            yield

    for h2 in range(4):
        gens = [run(2 * h2, 0, 0), run(2 * h2, 1, 1), run(2 * h2 + 1, 0, 2), run(2 * h2 + 1, 1, 3)]
        alive = [True] * 4
        while any(alive):
            for i in range(4):
                if alive[i]:
                    try:
                        next(gens[i])
                    except StopIteration:
                        alive[i] = False
    k.release(m)


def gdn_out(k, g, li, ntiles):
    nc = k.nc
    vec, act, pe, pool = nc.vector, nc.scalar, nc.tensor, nc.gpsimd
    m = k.mark()
    wz = k.sb([128, 8, 1024], BF16, "wz")
    wo = k.sb([128, 8, 1024], BF16, "wo")
    for kk in range(8):
        k.dma("pool", wz[:, kk, :], g.d_gwin[kk * 128:(kk + 1) * 128, 3072:4096], [wz[:]], [])
        k.dma("pool", wo[:, kk, :], g.d_gwout[kk * 128:(kk + 1) * 128, :], [wo[:]], [])
    ng = k.sb([128, 128], F32, "gng")
    bcast_row_load(k, ng[:], g.d_gng, 128)
    GT = k.sb([128, 1024], F32, "ggt")
    of = [k.sb([128, 1024], F32, "gof%d" % i) for i in range(2)]
    obw = [k.sb([128, 1024], F32, "gobw%d" % i) for i in range(2)]
    xt = [k.sb([128, 1024], F32, "gxt%d" % i) for i in range(2)]
    hT = [k.sb([128, 8, 128], BF16, "ghT%d" % i) for i in range(2)]
    sq = k.sb([128, 1024], F32, "gsq")
    zs = k.sb([128, 1024], F32, "gzs")
    st = k.sb([128, 24], F32, "gst")
    ogT = k.sb([128, 8, 128], BF16, "gogT")
    cur = -1
    for t in range(ntiles):
        r = 0 if t < L // 128 else 1
        if r != cur:
            cur = r
            bcast_row_load(k, GT[:], g.d_mod[li, r, 2], 1024)
        o, o2, x, h = of[t % 2], obw[t % 2], xt[t % 2], hT[t % 2]
        k.dma("sp", o[:], g.d_o[0][t * 128:(t + 1) * 128, :], [o[:]], [("x", ("o", 0, c)) for c in range(0, 72, 8)])
        k.dma("sp", o2[:], g.d_o[1][t * 128:(t + 1) * 128, :], [o2[:]], [("x", ("o", 1, c)) for c in range(0, 72, 8)])
        k.dma("sp", x[:], g.d_xs[t * 128:(t + 1) * 128, :], [x[:]], [("x", ("xs", t))])
        k.dma("sp", h[:], g.d_hT[t], [h[:]], [("x", ("hT", t))])
        for hf in range(2):
            ps = g.psb[hf]
            for kk in range(8):
                k.op("pe", lambda: pe.matmul(ps[:], lhsT=h[:, kk, :], rhs=wz[:, kk, hf * 512:(hf + 1) * 512], start=(kk == 0),
                                             stop=(kk == 7)), [ps[:]], [h[:], wz[:]], inc=(kk == 7))
            k.op("act", lambda: act.activation(out=zs[:, hf * 512:(hf + 1) * 512], in_=ps[:], func=AF.Silu), [zs[:]], [ps[:]])
        k.op("pool", lambda: pool.tensor_tensor(out=o[:], in0=o[:], in1=o2[:], op=ALU.add), [o[:]], [o[:], o2[:]])
        k.op("pool", lambda: pool.tensor_tensor(out=sq[:], in0=o[:], in1=o[:], op=ALU.mult), [sq[:]], [o[:]])
        k.op("dve", lambda: vec.tensor_reduce(out=st[:, 0:8], in_=sq[:].rearrange("p (h f) -> p h f", f=128), axis=AX.X, op=ALU.add),
             [("x", "gst0")], [sq[:]])
        k.op("act", lambda: act.activation(out=st[:, 8:16], in_=st[:, 0:8], func=AF.Sqrt, scale=1.0 / 128, bias=g.epsT), [("x", "gst1")],
             [("x", "gst0"), g.cst[:]])
        k.op("dve", lambda: vec.reciprocal(out=st[:, 16:24], in_=st[:, 8:16]), [("x", "gst2")], [("x", "gst1")])
        o3 = o[:].rearrange("p (h f) -> p h f", f=128)
        k.op("dve", lambda: vec.tensor_tensor(out=o3, in0=o3, in1=V(st[:, 16:17], (1, 8), (0, 128)), op=ALU.mult), [o[:]],
             [o[:], ("x", "gst2")])
        k.op("pool", lambda: pool.tensor_tensor(out=o3, in0=o3, in1=V(ng[:, 0:128], (0, 8), (1, 128)), op=ALU.mult), [o[:]], [o[:], ng[:]])
        k.op("dve", lambda: vec.tensor_tensor(out=o[:], in0=o[:], in1=zs[:], op=ALU.mult), [o[:]], [o[:], zs[:]])
        for kk in range(8):
            ps = g.psb[2 + kk // 4]
            k.op("pe", lambda: pe.transpose(ps[:, (kk % 4) * 128:(kk % 4 + 1) * 128], o[:, kk * 128:(kk + 1) * 128], g.ident), [ps[:]],
                 [o[:], g.cst[:]], inc=(kk % 4 == 3))
            if kk % 4 == 3:
                h0 = kk // 4
                k.op("act", lambda: act.copy(out=ogT[:, h0 * 4:h0 * 4 + 4, :], in_=ps[:].rearrange("p (a b) -> p a b", a=4)), [ogT[:]],
                     [ps[:]])
        for hf in range(2):
            ps = g.psb[4 + hf]
            for kk in range(8):
                k.op("pe", lambda: pe.matmul(ps[:], lhsT=ogT[:, kk, :], rhs=wo[:, kk, hf * 512:(hf + 1) * 512], start=(kk == 0),
                                             stop=(kk == 7)), [ps[:]], [ogT[:], wo[:]], inc=(kk == 7))
            sl = slice(hf * 512, (hf + 1) * 512)
            k.op("dve", lambda: vec.tensor_tensor(out=o2[:, sl], in0=ps[:], in1=GT[:, sl], op=ALU.mult), [o2[:]], [ps[:], GT[:]])
        k.op("pool", lambda: pool.tensor_tensor(out=o2[:], in0=o2[:], in1=x[:], op=ALU.add), [o2[:]], [o2[:], x[:]])
        k.dma("pool", g.d_xs[t * 128:(t + 1) * 128, :], o2[:], [("x", ("xs", t))], [o2[:]])
    k.release(m)


def host_cst2():
    r = np.ones((16, NTOK), np.float32)
    r[:, ::64] = 0.0
    return r


def prep_shared(inp):
    sh = {}
    sh["cst"] = host_consts()
    sh["cst2"] = host_cst2()
    sh["peer_wq"] = np.ascontiguousarray(inp["peer_w_query"], dtype=np.float32)
    sh["peer_sk"] = np.ascontiguousarray(inp["peer_sub_keys"], dtype=np.float32)
    u = np.asarray(inp["peer_u"], dtype=np.float32).reshape(2, 128, 128, 8, 128)
    sh["peer_ut"] = np.ascontiguousarray(u.transpose(0, 1, 4, 3, 2)).reshape(2, 128, 128, 1024)
    sh["peer_v"] = np.ascontiguousarray(inp["peer_v"], dtype=np.float32).reshape(2, 128, 128, 1024)
    sh["final_g"] = np.ascontiguousarray(inp["final_g"], dtype=np.float32)
    sh["ada_w"] = np.ascontiguousarray(inp["ada_w"], dtype=np.float32)
    sh["ada_b"] = np.ascontiguousarray(inp["ada_b"], dtype=np.float32)
    sh["norm1_g"] = np.ascontiguousarray(inp["norm1_g"], dtype=np.float32)
    sh["norm2_g"] = np.ascontiguousarray(inp["norm2_g"], dtype=np.float32)
    sh["gdn_w_in"] = np.ascontiguousarray(inp["gdn_w_in"][0], dtype=np.float32)
    cw = np.asarray(inp["gdn_conv_w"][0], dtype=np.float32)
    sh["gdn_conv"] = np.ascontiguousarray(cw.T.reshape(24, 128, 5))
    sh["gdn_a_log"] = np.ascontiguousarray(inp["gdn_a_log"][0], dtype=np.float32)
    sh["gdn_dt_bias"] = np.ascontiguousarray(inp["gdn_dt_bias"][0], dtype=np.float32)
    sh["gdn_norm_g"] = np.ascontiguousarray(inp["gdn_norm_g"][0], dtype=np.float32)
    sh["gdn_w_out"] = np.ascontiguousarray(inp["gdn_w_out"][0], dtype=np.float32)
    sh["swa_w_in"] = np.ascontiguousarray(inp["swa_w_in"][0], dtype=np.float32)
    sh["swa_w_out"] = np.ascontiguousarray(inp["swa_w_out"][0], dtype=np.float32)
    sh["swa_sinks"] = np.ascontiguousarray(inp["swa_sinks"][0], dtype=np.float32)
    sh["rope"] = host_rope()
    return sh


def prep_core(inp, b):
    pc = {}
    pc["x"] = np.ascontiguousarray(inp["x"][b], dtype=np.float32)
    pc["ctx"] = np.ascontiguousarray(inp["ctx"][b], dtype=np.float32)
    cc = np.stack([np.asarray(inp["c"][b], np.float32), np.asarray(inp["c_ctx"], np.float32)], axis=-1)
    pc["cT"] = np.ascontiguousarray(cc.reshape(8, 128, 2).transpose(1, 0, 2))
    return pc


def host_rope():
    rows = L // 64
    row = np.repeat(np.arange(rows), 64).astype(np.float32)
    col = np.tile(np.arange(64), rows).astype(np.float32)
    inv = (10000.0 ** (-np.arange(0, 32, 2, dtype=np.float32) / 32.0)).astype(np.float32)
    ar = row[:, None] * inv
    ac = col[:, None] * inv
    cos = np.concatenate([np.cos(ar), np.cos(ar), np.cos(ac), np.cos(ac)], axis=1)
    sin = np.concatenate([-np.sin(ar), np.sin(ar), -np.sin(ac), np.sin(ac)], axis=1)
    return np.ascontiguousarray(np.stack([cos, sin]).astype(np.float32))


def swa_phase(k, g, li):
    nc = k.nc
    vec, act, pe, pool = nc.vector, nc.scalar, nc.tensor, nc.gpsimd
    m = k.mark()
    NLT = L // 128
    win = k.sb([128, 8, 1536], BF16, "swin")
    wo = k.sb([128, 8, 1024], BF16, "swo")
    for kk in range(8):
        k.dma("pool", win[:, kk, :], g.d_swin[kk * 128:(kk + 1) * 128, :], [win[:]], [])
        k.dma("pool", wo[:, kk, :], g.d_swout[kk * 128:(kk + 1) * 128, :], [wo[:]], [])
    QT = k.sb([128, 8, L], BF16, "sQT")
    KT = k.sb([128, 4, NTOK], BF16, "sKT")
    V1 = k.sb([128, NT, 4, 65], BF16, "sV1")
    k.op("pool", lambda: pool.memset(V1[:], 1.0), [V1[:]], [])
    idb = k.sb([128, 128], BF16, "sidb")
    nmp = k.sb([128, 128], BF16, "snmp")
    nmn = k.sb([128, 128], BF16, "snmn")
    k.op("dve", lambda: vec.tensor_copy(out=idb[:], in_=g.ident), [idb[:]], [g.cst[:]])
    k.op("dve", lambda: vec.tensor_copy(out=nmp[:], in_=g.cst[:, C_NMP:C_NMP + 128]), [nmp[:]], [g.cst[:]])
    k.op("dve", lambda: vec.tensor_copy(out=nmn[:], in_=g.cst[:, C_NMN:C_NMN + 128]), [nmn[:]], [g.cst[:]])
    esink = k.sb([128, 16], F32, "sesink")
    bcast_row_load(k, esink[:], g.d_ssink, 16)
    k.op("act", lambda: act.activation(out=esink[:], in_=esink[:], func=AF.Exp), [esink[:]], [esink[:]])
    GT = k.sb([128, 1024], F32, "sgt")
    bcast_row_load(k, GT[:], g.d_mod[li, 0, 2], 1024)
    hT = [k.sb([128, 8, 128], BF16, "shT%d" % i) for i in range(2)]
    cs = [k.sb([128, 2, 64], F32, "scs%d" % i) for i in range(2)]
    tmps = [k.sb([128, 512], F32, "stmp%d" % i) for i in range(3)]
    tmp2s = [k.sb([128, 512], F32, "stmp2%d" % i) for i in range(3)]
    qrs = [k.sb([128, 1024], BF16, "sqr%d" % i) for i in range(2)]
    krs = [k.sb([128, 4, 2, 64], BF16, "skr%d" % i) for i in range(2)]
    pb = g.psb
    for t in range(NT):
        lat = t < NLT
        h = hT[t % 2]
        k.dma("sp", h[:], g.d_hT[t], [h[:]], [("x", ("hT", t))])
        if lat:
            c_ = cs[t % 2]
            k.dma("sp", c_[:], g.d_rope[:, t * 128:(t + 1) * 128, :].rearrange("a p e -> p a e"), [c_[:]], [])
        qr, kr = qrs[t % 2], krs[t % 2]
        banks = [0, 1, 2] if lat else [2]
        for b in banks:
            for kk in range(8):
                k.op("pe", lambda: pe.matmul(pb[b][:], lhsT=h[:, kk, :], rhs=win[:, kk, b * 512:(b + 1) * 512], start=(kk == 0),
                                             stop=(kk == 7)), [pb[b][:]], [h[:], win[:]], inc=(kk == 7))
        if lat:
            for b, H, dst in ((0, 8, qr[:, 0:512]), (1, 8, qr[:, 512:1024]), (2, 4, None)):
                W = H * 64
                tmp, tmp2 = tmps[b], tmp2s[b]
                src = pb[b][:, 0:W]
                for hf in range(2):
                    pv = V(pb[b][:, (1 - hf) * 16:(1 - hf) * 16 + 1], (32, 2 * H), (1, 16))
                    sv = V(c_[:, 1, hf * 16:hf * 16 + 1], (0, H), (32, 2), (1, 16))
                    ov = V(tmp[:, hf * 16:hf * 16 + 1], (32, 2 * H), (1, 16))
                    pv4 = V(pb[b][:, (1 - hf) * 16:(1 - hf) * 16 + 1], (64, H), (32, 2), (1, 16))
                    ov4 = V(tmp[:, hf * 16:hf * 16 + 1], (64, H), (32, 2), (1, 16))
                    k.op("dve", lambda: vec.tensor_tensor(out=ov4, in0=pv4, in1=sv, op=ALU.mult), [tmp[:]], [pb[b][:], c_[:]])
                k.op("dve", lambda: vec.tensor_tensor(out=tmp2[:, 0:W].rearrange("p (h e) -> p h e", e=64),
                                                      in0=src.rearrange("p (h e) -> p h e", e=64), in1=V(c_[:, 0, 0:1], (0, H), (1, 64)),
                                                      op=ALU.mult), [tmp2[:]], [pb[b][:], c_[:]])
                if b < 2:
                    k.op("pool", lambda: pool.tensor_tensor(out=dst, in0=tmp[:, 0:W], in1=tmp2[:, 0:W], op=ALU.add), [qr[:]],
                         [tmp[:], tmp2[:]])
                else:
                    for dup in range(2):
                        k.op("pool", lambda: pool.tensor_tensor(out=kr[:, :, dup, :], in0=tmp[:, 0:W].rearrange("p (h e) -> p h e", e=64),
                                                                in1=tmp2[:, 0:W].rearrange("p (h e) -> p h e", e=64), op=ALU.add),
                             [kr[:]], [tmp[:], tmp2[:]])
            psq = pb[3][:].bitcast(BF16)
            for pr in range(8):
                k.op("pe", lambda: pe.transpose(psq[:, pr * 128:(pr + 1) * 128], qr[:, pr * 128:(pr + 1) * 128], idb[:]), [pb[3][:]],
                     [qr[:], idb[:]], inc=(pr == 7))
            k.op("act", lambda: act.copy(out=QT[:, :, t * 128:(t + 1) * 128], in_=psq[:, 0:1024].rearrange("p (a b) -> p a b", a=8)),
                 [QT[:]], [pb[3][:]])
        else:
            for dup in range(2):
                k.op("act", lambda: act.copy(out=kr[:, :, dup, :], in_=pb[2][:, 0:256].rearrange("p (h e) -> p h e", e=64)), [kr[:]],
                     [pb[2][:]])
        psk = pb[4][:].bitcast(BF16)
        for j in range(4):
            k.op("pe", lambda: pe.transpose(psk[:, j * 128:(j + 1) * 128], kr[:, j].rearrange("p a e -> p (a e)"), idb[:]), [pb[4][:]],
                 [kr[:], idb[:]], inc=(j == 3))
        k.op("dve", lambda: vec.tensor_copy(out=KT[:, :, t * 128:(t + 1) * 128], in_=psk[:, 0:512].rearrange("p (a b) -> p a b", a=4)),
             [KT[:]], [pb[4][:]])
        k.op("act", lambda: act.copy(out=V1[:, t, :, 0:64], in_=pb[2][:, 256:512].rearrange("p (a b) -> p a b", a=4)), [V1[:]], [pb[2][:]])
    PT = [k.sb([128, 640], BF16, "sPT%d" % i) for i in range(2)]
    otm = k.sb([128, 1024], BF16, "sotm")
    den = k.sb([128, 8], F32, "sden")
    ogT = k.sb([128, 8, 128], BF16, "sogT")
    xt = [k.sb([128, 1024], F32, "sxt%d" % i) for i in range(2)]
    ysb = k.sb([128, 1024], F32, "sysb")
    psqT = pb[6][:].bitcast(BF16)
    n = 0
    for t in range(NLT):
        x = xt[t % 2]
        k.dma("sp", x[:], g.d_xs[t * 128:(t + 1) * 128, :], [x[:]], [("x", ("xs", t))])
        kbs = ([(t - 1, "p")] if t > 0 else []) + [(t, "c")] + ([(t + 1, "n")] if t < NLT - 1 else []) + [(NLT, "c"), (NLT + 1, "c")]
        nkb = len(kbs)
        for j in range(4):
            pso = pb[4 + j % 2]
            for hh in range(4):
                hq = 4 * j + hh
                pair, u = hq // 2, hq % 2
                sl = slice(u * 64, (u + 1) * 64)
                bs = (n % 2) * 2
                n += 1
                for i, (kt, kind) in enumerate(kbs):
                    dst = pb[bs + i // 4][:, (i % 4) * 128:(i % 4 + 1) * 128]
                    last = kind == "c"
                    k.op("pe", lambda: pe.matmul(dst, lhsT=KT[sl, j, kt * 128:(kt + 1) * 128], rhs=QT[sl, pair, t * 128:(t + 1) * 128],
                                                 start=True, stop=last), [pb[bs + i // 4][:]], [KT[:], QT[:]],
                         inc=(last and (i == nkb - 1 or i == 3)))
                    if not last:
                        mk_ = nmp if kind == "p" else nmn
                        k.op("pe", lambda: pe.matmul(dst, lhsT=idb[:], rhs=mk_[:], start=False, stop=True), [pb[bs + i // 4][:]],
                             [idb[:], mk_[:]], inc=(i == nkb - 1 or i == 3))
                P = PT[n % 2]
                n0 = min(nkb, 4) * 128
                k.op("act", lambda: act.activation(out=P[:, 0:n0], in_=pb[bs][:, 0:n0], func=AF.Exp, scale=0.125), [P[:]], [pb[bs][:]])
                if nkb > 4:
                    k.op("act", lambda: act.activation(out=P[:, 512:640], in_=pb[bs + 1][:, 0:128], func=AF.Exp, scale=0.125), [P[:]],
                         [pb[bs + 1][:]])
                for i, (kt, kind) in enumerate(kbs):
                    k.op("pe", lambda: pe.matmul(pso[:, hh * 128:hh * 128 + 65], lhsT=P[:, i * 128:(i + 1) * 128], rhs=V1[:, kt, j, :],
                                                 start=(i == 0), stop=(i == nkb - 1)), [pso[:]], [P[:], V1[:]],
                         inc=(i == nkb - 1 and hh == 3))
            k.op("dve", lambda: vec.tensor_tensor(out=den[:, 0:4], in0=V(pso[:, 64:65], (128, 4)), in1=esink[:, 4 * j:4 * j + 4], op=ALU.add),
                 [("x", "sden0")], [pso[:], esink[:]])
            k.op("dve", lambda: vec.reciprocal(out=den[:, 4:8], in_=den[:, 0:4]), [("x", "sden1")], [("x", "sden0")])
            k.op("dve", lambda: vec.tensor_tensor(out=otm[:, j * 256:(j + 1) * 256].rearrange("p (a e) -> p a e", e=64),
                                                  in0=V(pso[:, 0:1], (128, 4), (1, 64)), in1=V(den[:, 4:5], (1, 4), (0, 64)), op=ALU.mult),
                 [otm[:]], [pso[:], ("x", "sden1")])
        for kk in range(8):
            k.op("pe", lambda: pe.transpose(psqT[:, kk * 128:(kk + 1) * 128], otm[:, kk * 128:(kk + 1) * 128], idb[:]), [pb[6][:]],
                 [otm[:], idb[:]], inc=(kk == 7))
        k.op("act", lambda: act.copy(out=ogT[:], in_=psqT[:, 0:1024].rearrange("p (a b) -> p a b", a=8)), [ogT[:]], [pb[6][:]])
        for hf in range(2):
            ps = pb[7 - hf]
            for kk in range(8):
                k.op("pe", lambda: pe.matmul(ps[:], lhsT=ogT[:, kk, :], rhs=wo[:, kk, hf * 512:(hf + 1) * 512], start=(kk == 0), stop=(kk == 7)),
                     [ps[:]], [ogT[:], wo[:]], inc=(kk == 7))
            sl2 = slice(hf * 512, (hf + 1) * 512)
            k.op("dve", lambda: vec.tensor_tensor(out=ysb[:, sl2], in0=ps[:], in1=GT[:, sl2], op=ALU.mult), [ysb[:]], [ps[:], GT[:]])
        k.op("pool", lambda: pool.tensor_tensor(out=ysb[:], in0=ysb[:], in1=x[:], op=ALU.add), [ysb[:]], [ysb[:], x[:]])
        k.dma("pool", g.d_xs[t * 128:(t + 1) * 128, :], ysb[:], [("x", ("xs", t))], [ysb[:]])
    k.release(m)


FULL_PHASES = (("adaln",), ("gdn",), ("peer", 0, NT, False), ("swa",), ("peer", 1, L // 128, True))


def kernel(**inputs):
    k = build(phases=FULL_PHASES)
    sh = prep_shared(inputs)
    names = set(k.ext_inputs)
    in_maps = []
    for b in range(8):
        d_ = dict(sh)
        d_.update(prep_core(inputs, b))
        in_maps.append({a: v for a, v in d_.items() if a in names})
    res = run_bass_kernel_spmd(k.nc, in_maps, core_ids=list(range(8)))
    return np.stack([np.asarray(r["out"], dtype=np.float32) for r in res.results], axis=0)
```
